# Optimizing a Trainium2 kernel written in Bass

```python
import math
import jax, jax.numpy as jnp
from jax import lax
import numpy as np

D_MODEL = 1024
BATCH = 8
SEQ = 4096
DEPTH = 4

GRID_W = 64
HEAD_DIM = 64
A_Q_HEADS = 8
A_KV_HEADS = 2
ROPE_THETA = 10000.0
Q_BLOCK = 128
B_HEADS = 4
WIN_R = 8
WIN_C = 16
C_HEADS = 4
CONV_K = 5
GDN_CHUNK = 64
N_EXPERTS = 32
N_GROUPS = 8
TOP_K = 2
D_EXPERT = 512
DN_ALPHA = (2 * DEPTH) ** 0.25
DN_BETA = (8 * DEPTH) ** -0.25

A_Q_W = A_Q_HEADS * HEAD_DIM
A_KV_W = A_KV_HEADS * HEAD_DIM
B_W = B_HEADS * HEAD_DIM
C_W = C_HEADS * HEAD_DIM
IN_SPLITS = (A_Q_W, A_KV_W, A_KV_W,
             B_W, B_W, B_W,
             C_W, C_W, C_W, C_W,
             C_HEADS, C_HEADS, C_HEADS, C_HEADS,
             3 * D_MODEL)
IN_VALUE_BLOCKS = (2, 5, 8)
D_IN = sum(IN_SPLITS)

kernel_name = "hybrid_gqa_natten_gdn_moe_deepnorm"

F32 = jnp.float32


def _split_cols(t, sizes):
    idx = np.cumsum(sizes)[:-1].tolist()
    return jnp.split(t, idx, axis=-1)


def _layernorm(x, g, b, eps=1e-5):
    xf = x.astype(F32)
    mu = jnp.mean(xf, axis=-1, keepdims=True)
    xc = xf - mu
    var = jnp.mean(xc * xc, axis=-1, keepdims=True)
    return (xc * lax.rsqrt(var + eps) * g.astype(F32) + b.astype(F32)).astype(x.dtype)


def _rms(x, g, eps=1e-6):
    xf = x.astype(F32)
    y = xf * lax.rsqrt(jnp.mean(xf * xf, axis=-1, keepdims=True) + eps)
    return (y * g.astype(F32)).astype(x.dtype)


def _l2n(x, eps=1e-6):
    xf = x.astype(F32)
    return xf * lax.rsqrt(jnp.sum(xf * xf, axis=-1, keepdims=True) + eps)


def _axial_rope_tables(seq):
    t = jnp.arange(seq, dtype=jnp.int32)
    row = (t // GRID_W).astype(F32)
    col = (t % GRID_W).astype(F32)
    half = HEAD_DIM // 2
    inv = ROPE_THETA ** (-jnp.arange(0, half, 2, dtype=F32) / half)
    ang_r = row[:, None] * inv[None, :]
    ang_c = col[:, None] * inv[None, :]
    return (jnp.cos(ang_r), jnp.sin(ang_r), jnp.cos(ang_c), jnp.sin(ang_c))


def _rotate(x, cos, sin):
    x1, x2 = jnp.split(x, 2, axis=-1)
    c = cos[:, None, :]
    s = sin[:, None, :]
    return jnp.concatenate([x1 * c - x2 * s, x2 * c + x1 * s], axis=-1)


def _axial_rope(x, tables):
    cr, sr, cc, sc = tables
    xr, xc = jnp.split(x.astype(F32), 2, axis=-1)
    return jnp.concatenate([_rotate(xr, cr, sr), _rotate(xc, cc, sc)], axis=-1).astype(x.dtype)


def _global_gqa(q, k, v, q_g, k_g, tables):
    B_, S_, _ = q.shape
    q = q.reshape(B_, S_, A_Q_HEADS, HEAD_DIM)
    k = k.reshape(B_, S_, A_KV_HEADS, HEAD_DIM)
    v = v.reshape(B_, S_, A_KV_HEADS, HEAD_DIM)
    q = _axial_rope(_rms(q, q_g), tables) * (HEAD_DIM ** -0.5)
    k = _axial_rope(_rms(k, k_g), tables)
    grp = A_Q_HEADS // A_KV_HEADS
    nb = S_ // Q_BLOCK
    qb = q.reshape(B_, nb, Q_BLOCK, A_KV_HEADS, grp, HEAD_DIM).transpose(1, 0, 2, 3, 4, 5)

    def block(qi):
        s = jnp.einsum('bqhgd,bshd->bhgqs', qi, k).astype(F32)
        p = jax.nn.softmax(s, axis=-1).astype(v.dtype)
        return jnp.einsum('bhgqs,bshd->bqhgd', p, v)

    o = lax.map(block, qb)
    return o.transpose(1, 0, 2, 3, 4, 5).reshape(B_, S_, A_Q_W)


def _neighbourhood_attn(q, k, v, rpb):
    B_, S_, _ = q.shape
    rows = S_ // GRID_W
    wr = min(WIN_R, rows)
    shp = (B_, rows, GRID_W, B_HEADS, HEAD_DIM)
    q = q.reshape(shp) * (HEAD_DIM ** -0.5)
    k = k.reshape(shp)
    v = v.reshape(shp)
    c = jnp.arange(GRID_W, dtype=jnp.int32)
    col_idx = jnp.clip(c - WIN_C // 2, 0, GRID_W - WIN_C)[:, None] + jnp.arange(WIN_C, dtype=jnp.int32)[None, :]
    dc = col_idx - c[:, None] + (WIN_C - 1)
    rpb_c = rpb[:, :, dc]

    def row_block(args):
        q_r, r = args
        r0 = jnp.clip(r - wr // 2, 0, rows - wr)
        k_rows = lax.dynamic_slice_in_dim(k, r0, wr, axis=1)
        v_rows = lax.dynamic_slice_in_dim(v, r0, wr, axis=1)
        k_win = k_rows[:, :, col_idx]
        v_win = v_rows[:, :, col_idx]
        dr = r0 + jnp.arange(wr, dtype=jnp.int32) - r + (WIN_R - 1)
        bias = rpb_c[:, dr].transpose(0, 2, 1, 3)
        s = jnp.einsum('bchd,brcjhd->bhcrj', q_r, k_win).astype(F32) + bias.astype(F32)
        p = jax.nn.softmax(s, axis=(-2, -1)).astype(v.dtype)
        return jnp.einsum('bhcrj,brcjhd->bchd', p, v_win)

    q_rows = q.transpose(1, 0, 2, 3, 4)
    o = lax.map(row_block, (q_rows, jnp.arange(rows, dtype=jnp.int32)))
    return o.transpose(1, 0, 2, 3, 4).reshape(B_, S_, B_W)


def _short_conv(x, w):
    return lax.conv_general_dilated(
        x, w[:, None, :].astype(x.dtype), window_strides=(1,),
        padding=[(CONV_K // 2, CONV_K // 2)],
        dimension_numbers=('NWC', 'WIO', 'NWC'),
        feature_group_count=x.shape[-1])


def _gated_delta_chunked(q, k, v, g, beta):
    B_, S_, H, dk = q.shape
    dv = v.shape[-1]
    C = GDN_CHUNK
    N = S_ // C

    def chunks(t):
        return t.reshape(B_, N, C, H, -1).transpose(1, 0, 3, 2, 4)

    qc = chunks(q * (dk ** -0.5))
    kc = chunks(k)
    vc = chunks(v)
    gc = jnp.cumsum(chunks(g[..., None])[..., 0], axis=-1)
    bc = chunks(beta[..., None])
    incl = jnp.tril(jnp.ones((C, C), dtype=bool))
    strict = jnp.tril(jnp.ones((C, C), dtype=bool), -1)
    decay = jnp.exp(jnp.where(incl, gc[..., :, None] - gc[..., None, :], -jnp.inf))
    k_beta = kc * bc
    m = jnp.where(strict, jnp.einsum('nbhid,nbhjd->nbhij', k_beta, kc) * decay, 0.0)
    rhs = jnp.concatenate([vc * bc, k_beta * jnp.exp(gc)[..., None]], axis=-1)
    sol = lax.linalg.triangular_solve(m + jnp.eye(C, dtype=m.dtype), rhs,
                                      left_side=True, lower=True, unit_diagonal=True)
    u, w = sol[..., :dv], sol[..., dv:]
    qk = jnp.einsum('nbhid,nbhjd->nbhij', qc, kc) * decay

    def step(state, xs):
        q_i, k_i, u_i, w_i, g_i, qk_i = xs
        v_new = u_i - jnp.einsum('bhck,bhkv->bhcv', w_i, state)
        o = (jnp.einsum('bhck,bhkv->bhcv', q_i * jnp.exp(g_i)[..., None], state)
             + jnp.einsum('bhij,bhjv->bhiv', qk_i, v_new))
        g_last = g_i[..., -1:]
        state = (state * jnp.exp(g_last)[..., None]
                 + jnp.einsum('bhck,bhcv->bhkv', k_i * jnp.exp(g_last - g_i)[..., None], v_new))
        return state, o

    s0 = jnp.zeros((B_, H, dk, dv), F32)
    _, o = lax.scan(step, s0, (qc, kc, u, w, gc, qk))
    return o.transpose(1, 0, 3, 2, 4).reshape(B_, S_, H, dv)


def _gdn_bidir(q, k, v, z, b_f, b_b, a_f, a_b, conv_w, A_log, dt_bias, norm_g):
    B_, S_, _ = q.shape
    qkv = jax.nn.silu(_short_conv(jnp.concatenate([q, k, v], axis=-1), conv_w))
    q, k, v = jnp.split(qkv, 3, axis=-1)
    hs = (B_, S_, C_HEADS, HEAD_DIM)
    q = _l2n(q.reshape(hs))
    k = _l2n(k.reshape(hs))
    v = v.reshape(hs).astype(F32)

    def gates(bb, aa, d):
        beta = jax.nn.sigmoid(bb.astype(F32))
        g = -jnp.exp(A_log[d].astype(F32)) * jax.nn.softplus(aa.astype(F32) + dt_bias[d].astype(F32))
        return g, beta

    g_f, beta_f = gates(b_f, a_f, 0)
    g_b, beta_b = gates(b_b, a_b, 1)
    o_f = _gated_delta_chunked(q, k, v, g_f, beta_f)
    flip = lambda t: jnp.flip(t, axis=1)
    o_b = flip(_gated_delta_chunked(flip(q), flip(k), flip(v), flip(g_b), flip(beta_b)))
    o = _rms(o_f + o_b, norm_g) * jax.nn.silu(z.reshape(hs).astype(F32))
    return o.reshape(B_, S_, C_W).astype(z.dtype)


def _mixer(h, w_in, q_g, k_g, rpb, conv_w, A_log, dt_bias, gdn_g, wa, wb, wc, w_out, tables):
    p = h @ w_in
    (aq, ak, av, bq, bk, bv, cq, ck, cv, cz, cbf, cbb, caf, cab, gate) = _split_cols(p, IN_SPLITS)
    ya = _global_gqa(aq, ak, av, q_g, k_g, tables)
    yb = _neighbourhood_attn(bq, bk, bv, rpb)
    yc = _gdn_bidir(cq, ck, cv, cz, cbf, cbb, caf, cab, conv_w, A_log, dt_bias, gdn_g)
    ga, gb, gc = jnp.split(jax.nn.sigmoid(gate.astype(F32)).astype(h.dtype), 3, axis=-1)
    mix = ga * (ya @ wa) + gb * (yb @ wb) + gc * (yc @ wc)
    return mix @ w_out


def _moe(h, w_router, router_bias, w1, w3, w2):
    B_, S_, D = h.shape
    xt = h.reshape(B_ * S_, D)
    scores = jax.nn.sigmoid((xt @ w_router).astype(F32))
    sel = scores + router_bias.astype(F32)
    grp = sel.reshape(-1, N_GROUPS, N_EXPERTS // N_GROUPS)
    grp_score = jnp.sum(lax.top_k(grp, TOP_K)[0], axis=-1)
    best = jnp.argmax(grp_score, axis=-1)
    in_group = (jnp.arange(N_GROUPS)[None, :] == best[:, None])[:, :, None]
    masked = jnp.where(in_group, grp, -jnp.inf).reshape(-1, N_EXPERTS)
    _, eidx = lax.top_k(masked, TOP_K)
    w_sel = jnp.take_along_axis(scores, eidx, axis=-1)
    w_sel = w_sel / jnp.sum(w_sel, axis=-1, keepdims=True)
    comb = jnp.sum(jax.nn.one_hot(eidx, N_EXPERTS, dtype=F32) * w_sel[..., None], axis=1).astype(h.dtype)
    y = jnp.zeros_like(xt)
    for e in range(N_EXPERTS):
        hid = jax.nn.silu(xt @ w1[e]) * (xt @ w3[e])
        y = y + comb[:, e:e + 1] * (hid @ w2[e])
    return y.reshape(B_, S_, D)


def setup_inputs(seed: int = 0) -> dict:
    key = jax.random.key(seed)
    ks = jax.random.split(key, 32)
    D = D_MODEL

    def nrm(k, shape, scale):
        return jax.random.normal(k, shape, F32) * scale

    col_scale = jnp.concatenate([
        jnp.full((n,), DN_BETA if i in IN_VALUE_BLOCKS else 1.0, F32)
        for i, n in enumerate(IN_SPLITS)]) * (D ** -0.5)
    dt = jnp.exp(jax.random.uniform(ks[8], (DEPTH, 2, C_HEADS), F32, math.log(1e-3), math.log(1e-1)))
    return {
        "x": nrm(ks[0], (BATCH, SEQ, D), 1.0),
        "ln0_g": 1.0 + nrm(ks[1], (D,), 0.01),
        "ln0_b": nrm(ks[2], (D,), 0.01),
        "w_in": jax.random.normal(ks[3], (DEPTH, D, D_IN), F32) * col_scale,
        "q_norm_g": 1.0 + nrm(ks[4], (DEPTH, HEAD_DIM), 0.01),
        "k_norm_g": 1.0 + nrm(ks[5], (DEPTH, HEAD_DIM), 0.01),
        "na_rpb": nrm(ks[6], (DEPTH, B_HEADS, 2 * WIN_R - 1, 2 * WIN_C - 1), 0.02),
        "conv_w": nrm(ks[7], (DEPTH, CONV_K, 3 * C_W), CONV_K ** -0.5),
        "A_log": jnp.log(jax.random.uniform(ks[9], (DEPTH, 2, C_HEADS), F32, 1.0, 16.0)),
        "dt_bias": dt + jnp.log(-jnp.expm1(-dt)),
        "gdn_norm_g": 1.0 + nrm(ks[10], (DEPTH, HEAD_DIM), 0.01),
        "w_branch_a": nrm(ks[11], (DEPTH, A_Q_W, D), A_Q_W ** -0.5),
        "w_branch_b": nrm(ks[12], (DEPTH, B_W, D), B_W ** -0.5),
        "w_branch_c": nrm(ks[13], (DEPTH, C_W, D), C_W ** -0.5),
        "w_out": nrm(ks[14], (DEPTH, D, D), DN_BETA * D ** -0.5),
        "ln1_g": 1.0 + nrm(ks[15], (DEPTH, D), 0.01),
        "ln1_b": nrm(ks[16], (DEPTH, D), 0.01),
        "w_router": nrm(ks[17], (D, N_EXPERTS), D ** -0.5),
        "router_bias": nrm(ks[18], (N_EXPERTS,), 0.01),
        "w1": nrm(ks[19], (DEPTH, N_EXPERTS, D, D_EXPERT), D ** -0.5),
        "w3": nrm(ks[20], (DEPTH, N_EXPERTS, D, D_EXPERT), DN_BETA * D ** -0.5),
        "w2": nrm(ks[21], (DEPTH, N_EXPERTS, D_EXPERT, D), DN_BETA * D_EXPERT ** -0.5),
        "ln2_g": 1.0 + nrm(ks[22], (DEPTH, D), 0.01),
        "ln2_b": nrm(ks[23], (DEPTH, D), 0.01),
    }


def reference(x, ln0_g, ln0_b, w_in, q_norm_g, k_norm_g, na_rpb, conv_w, A_log, dt_bias,
              gdn_norm_g, w_branch_a, w_branch_b, w_branch_c, w_out, ln1_g, ln1_b,
              w_router, router_bias, w1, w3, w2, ln2_g, ln2_b):
    tables = _axial_rope_tables(x.shape[1])
    h = _layernorm(x, ln0_g, ln0_b)
    for l in range(DEPTH):
        mix = _mixer(h, w_in[l], q_norm_g[l], k_norm_g[l], na_rpb[l], conv_w[l], A_log[l], dt_bias[l],
                     gdn_norm_g[l], w_branch_a[l], w_branch_b[l], w_branch_c[l], w_out[l], tables)
        h = _layernorm(DN_ALPHA * h + mix, ln1_g[l], ln1_b[l])
        ffn = _moe(h, w_router, router_bias, w1[l], w3[l], w2[l])
        h = _layernorm(DN_ALPHA * h + ffn, ln2_g[l], ln2_b[l])
    return h
```

```python
import math
import numpy as np
from contextlib import ExitStack
import concourse.bass as bass
import concourse.mybir as mybir
from concourse.bass_utils import run_bass_kernel_spmd

F32 = mybir.dt.float32
BF16 = mybir.dt.bfloat16
I32 = mybir.dt.int32
AF = mybir.ActivationFunctionType
ALU = mybir.AluOpType
AX = mybir.AxisListType

ENGS = ("pe", "act", "dve", "pool", "sp")

D = 1024
KC = 8
DEPTH = 4
GRID_W = 64
DIN = 5648
NE = 32
DE = 512
ALPHA = (2 * DEPTH) ** 0.25
C_AQ, C_AK, C_AV = 0, 512, 640
C_BQ, C_BK, C_BV = 768, 1024, 1280
C_CQ, C_CK, C_CV, C_CZ = 1536, 1792, 2048, 2304
C_CG = 2560
C_GATE = 2576
NEG = -30000.0


class Prog:
    def __init__(self, nc):
        self.nc = nc
        self.es = ExitStack()
        self.sem = {e: self.es.enter_context(nc.semaphore("s_" + e)) for e in ENGS}
        self.cnt = {e: 0 for e in ENGS}
        self.dsem = {}
        self.known = {e: {} for e in ENGS}
        self.lastw = {}
        self.readers = {}
        self.queue = {e: [] for e in ENGS}
        self.pending = {e: [] for e in ENGS}
        self.nops = 0

    def close(self):
        self.es.close()

    def _deps(self, eng, r, w):
        toks = []
        for k in r:
            t = self.lastw.get(k)
            if t is not None:
                toks.append(t)
        for k in w:
            t = self.lastw.get(k)
            if t is not None:
                toks.append(t)
            toks.extend(self.readers.get(k, ()))
        need = {}
        for t in toks:
            key, val = t[0], t[1]
            if eng == "pe" and key == "pe":
                continue
            if val is None:
                raise RuntimeError("dependency on unresolved (inc=False) op")
            if need.get(key, 0) < val:
                need[key] = val
        waits = []
        kn = self.known[eng]
        for key, val in need.items():
            if kn.get(key, 0) < val:
                kn[key] = val
                waits.append((key, val))
        return waits

    def _mark(self, tok, r, w):
        for k in w:
            self.lastw[k] = tok
            self.readers[k] = []
        for k in r:
            self.readers.setdefault(k, []).append(tok)

    def op(self, eng, fn, r=(), w=(), inc=True):
        waits = self._deps(eng, r, w)
        if inc:
            self.cnt[eng] += 1
            tok = [eng, self.cnt[eng]]
            for p in self.pending[eng]:
                p[1] = self.cnt[eng]
            self.pending[eng] = []
        else:
            tok = [eng, None]
            self.pending[eng].append(tok)
        self._mark(tok, r, w)
        self.queue[eng].append((fn, waits, (eng, 1) if inc else None))
        self.nops += 1

    def dma(self, eng, out, in_, r=(), w=(), stream=None, **kw):
        assert stream is not None
        waits = self._deps(eng, r, w)
        if stream not in self.dsem:
            self.dsem[stream] = [self.es.enter_context(self.nc.semaphore("d%d" % len(self.dsem))), 0]
        ds = self.dsem[stream]
        ds[1] += 16
        tok = [("d", stream), ds[1]]
        self._mark(tok, r, w)
        self.queue[eng].append((lambda e, out=out, in_=in_, kw=kw: e.dma_start(out=out, in_=in_, **kw),
                                waits, (("d", stream), 16)))
        self.nops += 1

    def _semh(self, key):
        if isinstance(key, tuple):
            return self.dsem[key[1]][0]
        return self.sem[key]

    def wait_all(self, eng, keys):
        waits = self._deps(eng, keys, ())
        self.queue[eng].append((None, waits, None))

    def flush(self):
        nc = self.nc
        q = self.queue
        self.queue = {e: [] for e in ENGS}
        for e in ENGS:
            if self.pending[e]:
                raise RuntimeError("unresolved inc=False ops at flush on " + e)

        def run(engine, items):
            for fn, waits, inc in items:
                for key, val in waits:
                    engine.wait_ge(self._semh(key), val)
                if fn is None:
                    continue
                ins = fn(engine)
                if inc is not None:
                    ins.then_inc(self._semh(inc[0]), inc[1])

        with nc.Block() as block:
            if q["sp"]:
                @block.sync
                def _(e):
                    run(e, q["sp"])
            if q["pe"]:
                @block.tensor
                def _(e):
                    run(e, q["pe"])
            if q["act"]:
                @block.scalar
                def _(e):
                    run(e, q["act"])
            if q["dve"]:
                @block.vector
                def _(e):
                    run(e, q["dve"])
            if q["pool"]:
                @block.gpsimd
                def _(e):
                    run(e, q["pool"])


def bc(ap, shape):
    return ap.to_broadcast(shape)


class Builder:
    def __init__(self, S, L, dbg=(), parts=("A", "B", "C"), moe="dense"):
        self.S, self.L = S, L
        self.NT = S // 128
        self.NB = S // 512
        self.parts = parts
        self.moe = moe
        nc = self.nc = bass.Bass("TRN2", target_bir_lowering=False)
        self.P = Prog(nc)
        self.dbg = set(dbg)
        dt_in = lambda n, s, d=F32: nc.dram_tensor(n, list(s), d, kind="ExternalInput").ap()
        self.x = dt_in("x", [S, D])
        self.ln0 = dt_in("ln0", [2, D])
        self.w_in = dt_in("w_in", [L, D, DIN])
        self.qkg = dt_in("qkg", [L, 128, 2])
        self.bias_b = dt_in("bias_b", [L, 4, 64, 960])
        self.conv_w = dt_in("conv_w", [L, 128, 6, 5])
        self.gdn_ab = dt_in("gdn_ab", [L, 2, 8])
        self.gdn_g = dt_in("gdn_g", [L, 64])
        self.wa = dt_in("wa", [L, 512, D])
        self.wb = dt_in("wb", [L, 256, D])
        self.wc = dt_in("wc", [L, 256, D])
        self.wo = dt_in("wo", [L, D, D])
        self.ln1 = dt_in("ln1", [L, 2, D])
        self.ln2 = dt_in("ln2", [L, 2, D])
        self.w_router = dt_in("w_router", [D, NE])
        self.router_bias = dt_in("router_bias", [NE])
        self.w1 = dt_in("w1", [L, NE, D, DE])
        self.w3 = dt_in("w3", [L, NE, D, DE])
        self.w2 = dt_in("w2", [L, NE, DE, D])
        self.c_ident = dt_in("c_ident", [128, 128])
        self.c_blk = dt_in("c_blk", [128, 128])
        self.c_rot = dt_in("c_rot", [128, 128])
        self.c_cos = dt_in("c_cos", [128, S])
        self.c_sin = dt_in("c_sin", [128, S])
        self.c_gdn = dt_in("c_gdn", [64, 6, 8, 64])
        okind = "ExternalOutput"
        self.out = nc.dram_tensor("out", [S, D], F32, kind=okind).ap()

        def scr(n, s, d=F32):
            k = "ExternalOutput" if n in self.dbg else "Internal"
            return nc.dram_tensor(n, list(s), d, kind=k).ap()
        self.h_d = scr("h_d", [S, D])
        self.qT_d = scr("qT_d", [512, S], BF16)
        self.kT_d = scr("kT_d", [128, S], BF16)
        self.v_d = scr("v_d", [S, 128], BF16)
        self.bqT_d = scr("bqT_d", [256, S], BF16)
        self.bkT_d = scr("bkT_d", [256, S], BF16)
        self.bv_d = scr("bv_d", [S, 256], BF16)
        self.cT_d = scr("cT_d", [768, S])
        self.cz_d = scr("cz_d", [S, 272])
        self.gT_d = scr("gT_d", [3072, S], BF16)
        self.yaT_d = scr("yaT_d", [512, S], BF16)
        self.ybT_d = scr("ybT_d", [256, S], BF16)
        self.ycT_d = scr("ycT_d", [256, S], BF16)
        self.es = ExitStack()
        self.hT = self.sb(self.es, "hT", [128, KC, S], BF16)
        self.comb = self.sb(self.es, "comb", [128, self.NT, NE])
        self.ident = self.sb(self.es, "ident", [128, 128])
        self.identb = self.sb(self.es, "identb", [128, 128], BF16)

    def sb(self, es, n, s, d=F32):
        self.uid = getattr(self, "uid", 0) + 1
        return es.enter_context(self.nc.sbuf_tensor("sb%d_%s" % (self.uid, n), list(s), d))

    def ps(self, es, n, s, d=F32):
        self.uid = getattr(self, "uid", 0) + 1
        return es.enter_context(self.nc.psum_tensor("ps%d_%s" % (self.uid, n), list(s), d))

    def mm_group(self, out_ap, pairs, r, w):
        P = self.P
        n = len(pairs)
        for i, (l, rh) in enumerate(pairs):
            P.op("pe", lambda e, l=l, rh=rh, i=i: e.matmul(out_ap, l, rh, start=(i == 0), stop=(i == n - 1)),
                 r=r, w=w, inc=(i == n - 1))

    def load_consts(self):
        P = self.P
        P.dma("sp", self.ident[:], self.c_ident, w=["ident"], stream="ident")
        P.dma("pool", self.identb[:], self.c_ident, w=["identb"], stream="identb")

    def ln_tile(self, es_tiles, t, tres, gb, gbres, dst_d, tt, hT_out=True, router=None, pfx="ln"):
        P = self.P
        st, mv, sd, xn, pT = (es_tiles[k] for k in ("st", "mv", "sd", "xn", "pT"))
        P.op("dve", lambda e: e.bn_stats(out=st[:, 0:6], in_=t[:, 0:512]), r=[tres], w=[pfx + "st"])
        P.op("dve", lambda e: e.bn_stats(out=st[:, 6:12], in_=t[:, 512:1024]), r=[tres], w=[pfx + "st"])
        P.op("dve", lambda e: e.bn_aggr(out=mv[:], in_=st[:]), r=[pfx + "st"], w=[pfx + "mv"])
        P.op("act", lambda e: e.activation(out=sd[:, 0:1], in_=mv[:, 1:2], func=AF.Sqrt, bias=1e-5, scale=1.0),
             r=[pfx + "mv"], w=[pfx + "sd"])
        P.op("dve", lambda e: e.reciprocal(out=sd[:, 1:2], in_=sd[:, 0:1]), r=[pfx + "sd"], w=[pfx + "sd1"])
        P.op("dve", lambda e: e.scalar_tensor_tensor(out=sd[:, 2:3], in0=mv[:, 0:1], scalar=-1.0, in1=sd[:, 1:2],
                                                     op0=ALU.mult, op1=ALU.mult),
             r=[pfx + "mv", pfx + "sd1"], w=[pfx + "sd2"])
        P.op("act", lambda e: e.activation(out=xn[:], in_=t[:], func=AF.Identity, bias=sd[:, 2:3], scale=sd[:, 1:2]),
             r=[tres, pfx + "sd1", pfx + "sd2"], w=[pfx + "xn"])
        P.op("dve", lambda e: e.tensor_tensor(out=xn[:], in0=xn[:], in1=gb[:, 0, :], op=ALU.mult),
             r=[pfx + "xn", gbres], w=[pfx + "xn"])
        P.op("pool", lambda e: e.tensor_tensor(out=xn[:], in0=xn[:], in1=gb[:, 1, :], op=ALU.add),
             r=[pfx + "xn", gbres], w=[pfx + "xn"])
        P.dma("sp", dst_d[tt * 128:(tt + 1) * 128, :], xn[:], r=[pfx + "xn"], w=[("h_d", tt) if dst_d is self.h_d else "out"],
              stream=pfx + "xn")
        if hT_out:
            for kc in range(KC):
                P.op("pe", lambda e, kc=kc: e.transpose(pT[:, kc, :], xn[:, kc * 128:(kc + 1) * 128], self.ident[:]),
                     r=[pfx + "xn", "ident"], w=[pfx + "pT"], inc=(kc == KC - 1))
            P.op("act", lambda e: e.activation(out=self.hT[:, :, tt * 128:(tt + 1) * 128], in_=pT[:], func=AF.Copy),
                 r=[pfx + "pT"], w=[("hT", tt)])
            if router is not None:
                router(tt, pT, pfx + "pT")

    def ln_scratch(self, es, pfx="ln"):
        return dict(st=self.sb(es, pfx + "st", [128, 12]), mv=self.sb(es, pfx + "mv", [128, 2]),
                    sd=self.sb(es, pfx + "sd", [128, 4]), xn=self.sb(es, pfx + "xn", [128, D]),
                    pT=self.ps(es, pfx + "pT", [128, KC, 128]))

    def phase_ln0(self):
        P = self.P
        with ExitStack() as es:
            tiles = self.ln_scratch(es)
            gb = self.sb(es, "gb0", [128, 2, D])
            xt = [self.sb(es, "x%d" % i, [128, D]) for i in range(2)]
            P.dma("sp", gb[:], self.ln0.partition_broadcast(128), w=["gb0"], stream="gb0")
            for tt in range(self.NT):
                b = tt % 2
                P.dma("sp", xt[b][:], self.x[tt * 128:(tt + 1) * 128, :], w=["xt%d" % b], stream="xt%d" % b)
                self.ln_tile(tiles, xt[b], "xt%d" % b, gb, "gb0", self.h_d if self.L > 0 else self.out, tt)
            P.flush()

    def phase_inproj(self, l):
        P = self.P
        S, NB, NT = self.S, self.NB, self.NT
        w_l = self.w_in[l].rearrange("(kc p) n -> p kc n", p=128)
        with ExitStack() as es:
            wt = [self.sb(es, "wi%d" % i, [128, KC, 512], BF16) for i in range(2)]
            stg = [self.sb(es, "stg%d" % i, [128, 512]) for i in range(2)]
            stgb = [self.sb(es, "stgb%d" % i, [128, 512], BF16) for i in range(2)]
            pa = [self.ps(es, "pa%d" % i, [128, 512]) for i in range(2)]
            pb = self.ps(es, "pb", [128, 512])
            pc = self.ps(es, "pc", [128, 512])
            blk = self.sb(es, "blk", [128, 128], BF16)
            rot = self.sb(es, "rot", [128, 128], BF16)
            cos = self.sb(es, "cos", [128, S])
            sin = self.sb(es, "sin", [128, S])
            qkg = self.sb(es, "qkg", [128, 2])
            sq = self.sb(es, "sq", [128, 512], BF16)
            rs = self.sb(es, "rs", [128, 512])
            qn = self.sb(es, "qn", [128, 512], BF16)
            t1 = self.sb(es, "t1", [128, 512])
            t2 = self.sb(es, "t2", [128, 512])
            P.dma("pool", blk[:], self.c_blk, w=["blk"], stream="blk")
            P.dma("pool", rot[:], self.c_rot, w=["rot"], stream="rot")
            P.dma("sp", cos[:], self.c_cos, w=["cos"], stream="cos")
            P.dma("sp", sin[:], self.c_sin, w=["sin"], stream="sin")
            P.dma("sp", qkg[:], self.qkg[l], w=["qkg"], stream="qkg")
            cnt = {"w": 0, "o": 0, "p": 0}

            def load_w(c0, n):
                b = cnt["w"] % 2
                cnt["w"] += 1
                P.dma("pool", wt[b][:, :, 0:n], w_l[:, :, c0:c0 + n], w=["wi%d" % b], stream="wi%d" % b)
                return wt[b], "wi%d" % b

            def fm_block(c0, n, evac):
                w, wres = load_w(c0, n)
                for j in range(n // 128):
                    for tb in range(NB):
                        pp = cnt["p"] % 2
                        cnt["p"] += 1
                        self.mm_group(pa[pp][:], [(w[:, kc, j * 128:(j + 1) * 128], self.hT[:, kc, tb * 512:(tb + 1) * 512])
                                                   for kc in range(KC)],
                                      r=[wres] + [("hT", tb * 4 + i) for i in range(4)], w=["pa%d" % pp])
                        evac(pa[pp], "pa%d" % pp, c0 + j * 128, tb)

            def out_stage(bf):
                b = cnt["o"] % 2
                cnt["o"] += 1
                return (stgb[b], "stgb%d" % b) if bf else (stg[b], "stg%d" % b)

            def evac_aqk(p, pres, col, tb):
                isq = col < C_AK
                gcol = 0 if isq else 1
                tsl = slice(tb * 512, (tb + 1) * 512)
                P.op("act", lambda e: e.activation(out=sq[:], in_=p[:], func=AF.Square), r=[pres], w=["sq"])
                P.op("pe", lambda e: e.matmul(pb[:], blk[:], sq[:], start=True, stop=True), r=["blk", "sq"], w=["pb"])
                P.op("act", lambda e: e.activation(out=rs[:], in_=pb[:], func=AF.Sqrt, bias=(64e-6 if isq else 1e-6),
                                                   scale=(1.0 if isq else 1.0 / 64)), r=["pb"], w=["rs"])
                P.op("dve", lambda e: e.reciprocal(out=rs[:], in_=rs[:]), r=["rs"], w=["rs"])
                P.op("dve", lambda e: e.scalar_tensor_tensor(out=qn[:], in0=p[:], scalar=qkg[:, gcol:gcol + 1], in1=rs[:],
                                                             op0=ALU.mult, op1=ALU.mult),
                     r=[pres, "rs", "qkg"], w=["qn"])
                P.op("pe", lambda e: e.matmul(pc[:], rot[:], qn[:], start=True, stop=True), r=["rot", "qn"], w=["pc"])
                P.op("pool", lambda e: e.tensor_tensor(out=t1[:], in0=qn[:], in1=cos[:, tsl], op=ALU.mult),
                     r=["qn", "cos"], w=["t1"])
                P.op("dve", lambda e: e.tensor_tensor(out=t2[:], in0=pc[:], in1=sin[:, tsl], op=ALU.mult),
                     r=["pc", "sin"], w=["t2"])
                o, ores = out_stage(True)
                P.op("pool", lambda e: e.tensor_tensor(out=o[:], in0=t1[:], in1=t2[:], op=ALU.add),
                     r=["t1", "t2"], w=[ores])
                dst = self.qT_d[col:col + 128, tsl] if isq else self.kT_d[:, tsl]
                P.dma("sp", dst, o[:], r=[ores], w=["qkT_d"], stream=ores)

            if "A" in self.parts:
                fm_block(C_AQ, 512, evac_aqk)
                fm_block(C_AK, 128, evac_aqk)

            def evac_simple(dst_d, row0, bf, func=AF.Copy, scale=1.0):
                def ev(p, pres, col, tb):
                    o, ores = out_stage(bf)
                    P.op("act", lambda e: e.activation(out=o[:], in_=p[:], func=func, scale=scale), r=[pres], w=[ores])
                    r0 = col - row0
                    P.dma("sp", dst_d[r0:r0 + 128, tb * 512:(tb + 1) * 512], o[:], r=[ores], w=[("fm_d", id(dst_d))],
                          stream=ores)
                return ev

            if "B" in self.parts:
                fm_block(C_BQ, 256, evac_simple(self.bqT_d, C_BQ, True, scale=0.125))
                fm_block(C_BK, 256, evac_simple(self.bkT_d, C_BK, True))
            if "C" in self.parts:
                fm_block(C_CQ, 512, evac_simple(self.cT_d, C_CQ, False))
                fm_block(C_CV, 256, evac_simple(self.cT_d, C_CQ, False))
            for g in range(6):
                fm_block(C_GATE + g * 512, 512, evac_simple(self.gT_d, C_GATE, True, func=AF.Sigmoid))

            def tm_block(c0, n, dst_d, bf):
                w, wres = load_w(c0, n)
                for tt in range(NT):
                    pp = cnt["p"] % 2
                    cnt["p"] += 1
                    self.mm_group(pa[pp][:, 0:n], [(self.hT[:, kc, tt * 128:(tt + 1) * 128], w[:, kc, 0:n]) for kc in range(KC)],
                                  r=[wres, ("hT", tt)], w=["pa%d" % pp])
                    o, ores = out_stage(bf)
                    P.op("act", lambda e, o=o, pp=pp: e.activation(out=o[:, 0:n], in_=pa[pp][:, 0:n], func=AF.Copy),
                         r=["pa%d" % pp], w=[ores])
                    P.dma("sp", dst_d[tt * 128:(tt + 1) * 128, :], o[:, 0:n], r=[ores], w=[("tm_d", id(dst_d))], stream=ores)

            if "A" in self.parts:
                tm_block(C_AV, 128, self.v_d, True)
            if "B" in self.parts:
                tm_block(C_BV, 256, self.bv_d, True)
            if "C" in self.parts:
                tm_block(C_CZ, 272, self.cz_d, False)
            P.flush()

    def phase_attn_a(self, l):
        P = self.P
        S, NB, NT = self.S, self.NB, self.NT
        with ExitStack() as es:
            qh = [self.sb(es, "qh%d" % i, [64, S], BF16) for i in range(2)]
            kT = self.sb(es, "kT", [64, 2, S], BF16)
            vx = self.sb(es, "vx", [128, NT, 2, 65], BF16)
            pT = [self.sb(es, "pT%d" % i, [128, 512], BF16) for i in range(3)]
            onesr = self.sb(es, "onesr", [128, 64])
            rc = self.sb(es, "rc", [128, 512])
            bcs = self.sb(es, "bcs", [64, 512])
            ya = [self.sb(es, "ya%d" % i, [64, 512], BF16) for i in range(2)]
            sps = [self.ps(es, "sps%d" % i, [128, 512]) for i in range(3)]
            ops_ = [self.ps(es, "ops%d" % i, [128, 512]) for i in range(2)]
            bps = self.ps(es, "bps", [64, 512])
            P.dma("sp", kT[:], self.kT_d.rearrange("(g d) s -> d g s", d=64), r=["qkT_d"], w=["kT"], stream="kT")
            P.op("pool", lambda e: e.memset(vx[:], 1.0), w=["vx"])
            P.op("pool", lambda e: e.memset(onesr[:], 1.0), w=["onesr"])
            for g in range(2):
                P.dma("sp", vx[:, :, g, 0:64], self.v_d[:, g * 64:(g + 1) * 64].rearrange("(t p) d -> p t d", p=128),
                      r=[("tm_d", id(self.v_d))], w=["vx"], stream="vx")
            it = 0
            for hq in range(8):
                g = hq // 4
                qb_ = hq % 2
                P.dma("sp", qh[qb_][:], self.qT_d[hq * 64:(hq + 1) * 64, :], r=["qkT_d"], w=["qh%d" % qb_], stream="qh%d" % qb_)
                for qb in range(NB):
                    ob = (hq * NB + qb) % 2
                    for kt in range(NT):
                        b = it % 3
                        it += 1
                        P.op("pe", lambda e, b=b, kt=kt, g=g, qb_=qb_, qb=qb: e.matmul(
                            sps[b][:], kT[:, g, kt * 128:(kt + 1) * 128], qh[qb_][:, qb * 512:(qb + 1) * 512], start=True, stop=True),
                             r=["kT", "qh%d" % qb_], w=["sps%d" % b])
                        P.op("act", lambda e, b=b: e.activation(out=pT[b][:], in_=sps[b][:], func=AF.Exp),
                             r=["sps%d" % b], w=["pT%d" % b])
                        P.op("pe", lambda e, b=b, kt=kt, g=g, ob=ob: e.matmul(ops_[ob][0:65, :], vx[:, kt, g, :], pT[b][:],
                                                                  start=(kt == 0), stop=(kt == NT - 1)),
                             r=["vx", "pT%d" % b], w=["ops%d" % ob], inc=(kt == NT - 1))
                    P.op("dve", lambda e, ob=ob: e.reciprocal(out=rc[64:65, :], in_=ops_[ob][64:65, :]), r=["ops%d" % ob], w=["rc"])
                    P.op("pe", lambda e: e.matmul(bps[:], onesr[64:65, :], rc[64:65, :], start=True, stop=True),
                         r=["onesr", "rc"], w=["bps"])
                    P.op("act", lambda e: e.activation(out=bcs[:], in_=bps[:], func=AF.Copy), r=["bps"], w=["bcs"])
                    P.op("dve", lambda e, ob=ob: e.tensor_tensor(out=ya[ob][:], in0=ops_[ob][0:64, :], in1=bcs[:], op=ALU.mult),
                         r=["ops%d" % ob, "bcs"], w=["ya%d" % ob])
                    P.dma("sp", self.yaT_d[hq * 64:(hq + 1) * 64, qb * 512:(qb + 1) * 512], ya[ob][:], r=["ya%d" % ob],
                          w=["yaT_d"], stream="ya%d" % ob)
            P.flush()

    def phase_attn_b(self, l):
        P = self.P
        S, NT = self.S, self.NT
        rows = S // GRID_W
        wr_ = min(8, rows)
        with ExitStack() as es:
            qT = self.sb(es, "bqT", [64, 4, S], BF16)
            kT = self.sb(es, "bkT", [64, 4, S], BF16)
            v0 = self.sb(es, "bv0", [128, NT, 256], BF16)
            v1 = self.sb(es, "bv1", [128, NT, 256], BF16)
            bias = self.sb(es, "bbias", [64, 4, 960])
            sc = [self.sb(es, "bsc%d" % i, [64, 512]) for i in range(2)]
            pr = [self.sb(es, "bpr%d" % i, [64, 512], BF16) for i in range(2)]
            pn = [self.sb(es, "bpn%d" % i, [64, 512], BF16) for i in range(2)]
            st = [self.sb(es, "bst%d" % i, [64, 4]) for i in range(2)]
            pts = [self.sb(es, "bpts%d" % i, [128, 4, 64], BF16) for i in range(2)]
            yo = [self.sb(es, "byo%d" % i, [64, 4, 64], BF16) for i in range(2)]
            sp_ = [self.ps(es, "bsp%d" % i, [64, 512]) for i in range(2)]
            ptp = [self.ps(es, "bptp%d" % i, [128, 4, 64], BF16) for i in range(2)]
            op_ = [self.ps(es, "bop%d" % i, [64, 4, 64]) for i in range(2)]
            for h in range(4):
                P.dma("sp", qT[:, h, :], self.bqT_d[h * 64:(h + 1) * 64, :], r=[("fm_d", id(self.bqT_d))], w=["bqT"], stream="bqT")
                P.dma("sp", kT[:, h, :], self.bkT_d[h * 64:(h + 1) * 64, :], r=[("fm_d", id(self.bkT_d))], w=["bkT"], stream="bkT")
            P.dma("sp", v0[:], self.bv_d.rearrange("(t p) c -> p t c", p=128), r=[("tm_d", id(self.bv_d))], w=["bv0"], stream="bv0")
            P.dma("sp", v1[:, 0:NT - 1, :], self.bv_d[64:S - 64, :].rearrange("(t p) c -> p t c", p=128), r=[("tm_d", id(self.bv_d))],
                  w=["bv1"], stream="bv1")
            P.dma("sp", bias[:], self.bias_b[l].rearrange("h q k -> q h k"), w=["bbias"], stream="bbias")
            it = 0
            for r in range(rows):
                r0 = min(max(r - wr_ // 2, 0), rows - wr_)
                d0 = (r0 - r + 7) * 64
                k0 = r0 * 64
                ob = r % 2
                for h in range(4):
                    b = it % 2
                    it += 1
                    P.op("pe", lambda e, b=b, h=h, r=r, k0=k0: e.matmul(sp_[b][:], qT[:, h, r * 64:(r + 1) * 64], kT[:, h, k0:k0 + 512],
                                                                         start=True, stop=True), r=["bqT", "bkT"], w=["bsp%d" % b])
                    P.op("dve", lambda e, b=b, h=h, d0=d0: e.tensor_tensor(out=sc[b][:], in0=sp_[b][:], in1=bias[:, h, d0:d0 + 512], op=ALU.add),
                         r=["bsp%d" % b, "bbias"], w=["bsc%d" % b])
                    P.op("dve", lambda e, b=b: e.tensor_reduce(out=st[b][:, 0:1], in_=sc[b][:], axis=AX.X, op=ALU.max),
                         r=["bsc%d" % b], w=[("bst", b, 0)])
                    P.op("dve", lambda e, b=b: e.tensor_scalar(out=st[b][:, 1:2], in0=st[b][:, 0:1], scalar1=-1.0, scalar2=None, op0=ALU.mult),
                         r=[("bst", b, 0)], w=[("bst", b, 1)])
                    P.op("act", lambda e, b=b: e.activation(out=pr[b][:], in_=sc[b][:], func=AF.Exp, bias=st[b][:, 1:2], scale=1.0,
                                                            accum_out=st[b][:, 2:3]), r=["bsc%d" % b, ("bst", b, 1)], w=["bpr%d" % b, ("bst", b, 2)])
                    P.op("dve", lambda e, b=b: e.reciprocal(out=st[b][:, 3:4], in_=st[b][:, 2:3]), r=[("bst", b, 2)], w=[("bst", b, 3)])
                    P.op("pool", lambda e, b=b: e.tensor_scalar(out=pn[b][:], in0=pr[b][:], scalar1=st[b][:, 3:4], scalar2=None, op0=ALU.mult),
                         r=["bpr%d" % b, ("bst", b, 3)], w=["bpn%d" % b])
                    for kc in range(4):
                        P.op("pe", lambda e, b=b, kc=kc: e.transpose(ptp[b][:, kc, :], pn[b][:, kc * 128:(kc + 1) * 128], self.identb[0:64, 0:64]),
                             r=["bpn%d" % b, "identb"], w=["bptp%d" % b], inc=(kc == 3))
                    P.op("act", lambda e, b=b: e.activation(out=pts[b][:], in_=ptp[b][:], func=AF.Copy), r=["bptp%d" % b], w=["bpts%d" % b])
                    vsrc, vres, t0 = (v0, "bv0", r0 // 2) if r0 % 2 == 0 else (v1, "bv1", (r0 - 1) // 2)
                    for kc in range(4):
                        P.op("pe", lambda e, b=b, kc=kc, h=h, ob=ob, vsrc=vsrc, t0=t0: e.matmul(
                            op_[ob][:, h, :], vsrc[:, t0 + kc, h * 64:(h + 1) * 64], pts[b][:, kc, :], start=(kc == 0), stop=(kc == 3)),
                             r=[vres, "bpts%d" % b], w=["bop%d" % ob], inc=(kc == 3))
                P.op("act", lambda e, ob=ob: e.activation(out=yo[ob][:], in_=op_[ob][:], func=AF.Copy), r=["bop%d" % ob], w=["byo%d" % ob])
                P.dma("sp", self.ybT_d[:, r * 64:(r + 1) * 64].rearrange("(h d) q -> d h q", d=64), yo[ob][:], r=["byo%d" % ob],
                      w=["ybT_d"], stream="byo%d" % ob)
            P.flush()

    def phase_gdn(self, l):
        P = self.P
        S, NT, NB = self.S, self.NT, self.NB
        nc = self.nc
        if not hasattr(self, "cn_d"):
            mk = lambda n, s: nc.dram_tensor(n, list(s), F32, kind=("ExternalOutput" if n in self.dbg else "Internal")).ap()
            self.cn_d = mk("cn_d", [768, S])
            self.ktok_d = mk("ktok_d", [S, 256])
            self.vtok_d = mk("vtok_d", [S, 256])
            self.gates_d = mk("gates_d", [S, 16])
            self.o_d = mk("o_d", [2, S, 256])
        with ExitStack() as es:
            cw = self.sb(es, "cw", [128, 6, 5])
            x = self.sb(es, "gx", [128, S + 4])
            y = self.sb(es, "gy", [128, S])
            sq = self.sb(es, "gsq", [128, 512], BF16)
            rs = self.sb(es, "grs", [128, 512])
            blk = self.sb(es, "gblk", [128, 128], BF16)
            tk = [self.sb(es, "gtk%d" % i, [128, 512]) for i in range(2)]
            gin = self.sb(es, "gin", [128, NT, 16])
            gout = self.sb(es, "gout", [128, NT, 16])
            ab = self.sb(es, "gab", [128, 2, 8])
            pss = self.ps(es, "gpss", [128, 512])
            ptr = [self.ps(es, "gptr%d" % i, [128, 4, 128]) for i in range(2)]
            P.dma("sp", cw[:], self.conv_w[l], w=["cw"], stream="cw")
            P.dma("pool", blk[:], self.c_blk, w=["gblk"], stream="gblk")
            P.dma("sp", ab[:], self.gdn_ab[l].partition_broadcast(128), w=["gab"], stream="gab")
            P.op("pool", lambda e: e.memset(x[:, 0:2], 0.0), w=["gxp"])
            P.op("pool", lambda e: e.memset(x[:, S + 2:S + 4], 0.0), w=["gxp"])
            ti = 0
            import os
            for ch in range(6 if os.environ.get("GDBG", "") != "gates" else 0):
                P.dma("sp", x[:, 2:S + 2], self.cT_d[ch * 128:(ch + 1) * 128, :], r=[("fm_d", id(self.cT_d))], w=["gx"], stream="gx")
                P.op("act", lambda e, ch=ch: e.activation(out=y[:], in_=x[:, 0:S], func=AF.Identity, scale=cw[:, ch, 0:1]),
                     r=["gx", "gxp", "cw"], w=["gy"])
                for k in range(1, 5):
                    P.op("dve", lambda e, ch=ch, k=k: e.scalar_tensor_tensor(out=y[:], in0=x[:, k:k + S], scalar=cw[:, ch, k:k + 1], in1=y[:],
                                                                             op0=ALU.mult, op1=ALU.add), r=["gx", "gxp", "cw", "gy"], w=["gy"])
                P.op("act", lambda e: e.activation(out=y[:], in_=y[:], func=AF.Silu), r=["gy"], w=["gy"])
                if ch < 4:
                    isq = ch < 2
                    for tb in range(NB):
                        tsl = slice(tb * 512, (tb + 1) * 512)
                        P.op("act", lambda e, tsl=tsl: e.activation(out=sq[:], in_=y[:, tsl], func=AF.Square), r=["gy"], w=["gsq"])
                        P.op("pe", lambda e: e.matmul(pss[:], blk[:], sq[:], start=True, stop=True), r=["gblk", "gsq"], w=["gpss"])
                        P.op("act", lambda e, isq=isq: e.activation(out=rs[:], in_=pss[:], func=AF.Sqrt, bias=(64e-6 if isq else 1e-6),
                                                                    scale=(64.0 if isq else 1.0)), r=["gpss"], w=["grs"])
                        P.op("dve", lambda e: e.reciprocal(out=rs[:], in_=rs[:]), r=["grs"], w=["grs"])
                        P.op("dve", lambda e, tsl=tsl: e.tensor_tensor(out=y[:, tsl], in0=y[:, tsl], in1=rs[:], op=ALU.mult),
                             r=["gy", "grs"], w=["gy"])
                P.dma("sp", self.cn_d[ch * 128:(ch + 1) * 128, :], y[:], r=["gy"], w=["cn_d"], stream="gy")
                if ch >= 2:
                    dst = self.ktok_d if ch < 4 else self.vtok_d
                    for tb in range(NB):
                        b = ti % 2
                        ti += 1
                        for j in range(4):
                            tt = tb * 4 + j
                            P.op("pe", lambda e, b=b, j=j, tt=tt: e.transpose(ptr[b][:, j, :], y[:, tt * 128:(tt + 1) * 128], self.ident[:]),
                                 r=["gy", "ident"], w=["gptr%d" % b], inc=(j == 3))
                        P.op("act", lambda e, b=b: e.activation(out=tk[b][:], in_=ptr[b][:].rearrange("p a b -> p (a b)"), func=AF.Copy),
                             r=["gptr%d" % b], w=["gtk%d" % b])
                        P.dma("sp", dst[tb * 512:(tb + 1) * 512, (ch % 2) * 128:(ch % 2 + 1) * 128].rearrange("(j p) c -> p j c", p=128),
                              tk[b][:].rearrange("p (j c) -> p j c", c=128), r=["gtk%d" % b], w=["kvtok_d"], stream="gtk%d" % b)
            if os.environ.get("GDBG", "") == "conv":
                P.flush()
                return
            P.dma("sp", gin[:], self.cz_d[:, 256:272].rearrange("(t p) c -> p t c", p=128), r=[("tm_d", id(self.cz_d))], w=["gin"], stream="gin")
            P.op("act", lambda e: e.activation(out=gout[:, :, 0:8], in_=gin[:, :, 0:8], func=AF.Sigmoid), r=["gin"], w=["gout_b"])
            P.op("dve", lambda e: e.tensor_tensor(out=gin[:, :, 8:16], in0=gin[:, :, 8:16], in1=bc(ab[:, 1:2, :], [128, NT, 8]), op=ALU.add),
                 r=["gin", "gab"], w=["gin2"])
            P.op("act", lambda e: e.activation(out=gin[:, :, 8:16], in_=gin[:, :, 8:16], func=AF.Exp), r=["gin2"], w=["gin2"])
            P.op("act", lambda e: e.activation(out=gin[:, :, 8:16], in_=gin[:, :, 8:16], func=AF.Ln, bias=1.0, scale=1.0), r=["gin2"], w=["gin2"])
            P.op("act", lambda e: e.activation(out=ab[:, 0, :], in_=ab[:, 0, :], func=AF.Exp), r=["gab"], w=["gab0"])
            P.op("dve", lambda e: e.scalar_tensor_tensor(out=gout[:, :, 8:16], in0=gin[:, :, 8:16], scalar=-1.0, in1=bc(ab[:, 0:1, :], [128, NT, 8]),
                                                         op0=ALU.mult, op1=ALU.mult), r=["gin2", "gab0"], w=["gout_g"])
            P.dma("sp", self.gates_d.rearrange("(t p) c -> p t c", p=128), gout[:], r=["gout_b", "gout_g"], w=["gates_d"], stream="gout")
            P.flush()
        if getattr(self, "gdn_stop", None) == "prep":
            return
        NC = S // 64
        with ExitStack() as es:
            cst = self.sb(es, "gcst", [64, 6, 8, 64])
            NEGM, NEGMT, STRICT, ID8 = cst[:, 0], cst[:, 1], cst[:, 2], cst[:, 3]
            CUMF, CUMB, ONES = cst[:, 4, 0, :], cst[:, 4, 1, :], cst[:, 5, 0, :]
            ld = [dict(KT=self.sb(es, "gKT%d" % i, [64, 8, 64]), QT=self.sb(es, "gQT%d" % i, [64, 8, 64]),
                       Kt=self.sb(es, "gKt%d" % i, [64, 8, 64]), Vt=self.sb(es, "gVt%d" % i, [64, 8, 64]),
                       gb=self.sb(es, "ggb%d" % i, [64, 16])) for i in range(2)]
            T8 = lambda n: self.sb(es, n, [64, 8, 64])
            sm = self.sb(es, "gsm", [64, 48])
            diagG, Dm, DTm, eGr, t_a, t_b, SBm, qkTm, QgT = (T8(n) for n in ("gdiag", "gD", "gDT", "geGr", "gta", "gtb", "gSB", "gqkTm", "gQgT"))
            X = [T8("gX0"), T8("gX1")]
            XT = [T8("gXT0"), T8("gXT1")]
            PT, rv, rk, U, WT, Vn, Kd, St, ot = (T8(n) for n in ("gPT", "grv", "grk", "gU", "gWT", "gVn", "gKd", "gS", "gO"))
            psA = self.ps(es, "gpsA", [64, 16])
            pb = [self.ps(es, "gpb%d" % i, [64, 8, 64]) for i in range(1, 8)]
            psGrow, ps2, ps3, ps4, ps5, ps6, ps7 = pb
            fl = lambda t: t[:].rearrange("p c j -> p (c j)")
            P.dma("sp", cst[:], self.c_gdn, w=["gcst"], stream="gcst")
            P.op("pool", lambda e: e.memset(St[:], 0.0), w=["gS"])

            def mm8(ps, psres, lhs, lres, rhs, rres):
                for c in range(8):
                    P.op("pe", lambda e, c=c: e.matmul(ps[:, c, :], lhs[:, c, :], rhs[:, c, :], start=True, stop=True),
                         r=[lres, rres], w=[psres], inc=(c == 7))

            def bcg(col0):
                return lambda t: bc(t[:, col0:col0 + 8].unsqueeze(2), [64, 8, 64])

            _lim = int(os.environ.get('GLIM', '99'))
            for s_ in range(NC):
                a, b = s_, NC - 1 - s_
                L_ = ld[s_ % 2]
                sfx = str(s_ % 2)
                ra, rb_ = slice(a * 64, (a + 1) * 64), slice(b * 64, (b + 1) * 64)
                for nm, src, r0 in (("KT", self.cn_d, 256), ("QT", self.cn_d, 0)):
                    for half, rr in ((0, ra), (1, rb_)):
                        P.dma("sp", L_[nm][:, half * 4:(half + 1) * 4, :], src[r0:r0 + 256, rr].rearrange("(h d) t -> d h t", d=64),
                              r=["cn_d"], w=["g" + nm + sfx], stream="g" + nm + sfx)
                for nm, src in (("Kt", self.ktok_d), ("Vt", self.vtok_d)):
                    for half, rr in ((0, ra), (1, rb_)):
                        P.dma("sp", L_[nm][:, half * 4:(half + 1) * 4, :], src[rr, :].rearrange("t (h d) -> t h d", d=64),
                              r=["kvtok_d"], w=["g" + nm + sfx], stream="g" + nm + sfx)
                for c0, rr in ((0, ra), (4, rb_), (8, ra), (12, rb_)):
                    P.dma("sp", L_["gb"][:, c0:c0 + 4], self.gates_d[rr, c0:c0 + 4], r=["gates_d"], w=["ggb" + sfx], stream="ggb" + sfx)
                KT, QT, Kt, Vt, gb = L_["KT"], L_["QT"], L_["Kt"], L_["Vt"], L_["gb"]
                rKT, rQT, rKt, rVt, rgb = ("g" + n + sfx for n in ("KT", "QT", "Kt", "Vt", "gb"))
                P.op("pe", lambda e, gb=gb: e.matmul(psA[:, 0:4], CUMF, gb[:, 8:12], start=True, stop=True), r=["gcst", rgb], w=["gpsA"], inc=False)
                P.op("pe", lambda e, gb=gb: e.matmul(psA[:, 4:8], CUMB, gb[:, 12:16], start=True, stop=True), r=["gcst", rgb], w=["gpsA"], inc=False)
                P.op("pe", lambda e, gb=gb: e.matmul(psA[:, 8:16], ONES, gb[:, 8:16], start=True, stop=True), r=["gcst", rgb], w=["gpsA"])
                P.op("act", lambda e: e.activation(out=sm[:, 0:16], in_=psA[:], func=AF.Copy), r=["gpsA"], w=["gsmG"])
                P.op("act", lambda e: e.activation(out=sm[:, 16:32], in_=sm[:, 0:16], func=AF.Exp), r=["gsmG"], w=["gsmE"])
                P.op("dve", lambda e: e.tensor_tensor(out=sm[:, 32:40], in0=sm[:, 8:16], in1=sm[:, 0:8], op=ALU.subtract), r=["gsmG"], w=["gsmK"])
                P.op("act", lambda e: e.activation(out=sm[:, 32:40], in_=sm[:, 32:40], func=AF.Exp), r=["gsmK"], w=["gsmK"])
                P.op("dve", lambda e, gb=gb: e.tensor_tensor(out=sm[:, 40:48], in0=gb[:, 0:8], in1=sm[:, 16:24], op=ALU.mult), r=[rgb, "gsmE"], w=["gsmB"])
                Gb, eGtb, kdb, bkb = bcg(0)(sm), bcg(24)(sm), bcg(32)(sm), bcg(40)(sm)
                betab = bc(gb[:, 0:8].unsqueeze(2), [64, 8, 64])
                if _lim < 2:
                    continue
                P.op("pool", lambda e, Gb=Gb: e.tensor_tensor(out=diagG[:], in0=ID8, in1=Gb, op=ALU.mult), r=["gcst", "gsmG"], w=["gdiag"])
                P.op("pe", lambda e: e.matmul(fl(psGrow), ONES, fl(diagG), start=True, stop=True), r=["gcst", "gdiag"], w=["gpsGrow"])
                P.op("dve", lambda e: e.scalar_tensor_tensor(out=t_a[:], in0=psGrow[:], scalar=-1.0, in1=NEGM, op0=ALU.mult, op1=ALU.add),
                     r=["gpsGrow", "gcst"], w=["gta"])
                P.op("dve", lambda e, Gb=Gb: e.tensor_tensor(out=t_a[:], in0=t_a[:], in1=Gb, op=ALU.add), r=["gta", "gsmG"], w=["gta"])
                P.op("act", lambda e: e.activation(out=Dm[:], in_=t_a[:], func=AF.Exp), r=["gta"], w=["gD"])
                P.op("dve", lambda e: e.tensor_tensor(out=t_b[:], in0=psGrow[:], in1=NEGMT, op=ALU.add), r=["gpsGrow", "gcst"], w=["gtb"])
                P.op("dve", lambda e, Gb=Gb: e.tensor_tensor(out=t_b[:], in0=t_b[:], in1=Gb, op=ALU.subtract), r=["gtb", "gsmG"], w=["gtb"])
                P.op("act", lambda e: e.activation(out=DTm[:], in_=t_b[:], func=AF.Exp), r=["gtb"], w=["gDT"])
                P.op("act", lambda e: e.activation(out=eGr[:], in_=psGrow[:], func=AF.Exp), r=["gpsGrow"], w=["geGr"])
                P.op("pool", lambda e, QT=QT: e.tensor_tensor(out=QgT[:], in0=QT[:], in1=eGr[:], op=ALU.mult), r=[rQT, "geGr"], w=["gQgT"])
                if _lim < 4:
                    continue
                mm8(ps2, "gps2", KT, rKT, KT, rKT)
                mm8(ps3, "gps3", KT, rKT, QT, rQT)
                P.op("pool", lambda e, betab=betab: e.tensor_tensor(out=SBm[:], in0=STRICT, in1=betab, op=ALU.mult), r=["gcst", rgb], w=["gSB"])
                P.op("dve", lambda e: e.tensor_tensor(out=t_a[:], in0=ps2[:], in1=Dm[:], op=ALU.mult), r=["gps2", "gD"], w=["gta"])
                P.op("pool", lambda e: e.tensor_tensor(out=X[0][:], in0=t_a[:], in1=SBm[:], op=ALU.mult), r=["gta", "gSB"], w=["gX0"])
                P.op("dve", lambda e: e.tensor_tensor(out=qkTm[:], in0=ps3[:], in1=DTm[:], op=ALU.mult), r=["gps3", "gDT"], w=["gqkTm"])
                if _lim < 5:
                    continue
                for c in range(8):
                    P.op("pe", lambda e, c=c: e.matmul(ps2[:, c, :], X[0][:, c, :], cst[:, 3, c, :], start=True, stop=True), r=["gX0", "gcst"],
                         w=["gps2"], inc=(c == 7))
                P.op("act", lambda e: e.activation(out=XT[0][:], in_=ps2[:], func=AF.Copy), r=["gps2"], w=["gXT0"])
                P.op("dve", lambda e: e.scalar_tensor_tensor(out=PT[:], in0=ps2[:], scalar=-1.0, in1=ID8, op0=ALU.mult, op1=ALU.add),
                     r=["gps2", "gcst", "gXT0"], w=["gPT"])
                if _lim < 6:
                    continue
                for lv in range(1, 6):
                    ci, ni = (lv - 1) % 2, lv % 2
                    mm8(ps4, "gps4", XT[ci], "gXT%d" % ci, X[ci], "gX%d" % ci)
                    if lv < 5:
                        mm8(ps5, "gps5", X[ci], "gX%d" % ci, XT[ci], "gXT%d" % ci)
                    P.op("act", lambda e, ni=ni: e.activation(out=X[ni][:], in_=ps4[:], func=AF.Copy), r=["gps4"], w=["gX%d" % ni])
                    if lv < 5:
                        P.op("dve", lambda e, ni=ni: e.tensor_copy(out=XT[ni][:], in_=ps5[:]), r=["gps5"], w=["gXT%d" % ni])
                    mm8(ps6, "gps6", X[ni], "gX%d" % ni, PT, "gPT")
                    P.op("dve", lambda e: e.tensor_tensor(out=PT[:], in0=PT[:], in1=ps6[:], op=ALU.add), r=["gPT", "gps6"], w=["gPT"])
                if _lim < 7:
                    continue
                P.op("pool", lambda e, Vt=Vt, betab=betab: e.tensor_tensor(out=rv[:], in0=Vt[:], in1=betab, op=ALU.mult), r=[rVt, rgb], w=["grv"])
                P.op("pool", lambda e, Kt=Kt, bkb=bkb: e.tensor_tensor(out=rk[:], in0=Kt[:], in1=bkb, op=ALU.mult), r=[rKt, "gsmB"], w=["grk"])
                mm8(ps4, "gps4", PT, "gPT", rv, "grv")
                P.op("act", lambda e: e.activation(out=U[:], in_=ps4[:], func=AF.Copy), r=["gps4"], w=["gU"])
                mm8(ps5, "gps5", rk, "grk", PT, "gPT")
                P.op("dve", lambda e: e.tensor_copy(out=WT[:], in_=ps5[:]), r=["gps5"], w=["gWT"])
                if _lim < 8:
                    continue
                mm8(ps6, "gps6", WT, "gWT", St, "gS")
                P.op("dve", lambda e: e.scalar_tensor_tensor(out=Vn[:], in0=ps6[:], scalar=-1.0, in1=U[:], op0=ALU.mult, op1=ALU.add),
                     r=["gps6", "gU"], w=["gVn"])
                for c in range(8):
                    P.op("pe", lambda e, c=c: e.matmul(ps7[:, c, :], QgT[:, c, :], St[:, c, :], start=True, stop=False), r=["gQgT", "gS"], w=["gps7"],
                         inc=False)
                    P.op("pe", lambda e, c=c: e.matmul(ps7[:, c, :], qkTm[:, c, :], Vn[:, c, :], start=False, stop=True), r=["gqkTm", "gVn"],
                         w=["gps7"], inc=(c == 7))
                P.op("act", lambda e: e.activation(out=ot[:], in_=ps7[:], func=AF.Copy), r=["gps7"], w=["gO"])
                P.dma("sp", self.o_d[0, ra, :].rearrange("t (h d) -> t h d", d=64), ot[:, 0:4, :], r=["gO"], w=["o_d"], stream="gO")
                P.dma("sp", self.o_d[1, rb_, :].rearrange("t (h d) -> t h d", d=64), ot[:, 4:8, :], r=["gO"], w=["o_d"], stream="gO")
                P.op("pool", lambda e, Kt=Kt, kdb=kdb: e.tensor_tensor(out=Kd[:], in0=Kt[:], in1=kdb, op=ALU.mult), r=[rKt, "gsmK"], w=["gKd"])
                mm8(ps3, "gps3", Kd, "gKd", Vn, "gVn")
                P.op("pool", lambda e, eGtb=eGtb: e.tensor_tensor(out=St[:], in0=St[:], in1=eGtb, op=ALU.mult), r=["gS", "gsmE"], w=["gS"])
                P.op("dve", lambda e: e.tensor_tensor(out=St[:], in0=St[:], in1=ps3[:], op=ALU.add), r=["gS", "gps3"], w=["gS"])
            P.flush()
        if getattr(self, "gdn_stop", None) == "main":
            return
        with ExitStack() as es:
            gg = self.sb(es, "ggn", [128, 64])
            of = [self.sb(es, "gof%d" % i, [128, 4, 64]) for i in range(2)]
            obk = [self.sb(es, "gob%d" % i, [128, 4, 64]) for i in range(2)]
            zt = [self.sb(es, "gz%d" % i, [128, 4, 64]) for i in range(2)]
            sq2 = self.sb(es, "gsq2", [128, 4, 64])
            ms = self.sb(es, "gms", [128, 8])
            yb_ = [self.sb(es, "gyb%d" % i, [128, 2, 128], BF16) for i in range(2)]
            pt2 = [self.ps(es, "gpt2%d" % i, [128, 2, 128]) for i in range(2)]
            P.dma("sp", gg[:], self.gdn_g[l].partition_broadcast(128), w=["ggn"], stream="ggn")
            for tt in range(NT):
                b = tt % 2
                tr = slice(tt * 128, (tt + 1) * 128)
                P.dma("sp", of[b][:], self.o_d[0, tr, :].rearrange("t (h d) -> t h d", d=64), r=["o_d"], w=["gof%d" % b], stream="gof%d" % b)
                P.dma("sp", obk[b][:], self.o_d[1, tr, :].rearrange("t (h d) -> t h d", d=64), r=["o_d"], w=["gob%d" % b], stream="gob%d" % b)
                P.dma("sp", zt[b][:], self.cz_d[tr, 0:256].rearrange("t (h d) -> t h d", d=64), r=[("tm_d", id(self.cz_d))], w=["gz%d" % b],
                      stream="gz%d" % b)
                P.op("dve", lambda e, b=b: e.tensor_tensor(out=of[b][:], in0=of[b][:], in1=obk[b][:], op=ALU.add), r=["gof%d" % b, "gob%d" % b],
                     w=["gof%d" % b])
                P.op("act", lambda e, b=b: e.activation(out=sq2[:], in_=of[b][:], func=AF.Square), r=["gof%d" % b], w=["gsq2"])
                P.op("dve", lambda e: e.tensor_reduce(out=ms[:, 0:4], in_=sq2[:], axis=AX.X, op=ALU.add), r=["gsq2"], w=["gms"])
                P.op("act", lambda e: e.activation(out=ms[:, 4:8], in_=ms[:, 0:4], func=AF.Sqrt, bias=1e-6, scale=1.0 / 64), r=["gms"], w=["gms2"])
                P.op("dve", lambda e: e.reciprocal(out=ms[:, 4:8], in_=ms[:, 4:8]), r=["gms2"], w=["gms2"])
                P.op("dve", lambda e, b=b: e.tensor_tensor(out=of[b][:], in0=of[b][:], in1=bc(ms[:, 4:8].unsqueeze(2), [128, 4, 64]), op=ALU.mult),
                     r=["gof%d" % b, "gms2"], w=["gof%d" % b])
                P.op("pool", lambda e, b=b: e.tensor_tensor(out=of[b][:], in0=of[b][:], in1=bc(gg[:].unsqueeze(1), [128, 4, 64]), op=ALU.mult),
                     r=["gof%d" % b, "ggn"], w=["gof%d" % b])
                P.op("act", lambda e, b=b: e.activation(out=zt[b][:], in_=zt[b][:], func=AF.Silu), r=["gz%d" % b], w=["gz%d" % b])
                P.op("dve", lambda e, b=b: e.tensor_tensor(out=of[b][:], in0=of[b][:], in1=zt[b][:], op=ALU.mult), r=["gof%d" % b, "gz%d" % b],
                     w=["gof%d" % b])
                for j in range(2):
                    P.op("pe", lambda e, b=b, j=j: e.transpose(pt2[b][:, j, :], of[b][:, 2 * j:2 * j + 2, :].rearrange("p a d -> p (a d)"), self.ident[:]),
                         r=["gof%d" % b, "ident"], w=["gpt2%d" % b], inc=(j == 1))
                P.op("act", lambda e, b=b: e.activation(out=yb_[b][:], in_=pt2[b][:], func=AF.Copy), r=["gpt2%d" % b], w=["gyb%d" % b])
                P.dma("sp", self.ycT_d[:, tr].rearrange("(j p) t -> p j t", p=128), yb_[b][:], r=["gyb%d" % b], w=["ycT_d"], stream="gyb%d" % b)
            P.flush()

    def phase_merge(self, l):
        P = self.P
        S, NB, NT = self.S, self.NB, self.NT
        with ExitStack() as es:
            wa = self.sb(es, "wa", [128, 4, D], BF16)
            wb = self.sb(es, "wb", [128, 2, D], BF16)
            wc = self.sb(es, "wc", [128, 2, D], BF16)
            wo = self.sb(es, "wo", [128, 8, D], BF16)
            yT = self.sb(es, "yT", [128, 8, 512], BF16)
            gt = [self.sb(es, "gt%d" % i, [128, 3, 512], BF16) for i in range(2)]
            mixT = self.sb(es, "mixT", [128, 8, 512], BF16)
            m1 = self.sb(es, "m1", [128, 512])
            m2 = self.sb(es, "m2", [128, 512])
            m3 = self.sb(es, "m3", [128, 512])
            hold = [self.sb(es, "hold%d" % i, [128, D]) for i in range(2)]
            tsum = [self.sb(es, "tsum%d" % i, [128, D]) for i in range(2)]
            gb = self.sb(es, "gb1", [128, 2, D])
            pabc = [self.ps(es, "pabc%d" % i, [128, 512]) for i in range(3)]
            po = [self.ps(es, "po%d" % i, [128, 512]) for i in range(2)]
            tiles = self.ln_scratch(es, "l1")
            rt = self.router_setup(es) if not getattr(self, "no_router", False) else None
            P.dma("pool", wa[:], self.wa[l].rearrange("(kc p) n -> p kc n", p=128), w=["wa"], stream="wa")
            P.dma("pool", wb[:], self.wb[l].rearrange("(kc p) n -> p kc n", p=128), w=["wb"], stream="wb")
            P.dma("pool", wc[:], self.wc[l].rearrange("(kc p) n -> p kc n", p=128), w=["wc"], stream="wc")
            P.dma("pool", wo[:], self.wo[l].rearrange("(kc p) n -> p kc n", p=128), w=["wo"], stream="wo")
            P.dma("sp", gb[:], self.ln1[l].partition_broadcast(128), w=["gb1"], stream="gb1")
            if "B" not in self.parts:
                P.op("pool", lambda e: e.memset(yT[:, 4:6, :], 0.0), w=["yTb"])
            if "C" not in self.parts:
                P.op("pool", lambda e: e.memset(yT[:, 6:8, :], 0.0), w=["yTc"])
            for tb in range(NB):
                tsl = slice(tb * 512, (tb + 1) * 512)
                if "A" in self.parts:
                    P.dma("sp", yT[:, 0:4, :], self.yaT_d[:, tsl].rearrange("(kc p) s -> p kc s", p=128), r=["yaT_d"], w=["yTa"],
                          stream="yTa")
                else:
                    P.op("pool", lambda e: e.memset(yT[:, 0:4, :], 0.0), w=["yTa"])
                if "B" in self.parts:
                    P.dma("sp", yT[:, 4:6, :], self.ybT_d[:, tsl].rearrange("(kc p) s -> p kc s", p=128), r=["ybT_d"], w=["yTb"],
                          stream="yTb")
                if "C" in self.parts:
                    P.dma("sp", yT[:, 6:8, :], self.ycT_d[:, tsl].rearrange("(kc p) s -> p kc s", p=128), r=["ycT_d"], w=["yTc"],
                          stream="yTc")
                for dc in range(8):
                    b = dc % 2
                    P.dma("sp", gt[b][:], self.gT_d[:, tsl].rearrange("(t c p) s -> p t c s", p=128, t=3)[:, :, dc, :],
                          r=[("fm_d", id(self.gT_d))], w=["gt%d" % b], stream="gt%d" % b)
                    csl = slice(dc * 128, (dc + 1) * 128)
                    self.mm_group(pabc[0][:], [(wa[:, kc, csl], yT[:, kc, :]) for kc in range(4)], r=["wa", "yTa"], w=["pabc0"])
                    self.mm_group(pabc[1][:], [(wb[:, kc, csl], yT[:, 4 + kc, :]) for kc in range(2)], r=["wb", "yTb"], w=["pabc1"])
                    self.mm_group(pabc[2][:], [(wc[:, kc, csl], yT[:, 6 + kc, :]) for kc in range(2)], r=["wc", "yTc"], w=["pabc2"])
                    P.op("dve", lambda e, b=b: e.tensor_tensor(out=m1[:], in0=pabc[0][:], in1=gt[b][:, 0, :], op=ALU.mult),
                         r=["pabc0", "gt%d" % b], w=["m1"])
                    P.op("dve", lambda e, b=b: e.tensor_tensor(out=m2[:], in0=pabc[1][:], in1=gt[b][:, 1, :], op=ALU.mult),
                         r=["pabc1", "gt%d" % b], w=["m2"])
                    P.op("dve", lambda e, b=b: e.tensor_tensor(out=m3[:], in0=pabc[2][:], in1=gt[b][:, 2, :], op=ALU.mult),
                         r=["pabc2", "gt%d" % b], w=["m3"])
                    P.op("pool", lambda e: e.tensor_tensor(out=m1[:], in0=m1[:], in1=m2[:], op=ALU.add), r=["m1", "m2"], w=["m1"])
                    P.op("pool", lambda e, dc=dc: e.tensor_tensor(out=mixT[:, dc, :], in0=m1[:], in1=m3[:], op=ALU.add),
                         r=["m1", "m3"], w=[("mixT", dc)])
                for ti in range(4):
                    tt = tb * 4 + ti
                    hb = tt % 2
                    P.dma("sp", hold[hb][:], self.h_d[tt * 128:(tt + 1) * 128, :], r=[("h_d", tt)], w=["hold%d" % hb],
                          stream="hold%d" % hb)
                    for half in range(2):
                        self.mm_group(po[half][:], [(mixT[:, dc, ti * 128:(ti + 1) * 128], wo[:, dc, half * 512:(half + 1) * 512])
                                                    for dc in range(8)],
                                      r=["wo"] + [("mixT", dc) for dc in range(8)], w=["po%d" % half])
                        P.op("dve", lambda e, hb=hb, half=half: e.scalar_tensor_tensor(
                            out=tsum[hb][:, half * 512:(half + 1) * 512], in0=hold[hb][:, half * 512:(half + 1) * 512],
                            scalar=ALPHA, in1=po[half][:], op0=ALU.mult, op1=ALU.add),
                            r=["hold%d" % hb, "po%d" % half], w=["tsum%d" % hb])
                    self.ln_tile(tiles, tsum[hb], "tsum%d" % hb, gb, "gb1", self.h_d, tt, router=rt, pfx="l1")
            P.flush()

    def router_setup(self, es):
        P = self.P
        wr = self.sb(es, "wr", [128, KC, NE])
        wrh = self.sb(es, "wrh", [128, KC, NE], BF16)
        wrl = self.sb(es, "wrl", [128, KC, NE], BF16)
        rb = self.sb(es, "rb", [128, NE])
        hlo = self.sb(es, "hlo", [128, KC, 128], BF16)
        pl = self.ps(es, "pl", [128, NE])
        sc = self.sb(es, "r_sc", [128, NE])
        sel = self.sb(es, "r_sel", [128, NE])
        tmp = self.sb(es, "r_tmp", [128, NE])
        tmp2 = self.sb(es, "r_tmp2", [128, NE])
        g8 = self.sb(es, "r_g8", [128, 8, 4])
        oh1 = self.sb(es, "r_oh1", [128, NE])
        oh2 = self.sb(es, "r_oh2", [128, NE])
        sm = self.sb(es, "r_sm", [128, 8])
        P.dma("sp", wr[:], self.w_router.rearrange("(kc p) n -> p kc n", p=128), w=["wr"], stream="wr")
        P.dma("sp", rb[:], self.router_bias.partition_broadcast(128), w=["rb"], stream="rb")
        P.dma("pool", wrh[:], self.w_router.rearrange("(kc p) n -> p kc n", p=128), w=["wrh"], stream="wrh")
        P.op("dve", lambda e: e.tensor_tensor(out=wrl[:], in0=wr[:], in1=wrh[:], op=ALU.subtract), r=["wr", "wrh"], w=["wrl"])
        BIG = 1.0e4

        def router(tt, pT, pTres):
            tsl = slice(tt * 128, (tt + 1) * 128)
            P.op("dve", lambda e: e.tensor_tensor(out=hlo[:], in0=pT[:], in1=self.hT[:, :, tsl], op=ALU.subtract),
                 r=[pTres, ("hT", tt)], w=["hlo"])
            pairs = []
            for kc in range(KC):
                pairs += [(self.hT[:, kc, tsl], wrh[:, kc, :]), (self.hT[:, kc, tsl], wrl[:, kc, :]), (hlo[:, kc, :], wrh[:, kc, :])]
            self.mm_group(pl[:], pairs, r=["hlo", ("hT", tt), "wrh", "wrl"], w=["pl"])
            P.op("act", lambda e: e.activation(out=sc[:], in_=pl[:], func=AF.Sigmoid), r=["pl"], w=["r_sc"])
            P.op("dve", lambda e: e.tensor_tensor(out=sel[:], in0=sc[:], in1=rb[:], op=ALU.add), r=["r_sc", "rb"], w=["r_sel"])
            s3 = sel[:].rearrange("p (g k) -> p g k", k=4)
            t3 = tmp[:].rearrange("p (g k) -> p g k", k=4)
            P.op("dve", lambda e: e.tensor_reduce(out=sm[:, 0:8], in_=s3, axis=AX.X, op=ALU.max), r=["r_sel"], w=["r_sm"])
            P.op("dve", lambda e: e.tensor_tensor(out=t3, in0=s3, in1=bc(sm[:, 0:8].unsqueeze(2), [128, 8, 4]), op=ALU.is_equal),
                 r=["r_sel", "r_sm"], w=["r_tmp"])
            P.op("dve", lambda e: e.scalar_tensor_tensor(out=tmp[:], in0=tmp[:], scalar=-BIG, in1=sel[:], op0=ALU.mult, op1=ALU.add),
                 r=["r_tmp", "r_sel"], w=["r_tmp"])
            P.op("dve", lambda e: e.tensor_reduce(out=g8[:, :, 0], in_=t3, axis=AX.X, op=ALU.max), r=["r_tmp"], w=["r_g8"])
            P.op("dve", lambda e: e.tensor_tensor(out=g8[:, :, 1], in0=g8[:, :, 0], in1=sm[:, 0:8], op=ALU.add),
                 r=["r_g8", "r_sm"], w=["r_g8b"])
            P.op("dve", lambda e: e.tensor_reduce(out=sm[:, 0:1], in_=g8[:, :, 1], axis=AX.X, op=ALU.max), r=["r_g8b"], w=["r_sm1"])
            P.op("dve", lambda e: e.tensor_scalar(out=g8[:, :, 2], in0=g8[:, :, 1], scalar1=sm[:, 0:1], scalar2=None, op0=ALU.is_lt),
                 r=["r_g8b", "r_sm1"], w=["r_g8c"])
            P.op("dve", lambda e: e.scalar_tensor_tensor(out=t3, in0=bc(g8[:, :, 2:3], [128, 8, 4]), scalar=-BIG, in1=s3,
                                                         op0=ALU.mult, op1=ALU.add), r=["r_g8c", "r_sel"], w=["r_tmp"])
            P.op("dve", lambda e: e.tensor_reduce(out=sm[:, 1:2], in_=tmp[:], axis=AX.X, op=ALU.max), r=["r_tmp"], w=["r_sm2"])
            P.op("dve", lambda e: e.tensor_scalar(out=oh1[:], in0=tmp[:], scalar1=sm[:, 1:2], scalar2=None, op0=ALU.is_equal),
                 r=["r_tmp", "r_sm2"], w=["r_oh1"])
            P.op("dve", lambda e: e.scalar_tensor_tensor(out=tmp2[:], in0=oh1[:], scalar=-BIG, in1=tmp[:], op0=ALU.mult, op1=ALU.add),
                 r=["r_oh1", "r_tmp"], w=["r_tmp2"])
            P.op("dve", lambda e: e.tensor_reduce(out=sm[:, 2:3], in_=tmp2[:], axis=AX.X, op=ALU.max), r=["r_tmp2"], w=["r_sm3"])
            P.op("dve", lambda e: e.tensor_scalar(out=oh2[:], in0=tmp2[:], scalar1=sm[:, 2:3], scalar2=None, op0=ALU.is_equal),
                 r=["r_tmp2", "r_sm3"], w=["r_oh2"])
            P.op("dve", lambda e: e.tensor_tensor(out=oh1[:], in0=oh1[:], in1=oh2[:], op=ALU.add), r=["r_oh1", "r_oh2"], w=["r_oh1"])
            P.op("dve", lambda e: e.tensor_tensor(out=oh1[:], in0=oh1[:], in1=sc[:], op=ALU.mult), r=["r_oh1", "r_sc"], w=["r_oh1"])
            P.op("dve", lambda e: e.tensor_reduce(out=sm[:, 3:4], in_=oh1[:], axis=AX.X, op=ALU.add), r=["r_oh1"], w=["r_sm4"])
            P.op("dve", lambda e: e.reciprocal(out=sm[:, 4:5], in_=sm[:, 3:4]), r=["r_sm4"], w=["r_sm5"])
            P.op("dve", lambda e: e.tensor_scalar(out=self.comb[:, tt, :], in0=oh1[:], scalar1=sm[:, 4:5], scalar2=None, op0=ALU.mult),
                 r=["r_oh1", "r_sm5"], w=[("comb", tt)])
        return router

    def phase_moe_dense(self, l, last):
        P = self.P
        S, NB, NT = self.S, self.NB, self.NT
        G = min(8, NT)
        w1_l = self.w1[l].rearrange("e (kc p) n -> e p kc n", p=128)
        w3_l = self.w3[l].rearrange("e (kc p) n -> e p kc n", p=128)
        w2_l = self.w2[l].rearrange("e (kc p) n -> e p kc n", p=128)
        with ExitStack() as es:
            w1t = [self.sb(es, "w1t%d" % i, [128, KC, DE], BF16) for i in range(2)]
            w3t = [self.sb(es, "w3t%d" % i, [128, KC, DE], BF16) for i in range(2)]
            w2t = [self.sb(es, "w2t%d" % i, [128, 4, D], BF16) for i in range(2)]
            yacc = self.sb(es, "yacc", [128, G, D])
            hid = [self.sb(es, "hid%d" % i, [128, 4, 512], BF16) for i in range(2)]
            sl = [self.sb(es, "sl%d" % i, [128, 512], BF16) for i in range(2)]
            hold = [self.sb(es, "mhold%d" % i, [128, D]) for i in range(2)]
            gb = self.sb(es, "gb2", [128, 2, D])
            p1 = [self.ps(es, "p1_%d" % i, [128, 512]) for i in range(2)]
            p3 = [self.ps(es, "p3_%d" % i, [128, 512]) for i in range(2)]
            py = [self.ps(es, "py%d" % i, [128, 512]) for i in range(2)]
            tiles = self.ln_scratch(es, "l2")
            P.dma("sp", gb[:], self.ln2[l].partition_broadcast(128), w=["gb2"], stream="gb2")
            wi = 0
            for grp in range(NT // G):
                P.op("pool", lambda e: e.memset(yacc[:], 0.0), w=["yacc"])
                for ex in range(NE):
                    b = wi % 2
                    wi += 1
                    P.dma("pool", w1t[b][:], w1_l[ex], w=["w1t%d" % b], stream="w1t%d" % b)
                    P.dma("pool", w3t[b][:], w3_l[ex], w=["w3t%d" % b], stream="w3t%d" % b)
                    P.dma("pool", w2t[b][:], w2_l[ex], w=["w2t%d" % b], stream="w2t%d" % b)
                    for blk in range(G // 4):
                        tb = grp * (G // 4) + blk
                        hb = (ex * (G // 4) + blk) % 2
                        hres = [("hT", tb * 4 + i) for i in range(4)]
                        for fc in range(4):
                            pb_ = fc % 2
                            fsl = slice(fc * 128, (fc + 1) * 128)
                            self.mm_group(p1[pb_][:], [(w1t[b][:, kc, fsl], self.hT[:, kc, tb * 512:(tb + 1) * 512]) for kc in range(KC)],
                                          r=["w1t%d" % b] + hres, w=["p1_%d" % pb_])
                            self.mm_group(p3[pb_][:], [(w3t[b][:, kc, fsl], self.hT[:, kc, tb * 512:(tb + 1) * 512]) for kc in range(KC)],
                                          r=["w3t%d" % b] + hres, w=["p3_%d" % pb_])
                            P.op("act", lambda e, pb_=pb_: e.activation(out=sl[pb_][:], in_=p1[pb_][:], func=AF.Silu),
                                 r=["p1_%d" % pb_], w=["sl%d" % pb_])
                            P.op("dve", lambda e, pb_=pb_, hb=hb, fc=fc: e.tensor_tensor(out=hid[hb][:, fc, :], in0=sl[pb_][:],
                                                                                           in1=p3[pb_][:], op=ALU.mult),
                                 r=["sl%d" % pb_, "p3_%d" % pb_], w=[("hid", hb, fc)])
                        for ti in range(4):
                            gi = blk * 4 + ti
                            tt = tb * 4 + ti
                            for half in range(2):
                                hs = slice(half * 512, (half + 1) * 512)
                                self.mm_group(py[half][:], [(hid[hb][:, fc, ti * 128:(ti + 1) * 128], w2t[b][:, fc, hs]) for fc in range(4)],
                                              r=["w2t%d" % b] + [("hid", hb, fc) for fc in range(4)], w=["py%d" % half])
                                P.op("dve", lambda e, gi=gi, hs=hs, half=half, tt=tt, ex=ex: e.scalar_tensor_tensor(
                                    out=yacc[:, gi, hs], in0=py[half][:], scalar=self.comb[:, tt, ex:ex + 1], in1=yacc[:, gi, hs],
                                    op0=ALU.mult, op1=ALU.add), r=["py%d" % half, ("comb", tt), "yacc"], w=["yacc"])
                for gi in range(G):
                    tt = grp * G + gi
                    hb = tt % 2
                    P.dma("sp", hold[hb][:], self.h_d[tt * 128:(tt + 1) * 128, :], r=[("h_d", tt)], w=["mhold%d" % hb],
                          stream="mhold%d" % hb)
                    P.op("dve", lambda e, hb=hb, gi=gi: e.scalar_tensor_tensor(out=hold[hb][:], in0=hold[hb][:], scalar=ALPHA,
                                                                                 in1=yacc[:, gi, :], op0=ALU.mult, op1=ALU.add),
                         r=["mhold%d" % hb, "yacc"], w=["mhold%d" % hb])
                    self.ln_tile(tiles, hold[hb], "mhold%d" % hb, gb, "gb2", self.out if last else self.h_d, tt,
                                 hT_out=not last, pfx="l2")
            P.flush()

    def build(self, stop=None):
        P = self.P
        self.load_consts()
        self.phase_ln0()
        for l in range(self.L):
            if stop == "ln0":
                break
            self.phase_inproj(l)
            if stop == "inproj":
                break
            if "A" in self.parts:
                self.phase_attn_a(l)
            if "B" in self.parts:
                self.phase_attn_b(l)
            if "C" in self.parts:
                self.phase_gdn(l)
            if stop == "attn":
                break
            self.phase_merge(l)
            if stop == "merge":
                break
            self.phase_moe_dense(l, last=(l == self.L - 1))
        P.wait_all("sp", list(P.lastw.keys()))
        P.flush()
        self.es.close()
        P.close()
        return self.nc


def host_consts(S):
    c = {}
    c["c_ident"] = np.eye(128, dtype=np.float32)
    blk = np.zeros((128, 128), np.float32)
    blk[:64, :64] = 1.0
    blk[64:, 64:] = 1.0
    c["c_blk"] = blk
    R = np.zeros((64, 64), np.float32)
    for base in (0, 32):
        for i in range(16):
            R[base + i, base + 16 + i] = -1.0
            R[base + 16 + i, base + i] = 1.0
    RT = np.zeros((128, 128), np.float32)
    RT[:64, :64] = R.T
    RT[64:, 64:] = R.T
    c["c_rot"] = RT
    t = np.arange(S)
    row = (t // GRID_W).astype(np.float32)
    col = (t % GRID_W).astype(np.float32)
    inv = (10000.0 ** (-np.arange(0, 32, 2, dtype=np.float32) / 32)).astype(np.float32)
    ang_r = row[None, :] * inv[:, None]
    ang_c = col[None, :] * inv[:, None]
    cos64 = np.concatenate([np.cos(ang_r), np.cos(ang_r), np.cos(ang_c), np.cos(ang_c)], 0)
    sin64 = np.concatenate([np.sin(ang_r), np.sin(ang_r), np.sin(ang_c), np.sin(ang_c)], 0)
    c["c_cos"] = np.concatenate([cos64, cos64], 0).astype(np.float32)
    c["c_sin"] = np.concatenate([sin64, sin64], 0).astype(np.float32)
    g = np.zeros((64, 6, 8, 64), np.float32)
    i = np.arange(64)[:, None]
    j = np.arange(64)[None, :]
    for cc in range(8):
        fwd = cc < 4
        allow = (i >= j) if fwd else (i <= j)
        g[:, 0, cc, :] = np.where(allow, 0.0, NEG)
        g[:, 1, cc, :] = np.where(allow.T, 0.0, NEG)
        g[:, 2, cc, :] = ((i > j) if fwd else (i < j)).astype(np.float32)
        g[:, 3, cc, :] = (i == j).astype(np.float32)
    g[:, 4, 0, :] = (i <= j).astype(np.float32)
    g[:, 4, 1, :] = (i >= j).astype(np.float32)
    g[:, 5, 0, :] = 1.0
    c["c_gdn"] = g
    return c


def host_layout(inp, L):
    f = lambda a: np.ascontiguousarray(np.asarray(a, dtype=np.float32))
    m = {}
    m["ln0"] = f(np.stack([inp["ln0_g"], inp["ln0_b"]], 0))
    m["w_in"] = f(inp["w_in"][:L])
    qg = np.asarray(inp["q_norm_g"], np.float32)[:L]
    kg = np.asarray(inp["k_norm_g"], np.float32)[:L]
    m["qkg"] = f(np.stack([np.concatenate([qg, qg], 1), np.concatenate([kg, kg], 1)], 2))
    m["wa"] = f(inp["w_branch_a"][:L])
    m["wb"] = f(inp["w_branch_b"][:L])
    m["wc"] = f(inp["w_branch_c"][:L])
    m["wo"] = f(inp["w_out"][:L])
    m["ln1"] = f(np.stack([inp["ln1_g"][:L], inp["ln1_b"][:L]], 1))
    m["ln2"] = f(np.stack([inp["ln2_g"][:L], inp["ln2_b"][:L]], 1))
    m["w_router"] = f(inp["w_router"])
    m["router_bias"] = f(inp["router_bias"])
    m["w1"] = f(inp["w1"][:L])
    m["w3"] = f(inp["w3"][:L])
    m["w2"] = f(inp["w2"][:L])
    rpb = np.asarray(inp["na_rpb"], np.float32)[:L]
    c = np.arange(64)
    cs = np.clip(c - 8, 0, 48)
    kc_ = np.arange(64)
    inwin = (kc_[None, :] >= cs[:, None]) & (kc_[None, :] < cs[:, None] + 16)
    dc = np.clip(kc_[None, :] - c[:, None] + 15, 0, 30)
    g = rpb[:, :, :, dc]
    g = np.where(inwin[None, None, None], g, np.float32(NEG))
    m["bias_b"] = f(np.transpose(g, (0, 1, 3, 2, 4)).reshape(L, 4, 64, 960))
    cwv = np.asarray(inp["conv_w"], np.float32)[:L]
    m["conv_w"] = f(np.transpose(cwv.reshape(L, 5, 6, 128), (0, 3, 2, 1)))
    m["gdn_ab"] = f(np.stack([np.asarray(inp["A_log"], np.float32)[:L].reshape(L, 8),
                              np.asarray(inp["dt_bias"], np.float32)[:L].reshape(L, 8)], 1))
    m["gdn_g"] = f(inp["gdn_norm_g"][:L])
    return m


_CACHE = {}


def kernel(**inputs):
    S = 4096
    L = DEPTH
    x = np.asarray(inputs["x"], dtype=np.float32)
    nb = x.shape[0]
    key = (S, L)
    if key not in _CACHE:
        _CACHE[key] = Builder(S, L).build()
    nc = _CACHE[key]
    shared = host_layout(inputs, L)
    shared.update(host_consts(S))
    in_maps = []
    for b in range(nb):
        mm = dict(shared)
        mm["x"] = np.ascontiguousarray(x[b])
        in_maps.append(mm)
    res = run_bass_kernel_spmd(nc, in_maps, core_ids=list(range(nb)))
    return np.stack([np.asarray(r["out"], dtype=np.float32) for r in res.results], 0)
```

```python
import math
import numpy as np
from contextlib import ExitStack
import concourse.bass as bass
import concourse.mybir as mybir
from concourse.bass_utils import run_bass_kernel_spmd

F32 = mybir.dt.float32
BF16 = mybir.dt.bfloat16
I32 = mybir.dt.int32
AF = mybir.ActivationFunctionType
ALU = mybir.AluOpType
AX = mybir.AxisListType

ENGS = ("pe", "act", "dve", "pool", "sp")

D = 1024
KC = 8
DEPTH = 4
GRID_W = 64
DIN = 5648
NE = 32
DE = 512
ALPHA = (2 * DEPTH) ** 0.25
C_AQ, C_AK, C_AV = 0, 512, 640
C_BQ, C_BK, C_BV = 768, 1024, 1280
C_CQ, C_CK, C_CV, C_CZ = 1536, 1792, 2048, 2304
C_CG = 2560
C_GATE = 2576
NEG = -30000.0


class Prog:
    def __init__(self, nc):
        self.nc = nc
        self.es = ExitStack()
        self.sem = {e: self.es.enter_context(nc.semaphore("s_" + e)) for e in ENGS}
        self.cnt = {e: 0 for e in ENGS}
        self.dsem = {}
        self.known = {e: {} for e in ENGS}
        self.lastw = {}
        self.readers = {}
        self.queue = {e: [] for e in ENGS}
        self.pending = {e: [] for e in ENGS}
        self.nops = 0

    def close(self):
        self.es.close()

    def _deps(self, eng, r, w):
        toks = []
        for k in r:
            t = self.lastw.get(k)
            if t is not None:
                toks.append(t)
        for k in w:
            t = self.lastw.get(k)
            if t is not None:
                toks.append(t)
            toks.extend(self.readers.get(k, ()))
        need = {}
        for t in toks:
            key, val = t[0], t[1]
            if eng == "pe" and key == "pe":
                continue
            if val is None:
                raise RuntimeError("dependency on unresolved (inc=False) op")
            if need.get(key, 0) < val:
                need[key] = val
        waits = []
        kn = self.known[eng]
        for key, val in need.items():
            if kn.get(key, 0) < val:
                kn[key] = val
                waits.append((key, val))
        return waits

    def _mark(self, tok, r, w):
        for k in w:
            self.lastw[k] = tok
            self.readers[k] = []
        for k in r:
            self.readers.setdefault(k, []).append(tok)

    def op(self, eng, fn, r=(), w=(), inc=True):
        waits = self._deps(eng, r, w)
        if inc:
            self.cnt[eng] += 1
            tok = [eng, self.cnt[eng]]
            for p in self.pending[eng]:
                p[1] = self.cnt[eng]
            self.pending[eng] = []
        else:
            tok = [eng, None]
            self.pending[eng].append(tok)
        self._mark(tok, r, w)
        self.queue[eng].append((fn, waits, (eng, 1) if inc else None))
        self.nops += 1

    def dma(self, eng, out, in_, r=(), w=(), stream=None, **kw):
        assert stream is not None
        waits = self._deps(eng, r, w)
        if stream not in self.dsem:
            self.dsem[stream] = [self.es.enter_context(self.nc.semaphore("d%d" % len(self.dsem))), 0]
        ds = self.dsem[stream]
        ds[1] += 16
        tok = [("d", stream), ds[1]]
        self._mark(tok, r, w)
        self.queue[eng].append((lambda e, out=out, in_=in_, kw=kw: e.dma_start(out=out, in_=in_, **kw),
                                waits, (("d", stream), 16)))
        self.nops += 1

    def _semh(self, key):
        if isinstance(key, tuple):
            return self.dsem[key[1]][0]
        return self.sem[key]

    def wait_all(self, eng, keys):
        waits = self._deps(eng, keys, ())
        self.queue[eng].append((None, waits, None))

    def flush(self):
        nc = self.nc
        q = self.queue
        self.queue = {e: [] for e in ENGS}
        for e in ENGS:
            if self.pending[e]:
                raise RuntimeError("unresolved inc=False ops at flush on " + e)

        def run(engine, items):
            for fn, waits, inc in items:
                for key, val in waits:
                    engine.wait_ge(self._semh(key), val)
                if fn is None:
                    continue
                ins = fn(engine)
                if inc is not None:
                    ins.then_inc(self._semh(inc[0]), inc[1])

        with nc.Block() as block:
            if q["sp"]:
                @block.sync
                def _(e):
                    run(e, q["sp"])
            if q["pe"]:
                @block.tensor
                def _(e):
                    run(e, q["pe"])
            if q["act"]:
                @block.scalar
                def _(e):
                    run(e, q["act"])
            if q["dve"]:
                @block.vector
                def _(e):
                    run(e, q["dve"])
            if q["pool"]:
                @block.gpsimd
                def _(e):
                    run(e, q["pool"])


def bc(ap, shape):
    return ap.to_broadcast(shape)


class Builder:
    def __init__(self, S, L, dbg=(), parts=("A", "B", "C"), moe="dense"):
        self.S, self.L = S, L
        self.NT = S // 128
        self.NB = S // 512
        self.parts = parts
        self.moe = moe
        nc = self.nc = bass.Bass("TRN2", target_bir_lowering=False)
        self.P = Prog(nc)
        self.dbg = set(dbg)
        dt_in = lambda n, s, d=F32: nc.dram_tensor(n, list(s), d, kind="ExternalInput").ap()
        self.x = dt_in("x", [S, D])
        self.ln0 = dt_in("ln0", [2, D])
        self.w_in = dt_in("w_in", [L, D, DIN])
        self.qkg = dt_in("qkg", [L, 128, 2])
        self.bias_b = dt_in("bias_b", [L, 4, 64, 960])
        self.conv_w = dt_in("conv_w", [L, 128, 6, 5])
        self.gdn_ab = dt_in("gdn_ab", [L, 2, 8])
        self.gdn_g = dt_in("gdn_g", [L, 64])
        self.wa = dt_in("wa", [L, 512, D])
        self.wb = dt_in("wb", [L, 256, D])
        self.wc = dt_in("wc", [L, 256, D])
        self.wo = dt_in("wo", [L, D, D])
        self.ln1 = dt_in("ln1", [L, 2, D])
        self.ln2 = dt_in("ln2", [L, 2, D])
        self.w_router = dt_in("w_router", [D, NE])
        self.router_bias = dt_in("router_bias", [NE])
        self.w1 = dt_in("w1", [L, NE, D, DE])
        self.w3 = dt_in("w3", [L, NE, D, DE])
        self.w2 = dt_in("w2", [L, NE, DE, D])
        self.c_ident = dt_in("c_ident", [128, 128])
        self.c_blk = dt_in("c_blk", [128, 128])
        self.c_rot = dt_in("c_rot", [128, 128])
        self.c_cos = dt_in("c_cos", [128, S])
        self.c_sin = dt_in("c_sin", [128, S])
        self.c_gdn = dt_in("c_gdn", [64, 6, 8, 64])
        okind = "ExternalOutput"
        self.out = nc.dram_tensor("out", [S, D], F32, kind=okind).ap()

        def scr(n, s, d=F32):
            k = "ExternalOutput" if n in self.dbg else "Internal"
            return nc.dram_tensor(n, list(s), d, kind=k).ap()
        self.h_d = scr("h_d", [S, D])
        self.qT_d = scr("qT_d", [512, S], BF16)
        self.kT_d = scr("kT_d", [128, S], BF16)
        self.v_d = scr("v_d", [S, 128], BF16)
        self.bqT_d = scr("bqT_d", [256, S], BF16)
        self.bkT_d = scr("bkT_d", [256, S], BF16)
        self.bv_d = scr("bv_d", [S, 256], BF16)
        self.cT_d = scr("cT_d", [768, S])
        self.cz_d = scr("cz_d", [S, 272])
        self.gT_d = scr("gT_d", [3072, S], BF16)
        self.yaT_d = scr("yaT_d", [512, S], BF16)
        self.ybT_d = scr("ybT_d", [256, S], BF16)
        self.ycT_d = scr("ycT_d", [256, S], BF16)
        self.es = ExitStack()
        self.hT = self.sb(self.es, "hT", [128, KC, S], BF16)
        self.comb = self.sb(self.es, "comb", [128, self.NT, NE])
        self.ident = self.sb(self.es, "ident", [128, 128])
        self.identb = self.sb(self.es, "identb", [128, 128], BF16)

    def sb(self, es, n, s, d=F32):
        self.uid = getattr(self, "uid", 0) + 1
        return es.enter_context(self.nc.sbuf_tensor("sb%d_%s" % (self.uid, n), list(s), d))

    def ps(self, es, n, s, d=F32):
        self.uid = getattr(self, "uid", 0) + 1
        return es.enter_context(self.nc.psum_tensor("ps%d_%s" % (self.uid, n), list(s), d))

    def mm_group(self, out_ap, pairs, r, w):
        P = self.P
        n = len(pairs)
        for i, (l, rh) in enumerate(pairs):
            P.op("pe", lambda e, l=l, rh=rh, i=i: e.matmul(out_ap, l, rh, start=(i == 0), stop=(i == n - 1)),
                 r=r, w=w, inc=(i == n - 1))

    def load_consts(self):
        P = self.P
        P.dma("sp", self.ident[:], self.c_ident, w=["ident"], stream="ident")
        P.dma("pool", self.identb[:], self.c_ident, w=["identb"], stream="identb")

    def ln_tile(self, es_tiles, t, tres, gb, gbres, dst_d, tt, hT_out=True, router=None, pfx="ln"):
        P = self.P
        st, mv, sd, xn, pT = (es_tiles[k] for k in ("st", "mv", "sd", "xn", "pT"))
        P.op("dve", lambda e: e.bn_stats(out=st[:, 0:6], in_=t[:, 0:512]), r=[tres], w=[pfx + "st"])
        P.op("dve", lambda e: e.bn_stats(out=st[:, 6:12], in_=t[:, 512:1024]), r=[tres], w=[pfx + "st"])
        P.op("dve", lambda e: e.bn_aggr(out=mv[:], in_=st[:]), r=[pfx + "st"], w=[pfx + "mv"])
        P.op("act", lambda e: e.activation(out=sd[:, 0:1], in_=mv[:, 1:2], func=AF.Sqrt, bias=1e-5, scale=1.0),
             r=[pfx + "mv"], w=[pfx + "sd"])
        P.op("dve", lambda e: e.reciprocal(out=sd[:, 1:2], in_=sd[:, 0:1]), r=[pfx + "sd"], w=[pfx + "sd1"])
        P.op("dve", lambda e: e.scalar_tensor_tensor(out=sd[:, 2:3], in0=mv[:, 0:1], scalar=-1.0, in1=sd[:, 1:2],
                                                     op0=ALU.mult, op1=ALU.mult),
             r=[pfx + "mv", pfx + "sd1"], w=[pfx + "sd2"])
        P.op("act", lambda e: e.activation(out=xn[:], in_=t[:], func=AF.Identity, bias=sd[:, 2:3], scale=sd[:, 1:2]),
             r=[tres, pfx + "sd1", pfx + "sd2"], w=[pfx + "xn"])
        P.op("dve", lambda e: e.tensor_tensor(out=xn[:], in0=xn[:], in1=gb[:, 0, :], op=ALU.mult),
             r=[pfx + "xn", gbres], w=[pfx + "xn"])
        P.op("pool", lambda e: e.tensor_tensor(out=xn[:], in0=xn[:], in1=gb[:, 1, :], op=ALU.add),
             r=[pfx + "xn", gbres], w=[pfx + "xn"])
        P.dma("sp", dst_d[tt * 128:(tt + 1) * 128, :], xn[:], r=[pfx + "xn"], w=[("h_d", tt) if dst_d is self.h_d else "out"],
              stream=pfx + "xn")
        if hT_out:
            for kc in range(KC):
                P.op("pe", lambda e, kc=kc: e.transpose(pT[:, kc, :], xn[:, kc * 128:(kc + 1) * 128], self.ident[:]),
                     r=[pfx + "xn", "ident"], w=[pfx + "pT"], inc=(kc == KC - 1))
            P.op("act", lambda e: e.activation(out=self.hT[:, :, tt * 128:(tt + 1) * 128], in_=pT[:], func=AF.Copy),
                 r=[pfx + "pT"], w=[("hT", tt)])
            if router is not None:
                router(tt, pT, pfx + "pT")

    def ln_scratch(self, es, pfx="ln"):
        return dict(st=self.sb(es, pfx + "st", [128, 12]), mv=self.sb(es, pfx + "mv", [128, 2]),
                    sd=self.sb(es, pfx + "sd", [128, 4]), xn=self.sb(es, pfx + "xn", [128, D]),
                    pT=self.ps(es, pfx + "pT", [128, KC, 128]))

    def phase_ln0(self):
        P = self.P
        with ExitStack() as es:
            tiles = self.ln_scratch(es)
            gb = self.sb(es, "gb0", [128, 2, D])
            xt = [self.sb(es, "x%d" % i, [128, D]) for i in range(2)]
            P.dma("sp", gb[:], self.ln0.partition_broadcast(128), w=["gb0"], stream="gb0")
            for tt in range(self.NT):
                b = tt % 2
                P.dma("sp", xt[b][:], self.x[tt * 128:(tt + 1) * 128, :], w=["xt%d" % b], stream="xt%d" % b)
                self.ln_tile(tiles, xt[b], "xt%d" % b, gb, "gb0", self.h_d if self.L > 0 else self.out, tt)
            P.flush()

    def phase_inproj(self, l):
        P = self.P
        S, NB, NT = self.S, self.NB, self.NT
        w_l = self.w_in[l].rearrange("(kc p) n -> p kc n", p=128)
        with ExitStack() as es:
            wt = [self.sb(es, "wi%d" % i, [128, KC, 512], BF16) for i in range(2)]
            stg = [self.sb(es, "stg%d" % i, [128, 512]) for i in range(2)]
            stgb = [self.sb(es, "stgb%d" % i, [128, 512], BF16) for i in range(2)]
            pa = [self.ps(es, "pa%d" % i, [128, 512]) for i in range(2)]
            pb = self.ps(es, "pb", [128, 512])
            pc = self.ps(es, "pc", [128, 512])
            blk = self.sb(es, "blk", [128, 128], BF16)
            rot = self.sb(es, "rot", [128, 128], BF16)
            cos = self.sb(es, "cos", [128, S])
            sin = self.sb(es, "sin", [128, S])
            qkg = self.sb(es, "qkg", [128, 2])
            sq = self.sb(es, "sq", [128, 512], BF16)
            rs = self.sb(es, "rs", [128, 512])
            qn = self.sb(es, "qn", [128, 512], BF16)
            t1 = self.sb(es, "t1", [128, 512])
            t2 = self.sb(es, "t2", [128, 512])
            P.dma("pool", blk[:], self.c_blk, w=["blk"], stream="blk")
            P.dma("pool", rot[:], self.c_rot, w=["rot"], stream="rot")
            P.dma("sp", cos[:], self.c_cos, w=["cos"], stream="cos")
            P.dma("sp", sin[:], self.c_sin, w=["sin"], stream="sin")
            P.dma("sp", qkg[:], self.qkg[l], w=["qkg"], stream="qkg")
            cnt = {"w": 0, "o": 0, "p": 0}

            def load_w(c0, n):
                b = cnt["w"] % 2
                cnt["w"] += 1
                P.dma("pool", wt[b][:, :, 0:n], w_l[:, :, c0:c0 + n], w=["wi%d" % b], stream="wi%d" % b)
                return wt[b], "wi%d" % b

            def fm_block(c0, n, evac):
                w, wres = load_w(c0, n)
                for j in range(n // 128):
                    for tb in range(NB):
                        pp = cnt["p"] % 2
                        cnt["p"] += 1
                        self.mm_group(pa[pp][:], [(w[:, kc, j * 128:(j + 1) * 128], self.hT[:, kc, tb * 512:(tb + 1) * 512])
                                                   for kc in range(KC)],
                                      r=[wres] + [("hT", tb * 4 + i) for i in range(4)], w=["pa%d" % pp])
                        evac(pa[pp], "pa%d" % pp, c0 + j * 128, tb)

            def out_stage(bf):
                b = cnt["o"] % 2
                cnt["o"] += 1
                return (stgb[b], "stgb%d" % b) if bf else (stg[b], "stg%d" % b)

            def evac_aqk(p, pres, col, tb):
                isq = col < C_AK
                gcol = 0 if isq else 1
                tsl = slice(tb * 512, (tb + 1) * 512)
                P.op("act", lambda e: e.activation(out=sq[:], in_=p[:], func=AF.Square), r=[pres], w=["sq"])
                P.op("pe", lambda e: e.matmul(pb[:], blk[:], sq[:], start=True, stop=True), r=["blk", "sq"], w=["pb"])
                P.op("act", lambda e: e.activation(out=rs[:], in_=pb[:], func=AF.Sqrt, bias=(64e-6 if isq else 1e-6),
                                                   scale=(1.0 if isq else 1.0 / 64)), r=["pb"], w=["rs"])
                P.op("dve", lambda e: e.reciprocal(out=rs[:], in_=rs[:]), r=["rs"], w=["rs"])
                P.op("dve", lambda e: e.scalar_tensor_tensor(out=qn[:], in0=p[:], scalar=qkg[:, gcol:gcol + 1], in1=rs[:],
                                                             op0=ALU.mult, op1=ALU.mult),
                     r=[pres, "rs", "qkg"], w=["qn"])
                P.op("pe", lambda e: e.matmul(pc[:], rot[:], qn[:], start=True, stop=True), r=["rot", "qn"], w=["pc"])
                P.op("pool", lambda e: e.tensor_tensor(out=t1[:], in0=qn[:], in1=cos[:, tsl], op=ALU.mult),
                     r=["qn", "cos"], w=["t1"])
                P.op("dve", lambda e: e.tensor_tensor(out=t2[:], in0=pc[:], in1=sin[:, tsl], op=ALU.mult),
                     r=["pc", "sin"], w=["t2"])
                o, ores = out_stage(True)
                P.op("pool", lambda e: e.tensor_tensor(out=o[:], in0=t1[:], in1=t2[:], op=ALU.add),
                     r=["t1", "t2"], w=[ores])
                dst = self.qT_d[col:col + 128, tsl] if isq else self.kT_d[:, tsl]
                P.dma("sp", dst, o[:], r=[ores], w=["qkT_d"], stream=ores)

            if "A" in self.parts:
                fm_block(C_AQ, 512, evac_aqk)
                fm_block(C_AK, 128, evac_aqk)

            def evac_simple(dst_d, row0, bf, func=AF.Copy, scale=1.0):
                def ev(p, pres, col, tb):
                    o, ores = out_stage(bf)
                    P.op("act", lambda e: e.activation(out=o[:], in_=p[:], func=func, scale=scale), r=[pres], w=[ores])
                    r0 = col - row0
                    P.dma("sp", dst_d[r0:r0 + 128, tb * 512:(tb + 1) * 512], o[:], r=[ores], w=[("fm_d", id(dst_d))],
                          stream=ores)
                return ev

            if "B" in self.parts:
                fm_block(C_BQ, 256, evac_simple(self.bqT_d, C_BQ, True, scale=0.125))
                fm_block(C_BK, 256, evac_simple(self.bkT_d, C_BK, True))
            if "C" in self.parts:
                fm_block(C_CQ, 512, evac_simple(self.cT_d, C_CQ, False))
                fm_block(C_CV, 256, evac_simple(self.cT_d, C_CQ, False))
            for g in range(6):
                fm_block(C_GATE + g * 512, 512, evac_simple(self.gT_d, C_GATE, True, func=AF.Sigmoid))

            def tm_block(c0, n, dst_d, bf):
                w, wres = load_w(c0, n)
                for tt in range(NT):
                    pp = cnt["p"] % 2
                    cnt["p"] += 1
                    self.mm_group(pa[pp][:, 0:n], [(self.hT[:, kc, tt * 128:(tt + 1) * 128], w[:, kc, 0:n]) for kc in range(KC)],
                                  r=[wres, ("hT", tt)], w=["pa%d" % pp])
                    o, ores = out_stage(bf)
                    P.op("act", lambda e, o=o, pp=pp: e.activation(out=o[:, 0:n], in_=pa[pp][:, 0:n], func=AF.Copy),
                         r=["pa%d" % pp], w=[ores])
                    P.dma("sp", dst_d[tt * 128:(tt + 1) * 128, :], o[:, 0:n], r=[ores], w=[("tm_d", id(dst_d))], stream=ores)

            if "A" in self.parts:
                tm_block(C_AV, 128, self.v_d, True)
            if "B" in self.parts:
                tm_block(C_BV, 256, self.bv_d, True)
            if "C" in self.parts:
                tm_block(C_CZ, 272, self.cz_d, False)
            P.flush()

    def phase_attn_a(self, l):
        P = self.P
        S, NB, NT = self.S, self.NB, self.NT
        with ExitStack() as es:
            qh = [self.sb(es, "qh%d" % i, [64, S], BF16) for i in range(2)]
            kT = self.sb(es, "kT", [64, 2, S], BF16)
            vx = self.sb(es, "vx", [128, NT, 2, 65], BF16)
            pT = [self.sb(es, "pT%d" % i, [128, 512], BF16) for i in range(3)]
            onesr = self.sb(es, "onesr", [128, 64])
            rc = self.sb(es, "rc", [128, 512])
            bcs = self.sb(es, "bcs", [64, 512])
            ya = [self.sb(es, "ya%d" % i, [64, 512], BF16) for i in range(2)]
            sps = [self.ps(es, "sps%d" % i, [128, 512]) for i in range(3)]
            ops_ = [self.ps(es, "ops%d" % i, [128, 512]) for i in range(2)]
            bps = self.ps(es, "bps", [64, 512])
            P.dma("sp", kT[:], self.kT_d.rearrange("(g d) s -> d g s", d=64), r=["qkT_d"], w=["kT"], stream="kT")
            P.op("pool", lambda e: e.memset(vx[:], 1.0), w=["vx"])
            P.op("pool", lambda e: e.memset(onesr[:], 1.0), w=["onesr"])
            for g in range(2):
                P.dma("sp", vx[:, :, g, 0:64], self.v_d[:, g * 64:(g + 1) * 64].rearrange("(t p) d -> p t d", p=128),
                      r=[("tm_d", id(self.v_d))], w=["vx"], stream="vx")
            its = [(hq, qb, kt) for hq in range(8) for qb in range(NB) for kt in range(NT)]
            N_ = len(its)
            deferred = {}

            def emit_qk(j):
                hq, qb, kt = its[j]
                g, qb_, b = hq // 4, hq % 2, j % 3
                if qb == 0 and kt == 0:
                    for h2 in ([0, 1] if hq == 0 else [hq + 1]):
                        if h2 < 8:
                            P.dma("sp", qh[h2 % 2][:], self.qT_d[h2 * 64:(h2 + 1) * 64, :], r=["qkT_d"], w=["qh%d" % (h2 % 2)],
                                  stream="qh%d" % (h2 % 2))
                P.op("pe", lambda e: e.matmul(sps[b][:], kT[:, g, kt * 128:(kt + 1) * 128], qh[qb_][:, qb * 512:(qb + 1) * 512],
                                              start=True, stop=True), r=["kT", "qh%d" % qb_], w=["sps%d" % b])

            def tail(hq, qb, ob):
                P.op("pe", lambda e: e.matmul(bps[:], onesr[64:65, :], rc[64:65, :], start=True, stop=True),
                     r=["onesr", "rc"], w=["bps"])
                P.op("act", lambda e: e.activation(out=bcs[:], in_=bps[:], func=AF.Copy), r=["bps"], w=["bcs"])
                P.op("dve", lambda e: e.tensor_tensor(out=ya[ob][:], in0=ops_[ob][0:64, :], in1=bcs[:], op=ALU.mult),
                     r=["ops%d" % ob, "bcs"], w=["ya%d" % ob])
                P.dma("sp", self.yaT_d[hq * 64:(hq + 1) * 64, qb * 512:(qb + 1) * 512], ya[ob][:], r=["ya%d" % ob],
                      w=["yaT_d"], stream="ya%d" % ob)

            emit_qk(0)
            emit_qk(1)
            for j in range(N_):
                hq, qb, kt = its[j]
                g, b = hq // 4, j % 3
                ob = (hq * NB + qb) % 2
                if j + 2 < N_:
                    emit_qk(j + 2)
                P.op("act", lambda e, b=b: e.activation(out=pT[b][:], in_=sps[b][:], func=AF.Exp),
                     r=["sps%d" % b], w=["pT%d" % b])
                P.op("pe", lambda e, b=b, kt=kt, g=g, ob=ob: e.matmul(ops_[ob][0:65, :], vx[:, kt, g, :], pT[b][:],
                                                          start=(kt == 0), stop=(kt == NT - 1)),
                     r=["vx", "pT%d" % b], w=["ops%d" % ob], inc=(kt == NT - 1))
                if kt == NT - 1:
                    P.op("dve", lambda e, ob=ob: e.reciprocal(out=rc[64:65, :], in_=ops_[ob][64:65, :]), r=["ops%d" % ob], w=["rc"])
                    deferred[min(j + 2, N_ - 1)] = (hq, qb, ob)
                if j in deferred:
                    tail(*deferred.pop(j))
            assert not deferred
            P.flush()

    def phase_attn_b(self, l):
        P = self.P
        S, NT = self.S, self.NT
        rows = S // GRID_W
        wr_ = min(8, rows)
        NBUF = 4
        with ExitStack() as es:
            qT = self.sb(es, "bqT", [64, 4, S], BF16)
            kT = self.sb(es, "bkT", [64, 4, S], BF16)
            v0 = self.sb(es, "bv0", [128, NT, 256], BF16)
            v1 = self.sb(es, "bv1", [128, NT, 256], BF16)
            bias = self.sb(es, "bbias", [64, 4, 960])
            sc = [self.sb(es, "bsc%d" % i, [64, 512]) for i in range(NBUF)]
            pr = [self.sb(es, "bpr%d" % i, [64, 512], BF16) for i in range(NBUF)]
            st = [self.sb(es, "bst%d" % i, [64, 4]) for i in range(NBUF)]
            dg = [self.sb(es, "bdg%d" % i, [64, 64], BF16) for i in range(NBUF)]
            pts = [self.sb(es, "bpts%d" % i, [128, 4, 64], BF16) for i in range(2)]
            yo = [self.sb(es, "byo%d" % i, [64, 4, 64], BF16) for i in range(2)]
            sp_ = [self.ps(es, "bsp%d" % i, [64, 512]) for i in range(NBUF)]
            ptp = [self.ps(es, "bptp%d" % i, [128, 4, 64]) for i in range(2)]
            op_ = [self.ps(es, "bop%d" % i, [64, 4, 64]) for i in range(2)]
            for h in range(4):
                P.dma("sp", qT[:, h, :], self.bqT_d[h * 64:(h + 1) * 64, :], r=[("fm_d", id(self.bqT_d))], w=["bqT"], stream="bqT")
                P.dma("sp", kT[:, h, :], self.bkT_d[h * 64:(h + 1) * 64, :], r=[("fm_d", id(self.bkT_d))], w=["bkT"], stream="bkT")
            P.dma("sp", v0[:], self.bv_d.rearrange("(t p) c -> p t c", p=128), r=[("tm_d", id(self.bv_d))], w=["bv0"], stream="bv0")
            P.dma("sp", v1[:, 0:NT - 1, :], self.bv_d[64:S - 64, :].rearrange("(t p) c -> p t c", p=128), r=[("tm_d", id(self.bv_d))],
                  w=["bv1"], stream="bv1")
            P.dma("sp", bias[:], self.bias_b[l].rearrange("h q k -> q h k"), w=["bbias"], stream="bbias")
            its = [(r, h) for r in range(rows) for h in range(4)]
            N_ = len(its)

            def geo(r):
                r0 = min(max(r - wr_ // 2, 0), rows - wr_)
                return r0, (r0 - r + 7) * 64, r0 * 64

            def s1(i):
                r, h = its[i]
                b = i % NBUF
                r0, d0, k0 = geo(r)
                P.op("pe", lambda e: e.matmul(sp_[b][:], qT[:, h, r * 64:(r + 1) * 64], kT[:, h, k0:k0 + 512], start=True, stop=True),
                     r=["bqT", "bkT"], w=["bsp%d" % b])
                P.op("dve", lambda e: e.tensor_tensor(out=sc[b][:], in0=sp_[b][:], in1=bias[:, h, d0:d0 + 512], op=ALU.add),
                     r=["bsp%d" % b, "bbias"], w=["bsc%d" % b])
                P.op("dve", lambda e: e.tensor_reduce(out=st[b][:, 0:1], in_=sc[b][:], axis=AX.X, op=ALU.max),
                     r=["bsc%d" % b], w=[("bst", b, 0)])
                P.op("dve", lambda e: e.tensor_scalar(out=st[b][:, 1:2], in0=st[b][:, 0:1], scalar1=-1.0, scalar2=None, op0=ALU.mult),
                     r=[("bst", b, 0)], w=[("bst", b, 1)])
                P.op("act", lambda e: e.activation(out=pr[b][:], in_=sc[b][:], func=AF.Exp, bias=st[b][:, 1:2], scale=1.0,
                                                   accum_out=st[b][:, 2:3]), r=["bsc%d" % b, ("bst", b, 1)], w=["bpr%d" % b, ("bst", b, 2)])

            def s1b(i):
                b = i % NBUF
                P.op("dve", lambda e: e.reciprocal(out=st[b][:, 3:4], in_=st[b][:, 2:3]), r=[("bst", b, 2)], w=[("bst", b, 3)])
                P.op("dve", lambda e: e.tensor_scalar(out=dg[b][:], in0=self.identb[0:64, 0:64], scalar1=st[b][:, 3:4], scalar2=None,
                                                      op0=ALU.mult), r=["identb", ("bst", b, 3)], w=["bdg%d" % b])

            def s2(i):
                b, pb = i % NBUF, i % 2
                for kc in range(4):
                    P.op("pe", lambda e, kc=kc: e.matmul(ptp[pb][:, kc, :], pr[b][:, kc * 128:(kc + 1) * 128], dg[b][:], start=True, stop=True),
                         r=["bpr%d" % b, "bdg%d" % b], w=["bptp%d" % pb], inc=(kc == 3))
                P.op("act", lambda e: e.activation(out=pts[pb][:], in_=ptp[pb][:], func=AF.Copy), r=["bptp%d" % pb], w=["bpts%d" % pb])

            def s3(i):
                r, h = its[i]
                pb, ob = i % 2, r % 2
                r0, d0, k0 = geo(r)
                vsrc, vres, t0 = (v0, "bv0", r0 // 2) if r0 % 2 == 0 else (v1, "bv1", (r0 - 1) // 2)
                for kc in range(4):
                    P.op("pe", lambda e, kc=kc: e.matmul(op_[ob][:, h, :], vsrc[:, t0 + kc, h * 64:(h + 1) * 64], pts[pb][:, kc, :],
                                                         start=(kc == 0), stop=(kc == 3)),
                         r=[vres, "bpts%d" % pb], w=["bop%d" % ob], inc=(kc == 3))
                if h == 3:
                    P.op("act", lambda e: e.activation(out=yo[ob][:], in_=op_[ob][:], func=AF.Copy), r=["bop%d" % ob], w=["byo%d" % ob])
                    P.dma("sp", self.ybT_d[:, r * 64:(r + 1) * 64].rearrange("(h d) q -> d h q", d=64), yo[ob][:], r=["byo%d" % ob],
                          w=["ybT_d"], stream="byo%d" % ob)

            for t in range(N_ + 4):
                if t < N_:
                    s1(t)
                if 0 <= t - 1 < N_:
                    s1b(t - 1)
                if 0 <= t - 3 < N_:
                    s2(t - 3)
                if 0 <= t - 4 < N_:
                    s3(t - 4)
            P.flush()

    def phase_gdn(self, l):
        P = self.P
        S, NT, NB = self.S, self.NT, self.NB
        nc = self.nc
        if not hasattr(self, "cn_d"):
            mk = lambda n, s: nc.dram_tensor(n, list(s), F32, kind=("ExternalOutput" if n in self.dbg else "Internal")).ap()
            self.cn_d = mk("cn_d", [768, S])
            self.ktok_d = mk("ktok_d", [S, 256])
            self.vtok_d = mk("vtok_d", [S, 256])
            self.gates_d = mk("gates_d", [S, 16])
            self.o_d = mk("o_d", [2, S, 256])
        with ExitStack() as es:
            cw = self.sb(es, "cw", [128, 6, 5])
            x = self.sb(es, "gx", [128, S + 4])
            y = self.sb(es, "gy", [128, S])
            sq = self.sb(es, "gsq", [128, 512], BF16)
            rs = self.sb(es, "grs", [128, 512])
            blk = self.sb(es, "gblk", [128, 128], BF16)
            tk = [self.sb(es, "gtk%d" % i, [128, 512]) for i in range(2)]
            gin = self.sb(es, "gin", [128, NT, 16])
            gout = self.sb(es, "gout", [128, NT, 16])
            ab = self.sb(es, "gab", [128, 2, 8])
            pss = self.ps(es, "gpss", [128, 512])
            ptr = [self.ps(es, "gptr%d" % i, [128, 4, 128]) for i in range(2)]
            P.dma("sp", cw[:], self.conv_w[l], w=["cw"], stream="cw")
            P.dma("pool", blk[:], self.c_blk, w=["gblk"], stream="gblk")
            P.dma("sp", ab[:], self.gdn_ab[l].partition_broadcast(128), w=["gab"], stream="gab")
            P.op("pool", lambda e: e.memset(x[:, 0:2], 0.0), w=["gxp"])
            P.op("pool", lambda e: e.memset(x[:, S + 2:S + 4], 0.0), w=["gxp"])
            ti = 0
            import os
            for ch in range(6 if os.environ.get("GDBG", "") != "gates" else 0):
                P.dma("sp", x[:, 2:S + 2], self.cT_d[ch * 128:(ch + 1) * 128, :], r=[("fm_d", id(self.cT_d))], w=["gx"], stream="gx")
                P.op("act", lambda e, ch=ch: e.activation(out=y[:], in_=x[:, 0:S], func=AF.Identity, scale=cw[:, ch, 0:1]),
                     r=["gx", "gxp", "cw"], w=["gy"])
                for k in range(1, 5):
                    P.op("dve", lambda e, ch=ch, k=k: e.scalar_tensor_tensor(out=y[:], in0=x[:, k:k + S], scalar=cw[:, ch, k:k + 1], in1=y[:],
                                                                             op0=ALU.mult, op1=ALU.add), r=["gx", "gxp", "cw", "gy"], w=["gy"])
                P.op("act", lambda e: e.activation(out=y[:], in_=y[:], func=AF.Silu), r=["gy"], w=["gy"])
                if ch < 4:
                    isq = ch < 2
                    for tb in range(NB):
                        tsl = slice(tb * 512, (tb + 1) * 512)
                        P.op("act", lambda e, tsl=tsl: e.activation(out=sq[:], in_=y[:, tsl], func=AF.Square), r=["gy"], w=["gsq"])
                        P.op("pe", lambda e: e.matmul(pss[:], blk[:], sq[:], start=True, stop=True), r=["gblk", "gsq"], w=["gpss"])
                        P.op("act", lambda e, isq=isq: e.activation(out=rs[:], in_=pss[:], func=AF.Sqrt, bias=(64e-6 if isq else 1e-6),
                                                                    scale=(64.0 if isq else 1.0)), r=["gpss"], w=["grs"])
                        P.op("dve", lambda e: e.reciprocal(out=rs[:], in_=rs[:]), r=["grs"], w=["grs"])
                        P.op("dve", lambda e, tsl=tsl: e.tensor_tensor(out=y[:, tsl], in0=y[:, tsl], in1=rs[:], op=ALU.mult),
                             r=["gy", "grs"], w=["gy"])
                P.dma("sp", self.cn_d[ch * 128:(ch + 1) * 128, :], y[:], r=["gy"], w=["cn_d"], stream="gy")
                if ch >= 2:
                    dst = self.ktok_d if ch < 4 else self.vtok_d
                    for tb in range(NB):
                        b = ti % 2
                        ti += 1
                        for j in range(4):
                            tt = tb * 4 + j
                            P.op("pe", lambda e, b=b, j=j, tt=tt: e.transpose(ptr[b][:, j, :], y[:, tt * 128:(tt + 1) * 128], self.ident[:]),
                                 r=["gy", "ident"], w=["gptr%d" % b], inc=(j == 3))
                        P.op("act", lambda e, b=b: e.activation(out=tk[b][:], in_=ptr[b][:].rearrange("p a b -> p (a b)"), func=AF.Copy),
                             r=["gptr%d" % b], w=["gtk%d" % b])
                        P.dma("sp", dst[tb * 512:(tb + 1) * 512, (ch % 2) * 128:(ch % 2 + 1) * 128].rearrange("(j p) c -> p j c", p=128),
                              tk[b][:].rearrange("p (j c) -> p j c", c=128), r=["gtk%d" % b], w=["kvtok_d"], stream="gtk%d" % b)
            if os.environ.get("GDBG", "") == "conv":
                P.flush()
                return
            P.dma("sp", gin[:], self.cz_d[:, 256:272].rearrange("(t p) c -> p t c", p=128), r=[("tm_d", id(self.cz_d))], w=["gin"], stream="gin")
            P.op("act", lambda e: e.activation(out=gout[:, :, 0:8], in_=gin[:, :, 0:8], func=AF.Sigmoid), r=["gin"], w=["gout_b"])
            P.op("dve", lambda e: e.tensor_tensor(out=gin[:, :, 8:16], in0=gin[:, :, 8:16], in1=bc(ab[:, 1:2, :], [128, NT, 8]), op=ALU.add),
                 r=["gin", "gab"], w=["gin2"])
            P.op("act", lambda e: e.activation(out=gin[:, :, 8:16], in_=gin[:, :, 8:16], func=AF.Exp), r=["gin2"], w=["gin2"])
            P.op("act", lambda e: e.activation(out=gin[:, :, 8:16], in_=gin[:, :, 8:16], func=AF.Ln, bias=1.0, scale=1.0), r=["gin2"], w=["gin2"])
            P.op("act", lambda e: e.activation(out=ab[:, 0, :], in_=ab[:, 0, :], func=AF.Exp), r=["gab"], w=["gab0"])
            P.op("dve", lambda e: e.scalar_tensor_tensor(out=gout[:, :, 8:16], in0=gin[:, :, 8:16], scalar=-1.0, in1=bc(ab[:, 0:1, :], [128, NT, 8]),
                                                         op0=ALU.mult, op1=ALU.mult), r=["gin2", "gab0"], w=["gout_g"])
            P.dma("sp", self.gates_d.rearrange("(t p) c -> p t c", p=128), gout[:], r=["gout_b", "gout_g"], w=["gates_d"], stream="gout")
            P.flush()
        if getattr(self, "gdn_stop", None) == "prep":
            return
        NC = S // 64
        with ExitStack() as es:
            cst = self.sb(es, "gcst", [64, 6, 8, 64])
            NEGM, NEGMT, STRICT, ID8 = cst[:, 0], cst[:, 1], cst[:, 2], cst[:, 3]
            CUMF, CUMB, ONES = cst[:, 4, 0, :], cst[:, 4, 1, :], cst[:, 5, 0, :]
            ld = [dict(KT=self.sb(es, "gKT%d" % i, [64, 8, 64]), QT=self.sb(es, "gQT%d" % i, [64, 8, 64]),
                       Kt=self.sb(es, "gKt%d" % i, [64, 8, 64]), Vt=self.sb(es, "gVt%d" % i, [64, 8, 64]),
                       gb=self.sb(es, "ggb%d" % i, [64, 16])) for i in range(2)]
            T8 = lambda n: self.sb(es, n, [64, 8, 64])
            sm = self.sb(es, "gsm", [64, 48])
            diagG, Dm, DTm, eGr, t_a, t_b, SBm, qkTm, QgT = (T8(n) for n in ("gdiag", "gD", "gDT", "geGr", "gta", "gtb", "gSB", "gqkTm", "gQgT"))
            X = [T8("gX0"), T8("gX1")]
            XT = [T8("gXT0"), T8("gXT1")]
            PT, rv, rk, U, WT, Vn, Kd, St, ot = (T8(n) for n in ("gPT", "grv", "grk", "gU", "gWT", "gVn", "gKd", "gS", "gO"))
            psA = self.ps(es, "gpsA", [64, 16])
            pb = [self.ps(es, "gpb%d" % i, [64, 8, 64]) for i in range(1, 8)]
            psGrow, ps2, ps3, ps4, ps5, ps6, ps7 = pb
            fl = lambda t: t[:].rearrange("p c j -> p (c j)")
            P.dma("sp", cst[:], self.c_gdn, w=["gcst"], stream="gcst")
            P.op("pool", lambda e: e.memset(St[:], 0.0), w=["gS"])

            def mm8(ps, psres, lhs, lres, rhs, rres):
                for c in range(8):
                    P.op("pe", lambda e, c=c: e.matmul(ps[:, c, :], lhs[:, c, :], rhs[:, c, :], start=True, stop=True),
                         r=[lres, rres], w=[psres], inc=(c == 7))

            def bcg(col0):
                return lambda t: bc(t[:, col0:col0 + 8].unsqueeze(2), [64, 8, 64])

            _lim = int(os.environ.get('GLIM', '99'))
            for s_ in range(NC):
                a, b = s_, NC - 1 - s_
                L_ = ld[s_ % 2]
                sfx = str(s_ % 2)
                ra, rb_ = slice(a * 64, (a + 1) * 64), slice(b * 64, (b + 1) * 64)
                for nm, src, r0 in (("KT", self.cn_d, 256), ("QT", self.cn_d, 0)):
                    for half, rr in ((0, ra), (1, rb_)):
                        P.dma("sp", L_[nm][:, half * 4:(half + 1) * 4, :], src[r0:r0 + 256, rr].rearrange("(h d) t -> d h t", d=64),
                              r=["cn_d"], w=["g" + nm + sfx], stream="g" + nm + sfx)
                for nm, src in (("Kt", self.ktok_d), ("Vt", self.vtok_d)):
                    for half, rr in ((0, ra), (1, rb_)):
                        P.dma("sp", L_[nm][:, half * 4:(half + 1) * 4, :], src[rr, :].rearrange("t (h d) -> t h d", d=64),
                              r=["kvtok_d"], w=["g" + nm + sfx], stream="g" + nm + sfx)
                for c0, rr in ((0, ra), (4, rb_), (8, ra), (12, rb_)):
                    P.dma("sp", L_["gb"][:, c0:c0 + 4], self.gates_d[rr, c0:c0 + 4], r=["gates_d"], w=["ggb" + sfx], stream="ggb" + sfx)
                KT, QT, Kt, Vt, gb = L_["KT"], L_["QT"], L_["Kt"], L_["Vt"], L_["gb"]
                rKT, rQT, rKt, rVt, rgb = ("g" + n + sfx for n in ("KT", "QT", "Kt", "Vt", "gb"))
                P.op("pe", lambda e, gb=gb: e.matmul(psA[:, 0:4], CUMF, gb[:, 8:12], start=True, stop=True), r=["gcst", rgb], w=["gpsA"], inc=False)
                P.op("pe", lambda e, gb=gb: e.matmul(psA[:, 4:8], CUMB, gb[:, 12:16], start=True, stop=True), r=["gcst", rgb], w=["gpsA"], inc=False)
                P.op("pe", lambda e, gb=gb: e.matmul(psA[:, 8:16], ONES, gb[:, 8:16], start=True, stop=True), r=["gcst", rgb], w=["gpsA"])
                P.op("act", lambda e: e.activation(out=sm[:, 0:16], in_=psA[:], func=AF.Copy), r=["gpsA"], w=["gsmG"])
                P.op("act", lambda e: e.activation(out=sm[:, 16:32], in_=sm[:, 0:16], func=AF.Exp), r=["gsmG"], w=["gsmE"])
                P.op("dve", lambda e: e.tensor_tensor(out=sm[:, 32:40], in0=sm[:, 8:16], in1=sm[:, 0:8], op=ALU.subtract), r=["gsmG"], w=["gsmK"])
                P.op("act", lambda e: e.activation(out=sm[:, 32:40], in_=sm[:, 32:40], func=AF.Exp), r=["gsmK"], w=["gsmK"])
                P.op("dve", lambda e, gb=gb: e.tensor_tensor(out=sm[:, 40:48], in0=gb[:, 0:8], in1=sm[:, 16:24], op=ALU.mult), r=[rgb, "gsmE"], w=["gsmB"])
                Gb, eGtb, kdb, bkb = bcg(0)(sm), bcg(24)(sm), bcg(32)(sm), bcg(40)(sm)
                betab = bc(gb[:, 0:8].unsqueeze(2), [64, 8, 64])
                if _lim < 2:
                    continue
                P.op("pool", lambda e, Gb=Gb: e.tensor_tensor(out=diagG[:], in0=ID8, in1=Gb, op=ALU.mult), r=["gcst", "gsmG"], w=["gdiag"])
                P.op("pe", lambda e: e.matmul(fl(psGrow), ONES, fl(diagG), start=True, stop=True), r=["gcst", "gdiag"], w=["gpsGrow"])
                P.op("dve", lambda e: e.scalar_tensor_tensor(out=t_a[:], in0=psGrow[:], scalar=-1.0, in1=NEGM, op0=ALU.mult, op1=ALU.add),
                     r=["gpsGrow", "gcst"], w=["gta"])
                P.op("dve", lambda e, Gb=Gb: e.tensor_tensor(out=t_a[:], in0=t_a[:], in1=Gb, op=ALU.add), r=["gta", "gsmG"], w=["gta"])
                P.op("act", lambda e: e.activation(out=Dm[:], in_=t_a[:], func=AF.Exp), r=["gta"], w=["gD"])
                P.op("dve", lambda e: e.tensor_tensor(out=t_b[:], in0=psGrow[:], in1=NEGMT, op=ALU.add), r=["gpsGrow", "gcst"], w=["gtb"])
                P.op("dve", lambda e, Gb=Gb: e.tensor_tensor(out=t_b[:], in0=t_b[:], in1=Gb, op=ALU.subtract), r=["gtb", "gsmG"], w=["gtb"])
                P.op("act", lambda e: e.activation(out=DTm[:], in_=t_b[:], func=AF.Exp), r=["gtb"], w=["gDT"])
                P.op("act", lambda e: e.activation(out=eGr[:], in_=psGrow[:], func=AF.Exp), r=["gpsGrow"], w=["geGr"])
                P.op("pool", lambda e, QT=QT: e.tensor_tensor(out=QgT[:], in0=QT[:], in1=eGr[:], op=ALU.mult), r=[rQT, "geGr"], w=["gQgT"])
                if _lim < 4:
                    continue
                mm8(ps2, "gps2", KT, rKT, KT, rKT)
                mm8(ps3, "gps3", KT, rKT, QT, rQT)
                P.op("pool", lambda e, betab=betab: e.tensor_tensor(out=SBm[:], in0=STRICT, in1=betab, op=ALU.mult), r=["gcst", rgb], w=["gSB"])
                P.op("dve", lambda e: e.tensor_tensor(out=t_a[:], in0=ps2[:], in1=Dm[:], op=ALU.mult), r=["gps2", "gD"], w=["gta"])
                P.op("pool", lambda e: e.tensor_tensor(out=X[0][:], in0=t_a[:], in1=SBm[:], op=ALU.mult), r=["gta", "gSB"], w=["gX0"])
                P.op("dve", lambda e: e.tensor_tensor(out=qkTm[:], in0=ps3[:], in1=DTm[:], op=ALU.mult), r=["gps3", "gDT"], w=["gqkTm"])
                if _lim < 5:
                    continue
                for c in range(8):
                    P.op("pe", lambda e, c=c: e.matmul(ps2[:, c, :], X[0][:, c, :], cst[:, 3, c, :], start=True, stop=True), r=["gX0", "gcst"],
                         w=["gps2"], inc=(c == 7))
                P.op("act", lambda e: e.activation(out=XT[0][:], in_=ps2[:], func=AF.Copy), r=["gps2"], w=["gXT0"])
                P.op("dve", lambda e: e.scalar_tensor_tensor(out=PT[:], in0=ps2[:], scalar=-1.0, in1=ID8, op0=ALU.mult, op1=ALU.add),
                     r=["gps2", "gcst", "gXT0"], w=["gPT"])
                if _lim < 6:
                    continue
                for lv in range(1, 6):
                    ci, ni = (lv - 1) % 2, lv % 2
                    mm8(ps4, "gps4", XT[ci], "gXT%d" % ci, X[ci], "gX%d" % ci)
                    if lv < 5:
                        mm8(ps5, "gps5", X[ci], "gX%d" % ci, XT[ci], "gXT%d" % ci)
                    P.op("act", lambda e, ni=ni: e.activation(out=X[ni][:], in_=ps4[:], func=AF.Copy), r=["gps4"], w=["gX%d" % ni])
                    if lv < 5:
                        P.op("dve", lambda e, ni=ni: e.tensor_copy(out=XT[ni][:], in_=ps5[:]), r=["gps5"], w=["gXT%d" % ni])
                    mm8(ps6, "gps6", X[ni], "gX%d" % ni, PT, "gPT")
                    P.op("dve", lambda e: e.tensor_tensor(out=PT[:], in0=PT[:], in1=ps6[:], op=ALU.add), r=["gPT", "gps6"], w=["gPT"])
                if _lim < 7:
                    continue
                P.op("pool", lambda e, Vt=Vt, betab=betab: e.tensor_tensor(out=rv[:], in0=Vt[:], in1=betab, op=ALU.mult), r=[rVt, rgb], w=["grv"])
                P.op("pool", lambda e, Kt=Kt, bkb=bkb: e.tensor_tensor(out=rk[:], in0=Kt[:], in1=bkb, op=ALU.mult), r=[rKt, "gsmB"], w=["grk"])
                mm8(ps4, "gps4", PT, "gPT", rv, "grv")
                P.op("act", lambda e: e.activation(out=U[:], in_=ps4[:], func=AF.Copy), r=["gps4"], w=["gU"])
                mm8(ps5, "gps5", rk, "grk", PT, "gPT")
                P.op("dve", lambda e: e.tensor_copy(out=WT[:], in_=ps5[:]), r=["gps5"], w=["gWT"])
                if _lim < 8:
                    continue
                mm8(ps6, "gps6", WT, "gWT", St, "gS")
                P.op("dve", lambda e: e.scalar_tensor_tensor(out=Vn[:], in0=ps6[:], scalar=-1.0, in1=U[:], op0=ALU.mult, op1=ALU.add),
                     r=["gps6", "gU"], w=["gVn"])
                for c in range(8):
                    P.op("pe", lambda e, c=c: e.matmul(ps7[:, c, :], QgT[:, c, :], St[:, c, :], start=True, stop=False), r=["gQgT", "gS"], w=["gps7"],
                         inc=False)
                    P.op("pe", lambda e, c=c: e.matmul(ps7[:, c, :], qkTm[:, c, :], Vn[:, c, :], start=False, stop=True), r=["gqkTm", "gVn"],
                         w=["gps7"], inc=(c == 7))
                P.op("act", lambda e: e.activation(out=ot[:], in_=ps7[:], func=AF.Copy), r=["gps7"], w=["gO"])
                P.dma("sp", self.o_d[0, ra, :].rearrange("t (h d) -> t h d", d=64), ot[:, 0:4, :], r=["gO"], w=["o_d"], stream="gO")
                P.dma("sp", self.o_d[1, rb_, :].rearrange("t (h d) -> t h d", d=64), ot[:, 4:8, :], r=["gO"], w=["o_d"], stream="gO")
                P.op("pool", lambda e, Kt=Kt, kdb=kdb: e.tensor_tensor(out=Kd[:], in0=Kt[:], in1=kdb, op=ALU.mult), r=[rKt, "gsmK"], w=["gKd"])
                mm8(ps3, "gps3", Kd, "gKd", Vn, "gVn")
                P.op("pool", lambda e, eGtb=eGtb: e.tensor_tensor(out=St[:], in0=St[:], in1=eGtb, op=ALU.mult), r=["gS", "gsmE"], w=["gS"])
                P.op("dve", lambda e: e.tensor_tensor(out=St[:], in0=St[:], in1=ps3[:], op=ALU.add), r=["gS", "gps3"], w=["gS"])
            P.flush()
        if getattr(self, "gdn_stop", None) == "main":
            return
        with ExitStack() as es:
            gg = self.sb(es, "ggn", [128, 64])
            of = [self.sb(es, "gof%d" % i, [128, 4, 64]) for i in range(2)]
            obk = [self.sb(es, "gob%d" % i, [128, 4, 64]) for i in range(2)]
            zt = [self.sb(es, "gz%d" % i, [128, 4, 64]) for i in range(2)]
            sq2 = self.sb(es, "gsq2", [128, 4, 64])
            ms = self.sb(es, "gms", [128, 8])
            yb_ = [self.sb(es, "gyb%d" % i, [128, 2, 128], BF16) for i in range(2)]
            pt2 = [self.ps(es, "gpt2%d" % i, [128, 2, 128]) for i in range(2)]
            P.dma("sp", gg[:], self.gdn_g[l].partition_broadcast(128), w=["ggn"], stream="ggn")
            for tt in range(NT):
                b = tt % 2
                tr = slice(tt * 128, (tt + 1) * 128)
                P.dma("sp", of[b][:], self.o_d[0, tr, :].rearrange("t (h d) -> t h d", d=64), r=["o_d"], w=["gof%d" % b], stream="gof%d" % b)
                P.dma("sp", obk[b][:], self.o_d[1, tr, :].rearrange("t (h d) -> t h d", d=64), r=["o_d"], w=["gob%d" % b], stream="gob%d" % b)
                P.dma("sp", zt[b][:], self.cz_d[tr, 0:256].rearrange("t (h d) -> t h d", d=64), r=[("tm_d", id(self.cz_d))], w=["gz%d" % b],
                      stream="gz%d" % b)
                P.op("dve", lambda e, b=b: e.tensor_tensor(out=of[b][:], in0=of[b][:], in1=obk[b][:], op=ALU.add), r=["gof%d" % b, "gob%d" % b],
                     w=["gof%d" % b])
                P.op("act", lambda e, b=b: e.activation(out=sq2[:], in_=of[b][:], func=AF.Square), r=["gof%d" % b], w=["gsq2"])
                P.op("dve", lambda e: e.tensor_reduce(out=ms[:, 0:4], in_=sq2[:], axis=AX.X, op=ALU.add), r=["gsq2"], w=["gms"])
                P.op("act", lambda e: e.activation(out=ms[:, 4:8], in_=ms[:, 0:4], func=AF.Sqrt, bias=1e-6, scale=1.0 / 64), r=["gms"], w=["gms2"])
                P.op("dve", lambda e: e.reciprocal(out=ms[:, 4:8], in_=ms[:, 4:8]), r=["gms2"], w=["gms2"])
                P.op("dve", lambda e, b=b: e.tensor_tensor(out=of[b][:], in0=of[b][:], in1=bc(ms[:, 4:8].unsqueeze(2), [128, 4, 64]), op=ALU.mult),
                     r=["gof%d" % b, "gms2"], w=["gof%d" % b])
                P.op("pool", lambda e, b=b: e.tensor_tensor(out=of[b][:], in0=of[b][:], in1=bc(gg[:].unsqueeze(1), [128, 4, 64]), op=ALU.mult),
                     r=["gof%d" % b, "ggn"], w=["gof%d" % b])
                P.op("act", lambda e, b=b: e.activation(out=zt[b][:], in_=zt[b][:], func=AF.Silu), r=["gz%d" % b], w=["gz%d" % b])
                P.op("dve", lambda e, b=b: e.tensor_tensor(out=of[b][:], in0=of[b][:], in1=zt[b][:], op=ALU.mult), r=["gof%d" % b, "gz%d" % b],
                     w=["gof%d" % b])
                for j in range(2):
                    P.op("pe", lambda e, b=b, j=j: e.transpose(pt2[b][:, j, :], of[b][:, 2 * j:2 * j + 2, :].rearrange("p a d -> p (a d)"), self.ident[:]),
                         r=["gof%d" % b, "ident"], w=["gpt2%d" % b], inc=(j == 1))
                P.op("act", lambda e, b=b: e.activation(out=yb_[b][:], in_=pt2[b][:], func=AF.Copy), r=["gpt2%d" % b], w=["gyb%d" % b])
                P.dma("sp", self.ycT_d[:, tr].rearrange("(j p) t -> p j t", p=128), yb_[b][:], r=["gyb%d" % b], w=["ycT_d"], stream="gyb%d" % b)
            P.flush()

    def phase_merge(self, l):
        P = self.P
        S, NB, NT = self.S, self.NB, self.NT
        with ExitStack() as es:
            wa = self.sb(es, "wa", [128, 4, D], BF16)
            wb = self.sb(es, "wb", [128, 2, D], BF16)
            wc = self.sb(es, "wc", [128, 2, D], BF16)
            wo = self.sb(es, "wo", [128, 8, D], BF16)
            yT = self.sb(es, "yT", [128, 8, 512], BF16)
            gt = [self.sb(es, "gt%d" % i, [128, 3, 512], BF16) for i in range(2)]
            mixT = self.sb(es, "mixT", [128, 8, 512], BF16)
            m1 = self.sb(es, "m1", [128, 512])
            m2 = self.sb(es, "m2", [128, 512])
            m3 = self.sb(es, "m3", [128, 512])
            hold = [self.sb(es, "hold%d" % i, [128, D]) for i in range(2)]
            tsum = [self.sb(es, "tsum%d" % i, [128, D]) for i in range(2)]
            gb = self.sb(es, "gb1", [128, 2, D])
            pabc = [self.ps(es, "pabc%d" % i, [128, 512]) for i in range(3)]
            po = [self.ps(es, "po%d" % i, [128, 512]) for i in range(2)]
            tiles = self.ln_scratch(es, "l1")
            rt = self.router_setup(es) if not getattr(self, "no_router", False) else None
            P.dma("pool", wa[:], self.wa[l].rearrange("(kc p) n -> p kc n", p=128), w=["wa"], stream="wa")
            P.dma("pool", wb[:], self.wb[l].rearrange("(kc p) n -> p kc n", p=128), w=["wb"], stream="wb")
            P.dma("pool", wc[:], self.wc[l].rearrange("(kc p) n -> p kc n", p=128), w=["wc"], stream="wc")
            P.dma("pool", wo[:], self.wo[l].rearrange("(kc p) n -> p kc n", p=128), w=["wo"], stream="wo")
            P.dma("sp", gb[:], self.ln1[l].partition_broadcast(128), w=["gb1"], stream="gb1")
            if "B" not in self.parts:
                P.op("pool", lambda e: e.memset(yT[:, 4:6, :], 0.0), w=["yTb"])
            if "C" not in self.parts:
                P.op("pool", lambda e: e.memset(yT[:, 6:8, :], 0.0), w=["yTc"])
            for tb in range(NB):
                tsl = slice(tb * 512, (tb + 1) * 512)
                if "A" in self.parts:
                    P.dma("sp", yT[:, 0:4, :], self.yaT_d[:, tsl].rearrange("(kc p) s -> p kc s", p=128), r=["yaT_d"], w=["yTa"],
                          stream="yTa")
                else:
                    P.op("pool", lambda e: e.memset(yT[:, 0:4, :], 0.0), w=["yTa"])
                if "B" in self.parts:
                    P.dma("sp", yT[:, 4:6, :], self.ybT_d[:, tsl].rearrange("(kc p) s -> p kc s", p=128), r=["ybT_d"], w=["yTb"],
                          stream="yTb")
                if "C" in self.parts:
                    P.dma("sp", yT[:, 6:8, :], self.ycT_d[:, tsl].rearrange("(kc p) s -> p kc s", p=128), r=["ycT_d"], w=["yTc"],
                          stream="yTc")
                for dc in range(8):
                    b = dc % 2
                    P.dma("sp", gt[b][:], self.gT_d[:, tsl].rearrange("(t c p) s -> p t c s", p=128, t=3)[:, :, dc, :],
                          r=[("fm_d", id(self.gT_d))], w=["gt%d" % b], stream="gt%d" % b)
                    csl = slice(dc * 128, (dc + 1) * 128)
                    self.mm_group(pabc[0][:], [(wa[:, kc, csl], yT[:, kc, :]) for kc in range(4)], r=["wa", "yTa"], w=["pabc0"])
                    self.mm_group(pabc[1][:], [(wb[:, kc, csl], yT[:, 4 + kc, :]) for kc in range(2)], r=["wb", "yTb"], w=["pabc1"])
                    self.mm_group(pabc[2][:], [(wc[:, kc, csl], yT[:, 6 + kc, :]) for kc in range(2)], r=["wc", "yTc"], w=["pabc2"])
                    P.op("dve", lambda e, b=b: e.tensor_tensor(out=m1[:], in0=pabc[0][:], in1=gt[b][:, 0, :], op=ALU.mult),
                         r=["pabc0", "gt%d" % b], w=["m1"])
                    P.op("dve", lambda e, b=b: e.tensor_tensor(out=m2[:], in0=pabc[1][:], in1=gt[b][:, 1, :], op=ALU.mult),
                         r=["pabc1", "gt%d" % b], w=["m2"])
                    P.op("dve", lambda e, b=b: e.tensor_tensor(out=m3[:], in0=pabc[2][:], in1=gt[b][:, 2, :], op=ALU.mult),
                         r=["pabc2", "gt%d" % b], w=["m3"])
                    P.op("pool", lambda e: e.tensor_tensor(out=m1[:], in0=m1[:], in1=m2[:], op=ALU.add), r=["m1", "m2"], w=["m1"])
                    P.op("pool", lambda e, dc=dc: e.tensor_tensor(out=mixT[:, dc, :], in0=m1[:], in1=m3[:], op=ALU.add),
                         r=["m1", "m3"], w=[("mixT", dc)])
                for ti in range(4):
                    tt = tb * 4 + ti
                    hb = tt % 2
                    P.dma("sp", hold[hb][:], self.h_d[tt * 128:(tt + 1) * 128, :], r=[("h_d", tt)], w=["hold%d" % hb],
                          stream="hold%d" % hb)
                    for half in range(2):
                        self.mm_group(po[half][:], [(mixT[:, dc, ti * 128:(ti + 1) * 128], wo[:, dc, half * 512:(half + 1) * 512])
                                                    for dc in range(8)],
                                      r=["wo"] + [("mixT", dc) for dc in range(8)], w=["po%d" % half])
                        P.op("dve", lambda e, hb=hb, half=half: e.scalar_tensor_tensor(
                            out=tsum[hb][:, half * 512:(half + 1) * 512], in0=hold[hb][:, half * 512:(half + 1) * 512],
                            scalar=ALPHA, in1=po[half][:], op0=ALU.mult, op1=ALU.add),
                            r=["hold%d" % hb, "po%d" % half], w=["tsum%d" % hb])
                    self.ln_tile(tiles, tsum[hb], "tsum%d" % hb, gb, "gb1", self.h_d, tt, router=rt, pfx="l1")
            P.flush()

    def router_setup(self, es):
        P = self.P
        wr = self.sb(es, "wr", [128, KC, NE])
        wrh = self.sb(es, "wrh", [128, KC, NE], BF16)
        wrl = self.sb(es, "wrl", [128, KC, NE], BF16)
        rb = self.sb(es, "rb", [128, NE])
        hlo = self.sb(es, "hlo", [128, KC, 128], BF16)
        pl = self.ps(es, "pl", [128, NE])
        sc = self.sb(es, "r_sc", [128, NE])
        sel = self.sb(es, "r_sel", [128, NE])
        tmp = self.sb(es, "r_tmp", [128, NE])
        tmp2 = self.sb(es, "r_tmp2", [128, NE])
        g8 = self.sb(es, "r_g8", [128, 8, 4])
        oh1 = self.sb(es, "r_oh1", [128, NE])
        oh2 = self.sb(es, "r_oh2", [128, NE])
        sm = self.sb(es, "r_sm", [128, 8])
        P.dma("sp", wr[:], self.w_router.rearrange("(kc p) n -> p kc n", p=128), w=["wr"], stream="wr")
        P.dma("sp", rb[:], self.router_bias.partition_broadcast(128), w=["rb"], stream="rb")
        P.dma("pool", wrh[:], self.w_router.rearrange("(kc p) n -> p kc n", p=128), w=["wrh"], stream="wrh")
        P.op("dve", lambda e: e.tensor_tensor(out=wrl[:], in0=wr[:], in1=wrh[:], op=ALU.subtract), r=["wr", "wrh"], w=["wrl"])
        BIG = 1.0e4

        def router(tt, pT, pTres):
            tsl = slice(tt * 128, (tt + 1) * 128)
            P.op("dve", lambda e: e.tensor_tensor(out=hlo[:], in0=pT[:], in1=self.hT[:, :, tsl], op=ALU.subtract),
                 r=[pTres, ("hT", tt)], w=["hlo"])
            pairs = []
            for kc in range(KC):
                pairs += [(self.hT[:, kc, tsl], wrh[:, kc, :]), (self.hT[:, kc, tsl], wrl[:, kc, :]), (hlo[:, kc, :], wrh[:, kc, :])]
            self.mm_group(pl[:], pairs, r=["hlo", ("hT", tt), "wrh", "wrl"], w=["pl"])
            P.op("act", lambda e: e.activation(out=sc[:], in_=pl[:], func=AF.Sigmoid), r=["pl"], w=["r_sc"])
            P.op("dve", lambda e: e.tensor_tensor(out=sel[:], in0=sc[:], in1=rb[:], op=ALU.add), r=["r_sc", "rb"], w=["r_sel"])
            s3 = sel[:].rearrange("p (g k) -> p g k", k=4)
            t3 = tmp[:].rearrange("p (g k) -> p g k", k=4)
            P.op("dve", lambda e: e.tensor_reduce(out=sm[:, 0:8], in_=s3, axis=AX.X, op=ALU.max), r=["r_sel"], w=["r_sm"])
            P.op("dve", lambda e: e.tensor_tensor(out=t3, in0=s3, in1=bc(sm[:, 0:8].unsqueeze(2), [128, 8, 4]), op=ALU.is_equal),
                 r=["r_sel", "r_sm"], w=["r_tmp"])
            P.op("dve", lambda e: e.scalar_tensor_tensor(out=tmp[:], in0=tmp[:], scalar=-BIG, in1=sel[:], op0=ALU.mult, op1=ALU.add),
                 r=["r_tmp", "r_sel"], w=["r_tmp"])
            P.op("dve", lambda e: e.tensor_reduce(out=g8[:, :, 0], in_=t3, axis=AX.X, op=ALU.max), r=["r_tmp"], w=["r_g8"])
            P.op("dve", lambda e: e.tensor_tensor(out=g8[:, :, 1], in0=g8[:, :, 0], in1=sm[:, 0:8], op=ALU.add),
                 r=["r_g8", "r_sm"], w=["r_g8b"])
            P.op("dve", lambda e: e.tensor_reduce(out=sm[:, 0:1], in_=g8[:, :, 1], axis=AX.X, op=ALU.max), r=["r_g8b"], w=["r_sm1"])
            P.op("dve", lambda e: e.tensor_scalar(out=g8[:, :, 2], in0=g8[:, :, 1], scalar1=sm[:, 0:1], scalar2=None, op0=ALU.is_lt),
                 r=["r_g8b", "r_sm1"], w=["r_g8c"])
            P.op("dve", lambda e: e.scalar_tensor_tensor(out=t3, in0=bc(g8[:, :, 2:3], [128, 8, 4]), scalar=-BIG, in1=s3,
                                                         op0=ALU.mult, op1=ALU.add), r=["r_g8c", "r_sel"], w=["r_tmp"])
            P.op("dve", lambda e: e.tensor_reduce(out=sm[:, 1:2], in_=tmp[:], axis=AX.X, op=ALU.max), r=["r_tmp"], w=["r_sm2"])
            P.op("dve", lambda e: e.tensor_scalar(out=oh1[:], in0=tmp[:], scalar1=sm[:, 1:2], scalar2=None, op0=ALU.is_equal),
                 r=["r_tmp", "r_sm2"], w=["r_oh1"])
            P.op("dve", lambda e: e.scalar_tensor_tensor(out=tmp2[:], in0=oh1[:], scalar=-BIG, in1=tmp[:], op0=ALU.mult, op1=ALU.add),
                 r=["r_oh1", "r_tmp"], w=["r_tmp2"])
            P.op("dve", lambda e: e.tensor_reduce(out=sm[:, 2:3], in_=tmp2[:], axis=AX.X, op=ALU.max), r=["r_tmp2"], w=["r_sm3"])
            P.op("dve", lambda e: e.tensor_scalar(out=oh2[:], in0=tmp2[:], scalar1=sm[:, 2:3], scalar2=None, op0=ALU.is_equal),
                 r=["r_tmp2", "r_sm3"], w=["r_oh2"])
            P.op("dve", lambda e: e.tensor_tensor(out=oh1[:], in0=oh1[:], in1=oh2[:], op=ALU.add), r=["r_oh1", "r_oh2"], w=["r_oh1"])
            P.op("dve", lambda e: e.tensor_tensor(out=oh1[:], in0=oh1[:], in1=sc[:], op=ALU.mult), r=["r_oh1", "r_sc"], w=["r_oh1"])
            P.op("dve", lambda e: e.tensor_reduce(out=sm[:, 3:4], in_=oh1[:], axis=AX.X, op=ALU.add), r=["r_oh1"], w=["r_sm4"])
            P.op("dve", lambda e: e.reciprocal(out=sm[:, 4:5], in_=sm[:, 3:4]), r=["r_sm4"], w=["r_sm5"])
            P.op("dve", lambda e: e.tensor_scalar(out=self.comb[:, tt, :], in0=oh1[:], scalar1=sm[:, 4:5], scalar2=None, op0=ALU.mult),
                 r=["r_oh1", "r_sm5"], w=[("comb", tt)])
        return router

    def phase_moe_dense(self, l, last):
        P = self.P
        S, NB, NT = self.S, self.NB, self.NT
        G = min(8, NT)
        w1_l = self.w1[l].rearrange("e (kc p) n -> e p kc n", p=128)
        w3_l = self.w3[l].rearrange("e (kc p) n -> e p kc n", p=128)
        w2_l = self.w2[l].rearrange("e (kc p) n -> e p kc n", p=128)
        with ExitStack() as es:
            w1t = [self.sb(es, "w1t%d" % i, [128, KC, DE], BF16) for i in range(2)]
            w3t = [self.sb(es, "w3t%d" % i, [128, KC, DE], BF16) for i in range(2)]
            w2t = [self.sb(es, "w2t%d" % i, [128, 4, D], BF16) for i in range(2)]
            yacc = self.sb(es, "yacc", [128, G, D])
            hid = [self.sb(es, "hid%d" % i, [128, 4, 512], BF16) for i in range(2)]
            sl = [self.sb(es, "sl%d" % i, [128, 512], BF16) for i in range(2)]
            hold = [self.sb(es, "mhold%d" % i, [128, D]) for i in range(2)]
            gb = self.sb(es, "gb2", [128, 2, D])
            p1 = [self.ps(es, "p1_%d" % i, [128, 512]) for i in range(2)]
            p3 = [self.ps(es, "p3_%d" % i, [128, 512]) for i in range(2)]
            py = [self.ps(es, "py%d" % i, [128, 512]) for i in range(2)]
            tiles = self.ln_scratch(es, "l2")
            P.dma("sp", gb[:], self.ln2[l].partition_broadcast(128), w=["gb2"], stream="gb2")
            wi = 0
            for grp in range(NT // G):
                P.op("pool", lambda e: e.memset(yacc[:], 0.0), w=["yacc"])
                for ex in range(NE):
                    b = wi % 2
                    wi += 1
                    P.dma("pool", w1t[b][:], w1_l[ex], w=["w1t%d" % b], stream="w1t%d" % b)
                    P.dma("pool", w3t[b][:], w3_l[ex], w=["w3t%d" % b], stream="w3t%d" % b)
                    P.dma("pool", w2t[b][:], w2_l[ex], w=["w2t%d" % b], stream="w2t%d" % b)
                    for blk in range(G // 4):
                        tb = grp * (G // 4) + blk
                        hb = (ex * (G // 4) + blk) % 2
                        hres = [("hT", tb * 4 + i) for i in range(4)]
                        for fc in range(4):
                            pb_ = fc % 2
                            fsl = slice(fc * 128, (fc + 1) * 128)
                            self.mm_group(p1[pb_][:], [(w1t[b][:, kc, fsl], self.hT[:, kc, tb * 512:(tb + 1) * 512]) for kc in range(KC)],
                                          r=["w1t%d" % b] + hres, w=["p1_%d" % pb_])
                            self.mm_group(p3[pb_][:], [(w3t[b][:, kc, fsl], self.hT[:, kc, tb * 512:(tb + 1) * 512]) for kc in range(KC)],
                                          r=["w3t%d" % b] + hres, w=["p3_%d" % pb_])
                            P.op("act", lambda e, pb_=pb_: e.activation(out=sl[pb_][:], in_=p1[pb_][:], func=AF.Silu),
                                 r=["p1_%d" % pb_], w=["sl%d" % pb_])
                            P.op("dve", lambda e, pb_=pb_, hb=hb, fc=fc: e.tensor_tensor(out=hid[hb][:, fc, :], in0=sl[pb_][:],
                                                                                           in1=p3[pb_][:], op=ALU.mult),
                                 r=["sl%d" % pb_, "p3_%d" % pb_], w=[("hid", hb, fc)])
                        for ti in range(4):
                            gi = blk * 4 + ti
                            tt = tb * 4 + ti
                            for half in range(2):
                                hs = slice(half * 512, (half + 1) * 512)
                                self.mm_group(py[half][:], [(hid[hb][:, fc, ti * 128:(ti + 1) * 128], w2t[b][:, fc, hs]) for fc in range(4)],
                                              r=["w2t%d" % b] + [("hid", hb, fc) for fc in range(4)], w=["py%d" % half])
                                P.op("dve", lambda e, gi=gi, hs=hs, half=half, tt=tt, ex=ex: e.scalar_tensor_tensor(
                                    out=yacc[:, gi, hs], in0=py[half][:], scalar=self.comb[:, tt, ex:ex + 1], in1=yacc[:, gi, hs],
                                    op0=ALU.mult, op1=ALU.add), r=["py%d" % half, ("comb", tt), "yacc"], w=["yacc"])
                for gi in range(G):
                    tt = grp * G + gi
                    hb = tt % 2
                    P.dma("sp", hold[hb][:], self.h_d[tt * 128:(tt + 1) * 128, :], r=[("h_d", tt)], w=["mhold%d" % hb],
                          stream="mhold%d" % hb)
                    P.op("dve", lambda e, hb=hb, gi=gi: e.scalar_tensor_tensor(out=hold[hb][:], in0=hold[hb][:], scalar=ALPHA,
                                                                                 in1=yacc[:, gi, :], op0=ALU.mult, op1=ALU.add),
                         r=["mhold%d" % hb, "yacc"], w=["mhold%d" % hb])
                    self.ln_tile(tiles, hold[hb], "mhold%d" % hb, gb, "gb2", self.out if last else self.h_d, tt,
                                 hT_out=not last, pfx="l2")
            P.flush()

    def build(self, stop=None):
        P = self.P
        self.load_consts()
        self.phase_ln0()
        for l in range(self.L):
            if stop == "ln0":
                break
            self.phase_inproj(l)
            if stop == "inproj":
                break
            if "A" in self.parts:
                self.phase_attn_a(l)
            if "B" in self.parts:
                self.phase_attn_b(l)
            if "C" in self.parts:
                self.phase_gdn(l)
            if stop == "attn":
                break
            self.phase_merge(l)
            if stop == "merge":
                break
            self.phase_moe_dense(l, last=(l == self.L - 1))
        P.wait_all("sp", list(P.lastw.keys()))
        P.flush()
        self.es.close()
        P.close()
        return self.nc


def host_consts(S):
    c = {}
    c["c_ident"] = np.eye(128, dtype=np.float32)
    blk = np.zeros((128, 128), np.float32)
    blk[:64, :64] = 1.0
    blk[64:, 64:] = 1.0
    c["c_blk"] = blk
    R = np.zeros((64, 64), np.float32)
    for base in (0, 32):
        for i in range(16):
            R[base + i, base + 16 + i] = -1.0
            R[base + 16 + i, base + i] = 1.0
    RT = np.zeros((128, 128), np.float32)
    RT[:64, :64] = R.T
    RT[64:, 64:] = R.T
    c["c_rot"] = RT
    t = np.arange(S)
    row = (t // GRID_W).astype(np.float32)
    col = (t % GRID_W).astype(np.float32)
    inv = (10000.0 ** (-np.arange(0, 32, 2, dtype=np.float32) / 32)).astype(np.float32)
    ang_r = row[None, :] * inv[:, None]
    ang_c = col[None, :] * inv[:, None]
    cos64 = np.concatenate([np.cos(ang_r), np.cos(ang_r), np.cos(ang_c), np.cos(ang_c)], 0)
    sin64 = np.concatenate([np.sin(ang_r), np.sin(ang_r), np.sin(ang_c), np.sin(ang_c)], 0)
    c["c_cos"] = np.concatenate([cos64, cos64], 0).astype(np.float32)
    c["c_sin"] = np.concatenate([sin64, sin64], 0).astype(np.float32)
    g = np.zeros((64, 6, 8, 64), np.float32)
    i = np.arange(64)[:, None]
    j = np.arange(64)[None, :]
    for cc in range(8):
        fwd = cc < 4
        allow = (i >= j) if fwd else (i <= j)
        g[:, 0, cc, :] = np.where(allow, 0.0, NEG)
        g[:, 1, cc, :] = np.where(allow.T, 0.0, NEG)
        g[:, 2, cc, :] = ((i > j) if fwd else (i < j)).astype(np.float32)
        g[:, 3, cc, :] = (i == j).astype(np.float32)
    g[:, 4, 0, :] = (i <= j).astype(np.float32)
    g[:, 4, 1, :] = (i >= j).astype(np.float32)
    g[:, 5, 0, :] = 1.0
    c["c_gdn"] = g
    return c


def host_layout(inp, L):
    f = lambda a: np.ascontiguousarray(np.asarray(a, dtype=np.float32))
    m = {}
    m["ln0"] = f(np.stack([inp["ln0_g"], inp["ln0_b"]], 0))
    m["w_in"] = f(inp["w_in"][:L])
    qg = np.asarray(inp["q_norm_g"], np.float32)[:L]
    kg = np.asarray(inp["k_norm_g"], np.float32)[:L]
    m["qkg"] = f(np.stack([np.concatenate([qg, qg], 1), np.concatenate([kg, kg], 1)], 2))
    m["wa"] = f(inp["w_branch_a"][:L])
    m["wb"] = f(inp["w_branch_b"][:L])
    m["wc"] = f(inp["w_branch_c"][:L])
    m["wo"] = f(inp["w_out"][:L])
    m["ln1"] = f(np.stack([inp["ln1_g"][:L], inp["ln1_b"][:L]], 1))
    m["ln2"] = f(np.stack([inp["ln2_g"][:L], inp["ln2_b"][:L]], 1))
    m["w_router"] = f(inp["w_router"])
    m["router_bias"] = f(inp["router_bias"])
    m["w1"] = f(inp["w1"][:L])
    m["w3"] = f(inp["w3"][:L])
    m["w2"] = f(inp["w2"][:L])
    rpb = np.asarray(inp["na_rpb"], np.float32)[:L]
    c = np.arange(64)
    cs = np.clip(c - 8, 0, 48)
    kc_ = np.arange(64)
    inwin = (kc_[None, :] >= cs[:, None]) & (kc_[None, :] < cs[:, None] + 16)
    dc = np.clip(kc_[None, :] - c[:, None] + 15, 0, 30)
    g = rpb[:, :, :, dc]
    g = np.where(inwin[None, None, None], g, np.float32(NEG))
    m["bias_b"] = f(np.transpose(g, (0, 1, 3, 2, 4)).reshape(L, 4, 64, 960))
    cwv = np.asarray(inp["conv_w"], np.float32)[:L]
    m["conv_w"] = f(np.transpose(cwv.reshape(L, 5, 6, 128), (0, 3, 2, 1)))
    m["gdn_ab"] = f(np.stack([np.asarray(inp["A_log"], np.float32)[:L].reshape(L, 8),
                              np.asarray(inp["dt_bias"], np.float32)[:L].reshape(L, 8)], 1))
    m["gdn_g"] = f(inp["gdn_norm_g"][:L])
    return m


_CACHE = {}


def kernel(**inputs):
    S = 4096
    L = DEPTH
    x = np.asarray(inputs["x"], dtype=np.float32)
    nb = x.shape[0]
    key = (S, L)
    if key not in _CACHE:
        _CACHE[key] = Builder(S, L).build()
    nc = _CACHE[key]
    shared = host_layout(inputs, L)
    shared.update(host_consts(S))
    in_maps = []
    for b in range(nb):
        mm = dict(shared)
        mm["x"] = np.ascontiguousarray(x[b])
        in_maps.append(mm)
    res = run_bass_kernel_spmd(nc, in_maps, core_ids=list(range(nb)))
    return np.stack([np.asarray(r["out"], dtype=np.float32) for r in res.results], 0)
```

```python
import math
import numpy as np
from contextlib import ExitStack
import concourse.bass as bass
import concourse.mybir as mybir
from concourse.bass_utils import run_bass_kernel_spmd

F32 = mybir.dt.float32
BF16 = mybir.dt.bfloat16
I32 = mybir.dt.int32
AF = mybir.ActivationFunctionType
ALU = mybir.AluOpType
AX = mybir.AxisListType

ENGS = ("pe", "act", "dve", "pool", "sp")

D = 1024
KC = 8
DEPTH = 4
GRID_W = 64
DIN = 5648
NE = 32
DE = 512
ALPHA = (2 * DEPTH) ** 0.25
C_AQ, C_AK, C_AV = 0, 512, 640
C_BQ, C_BK, C_BV = 768, 1024, 1280
C_CQ, C_CK, C_CV, C_CZ = 1536, 1792, 2048, 2304
C_CG = 2560
C_GATE = 2576
NEG = -30000.0


class Prog:
    def __init__(self, nc):
        self.nc = nc
        self.es = ExitStack()
        self.sem = {e: self.es.enter_context(nc.semaphore("s_" + e)) for e in ENGS}
        self.cnt = {e: 0 for e in ENGS}
        self.dsem = {}
        self.known = {e: {} for e in ENGS}
        self.lastw = {}
        self.readers = {}
        self.queue = {e: [] for e in ENGS}
        self.pending = {e: [] for e in ENGS}
        self.nops = 0

    def close(self):
        self.es.close()

    def _deps(self, eng, r, w):
        toks = []
        for k in r:
            t = self.lastw.get(k)
            if t is not None:
                toks.append(t)
        for k in w:
            t = self.lastw.get(k)
            if t is not None:
                toks.append(t)
            toks.extend(self.readers.get(k, ()))
        need = {}
        for t in toks:
            key, val = t[0], t[1]
            if eng == "pe" and key == "pe":
                continue
            if val is None:
                raise RuntimeError("dependency on unresolved (inc=False) op")
            if need.get(key, 0) < val:
                need[key] = val
        waits = []
        kn = self.known[eng]
        for key, val in need.items():
            if kn.get(key, 0) < val:
                kn[key] = val
                waits.append((key, val))
        return waits

    def _mark(self, tok, r, w):
        for k in w:
            self.lastw[k] = tok
            self.readers[k] = []
        for k in r:
            self.readers.setdefault(k, []).append(tok)

    def op(self, eng, fn, r=(), w=(), inc=True):
        waits = self._deps(eng, r, w)
        if inc:
            self.cnt[eng] += 1
            tok = [eng, self.cnt[eng]]
            for p in self.pending[eng]:
                p[1] = self.cnt[eng]
            self.pending[eng] = []
        else:
            tok = [eng, None]
            self.pending[eng].append(tok)
        self._mark(tok, r, w)
        self.queue[eng].append((fn, waits, (eng, 1) if inc else None))
        self.nops += 1

    def dma(self, eng, out, in_, r=(), w=(), stream=None, **kw):
        assert stream is not None
        waits = self._deps(eng, r, w)
        if stream not in self.dsem:
            self.dsem[stream] = [self.es.enter_context(self.nc.semaphore("d%d" % len(self.dsem))), 0]
        ds = self.dsem[stream]
        ds[1] += 16
        tok = [("d", stream), ds[1]]
        self._mark(tok, r, w)
        self.queue[eng].append((lambda e, out=out, in_=in_, kw=kw: e.dma_start(out=out, in_=in_, **kw),
                                waits, (("d", stream), 16)))
        self.nops += 1

    def _semh(self, key):
        if isinstance(key, tuple):
            return self.dsem[key[1]][0]
        return self.sem[key]

    def wait_all(self, eng, keys):
        waits = self._deps(eng, keys, ())
        self.queue[eng].append((None, waits, None))

    def flush(self):
        nc = self.nc
        q = self.queue
        self.queue = {e: [] for e in ENGS}
        for e in ENGS:
            if self.pending[e]:
                raise RuntimeError("unresolved inc=False ops at flush on " + e)

        def run(engine, items):
            for fn, waits, inc in items:
                for key, val in waits:
                    engine.wait_ge(self._semh(key), val)
                if fn is None:
                    continue
                ins = fn(engine)
                if inc is not None:
                    ins.then_inc(self._semh(inc[0]), inc[1])

        with nc.Block() as block:
            if q["sp"]:
                @block.sync
                def _(e):
                    run(e, q["sp"])
            if q["pe"]:
                @block.tensor
                def _(e):
                    run(e, q["pe"])
            if q["act"]:
                @block.scalar
                def _(e):
                    run(e, q["act"])
            if q["dve"]:
                @block.vector
                def _(e):
                    run(e, q["dve"])
            if q["pool"]:
                @block.gpsimd
                def _(e):
                    run(e, q["pool"])


def bc(ap, shape):
    return ap.to_broadcast(shape)


class Builder:
    def __init__(self, S, L, dbg=(), parts=("A", "B", "C"), moe="dense"):
        self.S, self.L = S, L
        self.NT = S // 128
        self.NB = S // 512
        self.parts = parts
        self.moe = moe
        nc = self.nc = bass.Bass("TRN2", target_bir_lowering=False)
        self.P = Prog(nc)
        self.dbg = set(dbg)
        dt_in = lambda n, s, d=F32: nc.dram_tensor(n, list(s), d, kind="ExternalInput").ap()
        self.x = dt_in("x", [S, D])
        self.ln0 = dt_in("ln0", [2, D])
        self.w_in = dt_in("w_in", [L, D, DIN])
        self.qkg = dt_in("qkg", [L, 128, 2])
        self.bias_b = dt_in("bias_b", [L, 4, 64, 960])
        self.conv_w = dt_in("conv_w", [L, 128, 6, 5])
        self.gdn_ab = dt_in("gdn_ab", [L, 2, 8])
        self.gdn_g = dt_in("gdn_g", [L, 64])
        self.wa = dt_in("wa", [L, 512, D])
        self.wb = dt_in("wb", [L, 256, D])
        self.wc = dt_in("wc", [L, 256, D])
        self.wo = dt_in("wo", [L, D, D])
        self.ln1 = dt_in("ln1", [L, 2, D])
        self.ln2 = dt_in("ln2", [L, 2, D])
        self.w_router = dt_in("w_router", [D, NE])
        self.router_bias = dt_in("router_bias", [NE])
        self.w1 = dt_in("w1", [L, NE, D, DE])
        self.w3 = dt_in("w3", [L, NE, D, DE])
        self.w2 = dt_in("w2", [L, NE, DE, D])
        self.c_ident = dt_in("c_ident", [128, 128])
        self.c_blk = dt_in("c_blk", [128, 128])
        self.c_rot = dt_in("c_rot", [128, 128])
        self.c_cos = dt_in("c_cos", [128, S])
        self.c_sin = dt_in("c_sin", [128, S])
        self.c_gdn = dt_in("c_gdn", [128, 5, 4, 128])
        okind = "ExternalOutput"
        self.out = nc.dram_tensor("out", [S, D], F32, kind=okind).ap()

        def scr(n, s, d=F32):
            k = "ExternalOutput" if n in self.dbg else "Internal"
            return nc.dram_tensor(n, list(s), d, kind=k).ap()
        self.h_d = scr("h_d", [S, D])
        self.qT_d = scr("qT_d", [512, S], BF16)
        self.kT_d = scr("kT_d", [128, S], BF16)
        self.v_d = scr("v_d", [S, 128], BF16)
        self.bqT_d = scr("bqT_d", [256, S], BF16)
        self.bkT_d = scr("bkT_d", [256, S], BF16)
        self.bv_d = scr("bv_d", [S, 256], BF16)
        self.cT_d = scr("cT_d", [768, S])
        self.cz_d = scr("cz_d", [S, 272])
        self.gT_d = scr("gT_d", [3072, S], BF16)
        self.yaT_d = scr("yaT_d", [512, S], BF16)
        self.ybT_d = scr("ybT_d", [256, S], BF16)
        self.ycT_d = scr("ycT_d", [256, S], BF16)
        self.es = ExitStack()
        self.hT = self.sb(self.es, "hT", [128, KC, S], BF16)
        self.comb = self.sb(self.es, "comb", [128, self.NT, NE])
        self.ident = self.sb(self.es, "ident", [128, 128])
        self.identb = self.sb(self.es, "identb", [128, 128], BF16)

    def sb(self, es, n, s, d=F32):
        self.uid = getattr(self, "uid", 0) + 1
        return es.enter_context(self.nc.sbuf_tensor("sb%d_%s" % (self.uid, n), list(s), d))

    def ps(self, es, n, s, d=F32):
        self.uid = getattr(self, "uid", 0) + 1
        return es.enter_context(self.nc.psum_tensor("ps%d_%s" % (self.uid, n), list(s), d))

    def mm_group(self, out_ap, pairs, r, w):
        P = self.P
        n = len(pairs)
        for i, (l, rh) in enumerate(pairs):
            P.op("pe", lambda e, l=l, rh=rh, i=i: e.matmul(out_ap, l, rh, start=(i == 0), stop=(i == n - 1)),
                 r=r, w=w, inc=(i == n - 1))

    def load_consts(self):
        P = self.P
        P.dma("sp", self.ident[:], self.c_ident, w=["ident"], stream="ident")
        P.dma("pool", self.identb[:], self.c_ident, w=["identb"], stream="identb")

    def ln_tile(self, es_tiles, t, tres, gb, gbres, dst_d, tt, hT_out=True, router=None, pfx="ln"):
        P = self.P
        st, mv, sd, xn, pT = (es_tiles[k] for k in ("st", "mv", "sd", "xn", "pT"))
        P.op("dve", lambda e: e.bn_stats(out=st[:, 0:6], in_=t[:, 0:512]), r=[tres], w=[pfx + "st"])
        P.op("dve", lambda e: e.bn_stats(out=st[:, 6:12], in_=t[:, 512:1024]), r=[tres], w=[pfx + "st"])
        P.op("dve", lambda e: e.bn_aggr(out=mv[:], in_=st[:]), r=[pfx + "st"], w=[pfx + "mv"])
        P.op("act", lambda e: e.activation(out=sd[:, 0:1], in_=mv[:, 1:2], func=AF.Sqrt, bias=1e-5, scale=1.0),
             r=[pfx + "mv"], w=[pfx + "sd"])
        P.op("dve", lambda e: e.reciprocal(out=sd[:, 1:2], in_=sd[:, 0:1]), r=[pfx + "sd"], w=[pfx + "sd1"])
        P.op("dve", lambda e: e.scalar_tensor_tensor(out=sd[:, 2:3], in0=mv[:, 0:1], scalar=-1.0, in1=sd[:, 1:2],
                                                     op0=ALU.mult, op1=ALU.mult),
             r=[pfx + "mv", pfx + "sd1"], w=[pfx + "sd2"])
        P.op("act", lambda e: e.activation(out=xn[:], in_=t[:], func=AF.Identity, bias=sd[:, 2:3], scale=sd[:, 1:2]),
             r=[tres, pfx + "sd1", pfx + "sd2"], w=[pfx + "xn"])
        P.op("dve", lambda e: e.tensor_tensor(out=xn[:], in0=xn[:], in1=gb[:, 0, :], op=ALU.mult),
             r=[pfx + "xn", gbres], w=[pfx + "xn"])
        P.op("pool", lambda e: e.tensor_tensor(out=xn[:], in0=xn[:], in1=gb[:, 1, :], op=ALU.add),
             r=[pfx + "xn", gbres], w=[pfx + "xn"])
        P.dma("sp", dst_d[tt * 128:(tt + 1) * 128, :], xn[:], r=[pfx + "xn"], w=[("h_d", tt) if dst_d is self.h_d else "out"],
              stream=pfx + "xn")
        if hT_out:
            for kc in range(KC):
                P.op("pe", lambda e, kc=kc: e.transpose(pT[:, kc, :], xn[:, kc * 128:(kc + 1) * 128], self.ident[:]),
                     r=[pfx + "xn", "ident"], w=[pfx + "pT"], inc=(kc == KC - 1))
            P.op("act", lambda e: e.activation(out=self.hT[:, :, tt * 128:(tt + 1) * 128], in_=pT[:], func=AF.Copy),
                 r=[pfx + "pT"], w=[("hT", tt)])
            if router is not None:
                router(tt, pT, pfx + "pT")

    def ln_scratch(self, es, pfx="ln"):
        return dict(st=self.sb(es, pfx + "st", [128, 12]), mv=self.sb(es, pfx + "mv", [128, 2]),
                    sd=self.sb(es, pfx + "sd", [128, 4]), xn=self.sb(es, pfx + "xn", [128, D]),
                    pT=self.ps(es, pfx + "pT", [128, KC, 128]))

    def phase_ln0(self):
        P = self.P
        with ExitStack() as es:
            tiles = self.ln_scratch(es)
            gb = self.sb(es, "gb0", [128, 2, D])
            xt = [self.sb(es, "x%d" % i, [128, D]) for i in range(2)]
            P.dma("sp", gb[:], self.ln0.partition_broadcast(128), w=["gb0"], stream="gb0")
            for tt in range(self.NT):
                b = tt % 2
                P.dma("sp", xt[b][:], self.x[tt * 128:(tt + 1) * 128, :], w=["xt%d" % b], stream="xt%d" % b)
                self.ln_tile(tiles, xt[b], "xt%d" % b, gb, "gb0", self.h_d if self.L > 0 else self.out, tt)
            P.flush()

    def phase_inproj(self, l):
        P = self.P
        S, NB, NT = self.S, self.NB, self.NT
        w_l = self.w_in[l].rearrange("(kc p) n -> p kc n", p=128)
        with ExitStack() as es:
            wt = [self.sb(es, "wi%d" % i, [128, KC, 512], BF16) for i in range(2)]
            stg = [self.sb(es, "stg%d" % i, [128, 512]) for i in range(2)]
            stgb = [self.sb(es, "stgb%d" % i, [128, 512], BF16) for i in range(2)]
            pa = [self.ps(es, "pa%d" % i, [128, 512]) for i in range(2)]
            pb = self.ps(es, "pb", [128, 512])
            pc = self.ps(es, "pc", [128, 512])
            blk = self.sb(es, "blk", [128, 128], BF16)
            rot = self.sb(es, "rot", [128, 128], BF16)
            cos = self.sb(es, "cos", [128, S])
            sin = self.sb(es, "sin", [128, S])
            qkg = self.sb(es, "qkg", [128, 2])
            sq = self.sb(es, "sq", [128, 512], BF16)
            rs = self.sb(es, "rs", [128, 512])
            qn = self.sb(es, "qn", [128, 512], BF16)
            t1 = self.sb(es, "t1", [128, 512])
            t2 = self.sb(es, "t2", [128, 512])
            P.dma("pool", blk[:], self.c_blk, w=["blk"], stream="blk")
            P.dma("pool", rot[:], self.c_rot, w=["rot"], stream="rot")
            P.dma("sp", cos[:], self.c_cos, w=["cos"], stream="cos")
            P.dma("sp", sin[:], self.c_sin, w=["sin"], stream="sin")
            P.dma("sp", qkg[:], self.qkg[l], w=["qkg"], stream="qkg")
            cnt = {"w": 0, "o": 0, "p": 0}

            def load_w(c0, n):
                b = cnt["w"] % 2
                cnt["w"] += 1
                P.dma("pool", wt[b][:, :, 0:n], w_l[:, :, c0:c0 + n], w=["wi%d" % b], stream="wi%d" % b)
                return wt[b], "wi%d" % b

            def fm_block(c0, n, evac):
                w, wres = load_w(c0, n)
                for j in range(n // 128):
                    for tb in range(NB):
                        pp = cnt["p"] % 2
                        cnt["p"] += 1
                        self.mm_group(pa[pp][:], [(w[:, kc, j * 128:(j + 1) * 128], self.hT[:, kc, tb * 512:(tb + 1) * 512])
                                                   for kc in range(KC)],
                                      r=[wres] + [("hT", tb * 4 + i) for i in range(4)], w=["pa%d" % pp])
                        evac(pa[pp], "pa%d" % pp, c0 + j * 128, tb)

            def out_stage(bf):
                b = cnt["o"] % 2
                cnt["o"] += 1
                return (stgb[b], "stgb%d" % b) if bf else (stg[b], "stg%d" % b)

            def evac_aqk(p, pres, col, tb):
                isq = col < C_AK
                gcol = 0 if isq else 1
                tsl = slice(tb * 512, (tb + 1) * 512)
                P.op("act", lambda e: e.activation(out=sq[:], in_=p[:], func=AF.Square), r=[pres], w=["sq"])
                P.op("pe", lambda e: e.matmul(pb[:], blk[:], sq[:], start=True, stop=True), r=["blk", "sq"], w=["pb"])
                P.op("act", lambda e: e.activation(out=rs[:], in_=pb[:], func=AF.Sqrt, bias=(64e-6 if isq else 1e-6),
                                                   scale=(1.0 if isq else 1.0 / 64)), r=["pb"], w=["rs"])
                P.op("dve", lambda e: e.reciprocal(out=rs[:], in_=rs[:]), r=["rs"], w=["rs"])
                P.op("dve", lambda e: e.scalar_tensor_tensor(out=qn[:], in0=p[:], scalar=qkg[:, gcol:gcol + 1], in1=rs[:],
                                                             op0=ALU.mult, op1=ALU.mult),
                     r=[pres, "rs", "qkg"], w=["qn"])
                P.op("pe", lambda e: e.matmul(pc[:], rot[:], qn[:], start=True, stop=True), r=["rot", "qn"], w=["pc"])
                P.op("pool", lambda e: e.tensor_tensor(out=t1[:], in0=qn[:], in1=cos[:, tsl], op=ALU.mult),
                     r=["qn", "cos"], w=["t1"])
                P.op("dve", lambda e: e.tensor_tensor(out=t2[:], in0=pc[:], in1=sin[:, tsl], op=ALU.mult),
                     r=["pc", "sin"], w=["t2"])
                o, ores = out_stage(True)
                P.op("pool", lambda e: e.tensor_tensor(out=o[:], in0=t1[:], in1=t2[:], op=ALU.add),
                     r=["t1", "t2"], w=[ores])
                dst = self.qT_d[col:col + 128, tsl] if isq else self.kT_d[:, tsl]
                P.dma("sp", dst, o[:], r=[ores], w=["qkT_d"], stream=ores)

            if "A" in self.parts:
                fm_block(C_AQ, 512, evac_aqk)
                fm_block(C_AK, 128, evac_aqk)

            def evac_simple(dst_d, row0, bf, func=AF.Copy, scale=1.0):
                def ev(p, pres, col, tb):
                    o, ores = out_stage(bf)
                    P.op("act", lambda e: e.activation(out=o[:], in_=p[:], func=func, scale=scale), r=[pres], w=[ores])
                    r0 = col - row0
                    P.dma("sp", dst_d[r0:r0 + 128, tb * 512:(tb + 1) * 512], o[:], r=[ores], w=[("fm_d", id(dst_d))],
                          stream=ores)
                return ev

            if "B" in self.parts:
                fm_block(C_BQ, 256, evac_simple(self.bqT_d, C_BQ, True, scale=0.125))
                fm_block(C_BK, 256, evac_simple(self.bkT_d, C_BK, True))
            if "C" in self.parts:
                fm_block(C_CQ, 512, evac_simple(self.cT_d, C_CQ, False))
                fm_block(C_CV, 256, evac_simple(self.cT_d, C_CQ, False))
            for g in range(6):
                fm_block(C_GATE + g * 512, 512, evac_simple(self.gT_d, C_GATE, True, func=AF.Sigmoid))

            def tm_block(c0, n, dst_d, bf):
                w, wres = load_w(c0, n)
                for tt in range(NT):
                    pp = cnt["p"] % 2
                    cnt["p"] += 1
                    self.mm_group(pa[pp][:, 0:n], [(self.hT[:, kc, tt * 128:(tt + 1) * 128], w[:, kc, 0:n]) for kc in range(KC)],
                                  r=[wres, ("hT", tt)], w=["pa%d" % pp])
                    o, ores = out_stage(bf)
                    P.op("act", lambda e, o=o, pp=pp: e.activation(out=o[:, 0:n], in_=pa[pp][:, 0:n], func=AF.Copy),
                         r=["pa%d" % pp], w=[ores])
                    P.dma("sp", dst_d[tt * 128:(tt + 1) * 128, :], o[:, 0:n], r=[ores], w=[("tm_d", id(dst_d))], stream=ores)

            if "A" in self.parts:
                tm_block(C_AV, 128, self.v_d, True)
            if "B" in self.parts:
                tm_block(C_BV, 256, self.bv_d, True)
            if "C" in self.parts:
                tm_block(C_CZ, 272, self.cz_d, False)
            P.flush()

    def phase_attn_a(self, l):
        P = self.P
        S, NB, NT = self.S, self.NB, self.NT
        with ExitStack() as es:
            qh = [self.sb(es, "qh%d" % i, [128, S], BF16) for i in range(2)]
            kT = self.sb(es, "kT", [128, 2, S], BF16)
            vx = self.sb(es, "vx", [128, NT, 2, 128], BF16)
            pT = [self.sb(es, "pT%d" % i, [128, 512], BF16) for i in range(3)]
            onesr = self.sb(es, "onesr", [128, 64])
            rc = self.sb(es, "rc", [128, 512])
            bcs = self.sb(es, "bcs", [64, 512])
            ya = [self.sb(es, "ya%d" % i, [64, 512], BF16) for i in range(2)]
            sps = [self.ps(es, "sps%d" % i, [128, 512]) for i in range(3)]
            ops_ = [self.ps(es, "ops%d" % i, [128, 512]) for i in range(2)]
            bps = self.ps(es, "bps", [64, 512])
            P.op("pool", lambda e: e.memset(kT[64:128, :, :], 0.0), w=["kT"])
            for i in range(2):
                P.op("pool", lambda e, i=i: e.memset(qh[i][64:128, :], 0.0), w=["qh%d" % i])
            P.dma("sp", kT[0:64, :, :], self.kT_d.rearrange("(g d) s -> d g s", d=64), r=["qkT_d"], w=["kT"], stream="kT")
            P.op("pool", lambda e: e.memset(vx[:], 1.0), w=["vx"])
            P.op("pool", lambda e: e.memset(onesr[:], 1.0), w=["onesr"])
            for g in range(2):
                P.dma("sp", vx[:, :, g, 0:64], self.v_d[:, g * 64:(g + 1) * 64].rearrange("(t p) d -> p t d", p=128),
                      r=[("tm_d", id(self.v_d))], w=["vx"], stream="vx")
            its = [(hq, qb, kt) for hq in range(8) for qb in range(NB) for kt in range(NT)]
            N_ = len(its)
            deferred = {}

            def emit_qk(j):
                hq, qb, kt = its[j]
                g, qb_, b = hq // 4, hq % 2, j % 3
                if qb == 0 and kt == 0:
                    for h2 in ([0, 1] if hq == 0 else [hq + 1]):
                        if h2 < 8:
                            P.dma("sp", qh[h2 % 2][0:64, :], self.qT_d[h2 * 64:(h2 + 1) * 64, :], r=["qkT_d"], w=["qh%d" % (h2 % 2)],
                                  stream="qh%d" % (h2 % 2))
                P.op("pe", lambda e: e.matmul(sps[b][:], kT[:, g, kt * 128:(kt + 1) * 128], qh[qb_][:, qb * 512:(qb + 1) * 512],
                                              start=True, stop=True), r=["kT", "qh%d" % qb_], w=["sps%d" % b])

            def tail(hq, qb, ob):
                P.op("pe", lambda e: e.matmul(bps[:], onesr[64:65, :], rc[64:65, :], start=True, stop=True),
                     r=["onesr", "rc"], w=["bps"])
                P.op("act", lambda e: e.activation(out=bcs[:], in_=bps[:], func=AF.Copy), r=["bps"], w=["bcs"])
                P.op("dve", lambda e: e.tensor_tensor(out=ya[ob][:], in0=ops_[ob][0:64, :], in1=bcs[:], op=ALU.mult),
                     r=["ops%d" % ob, "bcs"], w=["ya%d" % ob])
                P.dma("sp", self.yaT_d[hq * 64:(hq + 1) * 64, qb * 512:(qb + 1) * 512], ya[ob][:], r=["ya%d" % ob],
                      w=["yaT_d"], stream="ya%d" % ob)

            emit_qk(0)
            emit_qk(1)
            for j in range(N_):
                hq, qb, kt = its[j]
                g, b = hq // 4, j % 3
                ob = (hq * NB + qb) % 2
                if j + 2 < N_:
                    emit_qk(j + 2)
                P.op("act", lambda e, b=b: e.activation(out=pT[b][:], in_=sps[b][:], func=AF.Exp),
                     r=["sps%d" % b], w=["pT%d" % b])
                P.op("pe", lambda e, b=b, kt=kt, g=g, ob=ob: e.matmul(ops_[ob][:, :], vx[:, kt, g, :], pT[b][:],
                                                          start=(kt == 0), stop=(kt == NT - 1)),
                     r=["vx", "pT%d" % b], w=["ops%d" % ob], inc=(kt == NT - 1))
                if kt == NT - 1:
                    P.op("dve", lambda e, ob=ob: e.reciprocal(out=rc[64:65, :], in_=ops_[ob][64:65, :]), r=["ops%d" % ob], w=["rc"])
                    deferred[min(j + 2, N_ - 1)] = (hq, qb, ob)
                if j in deferred:
                    tail(*deferred.pop(j))
            assert not deferred
            P.flush()

    def phase_attn_b(self, l):
        P = self.P
        S, NT = self.S, self.NT
        rows = S // GRID_W
        wr_ = min(8, rows)
        NBUF = 4
        with ExitStack() as es:
            qT = self.sb(es, "bqT", [64, 4, S], BF16)
            kT = self.sb(es, "bkT", [64, 4, S], BF16)
            v0 = self.sb(es, "bv0", [128, NT, 256], BF16)
            v1 = self.sb(es, "bv1", [128, NT, 256], BF16)
            bias = self.sb(es, "bbias", [64, 4, 960])
            sc = [self.sb(es, "bsc%d" % i, [64, 512]) for i in range(NBUF)]
            pr = [self.sb(es, "bpr%d" % i, [64, 512], BF16) for i in range(NBUF)]
            st = [self.sb(es, "bst%d" % i, [64, 4]) for i in range(NBUF)]
            dg = [self.sb(es, "bdg%d" % i, [64, 64], BF16) for i in range(NBUF)]
            pts = [self.sb(es, "bpts%d" % i, [128, 4, 64], BF16) for i in range(2)]
            yo = [self.sb(es, "byo%d" % i, [64, 4, 64], BF16) for i in range(2)]
            sp_ = [self.ps(es, "bsp%d" % i, [64, 512]) for i in range(NBUF)]
            ptp = [self.ps(es, "bptp%d" % i, [128, 4, 64]) for i in range(2)]
            op_ = [self.ps(es, "bop%d" % i, [64, 4, 64]) for i in range(2)]
            for h in range(4):
                P.dma("sp", qT[:, h, :], self.bqT_d[h * 64:(h + 1) * 64, :], r=[("fm_d", id(self.bqT_d))], w=["bqT"], stream="bqT")
                P.dma("sp", kT[:, h, :], self.bkT_d[h * 64:(h + 1) * 64, :], r=[("fm_d", id(self.bkT_d))], w=["bkT"], stream="bkT")
            P.dma("sp", v0[:], self.bv_d.rearrange("(t p) c -> p t c", p=128), r=[("tm_d", id(self.bv_d))], w=["bv0"], stream="bv0")
            P.dma("sp", v1[:, 0:NT - 1, :], self.bv_d[64:S - 64, :].rearrange("(t p) c -> p t c", p=128), r=[("tm_d", id(self.bv_d))],
                  w=["bv1"], stream="bv1")
            P.dma("sp", bias[:], self.bias_b[l].rearrange("h q k -> q h k"), w=["bbias"], stream="bbias")
            its = [(r, h) for r in range(rows) for h in range(4)]
            N_ = len(its)

            def geo(r):
                r0 = min(max(r - wr_ // 2, 0), rows - wr_)
                return r0, (r0 - r + 7) * 64, r0 * 64

            def s1(i):
                r, h = its[i]
                b = i % NBUF
                r0, d0, k0 = geo(r)
                P.op("pe", lambda e: e.matmul(sp_[b][:], qT[:, h, r * 64:(r + 1) * 64], kT[:, h, k0:k0 + 512], start=True, stop=True),
                     r=["bqT", "bkT"], w=["bsp%d" % b])
                P.op("dve", lambda e: e.tensor_tensor(out=sc[b][:], in0=sp_[b][:], in1=bias[:, h, d0:d0 + 512], op=ALU.add),
                     r=["bsp%d" % b, "bbias"], w=["bsc%d" % b])
                P.op("dve", lambda e: e.tensor_reduce(out=st[b][:, 0:1], in_=sc[b][:], axis=AX.X, op=ALU.max),
                     r=["bsc%d" % b], w=[("bst", b, 0)])
                P.op("dve", lambda e: e.tensor_scalar(out=st[b][:, 1:2], in0=st[b][:, 0:1], scalar1=-1.0, scalar2=None, op0=ALU.mult),
                     r=[("bst", b, 0)], w=[("bst", b, 1)])
                P.op("act", lambda e: e.activation(out=pr[b][:], in_=sc[b][:], func=AF.Exp, bias=st[b][:, 1:2], scale=1.0,
                                                   accum_out=st[b][:, 2:3]), r=["bsc%d" % b, ("bst", b, 1)], w=["bpr%d" % b, ("bst", b, 2)])

            def s1b(i):
                b = i % NBUF
                P.op("dve", lambda e: e.reciprocal(out=st[b][:, 3:4], in_=st[b][:, 2:3]), r=[("bst", b, 2)], w=[("bst", b, 3)])
                P.op("dve", lambda e: e.tensor_scalar(out=dg[b][:], in0=self.identb[0:64, 0:64], scalar1=st[b][:, 3:4], scalar2=None,
                                                      op0=ALU.mult), r=["identb", ("bst", b, 3)], w=["bdg%d" % b])

            def s2(i):
                b, pb = i % NBUF, i % 2
                for kc in range(4):
                    P.op("pe", lambda e, kc=kc: e.matmul(ptp[pb][:, kc, :], pr[b][:, kc * 128:(kc + 1) * 128], dg[b][:], start=True, stop=True),
                         r=["bpr%d" % b, "bdg%d" % b], w=["bptp%d" % pb], inc=(kc == 3))
                P.op("act", lambda e: e.activation(out=pts[pb][:], in_=ptp[pb][:], func=AF.Copy), r=["bptp%d" % pb], w=["bpts%d" % pb])

            def s3(i):
                r, h = its[i]
                pb, ob = i % 2, r % 2
                r0, d0, k0 = geo(r)
                vsrc, vres, t0 = (v0, "bv0", r0 // 2) if r0 % 2 == 0 else (v1, "bv1", (r0 - 1) // 2)
                for kc in range(4):
                    P.op("pe", lambda e, kc=kc: e.matmul(op_[ob][:, h, :], vsrc[:, t0 + kc, h * 64:(h + 1) * 64], pts[pb][:, kc, :],
                                                         start=(kc == 0), stop=(kc == 3)),
                         r=[vres, "bpts%d" % pb], w=["bop%d" % ob], inc=(kc == 3))
                if h == 3:
                    P.op("act", lambda e: e.activation(out=yo[ob][:], in_=op_[ob][:], func=AF.Copy), r=["bop%d" % ob], w=["byo%d" % ob])
                    P.dma("sp", self.ybT_d[:, r * 64:(r + 1) * 64].rearrange("(h d) q -> d h q", d=64), yo[ob][:], r=["byo%d" % ob],
                          w=["ybT_d"], stream="byo%d" % ob)

            for t in range(N_ + 4):
                if t < N_:
                    s1(t)
                if 0 <= t - 1 < N_:
                    s1b(t - 1)
                if 0 <= t - 3 < N_:
                    s2(t - 3)
                if 0 <= t - 4 < N_:
                    s3(t - 4)
            P.flush()

    def phase_gdn(self, l):
        P = self.P
        S, NT, NB = self.S, self.NT, self.NB
        nc = self.nc
        if not hasattr(self, "cn_d"):
            mk = lambda n, s: nc.dram_tensor(n, list(s), F32, kind=("ExternalOutput" if n in self.dbg else "Internal")).ap()
            self.cn_d = mk("cn_d", [768, S])
            self.ktok_d = mk("ktok_d", [S, 256])
            self.vtok_d = mk("vtok_d", [S, 256])
            self.gates_d = mk("gates_d", [S, 16])
            self.o_d = mk("o_d", [2, S, 256])
        with ExitStack() as es:
            cw = self.sb(es, "cw", [128, 6, 5])
            x = self.sb(es, "gx", [128, S + 4])
            y = self.sb(es, "gy", [128, S])
            sq = self.sb(es, "gsq", [128, 512], BF16)
            rs = self.sb(es, "grs", [128, 512])
            blk = self.sb(es, "gblk", [128, 128], BF16)
            tk = [self.sb(es, "gtk%d" % i, [128, 512]) for i in range(2)]
            gin = self.sb(es, "gin", [128, NT, 16])
            gout = self.sb(es, "gout", [128, NT, 16])
            ab = self.sb(es, "gab", [128, 2, 8])
            pss = self.ps(es, "gpss", [128, 512])
            ptr = [self.ps(es, "gptr%d" % i, [128, 4, 128]) for i in range(2)]
            P.dma("sp", cw[:], self.conv_w[l], w=["cw"], stream="cw")
            P.dma("pool", blk[:], self.c_blk, w=["gblk"], stream="gblk")
            P.dma("sp", ab[:], self.gdn_ab[l].partition_broadcast(128), w=["gab"], stream="gab")
            P.op("pool", lambda e: e.memset(x[:, 0:2], 0.0), w=["gxp"])
            P.op("pool", lambda e: e.memset(x[:, S + 2:S + 4], 0.0), w=["gxp"])
            ti = 0
            import os
            for ch in range(6 if os.environ.get("GDBG", "") != "gates" else 0):
                P.dma("sp", x[:, 2:S + 2], self.cT_d[ch * 128:(ch + 1) * 128, :], r=[("fm_d", id(self.cT_d))], w=["gx"], stream="gx")
                P.op("act", lambda e, ch=ch: e.activation(out=y[:], in_=x[:, 0:S], func=AF.Identity, scale=cw[:, ch, 0:1]),
                     r=["gx", "gxp", "cw"], w=["gy"])
                for k in range(1, 5):
                    P.op("dve", lambda e, ch=ch, k=k: e.scalar_tensor_tensor(out=y[:], in0=x[:, k:k + S], scalar=cw[:, ch, k:k + 1], in1=y[:],
                                                                             op0=ALU.mult, op1=ALU.add), r=["gx", "gxp", "cw", "gy"], w=["gy"])
                P.op("act", lambda e: e.activation(out=y[:], in_=y[:], func=AF.Silu), r=["gy"], w=["gy"])
                if ch < 4:
                    isq = ch < 2
                    for tb in range(NB):
                        tsl = slice(tb * 512, (tb + 1) * 512)
                        P.op("act", lambda e, tsl=tsl: e.activation(out=sq[:], in_=y[:, tsl], func=AF.Square), r=["gy"], w=["gsq"])
                        P.op("pe", lambda e: e.matmul(pss[:], blk[:], sq[:], start=True, stop=True), r=["gblk", "gsq"], w=["gpss"])
                        P.op("act", lambda e, isq=isq: e.activation(out=rs[:], in_=pss[:], func=AF.Sqrt, bias=(64e-6 if isq else 1e-6),
                                                                    scale=(64.0 if isq else 1.0)), r=["gpss"], w=["grs"])
                        P.op("dve", lambda e: e.reciprocal(out=rs[:], in_=rs[:]), r=["grs"], w=["grs"])
                        P.op("dve", lambda e, tsl=tsl: e.tensor_tensor(out=y[:, tsl], in0=y[:, tsl], in1=rs[:], op=ALU.mult),
                             r=["gy", "grs"], w=["gy"])
                P.dma("sp", self.cn_d[ch * 128:(ch + 1) * 128, :], y[:], r=["gy"], w=["cn_d"], stream="gy")
                if ch >= 2:
                    dst = self.ktok_d if ch < 4 else self.vtok_d
                    for tb in range(NB):
                        b = ti % 2
                        ti += 1
                        for j in range(4):
                            tt = tb * 4 + j
                            P.op("pe", lambda e, b=b, j=j, tt=tt: e.transpose(ptr[b][:, j, :], y[:, tt * 128:(tt + 1) * 128], self.ident[:]),
                                 r=["gy", "ident"], w=["gptr%d" % b], inc=(j == 3))
                        P.op("act", lambda e, b=b: e.activation(out=tk[b][:], in_=ptr[b][:].rearrange("p a b -> p (a b)"), func=AF.Copy),
                             r=["gptr%d" % b], w=["gtk%d" % b])
                        P.dma("sp", dst[tb * 512:(tb + 1) * 512, (ch % 2) * 128:(ch % 2 + 1) * 128].rearrange("(j p) c -> p j c", p=128),
                              tk[b][:].rearrange("p (j c) -> p j c", c=128), r=["gtk%d" % b], w=["kvtok_d"], stream="gtk%d" % b)
            if os.environ.get("GDBG", "") == "conv":
                P.flush()
                return
            P.dma("sp", gin[:], self.cz_d[:, 256:272].rearrange("(t p) c -> p t c", p=128), r=[("tm_d", id(self.cz_d))], w=["gin"], stream="gin")
            P.op("act", lambda e: e.activation(out=gout[:, :, 0:8], in_=gin[:, :, 0:8], func=AF.Sigmoid), r=["gin"], w=["gout_b"])
            P.op("dve", lambda e: e.tensor_tensor(out=gin[:, :, 8:16], in0=gin[:, :, 8:16], in1=bc(ab[:, 1:2, :], [128, NT, 8]), op=ALU.add),
                 r=["gin", "gab"], w=["gin2"])
            P.op("act", lambda e: e.activation(out=gin[:, :, 8:16], in_=gin[:, :, 8:16], func=AF.Exp), r=["gin2"], w=["gin2"])
            P.op("act", lambda e: e.activation(out=gin[:, :, 8:16], in_=gin[:, :, 8:16], func=AF.Ln, bias=1.0, scale=1.0), r=["gin2"], w=["gin2"])
            P.op("act", lambda e: e.activation(out=ab[:, 0, :], in_=ab[:, 0, :], func=AF.Exp), r=["gab"], w=["gab0"])
            P.op("dve", lambda e: e.scalar_tensor_tensor(out=gout[:, :, 8:16], in0=gin[:, :, 8:16], scalar=-1.0, in1=bc(ab[:, 0:1, :], [128, NT, 8]),
                                                         op0=ALU.mult, op1=ALU.mult), r=["gin2", "gab0"], w=["gout_g"])
            P.dma("sp", self.gates_d.rearrange("(t p) c -> p t c", p=128), gout[:], r=["gout_b", "gout_g"], w=["gates_d"], stream="gout")
            P.flush()
        if getattr(self, "gdn_stop", None) == "prep":
            return
        NC = S // 64
        with ExitStack() as es:
            cst = self.sb(es, "gcst", [128, 5, 4, 128])
            NEGM, NEGMT, STRICT, ID8 = cst[:, 0], cst[:, 1], cst[:, 2], cst[:, 3]
            CUM, ONES = cst[:, 4, 0, :], cst[:, 4, 1, :]
            T8 = lambda n: self.sb(es, n, [128, 4, 128])
            ld = [dict(KT=T8("gKT%d" % i), QT=T8("gQT%d" % i), Kt=T8("gKt%d" % i), Vt=T8("gVt%d" % i),
                       gb=self.sb(es, "ggb%d" % i, [128, 8])) for i in range(2)]
            sm = self.sb(es, "gsm", [128, 24])
            diagG, Dm, DTm, eGr, t_a, t_b, SBm, qkTm, QgT = (T8(n) for n in ("gdiag", "gD", "gDT", "geGr", "gta", "gtb", "gSB", "gqkTm", "gQgT"))
            X = [T8("gX0"), T8("gX1")]
            XT = [T8("gXT0"), T8("gXT1")]
            PT, rv, rk, U, WT, Vn, Kd, St, ot = (T8(n) for n in ("gPT", "grv", "grk", "gU", "gWT", "gVn", "gKd", "gS", "gO"))
            psA = self.ps(es, "gpsA", [128, 8])
            pb = [self.ps(es, "gpb%d" % i, [128, 4, 128]) for i in range(1, 8)]
            psGrow, ps2, ps3, ps4, ps5, ps6, ps7 = pb
            fl = lambda t: t[:].rearrange("p c j -> p (c j)")
            P.dma("sp", cst[:], self.c_gdn, w=["gcst"], stream="gcst")
            P.op("pool", lambda e: e.memset(St[:], 0.0), w=["gS"])
            for i in range(2):
                for nm in ("KT", "QT", "Kt", "Vt"):
                    P.op("pool", lambda e, t=ld[i][nm]: e.memset(t[:], 0.0), w=["g" + nm + str(i)])

            def mm8(ps, psres, lhs, lres, rhs, rres):
                for c in range(4):
                    P.op("pe", lambda e, c=c: e.matmul(ps[:, c, :], lhs[:, c, :], rhs[:, c, :], start=True, stop=True),
                         r=[lres, rres], w=[psres], inc=(c == 3))

            def bcg(col0):
                return lambda t: bc(t[:, col0:col0 + 4].unsqueeze(2), [128, 4, 128])

            for s_ in range(NC):
                a, b = s_, NC - 1 - s_
                L_ = ld[s_ % 2]
                sfx = str(s_ % 2)
                ra, rb_ = slice(a * 64, (a + 1) * 64), slice(b * 64, (b + 1) * 64)
                lo, hi = slice(0, 64), slice(64, 128)
                for nm, src_, r0 in (("KT", self.cn_d, 256), ("QT", self.cn_d, 0)):
                    for pp, rr in ((lo, ra), (hi, rb_)):
                        P.dma("sp", L_[nm][pp, :, pp], src_[r0:r0 + 256, rr].rearrange("(h d) t -> d h t", d=64),
                              r=["cn_d"], w=["g" + nm + sfx], stream="g" + nm + sfx)
                for nm, src_ in (("Kt", self.ktok_d), ("Vt", self.vtok_d)):
                    for pp, rr in ((lo, ra), (hi, rb_)):
                        P.dma("sp", L_[nm][pp, :, pp], src_[rr, :].rearrange("t (h d) -> t h d", d=64),
                              r=["kvtok_d"], w=["g" + nm + sfx], stream="g" + nm + sfx)
                for pp, rr, c0, s0 in ((lo, ra, 0, 0), (hi, rb_, 0, 4), (lo, ra, 4, 8), (hi, rb_, 4, 12)):
                    P.dma("sp", L_["gb"][pp, c0:c0 + 4], self.gates_d[rr, s0:s0 + 4], r=["gates_d"], w=["ggb" + sfx], stream="ggb" + sfx)
                KT, QT, Kt, Vt, gb = L_["KT"], L_["QT"], L_["Kt"], L_["Vt"], L_["gb"]
                rKT, rQT, rKt, rVt, rgb = ("g" + n + sfx for n in ("KT", "QT", "Kt", "Vt", "gb"))
                P.op("pe", lambda e, gb=gb: e.matmul(psA[:, 0:4], CUM, gb[:, 4:8], start=True, stop=True), r=["gcst", rgb], w=["gpsA"], inc=False)
                P.op("pe", lambda e, gb=gb: e.matmul(psA[:, 4:8], ONES, gb[:, 4:8], start=True, stop=True), r=["gcst", rgb], w=["gpsA"])
                P.op("act", lambda e: e.activation(out=sm[:, 0:8], in_=psA[:], func=AF.Copy), r=["gpsA"], w=["gsmG"])
                P.op("act", lambda e: e.activation(out=sm[:, 8:16], in_=sm[:, 0:8], func=AF.Exp), r=["gsmG"], w=["gsmE"])
                P.op("dve", lambda e: e.tensor_tensor(out=sm[:, 16:20], in0=sm[:, 4:8], in1=sm[:, 0:4], op=ALU.subtract), r=["gsmG"], w=["gsmK"])
                P.op("act", lambda e: e.activation(out=sm[:, 16:20], in_=sm[:, 16:20], func=AF.Exp), r=["gsmK"], w=["gsmK"])
                P.op("dve", lambda e, gb=gb: e.tensor_tensor(out=sm[:, 20:24], in0=gb[:, 0:4], in1=sm[:, 8:12], op=ALU.mult), r=[rgb, "gsmE"], w=["gsmB"])
                Gb, eGtb, kdb, bkb = bcg(0)(sm), bcg(12)(sm), bcg(16)(sm), bcg(20)(sm)
                betab = bc(gb[:, 0:4].unsqueeze(2), [128, 4, 128])
                P.op("pool", lambda e, Gb=Gb: e.tensor_tensor(out=diagG[:], in0=ID8, in1=Gb, op=ALU.mult), r=["gcst", "gsmG"], w=["gdiag"])
                P.op("pe", lambda e: e.matmul(fl(psGrow), ONES, fl(diagG), start=True, stop=True), r=["gcst", "gdiag"], w=["gpsGrow"])
                P.op("dve", lambda e: e.scalar_tensor_tensor(out=t_a[:], in0=psGrow[:], scalar=-1.0, in1=NEGM, op0=ALU.mult, op1=ALU.add),
                     r=["gpsGrow", "gcst"], w=["gta"])
                P.op("dve", lambda e, Gb=Gb: e.tensor_tensor(out=t_a[:], in0=t_a[:], in1=Gb, op=ALU.add), r=["gta", "gsmG"], w=["gta"])
                P.op("act", lambda e: e.activation(out=Dm[:], in_=t_a[:], func=AF.Exp), r=["gta"], w=["gD"])
                P.op("dve", lambda e: e.tensor_tensor(out=t_b[:], in0=psGrow[:], in1=NEGMT, op=ALU.add), r=["gpsGrow", "gcst"], w=["gtb"])
                P.op("dve", lambda e, Gb=Gb: e.tensor_tensor(out=t_b[:], in0=t_b[:], in1=Gb, op=ALU.subtract), r=["gtb", "gsmG"], w=["gtb"])
                P.op("act", lambda e: e.activation(out=DTm[:], in_=t_b[:], func=AF.Exp), r=["gtb"], w=["gDT"])
                P.op("act", lambda e: e.activation(out=eGr[:], in_=psGrow[:], func=AF.Exp), r=["gpsGrow"], w=["geGr"])
                P.op("pool", lambda e, QT=QT: e.tensor_tensor(out=QgT[:], in0=QT[:], in1=eGr[:], op=ALU.mult), r=[rQT, "geGr"], w=["gQgT"])
                mm8(ps2, "gps2", KT, rKT, KT, rKT)
                mm8(ps3, "gps3", KT, rKT, QT, rQT)
                P.op("pool", lambda e, betab=betab: e.tensor_tensor(out=SBm[:], in0=STRICT, in1=betab, op=ALU.mult), r=["gcst", rgb], w=["gSB"])
                P.op("dve", lambda e: e.tensor_tensor(out=t_a[:], in0=ps2[:], in1=Dm[:], op=ALU.mult), r=["gps2", "gD"], w=["gta"])
                P.op("pool", lambda e: e.tensor_tensor(out=X[0][:], in0=t_a[:], in1=SBm[:], op=ALU.mult), r=["gta", "gSB"], w=["gX0"])
                P.op("dve", lambda e: e.tensor_tensor(out=qkTm[:], in0=ps3[:], in1=DTm[:], op=ALU.mult), r=["gps3", "gDT"], w=["gqkTm"])
                mm8(ps2, "gps2", X[0], "gX0", cst[:, 3], "gcst")
                P.op("act", lambda e: e.activation(out=XT[0][:], in_=ps2[:], func=AF.Copy), r=["gps2"], w=["gXT0"])
                P.op("dve", lambda e: e.scalar_tensor_tensor(out=PT[:], in0=ps2[:], scalar=-1.0, in1=ID8, op0=ALU.mult, op1=ALU.add),
                     r=["gps2", "gcst", "gXT0"], w=["gPT"])
                for lv in range(1, 6):
                    ci, ni = (lv - 1) % 2, lv % 2
                    mm8(ps4, "gps4", XT[ci], "gXT%d" % ci, X[ci], "gX%d" % ci)
                    if lv < 5:
                        mm8(ps5, "gps5", X[ci], "gX%d" % ci, XT[ci], "gXT%d" % ci)
                    P.op("act", lambda e, ni=ni: e.activation(out=X[ni][:], in_=ps4[:], func=AF.Copy), r=["gps4"], w=["gX%d" % ni])
                    if lv < 5:
                        P.op("dve", lambda e, ni=ni: e.tensor_copy(out=XT[ni][:], in_=ps5[:]), r=["gps5"], w=["gXT%d" % ni])
                    mm8(ps6, "gps6", X[ni], "gX%d" % ni, PT, "gPT")
                    P.op("dve", lambda e: e.tensor_tensor(out=PT[:], in0=PT[:], in1=ps6[:], op=ALU.add), r=["gPT", "gps6"], w=["gPT"])
                P.op("pool", lambda e, Vt=Vt, betab=betab: e.tensor_tensor(out=rv[:], in0=Vt[:], in1=betab, op=ALU.mult), r=[rVt, rgb], w=["grv"])
                P.op("pool", lambda e, Kt=Kt, bkb=bkb: e.tensor_tensor(out=rk[:], in0=Kt[:], in1=bkb, op=ALU.mult), r=[rKt, "gsmB"], w=["grk"])
                mm8(ps4, "gps4", PT, "gPT", rv, "grv")
                P.op("act", lambda e: e.activation(out=U[:], in_=ps4[:], func=AF.Copy), r=["gps4"], w=["gU"])
                mm8(ps5, "gps5", rk, "grk", PT, "gPT")
                P.op("dve", lambda e: e.tensor_copy(out=WT[:], in_=ps5[:]), r=["gps5"], w=["gWT"])
                mm8(ps6, "gps6", WT, "gWT", St, "gS")
                P.op("dve", lambda e: e.scalar_tensor_tensor(out=Vn[:], in0=ps6[:], scalar=-1.0, in1=U[:], op0=ALU.mult, op1=ALU.add),
                     r=["gps6", "gU"], w=["gVn"])
                for c in range(4):
                    P.op("pe", lambda e, c=c: e.matmul(ps7[:, c, :], QgT[:, c, :], St[:, c, :], start=True, stop=False), r=["gQgT", "gS"], w=["gps7"],
                         inc=False)
                    P.op("pe", lambda e, c=c: e.matmul(ps7[:, c, :], qkTm[:, c, :], Vn[:, c, :], start=False, stop=True), r=["gqkTm", "gVn"],
                         w=["gps7"], inc=(c == 3))
                P.op("act", lambda e: e.activation(out=ot[:], in_=ps7[:], func=AF.Copy), r=["gps7"], w=["gO"])
                P.dma("sp", self.o_d[0, ra, :].rearrange("t (h d) -> t h d", d=64), ot[0:64, :, 0:64], r=["gO"], w=["o_d"], stream="gO")
                P.dma("sp", self.o_d[1, rb_, :].rearrange("t (h d) -> t h d", d=64), ot[64:128, :, 64:128], r=["gO"], w=["o_d"], stream="gO")
                P.op("pool", lambda e, Kt=Kt, kdb=kdb: e.tensor_tensor(out=Kd[:], in0=Kt[:], in1=kdb, op=ALU.mult), r=[rKt, "gsmK"], w=["gKd"])
                mm8(ps3, "gps3", Kd, "gKd", Vn, "gVn")
                P.op("pool", lambda e, eGtb=eGtb: e.tensor_tensor(out=St[:], in0=St[:], in1=eGtb, op=ALU.mult), r=["gS", "gsmE"], w=["gS"])
                P.op("dve", lambda e: e.tensor_tensor(out=St[:], in0=St[:], in1=ps3[:], op=ALU.add), r=["gS", "gps3"], w=["gS"])
            P.flush()
        if getattr(self, "gdn_stop", None) == "main":
            return
        with ExitStack() as es:
            gg = self.sb(es, "ggn", [128, 64])
            of = [self.sb(es, "gof%d" % i, [128, 4, 64]) for i in range(2)]
            obk = [self.sb(es, "gob%d" % i, [128, 4, 64]) for i in range(2)]
            zt = [self.sb(es, "gz%d" % i, [128, 4, 64]) for i in range(2)]
            sq2 = self.sb(es, "gsq2", [128, 4, 64])
            ms = self.sb(es, "gms", [128, 8])
            yb_ = [self.sb(es, "gyb%d" % i, [128, 2, 128], BF16) for i in range(2)]
            pt2 = [self.ps(es, "gpt2%d" % i, [128, 2, 128]) for i in range(2)]
            P.dma("sp", gg[:], self.gdn_g[l].partition_broadcast(128), w=["ggn"], stream="ggn")
            for tt in range(NT):
                b = tt % 2
                tr = slice(tt * 128, (tt + 1) * 128)
                P.dma("sp", of[b][:], self.o_d[0, tr, :].rearrange("t (h d) -> t h d", d=64), r=["o_d"], w=["gof%d" % b], stream="gof%d" % b)
                P.dma("sp", obk[b][:], self.o_d[1, tr, :].rearrange("t (h d) -> t h d", d=64), r=["o_d"], w=["gob%d" % b], stream="gob%d" % b)
                P.dma("sp", zt[b][:], self.cz_d[tr, 0:256].rearrange("t (h d) -> t h d", d=64), r=[("tm_d", id(self.cz_d))], w=["gz%d" % b],
                      stream="gz%d" % b)
                P.op("dve", lambda e, b=b: e.tensor_tensor(out=of[b][:], in0=of[b][:], in1=obk[b][:], op=ALU.add), r=["gof%d" % b, "gob%d" % b],
                     w=["gof%d" % b])
                P.op("act", lambda e, b=b: e.activation(out=sq2[:], in_=of[b][:], func=AF.Square), r=["gof%d" % b], w=["gsq2"])
                P.op("dve", lambda e: e.tensor_reduce(out=ms[:, 0:4], in_=sq2[:], axis=AX.X, op=ALU.add), r=["gsq2"], w=["gms"])
                P.op("act", lambda e: e.activation(out=ms[:, 4:8], in_=ms[:, 0:4], func=AF.Sqrt, bias=1e-6, scale=1.0 / 64), r=["gms"], w=["gms2"])
                P.op("dve", lambda e: e.reciprocal(out=ms[:, 4:8], in_=ms[:, 4:8]), r=["gms2"], w=["gms2"])
                P.op("dve", lambda e, b=b: e.tensor_tensor(out=of[b][:], in0=of[b][:], in1=bc(ms[:, 4:8].unsqueeze(2), [128, 4, 64]), op=ALU.mult),
                     r=["gof%d" % b, "gms2"], w=["gof%d" % b])
                P.op("pool", lambda e, b=b: e.tensor_tensor(out=of[b][:], in0=of[b][:], in1=bc(gg[:].unsqueeze(1), [128, 4, 64]), op=ALU.mult),
                     r=["gof%d" % b, "ggn"], w=["gof%d" % b])
                P.op("act", lambda e, b=b: e.activation(out=zt[b][:], in_=zt[b][:], func=AF.Silu), r=["gz%d" % b], w=["gz%d" % b])
                P.op("dve", lambda e, b=b: e.tensor_tensor(out=of[b][:], in0=of[b][:], in1=zt[b][:], op=ALU.mult), r=["gof%d" % b, "gz%d" % b],
                     w=["gof%d" % b])
                for j in range(2):
                    P.op("pe", lambda e, b=b, j=j: e.transpose(pt2[b][:, j, :], of[b][:, 2 * j:2 * j + 2, :].rearrange("p a d -> p (a d)"), self.ident[:]),
                         r=["gof%d" % b, "ident"], w=["gpt2%d" % b], inc=(j == 1))
                P.op("act", lambda e, b=b: e.activation(out=yb_[b][:], in_=pt2[b][:], func=AF.Copy), r=["gpt2%d" % b], w=["gyb%d" % b])
                P.dma("sp", self.ycT_d[:, tr].rearrange("(j p) t -> p j t", p=128), yb_[b][:], r=["gyb%d" % b], w=["ycT_d"], stream="gyb%d" % b)
            P.flush()

    def phase_merge(self, l):
        P = self.P
        S, NB, NT = self.S, self.NB, self.NT
        with ExitStack() as es:
            wa = self.sb(es, "wa", [128, 4, D], BF16)
            wb = self.sb(es, "wb", [128, 2, D], BF16)
            wc = self.sb(es, "wc", [128, 2, D], BF16)
            wo = self.sb(es, "wo", [128, 8, D], BF16)
            yT = self.sb(es, "yT", [128, 8, 512], BF16)
            gt = [self.sb(es, "gt%d" % i, [128, 3, 512], BF16) for i in range(2)]
            mixT = self.sb(es, "mixT", [128, 8, 512], BF16)
            m1 = self.sb(es, "m1", [128, 512])
            m2 = self.sb(es, "m2", [128, 512])
            m3 = self.sb(es, "m3", [128, 512])
            hold = [self.sb(es, "hold%d" % i, [128, D]) for i in range(2)]
            tsum = [self.sb(es, "tsum%d" % i, [128, D]) for i in range(2)]
            gb = self.sb(es, "gb1", [128, 2, D])
            pabc = [self.ps(es, "pabc%d" % i, [128, 512]) for i in range(3)]
            po = [self.ps(es, "po%d" % i, [128, 512]) for i in range(2)]
            tiles = self.ln_scratch(es, "l1")
            rt = self.router_setup(es) if not getattr(self, "no_router", False) else None
            P.dma("pool", wa[:], self.wa[l].rearrange("(kc p) n -> p kc n", p=128), w=["wa"], stream="wa")
            P.dma("pool", wb[:], self.wb[l].rearrange("(kc p) n -> p kc n", p=128), w=["wb"], stream="wb")
            P.dma("pool", wc[:], self.wc[l].rearrange("(kc p) n -> p kc n", p=128), w=["wc"], stream="wc")
            P.dma("pool", wo[:], self.wo[l].rearrange("(kc p) n -> p kc n", p=128), w=["wo"], stream="wo")
            P.dma("sp", gb[:], self.ln1[l].partition_broadcast(128), w=["gb1"], stream="gb1")
            if "B" not in self.parts:
                P.op("pool", lambda e: e.memset(yT[:, 4:6, :], 0.0), w=["yTb"])
            if "C" not in self.parts:
                P.op("pool", lambda e: e.memset(yT[:, 6:8, :], 0.0), w=["yTc"])
            for tb in range(NB):
                tsl = slice(tb * 512, (tb + 1) * 512)
                if "A" in self.parts:
                    P.dma("sp", yT[:, 0:4, :], self.yaT_d[:, tsl].rearrange("(kc p) s -> p kc s", p=128), r=["yaT_d"], w=["yTa"],
                          stream="yTa")
                else:
                    P.op("pool", lambda e: e.memset(yT[:, 0:4, :], 0.0), w=["yTa"])
                if "B" in self.parts:
                    P.dma("sp", yT[:, 4:6, :], self.ybT_d[:, tsl].rearrange("(kc p) s -> p kc s", p=128), r=["ybT_d"], w=["yTb"],
                          stream="yTb")
                if "C" in self.parts:
                    P.dma("sp", yT[:, 6:8, :], self.ycT_d[:, tsl].rearrange("(kc p) s -> p kc s", p=128), r=["ycT_d"], w=["yTc"],
                          stream="yTc")
                for dc in range(8):
                    b = dc % 2
                    P.dma("sp", gt[b][:], self.gT_d[:, tsl].rearrange("(t c p) s -> p t c s", p=128, t=3)[:, :, dc, :],
                          r=[("fm_d", id(self.gT_d))], w=["gt%d" % b], stream="gt%d" % b)
                    csl = slice(dc * 128, (dc + 1) * 128)
                    self.mm_group(pabc[0][:], [(wa[:, kc, csl], yT[:, kc, :]) for kc in range(4)], r=["wa", "yTa"], w=["pabc0"])
                    self.mm_group(pabc[1][:], [(wb[:, kc, csl], yT[:, 4 + kc, :]) for kc in range(2)], r=["wb", "yTb"], w=["pabc1"])
                    self.mm_group(pabc[2][:], [(wc[:, kc, csl], yT[:, 6 + kc, :]) for kc in range(2)], r=["wc", "yTc"], w=["pabc2"])
                    P.op("dve", lambda e, b=b: e.tensor_tensor(out=m1[:], in0=pabc[0][:], in1=gt[b][:, 0, :], op=ALU.mult),
                         r=["pabc0", "gt%d" % b], w=["m1"])
                    P.op("dve", lambda e, b=b: e.tensor_tensor(out=m2[:], in0=pabc[1][:], in1=gt[b][:, 1, :], op=ALU.mult),
                         r=["pabc1", "gt%d" % b], w=["m2"])
                    P.op("dve", lambda e, b=b: e.tensor_tensor(out=m3[:], in0=pabc[2][:], in1=gt[b][:, 2, :], op=ALU.mult),
                         r=["pabc2", "gt%d" % b], w=["m3"])
                    P.op("pool", lambda e: e.tensor_tensor(out=m1[:], in0=m1[:], in1=m2[:], op=ALU.add), r=["m1", "m2"], w=["m1"])
                    P.op("pool", lambda e, dc=dc: e.tensor_tensor(out=mixT[:, dc, :], in0=m1[:], in1=m3[:], op=ALU.add),
                         r=["m1", "m3"], w=[("mixT", dc)])
                for ti in range(4):
                    tt = tb * 4 + ti
                    hb = tt % 2
                    P.dma("sp", hold[hb][:], self.h_d[tt * 128:(tt + 1) * 128, :], r=[("h_d", tt)], w=["hold%d" % hb],
                          stream="hold%d" % hb)
                    for half in range(2):
                        self.mm_group(po[half][:], [(mixT[:, dc, ti * 128:(ti + 1) * 128], wo[:, dc, half * 512:(half + 1) * 512])
                                                    for dc in range(8)],
                                      r=["wo"] + [("mixT", dc) for dc in range(8)], w=["po%d" % half])
                        P.op("dve", lambda e, hb=hb, half=half: e.scalar_tensor_tensor(
                            out=tsum[hb][:, half * 512:(half + 1) * 512], in0=hold[hb][:, half * 512:(half + 1) * 512],
                            scalar=ALPHA, in1=po[half][:], op0=ALU.mult, op1=ALU.add),
                            r=["hold%d" % hb, "po%d" % half], w=["tsum%d" % hb])
                    self.ln_tile(tiles, tsum[hb], "tsum%d" % hb, gb, "gb1", self.h_d, tt, router=rt, pfx="l1")
            P.flush()

    def router_setup(self, es):
        P = self.P
        wr = self.sb(es, "wr", [128, KC, NE])
        wrh = self.sb(es, "wrh", [128, KC, NE], BF16)
        wrl = self.sb(es, "wrl", [128, KC, NE], BF16)
        rb = self.sb(es, "rb", [128, NE])
        hlo = self.sb(es, "hlo", [128, KC, 128], BF16)
        pl = self.ps(es, "pl", [128, NE])
        sc = self.sb(es, "r_sc", [128, NE])
        sel = self.sb(es, "r_sel", [128, NE])
        tmp = self.sb(es, "r_tmp", [128, NE])
        tmp2 = self.sb(es, "r_tmp2", [128, NE])
        g8 = self.sb(es, "r_g8", [128, 8, 4])
        oh1 = self.sb(es, "r_oh1", [128, NE])
        oh2 = self.sb(es, "r_oh2", [128, NE])
        sm = self.sb(es, "r_sm", [128, 8])
        P.dma("sp", wr[:], self.w_router.rearrange("(kc p) n -> p kc n", p=128), w=["wr"], stream="wr")
        P.dma("sp", rb[:], self.router_bias.partition_broadcast(128), w=["rb"], stream="rb")
        P.dma("pool", wrh[:], self.w_router.rearrange("(kc p) n -> p kc n", p=128), w=["wrh"], stream="wrh")
        P.op("dve", lambda e: e.tensor_tensor(out=wrl[:], in0=wr[:], in1=wrh[:], op=ALU.subtract), r=["wr", "wrh"], w=["wrl"])
        BIG = 1.0e4

        def router(tt, pT, pTres):
            tsl = slice(tt * 128, (tt + 1) * 128)
            P.op("dve", lambda e: e.tensor_tensor(out=hlo[:], in0=pT[:], in1=self.hT[:, :, tsl], op=ALU.subtract),
                 r=[pTres, ("hT", tt)], w=["hlo"])
            pairs = []
            for kc in range(KC):
                pairs += [(self.hT[:, kc, tsl], wrh[:, kc, :]), (self.hT[:, kc, tsl], wrl[:, kc, :]), (hlo[:, kc, :], wrh[:, kc, :])]
            self.mm_group(pl[:], pairs, r=["hlo", ("hT", tt), "wrh", "wrl"], w=["pl"])
            P.op("act", lambda e: e.activation(out=sc[:], in_=pl[:], func=AF.Sigmoid), r=["pl"], w=["r_sc"])
            P.op("dve", lambda e: e.tensor_tensor(out=sel[:], in0=sc[:], in1=rb[:], op=ALU.add), r=["r_sc", "rb"], w=["r_sel"])
            s3 = sel[:].rearrange("p (g k) -> p g k", k=4)
            t3 = tmp[:].rearrange("p (g k) -> p g k", k=4)
            P.op("dve", lambda e: e.tensor_reduce(out=sm[:, 0:8], in_=s3, axis=AX.X, op=ALU.max), r=["r_sel"], w=["r_sm"])
            P.op("dve", lambda e: e.tensor_tensor(out=t3, in0=s3, in1=bc(sm[:, 0:8].unsqueeze(2), [128, 8, 4]), op=ALU.is_equal),
                 r=["r_sel", "r_sm"], w=["r_tmp"])
            P.op("dve", lambda e: e.scalar_tensor_tensor(out=tmp[:], in0=tmp[:], scalar=-BIG, in1=sel[:], op0=ALU.mult, op1=ALU.add),
                 r=["r_tmp", "r_sel"], w=["r_tmp"])
            P.op("dve", lambda e: e.tensor_reduce(out=g8[:, :, 0], in_=t3, axis=AX.X, op=ALU.max), r=["r_tmp"], w=["r_g8"])
            P.op("dve", lambda e: e.tensor_tensor(out=g8[:, :, 1], in0=g8[:, :, 0], in1=sm[:, 0:8], op=ALU.add),
                 r=["r_g8", "r_sm"], w=["r_g8b"])
            P.op("dve", lambda e: e.tensor_reduce(out=sm[:, 0:1], in_=g8[:, :, 1], axis=AX.X, op=ALU.max), r=["r_g8b"], w=["r_sm1"])
            P.op("dve", lambda e: e.tensor_scalar(out=g8[:, :, 2], in0=g8[:, :, 1], scalar1=sm[:, 0:1], scalar2=None, op0=ALU.is_lt),
                 r=["r_g8b", "r_sm1"], w=["r_g8c"])
            P.op("dve", lambda e: e.scalar_tensor_tensor(out=t3, in0=bc(g8[:, :, 2:3], [128, 8, 4]), scalar=-BIG, in1=s3,
                                                         op0=ALU.mult, op1=ALU.add), r=["r_g8c", "r_sel"], w=["r_tmp"])
            P.op("dve", lambda e: e.tensor_reduce(out=sm[:, 1:2], in_=tmp[:], axis=AX.X, op=ALU.max), r=["r_tmp"], w=["r_sm2"])
            P.op("dve", lambda e: e.tensor_scalar(out=oh1[:], in0=tmp[:], scalar1=sm[:, 1:2], scalar2=None, op0=ALU.is_equal),
                 r=["r_tmp", "r_sm2"], w=["r_oh1"])
            P.op("dve", lambda e: e.scalar_tensor_tensor(out=tmp2[:], in0=oh1[:], scalar=-BIG, in1=tmp[:], op0=ALU.mult, op1=ALU.add),
                 r=["r_oh1", "r_tmp"], w=["r_tmp2"])
            P.op("dve", lambda e: e.tensor_reduce(out=sm[:, 2:3], in_=tmp2[:], axis=AX.X, op=ALU.max), r=["r_tmp2"], w=["r_sm3"])
            P.op("dve", lambda e: e.tensor_scalar(out=oh2[:], in0=tmp2[:], scalar1=sm[:, 2:3], scalar2=None, op0=ALU.is_equal),
                 r=["r_tmp2", "r_sm3"], w=["r_oh2"])
            P.op("dve", lambda e: e.tensor_tensor(out=oh1[:], in0=oh1[:], in1=oh2[:], op=ALU.add), r=["r_oh1", "r_oh2"], w=["r_oh1"])
            P.op("dve", lambda e: e.tensor_tensor(out=oh1[:], in0=oh1[:], in1=sc[:], op=ALU.mult), r=["r_oh1", "r_sc"], w=["r_oh1"])
            P.op("dve", lambda e: e.tensor_reduce(out=sm[:, 3:4], in_=oh1[:], axis=AX.X, op=ALU.add), r=["r_oh1"], w=["r_sm4"])
            P.op("dve", lambda e: e.reciprocal(out=sm[:, 4:5], in_=sm[:, 3:4]), r=["r_sm4"], w=["r_sm5"])
            P.op("dve", lambda e: e.tensor_scalar(out=self.comb[:, tt, :], in0=oh1[:], scalar1=sm[:, 4:5], scalar2=None, op0=ALU.mult),
                 r=["r_oh1", "r_sm5"], w=[("comb", tt)])
        return router

    def phase_moe_dense(self, l, last):
        P = self.P
        S, NB, NT = self.S, self.NB, self.NT
        G = min(8, NT)
        w1_l = self.w1[l].rearrange("e (kc p) n -> e p kc n", p=128)
        w3_l = self.w3[l].rearrange("e (kc p) n -> e p kc n", p=128)
        w2_l = self.w2[l].rearrange("e (kc p) n -> e p kc n", p=128)
        with ExitStack() as es:
            w1t = [self.sb(es, "w1t%d" % i, [128, KC, DE], BF16) for i in range(2)]
            w3t = [self.sb(es, "w3t%d" % i, [128, KC, DE], BF16) for i in range(2)]
            w2t = [self.sb(es, "w2t%d" % i, [128, 4, D], BF16) for i in range(2)]
            yacc = self.sb(es, "yacc", [128, G, D])
            hid = [self.sb(es, "hid%d" % i, [128, 4, 512], BF16) for i in range(2)]
            sl = [self.sb(es, "sl%d" % i, [128, 512], BF16) for i in range(2)]
            hold = [self.sb(es, "mhold%d" % i, [128, D]) for i in range(2)]
            gb = self.sb(es, "gb2", [128, 2, D])
            p1 = [self.ps(es, "p1_%d" % i, [128, 512]) for i in range(2)]
            p3 = [self.ps(es, "p3_%d" % i, [128, 512]) for i in range(2)]
            py = [self.ps(es, "py%d" % i, [128, 512]) for i in range(2)]
            tiles = self.ln_scratch(es, "l2")
            P.dma("sp", gb[:], self.ln2[l].partition_broadcast(128), w=["gb2"], stream="gb2")
            wi = 0
            for grp in range(NT // G):
                P.op("pool", lambda e: e.memset(yacc[:], 0.0), w=["yacc"])
                for ex in range(NE):
                    b = wi % 2
                    wi += 1
                    P.dma("pool", w1t[b][:], w1_l[ex], w=["w1t%d" % b], stream="w1t%d" % b)
                    P.dma("pool", w3t[b][:], w3_l[ex], w=["w3t%d" % b], stream="w3t%d" % b)
                    P.dma("pool", w2t[b][:], w2_l[ex], w=["w2t%d" % b], stream="w2t%d" % b)
                    for blk in range(G // 4):
                        tb = grp * (G // 4) + blk
                        hb = (ex * (G // 4) + blk) % 2
                        hres = [("hT", tb * 4 + i) for i in range(4)]
                        for fc in range(4):
                            pb_ = fc % 2
                            fsl = slice(fc * 128, (fc + 1) * 128)
                            self.mm_group(p1[pb_][:], [(w1t[b][:, kc, fsl], self.hT[:, kc, tb * 512:(tb + 1) * 512]) for kc in range(KC)],
                                          r=["w1t%d" % b] + hres, w=["p1_%d" % pb_])
                            self.mm_group(p3[pb_][:], [(w3t[b][:, kc, fsl], self.hT[:, kc, tb * 512:(tb + 1) * 512]) for kc in range(KC)],
                                          r=["w3t%d" % b] + hres, w=["p3_%d" % pb_])
                            P.op("act", lambda e, pb_=pb_: e.activation(out=sl[pb_][:], in_=p1[pb_][:], func=AF.Silu),
                                 r=["p1_%d" % pb_], w=["sl%d" % pb_])
                            P.op("dve", lambda e, pb_=pb_, hb=hb, fc=fc: e.tensor_tensor(out=hid[hb][:, fc, :], in0=sl[pb_][:],
                                                                                           in1=p3[pb_][:], op=ALU.mult),
                                 r=["sl%d" % pb_, "p3_%d" % pb_], w=[("hid", hb, fc)])
                        for ti in range(4):
                            gi = blk * 4 + ti
                            tt = tb * 4 + ti
                            for half in range(2):
                                hs = slice(half * 512, (half + 1) * 512)
                                self.mm_group(py[half][:], [(hid[hb][:, fc, ti * 128:(ti + 1) * 128], w2t[b][:, fc, hs]) for fc in range(4)],
                                              r=["w2t%d" % b] + [("hid", hb, fc) for fc in range(4)], w=["py%d" % half])
                                P.op("dve", lambda e, gi=gi, hs=hs, half=half, tt=tt, ex=ex: e.scalar_tensor_tensor(
                                    out=yacc[:, gi, hs], in0=py[half][:], scalar=self.comb[:, tt, ex:ex + 1], in1=yacc[:, gi, hs],
                                    op0=ALU.mult, op1=ALU.add), r=["py%d" % half, ("comb", tt), "yacc"], w=["yacc"])
                for gi in range(G):
                    tt = grp * G + gi
                    hb = tt % 2
                    P.dma("sp", hold[hb][:], self.h_d[tt * 128:(tt + 1) * 128, :], r=[("h_d", tt)], w=["mhold%d" % hb],
                          stream="mhold%d" % hb)
                    P.op("dve", lambda e, hb=hb, gi=gi: e.scalar_tensor_tensor(out=hold[hb][:], in0=hold[hb][:], scalar=ALPHA,
                                                                                 in1=yacc[:, gi, :], op0=ALU.mult, op1=ALU.add),
                         r=["mhold%d" % hb, "yacc"], w=["mhold%d" % hb])
                    self.ln_tile(tiles, hold[hb], "mhold%d" % hb, gb, "gb2", self.out if last else self.h_d, tt,
                                 hT_out=not last, pfx="l2")
            P.flush()

    def build(self, stop=None):
        P = self.P
        self.load_consts()
        self.phase_ln0()
        for l in range(self.L):
            if stop == "ln0":
                break
            self.phase_inproj(l)
            if stop == "inproj":
                break
            if "A" in self.parts:
                self.phase_attn_a(l)
            if "B" in self.parts:
                self.phase_attn_b(l)
            if "C" in self.parts:
                self.phase_gdn(l)
            if stop == "attn":
                break
            self.phase_merge(l)
            if stop == "merge":
                break
            self.phase_moe_dense(l, last=(l == self.L - 1))
        P.wait_all("sp", list(P.lastw.keys()))
        P.flush()
        self.es.close()
        P.close()
        return self.nc


def host_consts(S):
    c = {}
    c["c_ident"] = np.eye(128, dtype=np.float32)
    blk = np.zeros((128, 128), np.float32)
    blk[:64, :64] = 1.0
    blk[64:, 64:] = 1.0
    c["c_blk"] = blk
    R = np.zeros((64, 64), np.float32)
    for base in (0, 32):
        for i in range(16):
            R[base + i, base + 16 + i] = -1.0
            R[base + 16 + i, base + i] = 1.0
    RT = np.zeros((128, 128), np.float32)
    RT[:64, :64] = R.T
    RT[64:, 64:] = R.T
    c["c_rot"] = RT
    t = np.arange(S)
    row = (t // GRID_W).astype(np.float32)
    col = (t % GRID_W).astype(np.float32)
    inv = (10000.0 ** (-np.arange(0, 32, 2, dtype=np.float32) / 32)).astype(np.float32)
    ang_r = row[None, :] * inv[:, None]
    ang_c = col[None, :] * inv[:, None]
    cos64 = np.concatenate([np.cos(ang_r), np.cos(ang_r), np.cos(ang_c), np.cos(ang_c)], 0)
    sin64 = np.concatenate([np.sin(ang_r), np.sin(ang_r), np.sin(ang_c), np.sin(ang_c)], 0)
    c["c_cos"] = np.concatenate([cos64, cos64], 0).astype(np.float32)
    c["c_sin"] = np.concatenate([sin64, sin64], 0).astype(np.float32)
    g = np.zeros((128, 5, 4, 128), np.float32)
    i = np.arange(64)[:, None]
    j = np.arange(64)[None, :]
    g[:, 0] = NEG
    g[:, 1] = NEG
    lo, hi = slice(0, 64), slice(64, 128)
    for pp, fwd in ((lo, True), (hi, False)):
        allow = (i >= j) if fwd else (i <= j)
        for cc in range(4):
            g[pp, 0, cc, pp] = np.where(allow, 0.0, NEG)
            g[pp, 1, cc, pp] = np.where(allow.T, 0.0, NEG)
            g[pp, 2, cc, pp] = ((i > j) if fwd else (i < j)).astype(np.float32)
            g[pp, 3, cc, pp] = (i == j).astype(np.float32)
        g[pp, 4, 0, pp] = ((i <= j) if fwd else (i >= j)).astype(np.float32)
        g[pp, 4, 1, pp] = 1.0
    c["c_gdn"] = g
    return c


def host_layout(inp, L):
    f = lambda a: np.ascontiguousarray(np.asarray(a, dtype=np.float32))
    m = {}
    m["ln0"] = f(np.stack([inp["ln0_g"], inp["ln0_b"]], 0))
    m["w_in"] = f(inp["w_in"][:L])
    qg = np.asarray(inp["q_norm_g"], np.float32)[:L]
    kg = np.asarray(inp["k_norm_g"], np.float32)[:L]
    m["qkg"] = f(np.stack([np.concatenate([qg, qg], 1), np.concatenate([kg, kg], 1)], 2))
    m["wa"] = f(inp["w_branch_a"][:L])
    m["wb"] = f(inp["w_branch_b"][:L])
    m["wc"] = f(inp["w_branch_c"][:L])
    m["wo"] = f(inp["w_out"][:L])
    m["ln1"] = f(np.stack([inp["ln1_g"][:L], inp["ln1_b"][:L]], 1))
    m["ln2"] = f(np.stack([inp["ln2_g"][:L], inp["ln2_b"][:L]], 1))
    m["w_router"] = f(inp["w_router"])
    m["router_bias"] = f(inp["router_bias"])
    m["w1"] = f(inp["w1"][:L])
    m["w3"] = f(inp["w3"][:L])
    m["w2"] = f(inp["w2"][:L])
    rpb = np.asarray(inp["na_rpb"], np.float32)[:L]
    c = np.arange(64)
    cs = np.clip(c - 8, 0, 48)
    kc_ = np.arange(64)
    inwin = (kc_[None, :] >= cs[:, None]) & (kc_[None, :] < cs[:, None] + 16)
    dc = np.clip(kc_[None, :] - c[:, None] + 15, 0, 30)
    g = rpb[:, :, :, dc]
    g = np.where(inwin[None, None, None], g, np.float32(NEG))
    m["bias_b"] = f(np.transpose(g, (0, 1, 3, 2, 4)).reshape(L, 4, 64, 960))
    cwv = np.asarray(inp["conv_w"], np.float32)[:L]
    m["conv_w"] = f(np.transpose(cwv.reshape(L, 5, 6, 128), (0, 3, 2, 1)))
    m["gdn_ab"] = f(np.stack([np.asarray(inp["A_log"], np.float32)[:L].reshape(L, 8),
                              np.asarray(inp["dt_bias"], np.float32)[:L].reshape(L, 8)], 1))
    m["gdn_g"] = f(inp["gdn_norm_g"][:L])
    return m


_CACHE = {}


def kernel(**inputs):
    S = 4096
    L = DEPTH
    x = np.asarray(inputs["x"], dtype=np.float32)
    nb = x.shape[0]
    key = (S, L)
    if key not in _CACHE:
        _CACHE[key] = Builder(S, L).build()
    nc = _CACHE[key]
    shared = host_layout(inputs, L)
    shared.update(host_consts(S))
    in_maps = []
    for b in range(nb):
        mm = dict(shared)
        mm["x"] = np.ascontiguousarray(x[b])
        in_maps.append(mm)
    res = run_bass_kernel_spmd(nc, in_maps, core_ids=list(range(nb)))
    return np.stack([np.asarray(r["out"], dtype=np.float32) for r in res.results], 0)
```

```python
import math
import numpy as np
from contextlib import ExitStack
import concourse.bass as bass
import concourse.mybir as mybir
from concourse.bass_utils import run_bass_kernel_spmd

F32 = mybir.dt.float32
BF16 = mybir.dt.bfloat16
I32 = mybir.dt.int32
AF = mybir.ActivationFunctionType
ALU = mybir.AluOpType
AX = mybir.AxisListType

ENGS = ("pe", "act", "dve", "pool", "sp")

D = 1024
KC = 8
DEPTH = 4
GRID_W = 64
DIN = 5648
NE = 32
DE = 512
ALPHA = (2 * DEPTH) ** 0.25
C_AQ, C_AK, C_AV = 0, 512, 640
C_BQ, C_BK, C_BV = 768, 1024, 1280
C_CQ, C_CK, C_CV, C_CZ = 1536, 1792, 2048, 2304
C_CG = 2560
C_GATE = 2576
NEG = -30000.0


class Prog:
    def __init__(self, nc):
        self.nc = nc
        self.es = ExitStack()
        self.sem = {e: self.es.enter_context(nc.semaphore("s_" + e)) for e in ENGS}
        self.cnt = {e: 0 for e in ENGS}
        self.dsem = {}
        self.known = {e: {} for e in ENGS}
        self.lastw = {}
        self.readers = {}
        self.queue = {e: [] for e in ENGS}
        self.pending = {e: [] for e in ENGS}
        self.nops = 0

    def close(self):
        self.es.close()

    def _deps(self, eng, r, w):
        toks = []
        for k in r:
            t = self.lastw.get(k)
            if t is not None:
                toks.append(t)
        for k in w:
            t = self.lastw.get(k)
            if t is not None:
                toks.append(t)
            toks.extend(self.readers.get(k, ()))
        need = {}
        for t in toks:
            key, val = t[0], t[1]
            if eng == "pe" and key == "pe":
                continue
            if val is None:
                raise RuntimeError("dependency on unresolved (inc=False) op")
            if need.get(key, 0) < val:
                need[key] = val
        waits = []
        kn = self.known[eng]
        for key, val in need.items():
            if kn.get(key, 0) < val:
                kn[key] = val
                waits.append((key, val))
        return waits

    def _mark(self, tok, r, w):
        for k in w:
            self.lastw[k] = tok
            self.readers[k] = []
        for k in r:
            self.readers.setdefault(k, []).append(tok)

    rec = None

    def replay(self, item):
        kind, a, k = item
        (self.op if kind == "op" else self.dma)(*a, **k)

    def op(self, eng, fn, r=(), w=(), inc=True):
        if self.rec is not None:
            self.rec.append(("op", (eng, fn), dict(r=r, w=w, inc=inc)))
            return
        waits = self._deps(eng, r, w)
        if inc:
            self.cnt[eng] += 1
            tok = [eng, self.cnt[eng]]
            for p in self.pending[eng]:
                p[1] = self.cnt[eng]
            self.pending[eng] = []
        else:
            tok = [eng, None]
            self.pending[eng].append(tok)
        self._mark(tok, r, w)
        self.queue[eng].append((fn, waits, (eng, 1) if inc else None))
        self.nops += 1

    def dma(self, eng, out, in_, r=(), w=(), stream=None, **kw):
        assert stream is not None
        if self.rec is not None:
            self.rec.append(("dma", (eng, out, in_), dict(r=r, w=w, stream=stream, **kw)))
            return
        waits = self._deps(eng, r, w)
        if stream not in self.dsem:
            self.dsem[stream] = [self.es.enter_context(self.nc.semaphore("d%d" % len(self.dsem))), 0]
        ds = self.dsem[stream]
        ds[1] += 16
        tok = [("d", stream), ds[1]]
        self._mark(tok, r, w)
        self.queue[eng].append((lambda e, out=out, in_=in_, kw=kw: e.dma_start(out=out, in_=in_, **kw),
                                waits, (("d", stream), 16)))
        self.nops += 1

    def _semh(self, key):
        if isinstance(key, tuple):
            return self.dsem[key[1]][0]
        return self.sem[key]

    def wait_all(self, eng, keys):
        waits = self._deps(eng, keys, ())
        self.queue[eng].append((None, waits, None))

    def flush(self):
        nc = self.nc
        q = self.queue
        self.queue = {e: [] for e in ENGS}
        for e in ENGS:
            if self.pending[e]:
                raise RuntimeError("unresolved inc=False ops at flush on " + e)

        def run(engine, items):
            for fn, waits, inc in items:
                for key, val in waits:
                    engine.wait_ge(self._semh(key), val)
                if fn is None:
                    continue
                ins = fn(engine)
                if inc is not None:
                    ins.then_inc(self._semh(inc[0]), inc[1])

        with nc.Block() as block:
            if q["sp"]:
                @block.sync
                def _(e):
                    run(e, q["sp"])
            if q["pe"]:
                @block.tensor
                def _(e):
                    run(e, q["pe"])
            if q["act"]:
                @block.scalar
                def _(e):
                    run(e, q["act"])
            if q["dve"]:
                @block.vector
                def _(e):
                    run(e, q["dve"])
            if q["pool"]:
                @block.gpsimd
                def _(e):
                    run(e, q["pool"])


def bc(ap, shape):
    return ap.to_broadcast(shape)


class Builder:
    def __init__(self, S, L, dbg=(), parts=("A", "B", "C"), moe="dense"):
        self.S, self.L = S, L
        self.NT = S // 128
        self.NB = S // 512
        self.parts = parts
        self.moe = moe
        nc = self.nc = bass.Bass("TRN2", target_bir_lowering=False)
        self.P = Prog(nc)
        self.dbg = set(dbg)
        dt_in = lambda n, s, d=F32: nc.dram_tensor(n, list(s), d, kind="ExternalInput").ap()
        self.x = dt_in("x", [S, D])
        self.ln0 = dt_in("ln0", [2, D])
        self.w_in = dt_in("w_in", [L, D, DIN])
        self.qkg = dt_in("qkg", [L, 128, 2])
        self.bias_b = dt_in("bias_b", [L, 4, 64, 960])
        self.conv_w = dt_in("conv_w", [L, 128, 6, 5])
        self.gdn_ab = dt_in("gdn_ab", [L, 2, 8])
        self.gdn_g = dt_in("gdn_g", [L, 64])
        self.wa = dt_in("wa", [L, 512, D])
        self.wb = dt_in("wb", [L, 256, D])
        self.wc = dt_in("wc", [L, 256, D])
        self.wo = dt_in("wo", [L, D, D])
        self.ln1 = dt_in("ln1", [L, 2, D])
        self.ln2 = dt_in("ln2", [L, 2, D])
        self.w_router = dt_in("w_router", [D, NE])
        self.router_bias = dt_in("router_bias", [NE])
        self.w1 = dt_in("w1", [L, NE, D, DE])
        self.w3 = dt_in("w3", [L, NE, D, DE])
        self.w2 = dt_in("w2", [L, NE, DE, D])
        self.c_ident = dt_in("c_ident", [128, 128])
        self.c_blk = dt_in("c_blk", [128, 128])
        self.c_rot = dt_in("c_rot", [128, 128])
        self.c_cos = dt_in("c_cos", [128, S])
        self.c_sin = dt_in("c_sin", [128, S])
        self.c_gdn = dt_in("c_gdn", [128, 5, 4, 128])
        okind = "ExternalOutput"
        self.out = nc.dram_tensor("out", [S, D], F32, kind=okind).ap()

        def scr(n, s, d=F32):
            k = "ExternalOutput" if n in self.dbg else "Internal"
            return nc.dram_tensor(n, list(s), d, kind=k).ap()
        self.h_d = scr("h_d", [S, D])
        self.qT_d = scr("qT_d", [512, S], BF16)
        self.kT_d = scr("kT_d", [128, S], BF16)
        self.v_d = scr("v_d", [S, 128], BF16)
        self.bqT_d = scr("bqT_d", [256, S], BF16)
        self.bkT_d = scr("bkT_d", [256, S], BF16)
        self.bv_d = scr("bv_d", [S, 256], BF16)
        self.cT_d = scr("cT_d", [768, S])
        self.cz_d = scr("cz_d", [S, 272])
        self.gT_d = scr("gT_d", [3072, S], BF16)
        self.yaT_d = scr("yaT_d", [512, S], BF16)
        self.ybT_d = scr("ybT_d", [256, S], BF16)
        self.ycT_d = scr("ycT_d", [256, S], BF16)
        self.es = ExitStack()
        self.hT = self.sb(self.es, "hT", [128, KC, S], BF16)
        self.comb = self.sb(self.es, "comb", [128, self.NT, NE])
        self.ident = self.sb(self.es, "ident", [128, 128])
        self.identb = self.sb(self.es, "identb", [128, 128], BF16)

    def sb(self, es, n, s, d=F32):
        self.uid = getattr(self, "uid", 0) + 1
        return es.enter_context(self.nc.sbuf_tensor("sb%d_%s" % (self.uid, n), list(s), d))

    def ps(self, es, n, s, d=F32):
        self.uid = getattr(self, "uid", 0) + 1
        return es.enter_context(self.nc.psum_tensor("ps%d_%s" % (self.uid, n), list(s), d))

    def mm_group(self, out_ap, pairs, r, w):
        P = self.P
        n = len(pairs)
        for i, (l, rh) in enumerate(pairs):
            P.op("pe", lambda e, l=l, rh=rh, i=i: e.matmul(out_ap, l, rh, start=(i == 0), stop=(i == n - 1)),
                 r=r, w=w, inc=(i == n - 1))

    def load_consts(self):
        P = self.P
        P.dma("sp", self.ident[:], self.c_ident, w=["ident"], stream="ident")
        P.dma("pool", self.identb[:], self.c_ident, w=["identb"], stream="identb")

    def ln_tile(self, es_tiles, t, tres, gb, gbres, dst_d, tt, hT_out=True, router=None, pfx="ln"):
        P = self.P
        st, mv, sd, xn, pT = (es_tiles[k] for k in ("st", "mv", "sd", "xn", "pT"))
        P.op("dve", lambda e: e.bn_stats(out=st[:, 0:6], in_=t[:, 0:512]), r=[tres], w=[pfx + "st"])
        P.op("dve", lambda e: e.bn_stats(out=st[:, 6:12], in_=t[:, 512:1024]), r=[tres], w=[pfx + "st"])
        P.op("dve", lambda e: e.bn_aggr(out=mv[:], in_=st[:]), r=[pfx + "st"], w=[pfx + "mv"])
        P.op("act", lambda e: e.activation(out=sd[:, 0:1], in_=mv[:, 1:2], func=AF.Sqrt, bias=1e-5, scale=1.0),
             r=[pfx + "mv"], w=[pfx + "sd"])
        P.op("dve", lambda e: e.reciprocal(out=sd[:, 1:2], in_=sd[:, 0:1]), r=[pfx + "sd"], w=[pfx + "sd1"])
        P.op("dve", lambda e: e.scalar_tensor_tensor(out=sd[:, 2:3], in0=mv[:, 0:1], scalar=-1.0, in1=sd[:, 1:2],
                                                     op0=ALU.mult, op1=ALU.mult),
             r=[pfx + "mv", pfx + "sd1"], w=[pfx + "sd2"])
        P.op("act", lambda e: e.activation(out=xn[:], in_=t[:], func=AF.Identity, bias=sd[:, 2:3], scale=sd[:, 1:2]),
             r=[tres, pfx + "sd1", pfx + "sd2"], w=[pfx + "xn"])
        P.op("dve", lambda e: e.tensor_tensor(out=xn[:], in0=xn[:], in1=gb[:, 0, :], op=ALU.mult),
             r=[pfx + "xn", gbres], w=[pfx + "xn"])
        P.op("pool", lambda e: e.tensor_tensor(out=xn[:], in0=xn[:], in1=gb[:, 1, :], op=ALU.add),
             r=[pfx + "xn", gbres], w=[pfx + "xn"])
        P.dma("sp", dst_d[tt * 128:(tt + 1) * 128, :], xn[:], r=[pfx + "xn"], w=[("h_d", tt) if dst_d is self.h_d else "out"],
              stream=pfx + "xn")
        if hT_out:
            for kc in range(KC):
                P.op("pe", lambda e, kc=kc: e.transpose(pT[:, kc, :], xn[:, kc * 128:(kc + 1) * 128], self.ident[:]),
                     r=[pfx + "xn", "ident"], w=[pfx + "pT"], inc=(kc == KC - 1))
            P.op("act", lambda e: e.activation(out=self.hT[:, :, tt * 128:(tt + 1) * 128], in_=pT[:], func=AF.Copy),
                 r=[pfx + "pT"], w=[("hT", tt)])
            if router is not None:
                router(tt, pT, pfx + "pT")

    def ln_scratch(self, es, pfx="ln"):
        return dict(st=self.sb(es, pfx + "st", [128, 12]), mv=self.sb(es, pfx + "mv", [128, 2]),
                    sd=self.sb(es, pfx + "sd", [128, 4]), xn=self.sb(es, pfx + "xn", [128, D]),
                    pT=self.ps(es, pfx + "pT", [128, KC, 128]))

    def phase_ln0(self):
        P = self.P
        with ExitStack() as es:
            tiles = self.ln_scratch(es)
            gb = self.sb(es, "gb0", [128, 2, D])
            xt = [self.sb(es, "x%d" % i, [128, D]) for i in range(2)]
            P.dma("sp", gb[:], self.ln0.partition_broadcast(128), w=["gb0"], stream="gb0")
            for tt in range(self.NT):
                b = tt % 2
                P.dma("sp", xt[b][:], self.x[tt * 128:(tt + 1) * 128, :], w=["xt%d" % b], stream="xt%d" % b)
                self.ln_tile(tiles, xt[b], "xt%d" % b, gb, "gb0", self.h_d if self.L > 0 else self.out, tt)
            P.flush()

    def phase_inproj(self, l):
        P = self.P
        S, NB, NT = self.S, self.NB, self.NT
        w_l = self.w_in[l].rearrange("(kc p) n -> p kc n", p=128)
        with ExitStack() as es:
            wt = [self.sb(es, "wi%d" % i, [128, KC, 512], BF16) for i in range(2)]
            stg = [self.sb(es, "stg%d" % i, [128, 512]) for i in range(2)]
            stgb = [self.sb(es, "stgb%d" % i, [128, 512], BF16) for i in range(2)]
            pa = [self.ps(es, "pa%d" % i, [128, 512]) for i in range(2)]
            pb = self.ps(es, "pb", [128, 512])
            pc = self.ps(es, "pc", [128, 512])
            blk = self.sb(es, "blk", [128, 128], BF16)
            rot = self.sb(es, "rot", [128, 128], BF16)
            cos = self.sb(es, "cos", [128, S])
            sin = self.sb(es, "sin", [128, S])
            qkg = self.sb(es, "qkg", [128, 2])
            sq = self.sb(es, "sq", [128, 512], BF16)
            rs = self.sb(es, "rs", [128, 512])
            qn = self.sb(es, "qn", [128, 512], BF16)
            t1 = self.sb(es, "t1", [128, 512])
            t2 = self.sb(es, "t2", [128, 512])
            P.dma("pool", blk[:], self.c_blk, w=["blk"], stream="blk")
            P.dma("pool", rot[:], self.c_rot, w=["rot"], stream="rot")
            P.dma("sp", cos[:], self.c_cos, w=["cos"], stream="cos")
            P.dma("sp", sin[:], self.c_sin, w=["sin"], stream="sin")
            P.dma("sp", qkg[:], self.qkg[l], w=["qkg"], stream="qkg")
            cnt = {"w": 0, "o": 0, "p": 0}

            def load_w(c0, n):
                b = cnt["w"] % 2
                cnt["w"] += 1
                P.dma("pool", wt[b][:, :, 0:n], w_l[:, :, c0:c0 + n], w=["wi%d" % b], stream="wi%d" % b)
                return wt[b], "wi%d" % b

            def fm_block(c0, n, evac):
                w, wres = load_w(c0, n)
                for j in range(n // 128):
                    for tb in range(NB):
                        pp = cnt["p"] % 2
                        cnt["p"] += 1
                        self.mm_group(pa[pp][:], [(w[:, kc, j * 128:(j + 1) * 128], self.hT[:, kc, tb * 512:(tb + 1) * 512])
                                                   for kc in range(KC)],
                                      r=[wres] + [("hT", tb * 4 + i) for i in range(4)], w=["pa%d" % pp])
                        evac(pa[pp], "pa%d" % pp, c0 + j * 128, tb)

            def out_stage(bf):
                b = cnt["o"] % 2
                cnt["o"] += 1
                return (stgb[b], "stgb%d" % b) if bf else (stg[b], "stg%d" % b)

            def evac_aqk(p, pres, col, tb):
                isq = col < C_AK
                gcol = 0 if isq else 1
                tsl = slice(tb * 512, (tb + 1) * 512)
                P.op("act", lambda e: e.activation(out=sq[:], in_=p[:], func=AF.Square), r=[pres], w=["sq"])
                P.op("pe", lambda e: e.matmul(pb[:], blk[:], sq[:], start=True, stop=True), r=["blk", "sq"], w=["pb"])
                P.op("act", lambda e: e.activation(out=rs[:], in_=pb[:], func=AF.Sqrt, bias=(64e-6 if isq else 1e-6),
                                                   scale=(1.0 if isq else 1.0 / 64)), r=["pb"], w=["rs"])
                P.op("dve", lambda e: e.reciprocal(out=rs[:], in_=rs[:]), r=["rs"], w=["rs"])
                P.op("dve", lambda e: e.scalar_tensor_tensor(out=qn[:], in0=p[:], scalar=qkg[:, gcol:gcol + 1], in1=rs[:],
                                                             op0=ALU.mult, op1=ALU.mult),
                     r=[pres, "rs", "qkg"], w=["qn"])
                P.op("pe", lambda e: e.matmul(pc[:], rot[:], qn[:], start=True, stop=True), r=["rot", "qn"], w=["pc"])
                P.op("pool", lambda e: e.tensor_tensor(out=t1[:], in0=qn[:], in1=cos[:, tsl], op=ALU.mult),
                     r=["qn", "cos"], w=["t1"])
                P.op("dve", lambda e: e.tensor_tensor(out=t2[:], in0=pc[:], in1=sin[:, tsl], op=ALU.mult),
                     r=["pc", "sin"], w=["t2"])
                o, ores = out_stage(True)
                P.op("pool", lambda e: e.tensor_tensor(out=o[:], in0=t1[:], in1=t2[:], op=ALU.add),
                     r=["t1", "t2"], w=[ores])
                dst = self.qT_d[col:col + 128, tsl] if isq else self.kT_d[:, tsl]
                P.dma("sp", dst, o[:], r=[ores], w=["qkT_d"], stream=ores)

            if "A" in self.parts:
                fm_block(C_AQ, 512, evac_aqk)
                fm_block(C_AK, 128, evac_aqk)

            def evac_simple(dst_d, row0, bf, func=AF.Copy, scale=1.0):
                def ev(p, pres, col, tb):
                    o, ores = out_stage(bf)
                    P.op("act", lambda e: e.activation(out=o[:], in_=p[:], func=func, scale=scale), r=[pres], w=[ores])
                    r0 = col - row0
                    P.dma("sp", dst_d[r0:r0 + 128, tb * 512:(tb + 1) * 512], o[:], r=[ores], w=[("fm_d", id(dst_d))],
                          stream=ores)
                return ev

            if "B" in self.parts:
                fm_block(C_BQ, 256, evac_simple(self.bqT_d, C_BQ, True, scale=0.125))
                fm_block(C_BK, 256, evac_simple(self.bkT_d, C_BK, True))
            if "C" in self.parts:
                fm_block(C_CQ, 512, evac_simple(self.cT_d, C_CQ, False))
                fm_block(C_CV, 256, evac_simple(self.cT_d, C_CQ, False))
            for g in range(6):
                fm_block(C_GATE + g * 512, 512, evac_simple(self.gT_d, C_GATE, True, func=AF.Sigmoid))

            def tm_block(c0, n, dst_d, bf):
                w, wres = load_w(c0, n)
                for tt in range(NT):
                    pp = cnt["p"] % 2
                    cnt["p"] += 1
                    self.mm_group(pa[pp][:, 0:n], [(self.hT[:, kc, tt * 128:(tt + 1) * 128], w[:, kc, 0:n]) for kc in range(KC)],
                                  r=[wres, ("hT", tt)], w=["pa%d" % pp])
                    o, ores = out_stage(bf)
                    P.op("act", lambda e, o=o, pp=pp: e.activation(out=o[:, 0:n], in_=pa[pp][:, 0:n], func=AF.Copy),
                         r=["pa%d" % pp], w=[ores])
                    P.dma("sp", dst_d[tt * 128:(tt + 1) * 128, :], o[:, 0:n], r=[ores], w=[("tm_d", id(dst_d))], stream=ores)

            if "A" in self.parts:
                tm_block(C_AV, 128, self.v_d, True)
            if "B" in self.parts:
                tm_block(C_BV, 256, self.bv_d, True)
            if "C" in self.parts:
                tm_block(C_CZ, 272, self.cz_d, False)
            P.flush()

    def phase_attn_a(self, l):
        P = self.P
        S, NB, NT = self.S, self.NB, self.NT
        with ExitStack() as es:
            qh = [self.sb(es, "qh%d" % i, [128, S], BF16) for i in range(2)]
            kT = self.sb(es, "kT", [128, 2, S], BF16)
            vx = self.sb(es, "vx", [128, NT, 2, 128], BF16)
            pT = [self.sb(es, "pT%d" % i, [128, 512], BF16) for i in range(3)]
            onesr = self.sb(es, "onesr", [128, 64])
            rc = self.sb(es, "rc", [128, 512])
            bcs = self.sb(es, "bcs", [64, 512])
            ya = [self.sb(es, "ya%d" % i, [64, 512], BF16) for i in range(2)]
            sps = [self.ps(es, "sps%d" % i, [128, 512]) for i in range(3)]
            ops_ = [self.ps(es, "ops%d" % i, [128, 512]) for i in range(2)]
            bps = self.ps(es, "bps", [64, 512])
            P.op("pool", lambda e: e.memset(kT[64:128, :, :], 0.0), w=["kT"])
            for i in range(2):
                P.op("pool", lambda e, i=i: e.memset(qh[i][64:128, :], 0.0), w=["qh%d" % i])
            P.dma("sp", kT[0:64, :, :], self.kT_d.rearrange("(g d) s -> d g s", d=64), r=["qkT_d"], w=["kT"], stream="kT")
            P.op("pool", lambda e: e.memset(vx[:], 1.0), w=["vx"])
            P.op("pool", lambda e: e.memset(onesr[:], 1.0), w=["onesr"])
            for g in range(2):
                P.dma("sp", vx[:, :, g, 0:64], self.v_d[:, g * 64:(g + 1) * 64].rearrange("(t p) d -> p t d", p=128),
                      r=[("tm_d", id(self.v_d))], w=["vx"], stream="vx")
            its = [(hq, qb, kt) for hq in range(8) for qb in range(NB) for kt in range(NT)]
            N_ = len(its)
            deferred = {}

            def emit_qk(j):
                hq, qb, kt = its[j]
                g, qb_, b = hq // 4, hq % 2, j % 3
                if qb == 0 and kt == 0:
                    for h2 in ([0, 1] if hq == 0 else [hq + 1]):
                        if h2 < 8:
                            P.dma("sp", qh[h2 % 2][0:64, :], self.qT_d[h2 * 64:(h2 + 1) * 64, :], r=["qkT_d"], w=["qh%d" % (h2 % 2)],
                                  stream="qh%d" % (h2 % 2))
                P.op("pe", lambda e: e.matmul(sps[b][:], kT[:, g, kt * 128:(kt + 1) * 128], qh[qb_][:, qb * 512:(qb + 1) * 512],
                                              start=True, stop=True), r=["kT", "qh%d" % qb_], w=["sps%d" % b])

            def tail(hq, qb, ob):
                P.op("pe", lambda e: e.matmul(bps[:], onesr[64:65, :], rc[64:65, :], start=True, stop=True),
                     r=["onesr", "rc"], w=["bps"])
                P.op("act", lambda e: e.activation(out=bcs[:], in_=bps[:], func=AF.Copy), r=["bps"], w=["bcs"])
                P.op("dve", lambda e: e.tensor_tensor(out=ya[ob][:], in0=ops_[ob][0:64, :], in1=bcs[:], op=ALU.mult),
                     r=["ops%d" % ob, "bcs"], w=["ya%d" % ob])
                P.dma("sp", self.yaT_d[hq * 64:(hq + 1) * 64, qb * 512:(qb + 1) * 512], ya[ob][:], r=["ya%d" % ob],
                      w=["yaT_d"], stream="ya%d" % ob)

            emit_qk(0)
            emit_qk(1)
            for j in range(N_):
                hq, qb, kt = its[j]
                g, b = hq // 4, j % 3
                ob = (hq * NB + qb) % 2
                if j + 2 < N_:
                    emit_qk(j + 2)
                P.op("act", lambda e, b=b: e.activation(out=pT[b][:], in_=sps[b][:], func=AF.Exp),
                     r=["sps%d" % b], w=["pT%d" % b])
                P.op("pe", lambda e, b=b, kt=kt, g=g, ob=ob: e.matmul(ops_[ob][:, :], vx[:, kt, g, :], pT[b][:],
                                                          start=(kt == 0), stop=(kt == NT - 1)),
                     r=["vx", "pT%d" % b], w=["ops%d" % ob], inc=(kt == NT - 1))
                if kt == NT - 1:
                    P.op("dve", lambda e, ob=ob: e.reciprocal(out=rc[64:65, :], in_=ops_[ob][64:65, :]), r=["ops%d" % ob], w=["rc"])
                    deferred[min(j + 2, N_ - 1)] = (hq, qb, ob)
                if j in deferred:
                    tail(*deferred.pop(j))
            assert not deferred
            P.flush()

    def phase_attn_b(self, l):
        P = self.P
        S, NT = self.S, self.NT
        rows = S // GRID_W
        wr_ = min(8, rows)
        NBUF = 4
        with ExitStack() as es:
            qT = self.sb(es, "bqT", [64, 4, S], BF16)
            kT = self.sb(es, "bkT", [64, 4, S], BF16)
            v0 = self.sb(es, "bv0", [128, NT, 256], BF16)
            v1 = self.sb(es, "bv1", [128, NT, 256], BF16)
            bias = self.sb(es, "bbias", [64, 4, 960])
            sc = [self.sb(es, "bsc%d" % i, [64, 512]) for i in range(NBUF)]
            pr = [self.sb(es, "bpr%d" % i, [64, 512], BF16) for i in range(NBUF)]
            st = [self.sb(es, "bst%d" % i, [64, 4]) for i in range(NBUF)]
            dg = [self.sb(es, "bdg%d" % i, [64, 64], BF16) for i in range(NBUF)]
            pts = [self.sb(es, "bpts%d" % i, [128, 4, 64], BF16) for i in range(2)]
            yo = [self.sb(es, "byo%d" % i, [64, 4, 64], BF16) for i in range(2)]
            sp_ = [self.ps(es, "bsp%d" % i, [64, 512]) for i in range(NBUF)]
            ptp = [self.ps(es, "bptp%d" % i, [128, 4, 64]) for i in range(2)]
            op_ = [self.ps(es, "bop%d" % i, [64, 4, 64]) for i in range(2)]
            for h in range(4):
                P.dma("sp", qT[:, h, :], self.bqT_d[h * 64:(h + 1) * 64, :], r=[("fm_d", id(self.bqT_d))], w=["bqT"], stream="bqT")
                P.dma("sp", kT[:, h, :], self.bkT_d[h * 64:(h + 1) * 64, :], r=[("fm_d", id(self.bkT_d))], w=["bkT"], stream="bkT")
            P.dma("sp", v0[:], self.bv_d.rearrange("(t p) c -> p t c", p=128), r=[("tm_d", id(self.bv_d))], w=["bv0"], stream="bv0")
            P.dma("sp", v1[:, 0:NT - 1, :], self.bv_d[64:S - 64, :].rearrange("(t p) c -> p t c", p=128), r=[("tm_d", id(self.bv_d))],
                  w=["bv1"], stream="bv1")
            P.dma("sp", bias[:], self.bias_b[l].rearrange("h q k -> q h k"), w=["bbias"], stream="bbias")
            its = [(r, h) for r in range(rows) for h in range(4)]
            N_ = len(its)

            def geo(r):
                r0 = min(max(r - wr_ // 2, 0), rows - wr_)
                return r0, (r0 - r + 7) * 64, r0 * 64

            def s1(i):
                r, h = its[i]
                b = i % NBUF
                r0, d0, k0 = geo(r)
                P.op("pe", lambda e: e.matmul(sp_[b][:], qT[:, h, r * 64:(r + 1) * 64], kT[:, h, k0:k0 + 512], start=True, stop=True),
                     r=["bqT", "bkT"], w=["bsp%d" % b])
                P.op("dve", lambda e: e.tensor_tensor(out=sc[b][:], in0=sp_[b][:], in1=bias[:, h, d0:d0 + 512], op=ALU.add),
                     r=["bsp%d" % b, "bbias"], w=["bsc%d" % b])
                P.op("dve", lambda e: e.tensor_reduce(out=st[b][:, 0:1], in_=sc[b][:], axis=AX.X, op=ALU.max),
                     r=["bsc%d" % b], w=[("bst", b, 0)])
                P.op("dve", lambda e: e.tensor_scalar(out=st[b][:, 1:2], in0=st[b][:, 0:1], scalar1=-1.0, scalar2=None, op0=ALU.mult),
                     r=[("bst", b, 0)], w=[("bst", b, 1)])
                P.op("act", lambda e: e.activation(out=pr[b][:], in_=sc[b][:], func=AF.Exp, bias=st[b][:, 1:2], scale=1.0,
                                                   accum_out=st[b][:, 2:3]), r=["bsc%d" % b, ("bst", b, 1)], w=["bpr%d" % b, ("bst", b, 2)])

            def s1b(i):
                b = i % NBUF
                P.op("dve", lambda e: e.reciprocal(out=st[b][:, 3:4], in_=st[b][:, 2:3]), r=[("bst", b, 2)], w=[("bst", b, 3)])
                P.op("dve", lambda e: e.tensor_scalar(out=dg[b][:], in0=self.identb[0:64, 0:64], scalar1=st[b][:, 3:4], scalar2=None,
                                                      op0=ALU.mult), r=["identb", ("bst", b, 3)], w=["bdg%d" % b])

            def s2(i):
                b, pb = i % NBUF, i % 2
                for kc in range(4):
                    P.op("pe", lambda e, kc=kc: e.matmul(ptp[pb][:, kc, :], pr[b][:, kc * 128:(kc + 1) * 128], dg[b][:], start=True, stop=True),
                         r=["bpr%d" % b, "bdg%d" % b], w=["bptp%d" % pb], inc=(kc == 3))
                P.op("act", lambda e: e.activation(out=pts[pb][:], in_=ptp[pb][:], func=AF.Copy), r=["bptp%d" % pb], w=["bpts%d" % pb])

            def s3(i):
                r, h = its[i]
                pb, ob = i % 2, r % 2
                r0, d0, k0 = geo(r)
                vsrc, vres, t0 = (v0, "bv0", r0 // 2) if r0 % 2 == 0 else (v1, "bv1", (r0 - 1) // 2)
                for kc in range(4):
                    P.op("pe", lambda e, kc=kc: e.matmul(op_[ob][:, h, :], vsrc[:, t0 + kc, h * 64:(h + 1) * 64], pts[pb][:, kc, :],
                                                         start=(kc == 0), stop=(kc == 3)),
                         r=[vres, "bpts%d" % pb], w=["bop%d" % ob], inc=(kc == 3))
                if h == 3:
                    P.op("act", lambda e: e.activation(out=yo[ob][:], in_=op_[ob][:], func=AF.Copy), r=["bop%d" % ob], w=["byo%d" % ob])
                    P.dma("sp", self.ybT_d[:, r * 64:(r + 1) * 64].rearrange("(h d) q -> d h q", d=64), yo[ob][:], r=["byo%d" % ob],
                          w=["ybT_d"], stream="byo%d" % ob)

            for t in range(N_ + 4):
                if t < N_:
                    s1(t)
                if 0 <= t - 1 < N_:
                    s1b(t - 1)
                if 0 <= t - 3 < N_:
                    s2(t - 3)
                if 0 <= t - 4 < N_:
                    s3(t - 4)
            P.flush()

    def phase_gdn(self, l):
        P = self.P
        S, NT, NB = self.S, self.NT, self.NB
        nc = self.nc
        if not hasattr(self, "cn_d"):
            mk = lambda n, s: nc.dram_tensor(n, list(s), F32, kind=("ExternalOutput" if n in self.dbg else "Internal")).ap()
            self.cn_d = mk("cn_d", [768, S])
            self.ktok_d = mk("ktok_d", [S, 256])
            self.vtok_d = mk("vtok_d", [S, 256])
            self.gates_d = mk("gates_d", [S, 16])
            self.o_d = mk("o_d", [2, S, 256])
        with ExitStack() as es:
            cw = self.sb(es, "cw", [128, 6, 5])
            x = self.sb(es, "gx", [128, S + 4])
            y = self.sb(es, "gy", [128, S])
            sq = self.sb(es, "gsq", [128, 512], BF16)
            rs = self.sb(es, "grs", [128, 512])
            blk = self.sb(es, "gblk", [128, 128], BF16)
            tk = [self.sb(es, "gtk%d" % i, [128, 512]) for i in range(2)]
            gin = self.sb(es, "gin", [128, NT, 16])
            gout = self.sb(es, "gout", [128, NT, 16])
            ab = self.sb(es, "gab", [128, 2, 8])
            pss = self.ps(es, "gpss", [128, 512])
            ptr = [self.ps(es, "gptr%d" % i, [128, 4, 128]) for i in range(2)]
            P.dma("sp", cw[:], self.conv_w[l], w=["cw"], stream="cw")
            P.dma("pool", blk[:], self.c_blk, w=["gblk"], stream="gblk")
            P.dma("sp", ab[:], self.gdn_ab[l].partition_broadcast(128), w=["gab"], stream="gab")
            P.op("pool", lambda e: e.memset(x[:, 0:2], 0.0), w=["gxp"])
            P.op("pool", lambda e: e.memset(x[:, S + 2:S + 4], 0.0), w=["gxp"])
            ti = 0
            import os
            for ch in range(6 if os.environ.get("GDBG", "") != "gates" else 0):
                P.dma("sp", x[:, 2:S + 2], self.cT_d[ch * 128:(ch + 1) * 128, :], r=[("fm_d", id(self.cT_d))], w=["gx"], stream="gx")
                P.op("act", lambda e, ch=ch: e.activation(out=y[:], in_=x[:, 0:S], func=AF.Identity, scale=cw[:, ch, 0:1]),
                     r=["gx", "gxp", "cw"], w=["gy"])
                for k in range(1, 5):
                    P.op("dve", lambda e, ch=ch, k=k: e.scalar_tensor_tensor(out=y[:], in0=x[:, k:k + S], scalar=cw[:, ch, k:k + 1], in1=y[:],
                                                                             op0=ALU.mult, op1=ALU.add), r=["gx", "gxp", "cw", "gy"], w=["gy"])
                P.op("act", lambda e: e.activation(out=y[:], in_=y[:], func=AF.Silu), r=["gy"], w=["gy"])
                if ch < 4:
                    isq = ch < 2
                    for tb in range(NB):
                        tsl = slice(tb * 512, (tb + 1) * 512)
                        P.op("act", lambda e, tsl=tsl: e.activation(out=sq[:], in_=y[:, tsl], func=AF.Square), r=["gy"], w=["gsq"])
                        P.op("pe", lambda e: e.matmul(pss[:], blk[:], sq[:], start=True, stop=True), r=["gblk", "gsq"], w=["gpss"])
                        P.op("act", lambda e, isq=isq: e.activation(out=rs[:], in_=pss[:], func=AF.Sqrt, bias=(64e-6 if isq else 1e-6),
                                                                    scale=(64.0 if isq else 1.0)), r=["gpss"], w=["grs"])
                        P.op("dve", lambda e: e.reciprocal(out=rs[:], in_=rs[:]), r=["grs"], w=["grs"])
                        P.op("dve", lambda e, tsl=tsl: e.tensor_tensor(out=y[:, tsl], in0=y[:, tsl], in1=rs[:], op=ALU.mult),
                             r=["gy", "grs"], w=["gy"])
                P.dma("sp", self.cn_d[ch * 128:(ch + 1) * 128, :], y[:], r=["gy"], w=["cn_d"], stream="gy")
                if ch >= 2:
                    dst = self.ktok_d if ch < 4 else self.vtok_d
                    for tb in range(NB):
                        b = ti % 2
                        ti += 1
                        for j in range(4):
                            tt = tb * 4 + j
                            P.op("pe", lambda e, b=b, j=j, tt=tt: e.transpose(ptr[b][:, j, :], y[:, tt * 128:(tt + 1) * 128], self.ident[:]),
                                 r=["gy", "ident"], w=["gptr%d" % b], inc=(j == 3))
                        P.op("act", lambda e, b=b: e.activation(out=tk[b][:], in_=ptr[b][:].rearrange("p a b -> p (a b)"), func=AF.Copy),
                             r=["gptr%d" % b], w=["gtk%d" % b])
                        P.dma("sp", dst[tb * 512:(tb + 1) * 512, (ch % 2) * 128:(ch % 2 + 1) * 128].rearrange("(j p) c -> p j c", p=128),
                              tk[b][:].rearrange("p (j c) -> p j c", c=128), r=["gtk%d" % b], w=["kvtok_d"], stream="gtk%d" % b)
            if os.environ.get("GDBG", "") == "conv":
                P.flush()
                return
            P.dma("sp", gin[:], self.cz_d[:, 256:272].rearrange("(t p) c -> p t c", p=128), r=[("tm_d", id(self.cz_d))], w=["gin"], stream="gin")
            P.op("act", lambda e: e.activation(out=gout[:, :, 0:8], in_=gin[:, :, 0:8], func=AF.Sigmoid), r=["gin"], w=["gout_b"])
            P.op("dve", lambda e: e.tensor_tensor(out=gin[:, :, 8:16], in0=gin[:, :, 8:16], in1=bc(ab[:, 1:2, :], [128, NT, 8]), op=ALU.add),
                 r=["gin", "gab"], w=["gin2"])
            P.op("act", lambda e: e.activation(out=gin[:, :, 8:16], in_=gin[:, :, 8:16], func=AF.Exp), r=["gin2"], w=["gin2"])
            P.op("act", lambda e: e.activation(out=gin[:, :, 8:16], in_=gin[:, :, 8:16], func=AF.Ln, bias=1.0, scale=1.0), r=["gin2"], w=["gin2"])
            P.op("act", lambda e: e.activation(out=ab[:, 0, :], in_=ab[:, 0, :], func=AF.Exp), r=["gab"], w=["gab0"])
            P.op("dve", lambda e: e.scalar_tensor_tensor(out=gout[:, :, 8:16], in0=gin[:, :, 8:16], scalar=-1.0, in1=bc(ab[:, 0:1, :], [128, NT, 8]),
                                                         op0=ALU.mult, op1=ALU.mult), r=["gin2", "gab0"], w=["gout_g"])
            P.dma("sp", self.gates_d.rearrange("(t p) c -> p t c", p=128), gout[:], r=["gout_b", "gout_g"], w=["gates_d"], stream="gout")
            P.flush()
        if getattr(self, "gdn_stop", None) == "prep":
            return
        NC = S // 64
        with ExitStack() as es:
            cst = self.sb(es, "gcst", [128, 5, 4, 128])
            NEGM, NEGMT, STRICT, ID8 = cst[:, 0], cst[:, 1], cst[:, 2], cst[:, 3]
            CUM, ONES = cst[:, 4, 0, :], cst[:, 4, 1, :]
            T8 = lambda n: self.sb(es, n, [128, 4, 128])
            ld = [dict(KT=T8("gKT%d" % i), QT=T8("gQT%d" % i), Kt=T8("gKt%d" % i), Vt=T8("gVt%d" % i),
                       gb=self.sb(es, "ggb%d" % i, [128, 8])) for i in range(2)]
            TN = ("gdiag", "gD", "gDT", "geGr", "gta", "gtb", "gSB", "gqkTm", "gQgT", "gX0", "gX1", "gXT0", "gXT1",
                  "gPT", "grv", "grk", "gU", "gWT", "gVn", "gKd", "gO")
            WT_ = [{n: T8(n + "_%d" % p) for n in TN} for p in range(2)]
            SM = [self.sb(es, "gsm%d" % p, [128, 24]) for p in range(2)]
            St = T8("gS")
            psAA = self.ps(es, "gpsA", [128, 2, 8])
            BK = [[self.ps(es, "gpb%d_%d" % (p, i), [128, 4, 128]) for i in range(3)] for p in range(2)]
            fl = lambda t: t[:].rearrange("p c j -> p (c j)")
            P.dma("sp", cst[:], self.c_gdn, w=["gcst"], stream="gcst")
            P.op("pool", lambda e: e.memset(St[:], 0.0), w=["gS"])
            for i in range(2):
                for nm in ("KT", "QT", "Kt", "Vt"):
                    P.op("pool", lambda e, t=ld[i][nm]: e.memset(t[:], 0.0), w=["g" + nm + str(i)])

            def bcg(t, col0):
                return bc(t[:, col0:col0 + 4].unsqueeze(2), [128, 4, 128])

            def body(s_):
                a, b = s_, NC - 1 - s_
                p = s_ % 2
                L_ = ld[p]
                sfx = str(p)
                W = WT_[p]
                R = lambda n: n + "_" + sfx
                diagG, Dm, DTm, eGr, t_a, t_b, SBm, qkTm, QgT = (W[n] for n in ("gdiag", "gD", "gDT", "geGr", "gta", "gtb", "gSB", "gqkTm", "gQgT"))
                X, XT = [W["gX0"], W["gX1"]], [W["gXT0"], W["gXT1"]]
                PT, rv, rk, U, WTt, Vn, Kd, ot = (W[n] for n in ("gPT", "grv", "grk", "gU", "gWT", "gVn", "gKd", "gO"))
                sm = SM[p]
                psA = psAA[:, p, :]
                B0, B1, B2 = BK[p]
                rB0, rB1, rB2, rA = R("gB0"), R("gB1"), R("gB2"), R("gpsA")

                def mm8(ps, psres, lhs, lres, rhs, rres):
                    for c in range(4):
                        P.op("pe", lambda e, c=c: e.matmul(ps[:, c, :], lhs[:, c, :], rhs[:, c, :], start=True, stop=True),
                             r=[lres, rres], w=[psres], inc=(c == 3))

                ra, rb_ = slice(a * 64, (a + 1) * 64), slice(b * 64, (b + 1) * 64)
                lo, hi = slice(0, 64), slice(64, 128)
                for nm, src_, r0 in (("KT", self.cn_d, 256), ("QT", self.cn_d, 0)):
                    for pp, rr in ((lo, ra), (hi, rb_)):
                        P.dma("sp", L_[nm][pp, :, pp], src_[r0:r0 + 256, rr].rearrange("(h d) t -> d h t", d=64),
                              r=["cn_d"], w=["g" + nm + sfx], stream="g" + nm + sfx)
                for nm, src_ in (("Kt", self.ktok_d), ("Vt", self.vtok_d)):
                    for pp, rr in ((lo, ra), (hi, rb_)):
                        P.dma("sp", L_[nm][pp, :, pp], src_[rr, :].rearrange("t (h d) -> t h d", d=64),
                              r=["kvtok_d"], w=["g" + nm + sfx], stream="g" + nm + sfx)
                for pp, rr, c0, s0 in ((lo, ra, 0, 0), (hi, rb_, 0, 4), (lo, ra, 4, 8), (hi, rb_, 4, 12)):
                    P.dma("sp", L_["gb"][pp, c0:c0 + 4], self.gates_d[rr, s0:s0 + 4], r=["gates_d"], w=["ggb" + sfx], stream="ggb" + sfx)
                KT, QT, Kt, Vt, gb = L_["KT"], L_["QT"], L_["Kt"], L_["Vt"], L_["gb"]
                rKT, rQT, rKt, rVt, rgb = ("g" + n + sfx for n in ("KT", "QT", "Kt", "Vt", "gb"))
                P.op("pe", lambda e: e.matmul(psA[:, 0:4], CUM, gb[:, 4:8], start=True, stop=True), r=["gcst", rgb], w=[rA], inc=False)
                P.op("pe", lambda e: e.matmul(psA[:, 4:8], ONES, gb[:, 4:8], start=True, stop=True), r=["gcst", rgb], w=[rA])
                P.op("act", lambda e: e.activation(out=sm[:, 0:8], in_=psA, func=AF.Copy), r=[rA], w=[R("gsmG")])
                P.op("act", lambda e: e.activation(out=sm[:, 8:16], in_=sm[:, 0:8], func=AF.Exp), r=[R("gsmG")], w=[R("gsmE")])
                P.op("dve", lambda e: e.tensor_tensor(out=sm[:, 16:20], in0=sm[:, 4:8], in1=sm[:, 0:4], op=ALU.subtract), r=[R("gsmG")], w=[R("gsmK")])
                P.op("act", lambda e: e.activation(out=sm[:, 16:20], in_=sm[:, 16:20], func=AF.Exp), r=[R("gsmK")], w=[R("gsmK")])
                P.op("dve", lambda e: e.tensor_tensor(out=sm[:, 20:24], in0=gb[:, 0:4], in1=sm[:, 8:12], op=ALU.mult), r=[rgb, R("gsmE")], w=[R("gsmB")])
                Gb, eGtb, kdb, bkb = bcg(sm, 0), bcg(sm, 12), bcg(sm, 16), bcg(sm, 20)
                betab = bc(gb[:, 0:4].unsqueeze(2), [128, 4, 128])
                P.op("pool", lambda e: e.tensor_tensor(out=diagG[:], in0=ID8, in1=Gb, op=ALU.mult), r=["gcst", R("gsmG")], w=[R("gdiag")])
                P.op("pe", lambda e: e.matmul(fl(B0), ONES, fl(diagG), start=True, stop=True), r=["gcst", R("gdiag")], w=[rB0])
                P.op("dve", lambda e: e.scalar_tensor_tensor(out=t_a[:], in0=B0[:], scalar=-1.0, in1=NEGM, op0=ALU.mult, op1=ALU.add),
                     r=[rB0, "gcst"], w=[R("gta")])
                P.op("dve", lambda e: e.tensor_tensor(out=t_a[:], in0=t_a[:], in1=Gb, op=ALU.add), r=[R("gta"), R("gsmG")], w=[R("gta")])
                P.op("act", lambda e: e.activation(out=Dm[:], in_=t_a[:], func=AF.Exp), r=[R("gta")], w=[R("gD")])
                P.op("dve", lambda e: e.tensor_tensor(out=t_b[:], in0=B0[:], in1=NEGMT, op=ALU.add), r=[rB0, "gcst"], w=[R("gtb")])
                P.op("dve", lambda e: e.tensor_tensor(out=t_b[:], in0=t_b[:], in1=Gb, op=ALU.subtract), r=[R("gtb"), R("gsmG")], w=[R("gtb")])
                P.op("act", lambda e: e.activation(out=DTm[:], in_=t_b[:], func=AF.Exp), r=[R("gtb")], w=[R("gDT")])
                P.op("act", lambda e: e.activation(out=eGr[:], in_=B0[:], func=AF.Exp), r=[rB0], w=[R("geGr")])
                P.op("pool", lambda e: e.tensor_tensor(out=QgT[:], in0=QT[:], in1=eGr[:], op=ALU.mult), r=[rQT, R("geGr")], w=[R("gQgT")])
                mm8(B1, rB1, KT, rKT, KT, rKT)
                mm8(B2, rB2, KT, rKT, QT, rQT)
                P.op("pool", lambda e: e.tensor_tensor(out=SBm[:], in0=STRICT, in1=betab, op=ALU.mult), r=["gcst", rgb], w=[R("gSB")])
                P.op("dve", lambda e: e.tensor_tensor(out=t_a[:], in0=B1[:], in1=Dm[:], op=ALU.mult), r=[rB1, R("gD")], w=[R("gta")])
                P.op("pool", lambda e: e.tensor_tensor(out=X[0][:], in0=t_a[:], in1=SBm[:], op=ALU.mult), r=[R("gta"), R("gSB")], w=[R("gX0")])
                P.op("dve", lambda e: e.tensor_tensor(out=qkTm[:], in0=B2[:], in1=DTm[:], op=ALU.mult), r=[rB2, R("gDT")], w=[R("gqkTm")])
                mm8(B1, rB1, X[0], R("gX0"), cst[:, 3], "gcst")
                P.op("act", lambda e: e.activation(out=XT[0][:], in_=B1[:], func=AF.Copy), r=[rB1], w=[R("gXT0")])
                P.op("dve", lambda e: e.scalar_tensor_tensor(out=PT[:], in0=B1[:], scalar=-1.0, in1=ID8, op0=ALU.mult, op1=ALU.add),
                     r=[rB1, "gcst", R("gXT0")], w=[R("gPT")])
                for lv in range(1, 6):
                    ci, ni = (lv - 1) % 2, lv % 2
                    mm8(B0, rB0, XT[ci], R("gXT%d" % ci), X[ci], R("gX%d" % ci))
                    if lv < 5:
                        mm8(B2, rB2, X[ci], R("gX%d" % ci), XT[ci], R("gXT%d" % ci))
                    P.op("act", lambda e, ni=ni: e.activation(out=X[ni][:], in_=B0[:], func=AF.Copy), r=[rB0], w=[R("gX%d" % ni)])
                    if lv < 5:
                        P.op("dve", lambda e, ni=ni: e.tensor_copy(out=XT[ni][:], in_=B2[:]), r=[rB2], w=[R("gXT%d" % ni)])
                    mm8(B1, rB1, X[ni], R("gX%d" % ni), PT, R("gPT"))
                    P.op("dve", lambda e: e.tensor_tensor(out=PT[:], in0=PT[:], in1=B1[:], op=ALU.add), r=[R("gPT"), rB1], w=[R("gPT")])
                P.op("pool", lambda e: e.tensor_tensor(out=rv[:], in0=Vt[:], in1=betab, op=ALU.mult), r=[rVt, rgb], w=[R("grv")])
                P.op("pool", lambda e: e.tensor_tensor(out=rk[:], in0=Kt[:], in1=bkb, op=ALU.mult), r=[rKt, R("gsmB")], w=[R("grk")])
                P.op("pool", lambda e: e.tensor_tensor(out=Kd[:], in0=Kt[:], in1=kdb, op=ALU.mult), r=[rKt, R("gsmK")], w=[R("gKd")])
                mm8(B0, rB0, PT, R("gPT"), rv, R("grv"))
                P.op("act", lambda e: e.activation(out=U[:], in_=B0[:], func=AF.Copy), r=[rB0], w=[R("gU")])
                mm8(B2, rB2, rk, R("grk"), PT, R("gPT"))
                P.op("dve", lambda e: e.tensor_copy(out=WTt[:], in_=B2[:]), r=[rB2], w=[R("gWT")])
                mark.append(len(P.rec))
                mm8(B1, rB1, WTt, R("gWT"), St, "gS")
                P.op("dve", lambda e: e.scalar_tensor_tensor(out=Vn[:], in0=B1[:], scalar=-1.0, in1=U[:], op0=ALU.mult, op1=ALU.add),
                     r=[rB1, R("gU")], w=[R("gVn")])
                for c in range(4):
                    P.op("pe", lambda e, c=c: e.matmul(B0[:, c, :], QgT[:, c, :], St[:, c, :], start=True, stop=False), r=[R("gQgT"), "gS"], w=[rB0],
                         inc=False)
                    P.op("pe", lambda e, c=c: e.matmul(B0[:, c, :], qkTm[:, c, :], Vn[:, c, :], start=False, stop=True), r=[R("gqkTm"), R("gVn")],
                         w=[rB0], inc=(c == 3))
                mm8(B2, rB2, Kd, R("gKd"), Vn, R("gVn"))
                P.op("pool", lambda e: e.tensor_tensor(out=St[:], in0=St[:], in1=eGtb, op=ALU.mult), r=["gS", R("gsmE")], w=["gS"])
                P.op("dve", lambda e: e.tensor_tensor(out=St[:], in0=St[:], in1=B2[:], op=ALU.add), r=["gS", rB2], w=["gS"])
                P.op("act", lambda e: e.activation(out=ot[:], in_=B0[:], func=AF.Copy), r=[rB0], w=[R("gO")])
                P.dma("sp", self.o_d[0, ra, :].rearrange("t (h d) -> t h d", d=64), ot[0:64, :, 0:64], r=[R("gO")], w=["o_d"], stream="gO" + sfx)
                P.dma("sp", self.o_d[1, rb_, :].rearrange("t (h d) -> t h d", d=64), ot[64:128, :, 64:128], r=[R("gO")], w=["o_d"], stream="gO" + sfx)

            streams = [[], []]
            for s_ in range(NC):
                P.rec = []
                mark = []
                body(s_)
                items, P.rec = P.rec, None
                for k, it_ in enumerate(items):
                    streams[s_ % 2].append((s_, k >= mark[0], it_))
            per = len(streams[0]) // ((NC + 1) // 2)
            delay = per // 2
            merged = []
            i0 = i1 = 0
            n0, n1 = len(streams[0]), len(streams[1])
            t = 0
            while i0 < n0 or i1 < n1:
                if i0 < n0:
                    merged.append(streams[0][i0]); i0 += 1
                if t >= delay and i1 < n1:
                    merged.append(streams[1][i1]); i1 += 1
                t += 1
            last_rec = -1
            for s_, isrec, it_ in merged:
                if isrec:
                    assert s_ >= last_rec, "recurrence emitted out of chunk order"
                    last_rec = s_
                P.replay(it_)
            P.flush()
        if getattr(self, "gdn_stop", None) == "main":
            return
        with ExitStack() as es:
            gg = self.sb(es, "ggn", [128, 64])
            of = [self.sb(es, "gof%d" % i, [128, 4, 64]) for i in range(2)]
            obk = [self.sb(es, "gob%d" % i, [128, 4, 64]) for i in range(2)]
            zt = [self.sb(es, "gz%d" % i, [128, 4, 64]) for i in range(2)]
            sq2 = self.sb(es, "gsq2", [128, 4, 64])
            ms = self.sb(es, "gms", [128, 8])
            yb_ = [self.sb(es, "gyb%d" % i, [128, 2, 128], BF16) for i in range(2)]
            pt2 = [self.ps(es, "gpt2%d" % i, [128, 2, 128]) for i in range(2)]
            P.dma("sp", gg[:], self.gdn_g[l].partition_broadcast(128), w=["ggn"], stream="ggn")
            for tt in range(NT):
                b = tt % 2
                tr = slice(tt * 128, (tt + 1) * 128)
                P.dma("sp", of[b][:], self.o_d[0, tr, :].rearrange("t (h d) -> t h d", d=64), r=["o_d"], w=["gof%d" % b], stream="gof%d" % b)
                P.dma("sp", obk[b][:], self.o_d[1, tr, :].rearrange("t (h d) -> t h d", d=64), r=["o_d"], w=["gob%d" % b], stream="gob%d" % b)
                P.dma("sp", zt[b][:], self.cz_d[tr, 0:256].rearrange("t (h d) -> t h d", d=64), r=[("tm_d", id(self.cz_d))], w=["gz%d" % b],
                      stream="gz%d" % b)
                P.op("dve", lambda e, b=b: e.tensor_tensor(out=of[b][:], in0=of[b][:], in1=obk[b][:], op=ALU.add), r=["gof%d" % b, "gob%d" % b],
                     w=["gof%d" % b])
                P.op("act", lambda e, b=b: e.activation(out=sq2[:], in_=of[b][:], func=AF.Square), r=["gof%d" % b], w=["gsq2"])
                P.op("dve", lambda e: e.tensor_reduce(out=ms[:, 0:4], in_=sq2[:], axis=AX.X, op=ALU.add), r=["gsq2"], w=["gms"])
                P.op("act", lambda e: e.activation(out=ms[:, 4:8], in_=ms[:, 0:4], func=AF.Sqrt, bias=1e-6, scale=1.0 / 64), r=["gms"], w=["gms2"])
                P.op("dve", lambda e: e.reciprocal(out=ms[:, 4:8], in_=ms[:, 4:8]), r=["gms2"], w=["gms2"])
                P.op("dve", lambda e, b=b: e.tensor_tensor(out=of[b][:], in0=of[b][:], in1=bc(ms[:, 4:8].unsqueeze(2), [128, 4, 64]), op=ALU.mult),
                     r=["gof%d" % b, "gms2"], w=["gof%d" % b])
                P.op("pool", lambda e, b=b: e.tensor_tensor(out=of[b][:], in0=of[b][:], in1=bc(gg[:].unsqueeze(1), [128, 4, 64]), op=ALU.mult),
                     r=["gof%d" % b, "ggn"], w=["gof%d" % b])
                P.op("act", lambda e, b=b: e.activation(out=zt[b][:], in_=zt[b][:], func=AF.Silu), r=["gz%d" % b], w=["gz%d" % b])
                P.op("dve", lambda e, b=b: e.tensor_tensor(out=of[b][:], in0=of[b][:], in1=zt[b][:], op=ALU.mult), r=["gof%d" % b, "gz%d" % b],
                     w=["gof%d" % b])
                for j in range(2):
                    P.op("pe", lambda e, b=b, j=j: e.transpose(pt2[b][:, j, :], of[b][:, 2 * j:2 * j + 2, :].rearrange("p a d -> p (a d)"), self.ident[:]),
                         r=["gof%d" % b, "ident"], w=["gpt2%d" % b], inc=(j == 1))
                P.op("act", lambda e, b=b: e.activation(out=yb_[b][:], in_=pt2[b][:], func=AF.Copy), r=["gpt2%d" % b], w=["gyb%d" % b])
                P.dma("sp", self.ycT_d[:, tr].rearrange("(j p) t -> p j t", p=128), yb_[b][:], r=["gyb%d" % b], w=["ycT_d"], stream="gyb%d" % b)
            P.flush()

    def phase_merge(self, l):
        P = self.P
        S, NB, NT = self.S, self.NB, self.NT
        with ExitStack() as es:
            wa = self.sb(es, "wa", [128, 4, D], BF16)
            wb = self.sb(es, "wb", [128, 2, D], BF16)
            wc = self.sb(es, "wc", [128, 2, D], BF16)
            wo = self.sb(es, "wo", [128, 8, D], BF16)
            yT = self.sb(es, "yT", [128, 8, 512], BF16)
            gt = [self.sb(es, "gt%d" % i, [128, 3, 512], BF16) for i in range(2)]
            mixT = self.sb(es, "mixT", [128, 8, 512], BF16)
            m1 = self.sb(es, "m1", [128, 512])
            m2 = self.sb(es, "m2", [128, 512])
            m3 = self.sb(es, "m3", [128, 512])
            hold = [self.sb(es, "hold%d" % i, [128, D]) for i in range(2)]
            tsum = [self.sb(es, "tsum%d" % i, [128, D]) for i in range(2)]
            gb = self.sb(es, "gb1", [128, 2, D])
            pabc = [self.ps(es, "pabc%d" % i, [128, 512]) for i in range(3)]
            po = [self.ps(es, "po%d" % i, [128, 512]) for i in range(2)]
            tiles = self.ln_scratch(es, "l1")
            rt = self.router_setup(es) if not getattr(self, "no_router", False) else None
            P.dma("pool", wa[:], self.wa[l].rearrange("(kc p) n -> p kc n", p=128), w=["wa"], stream="wa")
            P.dma("pool", wb[:], self.wb[l].rearrange("(kc p) n -> p kc n", p=128), w=["wb"], stream="wb")
            P.dma("pool", wc[:], self.wc[l].rearrange("(kc p) n -> p kc n", p=128), w=["wc"], stream="wc")
            P.dma("pool", wo[:], self.wo[l].rearrange("(kc p) n -> p kc n", p=128), w=["wo"], stream="wo")
            P.dma("sp", gb[:], self.ln1[l].partition_broadcast(128), w=["gb1"], stream="gb1")
            if "B" not in self.parts:
                P.op("pool", lambda e: e.memset(yT[:, 4:6, :], 0.0), w=["yTb"])
            if "C" not in self.parts:
                P.op("pool", lambda e: e.memset(yT[:, 6:8, :], 0.0), w=["yTc"])
            for tb in range(NB):
                tsl = slice(tb * 512, (tb + 1) * 512)
                if "A" in self.parts:
                    P.dma("sp", yT[:, 0:4, :], self.yaT_d[:, tsl].rearrange("(kc p) s -> p kc s", p=128), r=["yaT_d"], w=["yTa"],
                          stream="yTa")
                else:
                    P.op("pool", lambda e: e.memset(yT[:, 0:4, :], 0.0), w=["yTa"])
                if "B" in self.parts:
                    P.dma("sp", yT[:, 4:6, :], self.ybT_d[:, tsl].rearrange("(kc p) s -> p kc s", p=128), r=["ybT_d"], w=["yTb"],
                          stream="yTb")
                if "C" in self.parts:
                    P.dma("sp", yT[:, 6:8, :], self.ycT_d[:, tsl].rearrange("(kc p) s -> p kc s", p=128), r=["ycT_d"], w=["yTc"],
                          stream="yTc")
                for dc in range(8):
                    b = dc % 2
                    P.dma("sp", gt[b][:], self.gT_d[:, tsl].rearrange("(t c p) s -> p t c s", p=128, t=3)[:, :, dc, :],
                          r=[("fm_d", id(self.gT_d))], w=["gt%d" % b], stream="gt%d" % b)
                    csl = slice(dc * 128, (dc + 1) * 128)
                    self.mm_group(pabc[0][:], [(wa[:, kc, csl], yT[:, kc, :]) for kc in range(4)], r=["wa", "yTa"], w=["pabc0"])
                    self.mm_group(pabc[1][:], [(wb[:, kc, csl], yT[:, 4 + kc, :]) for kc in range(2)], r=["wb", "yTb"], w=["pabc1"])
                    self.mm_group(pabc[2][:], [(wc[:, kc, csl], yT[:, 6 + kc, :]) for kc in range(2)], r=["wc", "yTc"], w=["pabc2"])
                    P.op("dve", lambda e, b=b: e.tensor_tensor(out=m1[:], in0=pabc[0][:], in1=gt[b][:, 0, :], op=ALU.mult),
                         r=["pabc0", "gt%d" % b], w=["m1"])
                    P.op("dve", lambda e, b=b: e.tensor_tensor(out=m2[:], in0=pabc[1][:], in1=gt[b][:, 1, :], op=ALU.mult),
                         r=["pabc1", "gt%d" % b], w=["m2"])
                    P.op("dve", lambda e, b=b: e.tensor_tensor(out=m3[:], in0=pabc[2][:], in1=gt[b][:, 2, :], op=ALU.mult),
                         r=["pabc2", "gt%d" % b], w=["m3"])
                    P.op("pool", lambda e: e.tensor_tensor(out=m1[:], in0=m1[:], in1=m2[:], op=ALU.add), r=["m1", "m2"], w=["m1"])
                    P.op("pool", lambda e, dc=dc: e.tensor_tensor(out=mixT[:, dc, :], in0=m1[:], in1=m3[:], op=ALU.add),
                         r=["m1", "m3"], w=[("mixT", dc)])
                for ti in range(4):
                    tt = tb * 4 + ti
                    hb = tt % 2
                    P.dma("sp", hold[hb][:], self.h_d[tt * 128:(tt + 1) * 128, :], r=[("h_d", tt)], w=["hold%d" % hb],
                          stream="hold%d" % hb)
                    for half in range(2):
                        self.mm_group(po[half][:], [(mixT[:, dc, ti * 128:(ti + 1) * 128], wo[:, dc, half * 512:(half + 1) * 512])
                                                    for dc in range(8)],
                                      r=["wo"] + [("mixT", dc) for dc in range(8)], w=["po%d" % half])
                        P.op("dve", lambda e, hb=hb, half=half: e.scalar_tensor_tensor(
                            out=tsum[hb][:, half * 512:(half + 1) * 512], in0=hold[hb][:, half * 512:(half + 1) * 512],
                            scalar=ALPHA, in1=po[half][:], op0=ALU.mult, op1=ALU.add),
                            r=["hold%d" % hb, "po%d" % half], w=["tsum%d" % hb])
                    self.ln_tile(tiles, tsum[hb], "tsum%d" % hb, gb, "gb1", self.h_d, tt, router=rt, pfx="l1")
            P.flush()

    def router_setup(self, es):
        P = self.P
        wr = self.sb(es, "wr", [128, KC, NE])
        wrh = self.sb(es, "wrh", [128, KC, NE], BF16)
        wrl = self.sb(es, "wrl", [128, KC, NE], BF16)
        rb = self.sb(es, "rb", [128, NE])
        hlo = self.sb(es, "hlo", [128, KC, 128], BF16)
        pl = self.ps(es, "pl", [128, NE])
        sc = self.sb(es, "r_sc", [128, NE])
        sel = self.sb(es, "r_sel", [128, NE])
        tmp = self.sb(es, "r_tmp", [128, NE])
        tmp2 = self.sb(es, "r_tmp2", [128, NE])
        g8 = self.sb(es, "r_g8", [128, 8, 4])
        oh1 = self.sb(es, "r_oh1", [128, NE])
        oh2 = self.sb(es, "r_oh2", [128, NE])
        sm = self.sb(es, "r_sm", [128, 8])
        P.dma("sp", wr[:], self.w_router.rearrange("(kc p) n -> p kc n", p=128), w=["wr"], stream="wr")
        P.dma("sp", rb[:], self.router_bias.partition_broadcast(128), w=["rb"], stream="rb")
        P.dma("pool", wrh[:], self.w_router.rearrange("(kc p) n -> p kc n", p=128), w=["wrh"], stream="wrh")
        P.op("dve", lambda e: e.tensor_tensor(out=wrl[:], in0=wr[:], in1=wrh[:], op=ALU.subtract), r=["wr", "wrh"], w=["wrl"])
        BIG = 1.0e4

        def router(tt, pT, pTres):
            tsl = slice(tt * 128, (tt + 1) * 128)
            P.op("dve", lambda e: e.tensor_tensor(out=hlo[:], in0=pT[:], in1=self.hT[:, :, tsl], op=ALU.subtract),
                 r=[pTres, ("hT", tt)], w=["hlo"])
            pairs = []
            for kc in range(KC):
                pairs += [(self.hT[:, kc, tsl], wrh[:, kc, :]), (self.hT[:, kc, tsl], wrl[:, kc, :]), (hlo[:, kc, :], wrh[:, kc, :])]
            self.mm_group(pl[:], pairs, r=["hlo", ("hT", tt), "wrh", "wrl"], w=["pl"])
            P.op("act", lambda e: e.activation(out=sc[:], in_=pl[:], func=AF.Sigmoid), r=["pl"], w=["r_sc"])
            P.op("dve", lambda e: e.tensor_tensor(out=sel[:], in0=sc[:], in1=rb[:], op=ALU.add), r=["r_sc", "rb"], w=["r_sel"])
            s3 = sel[:].rearrange("p (g k) -> p g k", k=4)
            t3 = tmp[:].rearrange("p (g k) -> p g k", k=4)
            P.op("dve", lambda e: e.tensor_reduce(out=sm[:, 0:8], in_=s3, axis=AX.X, op=ALU.max), r=["r_sel"], w=["r_sm"])
            P.op("dve", lambda e: e.tensor_tensor(out=t3, in0=s3, in1=bc(sm[:, 0:8].unsqueeze(2), [128, 8, 4]), op=ALU.is_equal),
                 r=["r_sel", "r_sm"], w=["r_tmp"])
            P.op("dve", lambda e: e.scalar_tensor_tensor(out=tmp[:], in0=tmp[:], scalar=-BIG, in1=sel[:], op0=ALU.mult, op1=ALU.add),
                 r=["r_tmp", "r_sel"], w=["r_tmp"])
            P.op("dve", lambda e: e.tensor_reduce(out=g8[:, :, 0], in_=t3, axis=AX.X, op=ALU.max), r=["r_tmp"], w=["r_g8"])
            P.op("dve", lambda e: e.tensor_tensor(out=g8[:, :, 1], in0=g8[:, :, 0], in1=sm[:, 0:8], op=ALU.add),
                 r=["r_g8", "r_sm"], w=["r_g8b"])
            P.op("dve", lambda e: e.tensor_reduce(out=sm[:, 0:1], in_=g8[:, :, 1], axis=AX.X, op=ALU.max), r=["r_g8b"], w=["r_sm1"])
            P.op("dve", lambda e: e.tensor_scalar(out=g8[:, :, 2], in0=g8[:, :, 1], scalar1=sm[:, 0:1], scalar2=None, op0=ALU.is_lt),
                 r=["r_g8b", "r_sm1"], w=["r_g8c"])
            P.op("dve", lambda e: e.scalar_tensor_tensor(out=t3, in0=bc(g8[:, :, 2:3], [128, 8, 4]), scalar=-BIG, in1=s3,
                                                         op0=ALU.mult, op1=ALU.add), r=["r_g8c", "r_sel"], w=["r_tmp"])
            P.op("dve", lambda e: e.tensor_reduce(out=sm[:, 1:2], in_=tmp[:], axis=AX.X, op=ALU.max), r=["r_tmp"], w=["r_sm2"])
            P.op("dve", lambda e: e.tensor_scalar(out=oh1[:], in0=tmp[:], scalar1=sm[:, 1:2], scalar2=None, op0=ALU.is_equal),
                 r=["r_tmp", "r_sm2"], w=["r_oh1"])
            P.op("dve", lambda e: e.scalar_tensor_tensor(out=tmp2[:], in0=oh1[:], scalar=-BIG, in1=tmp[:], op0=ALU.mult, op1=ALU.add),
                 r=["r_oh1", "r_tmp"], w=["r_tmp2"])
            P.op("dve", lambda e: e.tensor_reduce(out=sm[:, 2:3], in_=tmp2[:], axis=AX.X, op=ALU.max), r=["r_tmp2"], w=["r_sm3"])
            P.op("dve", lambda e: e.tensor_scalar(out=oh2[:], in0=tmp2[:], scalar1=sm[:, 2:3], scalar2=None, op0=ALU.is_equal),
                 r=["r_tmp2", "r_sm3"], w=["r_oh2"])
            P.op("dve", lambda e: e.tensor_tensor(out=oh1[:], in0=oh1[:], in1=oh2[:], op=ALU.add), r=["r_oh1", "r_oh2"], w=["r_oh1"])
            P.op("dve", lambda e: e.tensor_tensor(out=oh1[:], in0=oh1[:], in1=sc[:], op=ALU.mult), r=["r_oh1", "r_sc"], w=["r_oh1"])
            P.op("dve", lambda e: e.tensor_reduce(out=sm[:, 3:4], in_=oh1[:], axis=AX.X, op=ALU.add), r=["r_oh1"], w=["r_sm4"])
            P.op("dve", lambda e: e.reciprocal(out=sm[:, 4:5], in_=sm[:, 3:4]), r=["r_sm4"], w=["r_sm5"])
            P.op("dve", lambda e: e.tensor_scalar(out=self.comb[:, tt, :], in0=oh1[:], scalar1=sm[:, 4:5], scalar2=None, op0=ALU.mult),
                 r=["r_oh1", "r_sm5"], w=[("comb", tt)])
        return router

    def phase_moe_dense(self, l, last):
        P = self.P
        S, NB, NT = self.S, self.NB, self.NT
        G = min(8, NT)
        w1_l = self.w1[l].rearrange("e (kc p) n -> e p kc n", p=128)
        w3_l = self.w3[l].rearrange("e (kc p) n -> e p kc n", p=128)
        w2_l = self.w2[l].rearrange("e (kc p) n -> e p kc n", p=128)
        with ExitStack() as es:
            w1t = [self.sb(es, "w1t%d" % i, [128, KC, DE], BF16) for i in range(2)]
            w3t = [self.sb(es, "w3t%d" % i, [128, KC, DE], BF16) for i in range(2)]
            w2t = [self.sb(es, "w2t%d" % i, [128, 4, D], BF16) for i in range(2)]
            yacc = self.sb(es, "yacc", [128, G, D])
            hid = [self.sb(es, "hid%d" % i, [128, 4, 512], BF16) for i in range(2)]
            sl = [self.sb(es, "sl%d" % i, [128, 512], BF16) for i in range(2)]
            hold = [self.sb(es, "mhold%d" % i, [128, D]) for i in range(2)]
            gb = self.sb(es, "gb2", [128, 2, D])
            p1 = [self.ps(es, "p1_%d" % i, [128, 512]) for i in range(2)]
            p3 = [self.ps(es, "p3_%d" % i, [128, 512]) for i in range(2)]
            py = [self.ps(es, "py%d" % i, [128, 512]) for i in range(2)]
            tiles = self.ln_scratch(es, "l2")
            P.dma("sp", gb[:], self.ln2[l].partition_broadcast(128), w=["gb2"], stream="gb2")
            wi = 0
            for grp in range(NT // G):
                P.op("pool", lambda e: e.memset(yacc[:], 0.0), w=["yacc"])
                for ex in range(NE):
                    b = wi % 2
                    wi += 1
                    P.dma("pool", w1t[b][:], w1_l[ex], w=["w1t%d" % b], stream="w1t%d" % b)
                    P.dma("pool", w3t[b][:], w3_l[ex], w=["w3t%d" % b], stream="w3t%d" % b)
                    P.dma("pool", w2t[b][:], w2_l[ex], w=["w2t%d" % b], stream="w2t%d" % b)
                    for blk in range(G // 4):
                        tb = grp * (G // 4) + blk
                        hb = (ex * (G // 4) + blk) % 2
                        hres = [("hT", tb * 4 + i) for i in range(4)]
                        for fc in range(4):
                            pb_ = fc % 2
                            fsl = slice(fc * 128, (fc + 1) * 128)
                            self.mm_group(p1[pb_][:], [(w1t[b][:, kc, fsl], self.hT[:, kc, tb * 512:(tb + 1) * 512]) for kc in range(KC)],
                                          r=["w1t%d" % b] + hres, w=["p1_%d" % pb_])
                            self.mm_group(p3[pb_][:], [(w3t[b][:, kc, fsl], self.hT[:, kc, tb * 512:(tb + 1) * 512]) for kc in range(KC)],
                                          r=["w3t%d" % b] + hres, w=["p3_%d" % pb_])
                            P.op("act", lambda e, pb_=pb_: e.activation(out=sl[pb_][:], in_=p1[pb_][:], func=AF.Silu),
                                 r=["p1_%d" % pb_], w=["sl%d" % pb_])
                            P.op("dve", lambda e, pb_=pb_, hb=hb, fc=fc: e.tensor_tensor(out=hid[hb][:, fc, :], in0=sl[pb_][:],
                                                                                           in1=p3[pb_][:], op=ALU.mult),
                                 r=["sl%d" % pb_, "p3_%d" % pb_], w=[("hid", hb, fc)])
                        for ti in range(4):
                            gi = blk * 4 + ti
                            tt = tb * 4 + ti
                            for half in range(2):
                                hs = slice(half * 512, (half + 1) * 512)
                                self.mm_group(py[half][:], [(hid[hb][:, fc, ti * 128:(ti + 1) * 128], w2t[b][:, fc, hs]) for fc in range(4)],
                                              r=["w2t%d" % b] + [("hid", hb, fc) for fc in range(4)], w=["py%d" % half])
                                P.op("dve", lambda e, gi=gi, hs=hs, half=half, tt=tt, ex=ex: e.scalar_tensor_tensor(
                                    out=yacc[:, gi, hs], in0=py[half][:], scalar=self.comb[:, tt, ex:ex + 1], in1=yacc[:, gi, hs],
                                    op0=ALU.mult, op1=ALU.add), r=["py%d" % half, ("comb", tt), "yacc"], w=["yacc"])
                for gi in range(G):
                    tt = grp * G + gi
                    hb = tt % 2
                    P.dma("sp", hold[hb][:], self.h_d[tt * 128:(tt + 1) * 128, :], r=[("h_d", tt)], w=["mhold%d" % hb],
                          stream="mhold%d" % hb)
                    P.op("dve", lambda e, hb=hb, gi=gi: e.scalar_tensor_tensor(out=hold[hb][:], in0=hold[hb][:], scalar=ALPHA,
                                                                                 in1=yacc[:, gi, :], op0=ALU.mult, op1=ALU.add),
                         r=["mhold%d" % hb, "yacc"], w=["mhold%d" % hb])
                    self.ln_tile(tiles, hold[hb], "mhold%d" % hb, gb, "gb2", self.out if last else self.h_d, tt,
                                 hT_out=not last, pfx="l2")
            P.flush()

    def build(self, stop=None):
        P = self.P
        self.load_consts()
        self.phase_ln0()
        for l in range(self.L):
            if stop == "ln0":
                break
            self.phase_inproj(l)
            if stop == "inproj":
                break
            if "A" in self.parts:
                self.phase_attn_a(l)
            if "B" in self.parts:
                self.phase_attn_b(l)
            if "C" in self.parts:
                self.phase_gdn(l)
            if stop == "attn":
                break
            self.phase_merge(l)
            if stop == "merge":
                break
            self.phase_moe_dense(l, last=(l == self.L - 1))
        P.wait_all("sp", list(P.lastw.keys()))
        P.flush()
        self.es.close()
        P.close()
        return self.nc


def host_consts(S):
    c = {}
    c["c_ident"] = np.eye(128, dtype=np.float32)
    blk = np.zeros((128, 128), np.float32)
    blk[:64, :64] = 1.0
    blk[64:, 64:] = 1.0
    c["c_blk"] = blk
    R = np.zeros((64, 64), np.float32)
    for base in (0, 32):
        for i in range(16):
            R[base + i, base + 16 + i] = -1.0
            R[base + 16 + i, base + i] = 1.0
    RT = np.zeros((128, 128), np.float32)
    RT[:64, :64] = R.T
    RT[64:, 64:] = R.T
    c["c_rot"] = RT
    t = np.arange(S)
    row = (t // GRID_W).astype(np.float32)
    col = (t % GRID_W).astype(np.float32)
    inv = (10000.0 ** (-np.arange(0, 32, 2, dtype=np.float32) / 32)).astype(np.float32)
    ang_r = row[None, :] * inv[:, None]
    ang_c = col[None, :] * inv[:, None]
    cos64 = np.concatenate([np.cos(ang_r), np.cos(ang_r), np.cos(ang_c), np.cos(ang_c)], 0)
    sin64 = np.concatenate([np.sin(ang_r), np.sin(ang_r), np.sin(ang_c), np.sin(ang_c)], 0)
    c["c_cos"] = np.concatenate([cos64, cos64], 0).astype(np.float32)
    c["c_sin"] = np.concatenate([sin64, sin64], 0).astype(np.float32)
    g = np.zeros((128, 5, 4, 128), np.float32)
    i = np.arange(64)[:, None]
    j = np.arange(64)[None, :]
    g[:, 0] = NEG
    g[:, 1] = NEG
    lo, hi = slice(0, 64), slice(64, 128)
    for pp, fwd in ((lo, True), (hi, False)):
        allow = (i >= j) if fwd else (i <= j)
        for cc in range(4):
            g[pp, 0, cc, pp] = np.where(allow, 0.0, NEG)
            g[pp, 1, cc, pp] = np.where(allow.T, 0.0, NEG)
            g[pp, 2, cc, pp] = ((i > j) if fwd else (i < j)).astype(np.float32)
            g[pp, 3, cc, pp] = (i == j).astype(np.float32)
        g[pp, 4, 0, pp] = ((i <= j) if fwd else (i >= j)).astype(np.float32)
        g[pp, 4, 1, pp] = 1.0
    c["c_gdn"] = g
    return c


def host_layout(inp, L):
    f = lambda a: np.ascontiguousarray(np.asarray(a, dtype=np.float32))
    m = {}
    m["ln0"] = f(np.stack([inp["ln0_g"], inp["ln0_b"]], 0))
    m["w_in"] = f(inp["w_in"][:L])
    qg = np.asarray(inp["q_norm_g"], np.float32)[:L]
    kg = np.asarray(inp["k_norm_g"], np.float32)[:L]
    m["qkg"] = f(np.stack([np.concatenate([qg, qg], 1), np.concatenate([kg, kg], 1)], 2))
    m["wa"] = f(inp["w_branch_a"][:L])
    m["wb"] = f(inp["w_branch_b"][:L])
    m["wc"] = f(inp["w_branch_c"][:L])
    m["wo"] = f(inp["w_out"][:L])
    m["ln1"] = f(np.stack([inp["ln1_g"][:L], inp["ln1_b"][:L]], 1))
    m["ln2"] = f(np.stack([inp["ln2_g"][:L], inp["ln2_b"][:L]], 1))
    m["w_router"] = f(inp["w_router"])
    m["router_bias"] = f(inp["router_bias"])
    m["w1"] = f(inp["w1"][:L])
    m["w3"] = f(inp["w3"][:L])
    m["w2"] = f(inp["w2"][:L])
    rpb = np.asarray(inp["na_rpb"], np.float32)[:L]
    c = np.arange(64)
    cs = np.clip(c - 8, 0, 48)
    kc_ = np.arange(64)
    inwin = (kc_[None, :] >= cs[:, None]) & (kc_[None, :] < cs[:, None] + 16)
    dc = np.clip(kc_[None, :] - c[:, None] + 15, 0, 30)
    g = rpb[:, :, :, dc]
    g = np.where(inwin[None, None, None], g, np.float32(NEG))
    m["bias_b"] = f(np.transpose(g, (0, 1, 3, 2, 4)).reshape(L, 4, 64, 960))
    cwv = np.asarray(inp["conv_w"], np.float32)[:L]
    m["conv_w"] = f(np.transpose(cwv.reshape(L, 5, 6, 128), (0, 3, 2, 1)))
    m["gdn_ab"] = f(np.stack([np.asarray(inp["A_log"], np.float32)[:L].reshape(L, 8),
                              np.asarray(inp["dt_bias"], np.float32)[:L].reshape(L, 8)], 1))
    m["gdn_g"] = f(inp["gdn_norm_g"][:L])
    return m


_CACHE = {}


def kernel(**inputs):
    S = 4096
    L = DEPTH
    x = np.asarray(inputs["x"], dtype=np.float32)
    nb = x.shape[0]
    key = (S, L)
    if key not in _CACHE:
        _CACHE[key] = Builder(S, L).build()
    nc = _CACHE[key]
    shared = host_layout(inputs, L)
    shared.update(host_consts(S))
    in_maps = []
    for b in range(nb):
        mm = dict(shared)
        mm["x"] = np.ascontiguousarray(x[b])
        in_maps.append(mm)
    res = run_bass_kernel_spmd(nc, in_maps, core_ids=list(range(nb)))
    return np.stack([np.asarray(r["out"], dtype=np.float32) for r in res.results], 0)
```

```python
import math
import numpy as np
from contextlib import ExitStack
import concourse.bass as bass
import concourse.mybir as mybir
from concourse.bass_utils import run_bass_kernel_spmd

F32 = mybir.dt.float32
BF16 = mybir.dt.bfloat16
I32 = mybir.dt.int32
AF = mybir.ActivationFunctionType
ALU = mybir.AluOpType
AX = mybir.AxisListType

ENGS = ("pe", "act", "dve", "pool", "sp")

D = 1024
KC = 8
DEPTH = 4
GRID_W = 64
DIN = 5648
NE = 32
DE = 512
ALPHA = (2 * DEPTH) ** 0.25
C_AQ, C_AK, C_AV = 0, 512, 640
C_BQ, C_BK, C_BV = 768, 1024, 1280
C_CQ, C_CK, C_CV, C_CZ = 1536, 1792, 2048, 2304
C_CG = 2560
C_GATE = 2576
NEG = -30000.0


class Prog:
    def __init__(self, nc):
        self.nc = nc
        self.es = ExitStack()
        self.sem = {e: self.es.enter_context(nc.semaphore("s_" + e)) for e in ENGS}
        self.cnt = {e: 0 for e in ENGS}
        self.dsem = {}
        self.known = {e: {} for e in ENGS}
        self.lastw = {}
        self.readers = {}
        self.queue = {e: [] for e in ENGS}
        self.pending = {e: [] for e in ENGS}
        self.nops = 0

    def close(self):
        self.es.close()

    def _deps(self, eng, r, w):
        toks = []
        for k in r:
            t = self.lastw.get(k)
            if t is not None:
                toks.append(t)
        for k in w:
            t = self.lastw.get(k)
            if t is not None:
                toks.append(t)
            toks.extend(self.readers.get(k, ()))
        need = {}
        for t in toks:
            key, val = t[0], t[1]
            if eng == "pe" and key == "pe":
                continue
            if val is None:
                raise RuntimeError("dependency on unresolved (inc=False) op")
            if need.get(key, 0) < val:
                need[key] = val
        waits = []
        kn = self.known[eng]
        for key, val in need.items():
            if kn.get(key, 0) < val:
                kn[key] = val
                waits.append((key, val))
        return waits

    def _mark(self, tok, r, w):
        for k in w:
            self.lastw[k] = tok
            self.readers[k] = []
        for k in r:
            self.readers.setdefault(k, []).append(tok)

    rec = None

    def replay(self, item):
        kind, a, k = item
        (self.op if kind == "op" else self.dma)(*a, **k)

    def op(self, eng, fn, r=(), w=(), inc=True):
        if self.rec is not None:
            self.rec.append(("op", (eng, fn), dict(r=r, w=w, inc=inc)))
            return
        waits = self._deps(eng, r, w)
        if inc:
            self.cnt[eng] += 1
            tok = [eng, self.cnt[eng]]
            for p in self.pending[eng]:
                p[1] = self.cnt[eng]
            self.pending[eng] = []
        else:
            tok = [eng, None]
            self.pending[eng].append(tok)
        self._mark(tok, r, w)
        self.queue[eng].append((fn, waits, (eng, 1) if inc else None))
        self.nops += 1

    def dma(self, eng, out, in_, r=(), w=(), stream=None, **kw):
        assert stream is not None
        if self.rec is not None:
            self.rec.append(("dma", (eng, out, in_), dict(r=r, w=w, stream=stream, **kw)))
            return
        waits = self._deps(eng, r, w)
        if stream not in self.dsem:
            self.dsem[stream] = [self.es.enter_context(self.nc.semaphore("d%d" % len(self.dsem))), 0]
        ds = self.dsem[stream]
        ds[1] += 16
        tok = [("d", stream), ds[1]]
        self._mark(tok, r, w)
        self.queue[eng].append((lambda e, out=out, in_=in_, kw=kw: e.dma_start(out=out, in_=in_, **kw),
                                waits, (("d", stream), 16)))
        self.nops += 1

    def _semh(self, key):
        if isinstance(key, tuple):
            return self.dsem[key[1]][0]
        return self.sem[key]

    def wait_all(self, eng, keys):
        waits = self._deps(eng, keys, ())
        self.queue[eng].append((None, waits, None))

    def flush(self):
        nc = self.nc
        q = self.queue
        self.queue = {e: [] for e in ENGS}
        for e in ENGS:
            if self.pending[e]:
                raise RuntimeError("unresolved inc=False ops at flush on " + e)

        def run(engine, items):
            for fn, waits, inc in items:
                for key, val in waits:
                    engine.wait_ge(self._semh(key), val)
                if fn is None:
                    continue
                ins = fn(engine)
                if inc is not None:
                    ins.then_inc(self._semh(inc[0]), inc[1])

        with nc.Block() as block:
            if q["sp"]:
                @block.sync
                def _(e):
                    run(e, q["sp"])
            if q["pe"]:
                @block.tensor
                def _(e):
                    run(e, q["pe"])
            if q["act"]:
                @block.scalar
                def _(e):
                    run(e, q["act"])
            if q["dve"]:
                @block.vector
                def _(e):
                    run(e, q["dve"])
            if q["pool"]:
                @block.gpsimd
                def _(e):
                    run(e, q["pool"])


def bc(ap, shape):
    return ap.to_broadcast(shape)


class Builder:
    def __init__(self, S, L, dbg=(), parts=("A", "B", "C"), moe="dense"):
        self.S, self.L = S, L
        self.NT = S // 128
        self.NB = S // 512
        self.parts = parts
        self.moe = moe
        nc = self.nc = bass.Bass("TRN2", target_bir_lowering=False)
        self.P = Prog(nc)
        self.dbg = set(dbg)
        dt_in = lambda n, s, d=F32: nc.dram_tensor(n, list(s), d, kind="ExternalInput").ap()
        self.x = dt_in("x", [S, D])
        self.ln0 = dt_in("ln0", [2, D])
        self.w_in = dt_in("w_in", [L, D, DIN])
        self.qkg = dt_in("qkg", [L, 128, 2])
        self.bias_b = dt_in("bias_b", [L, 4, 64, 960])
        self.conv_w = dt_in("conv_w", [L, 128, 6, 5])
        self.gdn_ab = dt_in("gdn_ab", [L, 2, 8])
        self.gdn_g = dt_in("gdn_g", [L, 64])
        self.wa = dt_in("wa", [L, 512, D])
        self.wb = dt_in("wb", [L, 256, D])
        self.wc = dt_in("wc", [L, 256, D])
        self.wo = dt_in("wo", [L, D, D])
        self.ln1 = dt_in("ln1", [L, 2, D])
        self.ln2 = dt_in("ln2", [L, 2, D])
        self.w_router = dt_in("w_router", [D, NE])
        self.router_bias = dt_in("router_bias", [NE])
        self.w1 = dt_in("w1", [L, NE, D, DE])
        self.w3 = dt_in("w3", [L, NE, D, DE])
        self.w2 = dt_in("w2", [L, NE, DE, D])
        self.c_ident = dt_in("c_ident", [128, 128])
        self.c_blk = dt_in("c_blk", [128, 128])
        self.c_rot = dt_in("c_rot", [128, 128])
        self.c_cos = dt_in("c_cos", [128, S])
        self.c_sin = dt_in("c_sin", [128, S])
        self.c_gdn = dt_in("c_gdn", [128, 5, 4, 128])
        okind = "ExternalOutput"
        self.out = nc.dram_tensor("out", [S, D], F32, kind=okind).ap()

        def scr(n, s, d=F32):
            k = "ExternalOutput" if n in self.dbg else "Internal"
            return nc.dram_tensor(n, list(s), d, kind=k).ap()
        self.h_d = scr("h_d", [S, D])
        self.qT_d = scr("qT_d", [512, S], BF16)
        self.kT_d = scr("kT_d", [128, S], BF16)
        self.v_d = scr("v_d", [S, 128], BF16)
        self.bqT_d = scr("bqT_d", [256, S], BF16)
        self.bkT_d = scr("bkT_d", [256, S], BF16)
        self.bv_d = scr("bv_d", [S, 256], BF16)
        self.cT_d = scr("cT_d", [768, S])
        self.cz_d = scr("cz_d", [S, 272])
        self.gT_d = scr("gT_d", [3072, S], BF16)
        self.yaT_d = scr("yaT_d", [512, S], BF16)
        self.ybT_d = scr("ybT_d", [256, S], BF16)
        self.ycT_d = scr("ycT_d", [256, S], BF16)
        self.es = ExitStack()
        self.hT = self.sb(self.es, "hT", [128, KC, S], BF16)
        self.comb = self.sb(self.es, "comb", [128, self.NT, NE])
        self.ident = self.sb(self.es, "ident", [128, 128])
        self.identb = self.sb(self.es, "identb", [128, 128], BF16)

    def sb(self, es, n, s, d=F32):
        self.uid = getattr(self, "uid", 0) + 1
        return es.enter_context(self.nc.sbuf_tensor("sb%d_%s" % (self.uid, n), list(s), d))

    def ps(self, es, n, s, d=F32):
        self.uid = getattr(self, "uid", 0) + 1
        return es.enter_context(self.nc.psum_tensor("ps%d_%s" % (self.uid, n), list(s), d))

    def mm_group(self, out_ap, pairs, r, w):
        P = self.P
        n = len(pairs)
        for i, (l, rh) in enumerate(pairs):
            P.op("pe", lambda e, l=l, rh=rh, i=i: e.matmul(out_ap, l, rh, start=(i == 0), stop=(i == n - 1)),
                 r=r, w=w, inc=(i == n - 1))

    def load_consts(self):
        P = self.P
        P.dma("sp", self.ident[:], self.c_ident, w=["ident"], stream="ident")
        P.dma("pool", self.identb[:], self.c_ident, w=["identb"], stream="identb")

    def ln_tile(self, es_tiles, t, tres, gb, gbres, dst_d, tt, hT_out=True, router=None, pfx="ln"):
        P = self.P
        st, mv, sd, xn, pT = (es_tiles[k] for k in ("st", "mv", "sd", "xn", "pT"))
        P.op("dve", lambda e: e.bn_stats(out=st[:, 0:6], in_=t[:, 0:512]), r=[tres], w=[pfx + "st"])
        P.op("dve", lambda e: e.bn_stats(out=st[:, 6:12], in_=t[:, 512:1024]), r=[tres], w=[pfx + "st"])
        P.op("dve", lambda e: e.bn_aggr(out=mv[:], in_=st[:]), r=[pfx + "st"], w=[pfx + "mv"])
        P.op("act", lambda e: e.activation(out=sd[:, 0:1], in_=mv[:, 1:2], func=AF.Sqrt, bias=1e-5, scale=1.0),
             r=[pfx + "mv"], w=[pfx + "sd"])
        P.op("dve", lambda e: e.reciprocal(out=sd[:, 1:2], in_=sd[:, 0:1]), r=[pfx + "sd"], w=[pfx + "sd1"])
        P.op("dve", lambda e: e.scalar_tensor_tensor(out=sd[:, 2:3], in0=mv[:, 0:1], scalar=-1.0, in1=sd[:, 1:2],
                                                     op0=ALU.mult, op1=ALU.mult),
             r=[pfx + "mv", pfx + "sd1"], w=[pfx + "sd2"])
        P.op("act", lambda e: e.activation(out=xn[:], in_=t[:], func=AF.Identity, bias=sd[:, 2:3], scale=sd[:, 1:2]),
             r=[tres, pfx + "sd1", pfx + "sd2"], w=[pfx + "xn"])
        P.op("dve", lambda e: e.tensor_tensor(out=xn[:], in0=xn[:], in1=gb[:, 0, :], op=ALU.mult),
             r=[pfx + "xn", gbres], w=[pfx + "xn"])
        P.op("pool", lambda e: e.tensor_tensor(out=xn[:], in0=xn[:], in1=gb[:, 1, :], op=ALU.add),
             r=[pfx + "xn", gbres], w=[pfx + "xn"])
        P.dma("sp", dst_d[tt * 128:(tt + 1) * 128, :], xn[:], r=[pfx + "xn"], w=[("h_d", tt) if dst_d is self.h_d else "out"],
              stream=pfx + "xn")
        if hT_out:
            for kc in range(KC):
                P.op("pe", lambda e, kc=kc: e.transpose(pT[:, kc, :], xn[:, kc * 128:(kc + 1) * 128], self.ident[:]),
                     r=[pfx + "xn", "ident"], w=[pfx + "pT"], inc=(kc == KC - 1))
            P.op("act", lambda e: e.activation(out=self.hT[:, :, tt * 128:(tt + 1) * 128], in_=pT[:], func=AF.Copy),
                 r=[pfx + "pT"], w=[("hT", tt)])
            if router is not None:
                router(tt, pT, pfx + "pT")

    def ln_scratch(self, es, pfx="ln"):
        return dict(st=self.sb(es, pfx + "st", [128, 12]), mv=self.sb(es, pfx + "mv", [128, 2]),
                    sd=self.sb(es, pfx + "sd", [128, 4]), xn=self.sb(es, pfx + "xn", [128, D]),
                    pT=self.ps(es, pfx + "pT", [128, KC, 128]))

    def phase_ln0(self):
        P = self.P
        with ExitStack() as es:
            tiles = self.ln_scratch(es)
            gb = self.sb(es, "gb0", [128, 2, D])
            xt = [self.sb(es, "x%d" % i, [128, D]) for i in range(2)]
            P.dma("sp", gb[:], self.ln0.partition_broadcast(128), w=["gb0"], stream="gb0")
            for tt in range(self.NT):
                b = tt % 2
                P.dma("sp", xt[b][:], self.x[tt * 128:(tt + 1) * 128, :], w=["xt%d" % b], stream="xt%d" % b)
                self.ln_tile(tiles, xt[b], "xt%d" % b, gb, "gb0", self.h_d if self.L > 0 else self.out, tt)
            P.flush()

    def phase_inproj(self, l):
        P = self.P
        S, NB, NT = self.S, self.NB, self.NT
        w_l = self.w_in[l].rearrange("(kc p) n -> p kc n", p=128)
        with ExitStack() as es:
            wt = [self.sb(es, "wi%d" % i, [128, KC, 512], BF16) for i in range(2)]
            stg = [self.sb(es, "stg%d" % i, [128, 512]) for i in range(2)]
            stgb = [self.sb(es, "stgb%d" % i, [128, 512], BF16) for i in range(2)]
            pa = [self.ps(es, "pa%d" % i, [128, 512]) for i in range(2)]
            pb = self.ps(es, "pb", [128, 512])
            pc = self.ps(es, "pc", [128, 512])
            blk = self.sb(es, "blk", [128, 128], BF16)
            rot = self.sb(es, "rot", [128, 128], BF16)
            cos = self.sb(es, "cos", [128, S])
            sin = self.sb(es, "sin", [128, S])
            qkg = self.sb(es, "qkg", [128, 2])
            sq = self.sb(es, "sq", [128, 512], BF16)
            rs = self.sb(es, "rs", [128, 512])
            qn = self.sb(es, "qn", [128, 512], BF16)
            t1 = self.sb(es, "t1", [128, 512])
            t2 = self.sb(es, "t2", [128, 512])
            P.dma("pool", blk[:], self.c_blk, w=["blk"], stream="blk")
            P.dma("pool", rot[:], self.c_rot, w=["rot"], stream="rot")
            P.dma("sp", cos[:], self.c_cos, w=["cos"], stream="cos")
            P.dma("sp", sin[:], self.c_sin, w=["sin"], stream="sin")
            P.dma("sp", qkg[:], self.qkg[l], w=["qkg"], stream="qkg")
            cnt = {"w": 0, "o": 0, "p": 0}

            def load_w(c0, n):
                b = cnt["w"] % 2
                cnt["w"] += 1
                P.dma("pool", wt[b][:, :, 0:n], w_l[:, :, c0:c0 + n], w=["wi%d" % b], stream="wi%d" % b)
                return wt[b], "wi%d" % b

            def fm_block(c0, n, evac):
                w, wres = load_w(c0, n)
                for j in range(n // 128):
                    for tb in range(NB):
                        pp = cnt["p"] % 2
                        cnt["p"] += 1
                        self.mm_group(pa[pp][:], [(w[:, kc, j * 128:(j + 1) * 128], self.hT[:, kc, tb * 512:(tb + 1) * 512])
                                                   for kc in range(KC)],
                                      r=[wres] + [("hT", tb * 4 + i) for i in range(4)], w=["pa%d" % pp])
                        evac(pa[pp], "pa%d" % pp, c0 + j * 128, tb)

            def out_stage(bf):
                b = cnt["o"] % 2
                cnt["o"] += 1
                return (stgb[b], "stgb%d" % b) if bf else (stg[b], "stg%d" % b)

            def evac_aqk(p, pres, col, tb):
                isq = col < C_AK
                gcol = 0 if isq else 1
                tsl = slice(tb * 512, (tb + 1) * 512)
                P.op("act", lambda e: e.activation(out=sq[:], in_=p[:], func=AF.Square), r=[pres], w=["sq"])
                P.op("pe", lambda e: e.matmul(pb[:], blk[:], sq[:], start=True, stop=True), r=["blk", "sq"], w=["pb"])
                P.op("act", lambda e: e.activation(out=rs[:], in_=pb[:], func=AF.Sqrt, bias=(64e-6 if isq else 1e-6),
                                                   scale=(1.0 if isq else 1.0 / 64)), r=["pb"], w=["rs"])
                P.op("dve", lambda e: e.reciprocal(out=rs[:], in_=rs[:]), r=["rs"], w=["rs"])
                P.op("dve", lambda e: e.scalar_tensor_tensor(out=qn[:], in0=p[:], scalar=qkg[:, gcol:gcol + 1], in1=rs[:],
                                                             op0=ALU.mult, op1=ALU.mult),
                     r=[pres, "rs", "qkg"], w=["qn"])
                P.op("pe", lambda e: e.matmul(pc[:], rot[:], qn[:], start=True, stop=True), r=["rot", "qn"], w=["pc"])
                P.op("pool", lambda e: e.tensor_tensor(out=t1[:], in0=qn[:], in1=cos[:, tsl], op=ALU.mult),
                     r=["qn", "cos"], w=["t1"])
                P.op("dve", lambda e: e.tensor_tensor(out=t2[:], in0=pc[:], in1=sin[:, tsl], op=ALU.mult),
                     r=["pc", "sin"], w=["t2"])
                o, ores = out_stage(True)
                P.op("pool", lambda e: e.tensor_tensor(out=o[:], in0=t1[:], in1=t2[:], op=ALU.add),
                     r=["t1", "t2"], w=[ores])
                dst = self.qT_d[col:col + 128, tsl] if isq else self.kT_d[:, tsl]
                P.dma("sp", dst, o[:], r=[ores], w=["qkT_d"], stream=ores)

            if "A" in self.parts:
                fm_block(C_AQ, 512, evac_aqk)
                fm_block(C_AK, 128, evac_aqk)

            def evac_simple(dst_d, row0, bf, func=AF.Copy, scale=1.0):
                def ev(p, pres, col, tb):
                    o, ores = out_stage(bf)
                    P.op("act", lambda e: e.activation(out=o[:], in_=p[:], func=func, scale=scale), r=[pres], w=[ores])
                    r0 = col - row0
                    P.dma("sp", dst_d[r0:r0 + 128, tb * 512:(tb + 1) * 512], o[:], r=[ores], w=[("fm_d", id(dst_d))],
                          stream=ores)
                return ev

            if "B" in self.parts:
                fm_block(C_BQ, 256, evac_simple(self.bqT_d, C_BQ, True, scale=0.125))
                fm_block(C_BK, 256, evac_simple(self.bkT_d, C_BK, True))
            if "C" in self.parts:
                fm_block(C_CQ, 512, evac_simple(self.cT_d, C_CQ, False))
                fm_block(C_CV, 256, evac_simple(self.cT_d, C_CQ, False))
            for g in range(6):
                fm_block(C_GATE + g * 512, 512, evac_simple(self.gT_d, C_GATE, True, func=AF.Sigmoid))

            def tm_block(c0, n, dst_d, bf):
                w, wres = load_w(c0, n)
                for tt in range(NT):
                    pp = cnt["p"] % 2
                    cnt["p"] += 1
                    self.mm_group(pa[pp][:, 0:n], [(self.hT[:, kc, tt * 128:(tt + 1) * 128], w[:, kc, 0:n]) for kc in range(KC)],
                                  r=[wres, ("hT", tt)], w=["pa%d" % pp])
                    o, ores = out_stage(bf)
                    P.op("act", lambda e, o=o, pp=pp: e.activation(out=o[:, 0:n], in_=pa[pp][:, 0:n], func=AF.Copy),
                         r=["pa%d" % pp], w=[ores])
                    P.dma("sp", dst_d[tt * 128:(tt + 1) * 128, :], o[:, 0:n], r=[ores], w=[("tm_d", id(dst_d))], stream=ores)

            if "A" in self.parts:
                tm_block(C_AV, 128, self.v_d, True)
            if "B" in self.parts:
                tm_block(C_BV, 256, self.bv_d, True)
            if "C" in self.parts:
                tm_block(C_CZ, 272, self.cz_d, False)
            P.flush()

    def phase_attn_a(self, l):
        P = self.P
        S, NB, NT = self.S, self.NB, self.NT
        with ExitStack() as es:
            qh = [self.sb(es, "qh%d" % i, [128, S], BF16) for i in range(2)]
            kT = self.sb(es, "kT", [128, 2, S], BF16)
            vx = self.sb(es, "vx", [128, NT, 2, 128], BF16)
            pT = [self.sb(es, "pT%d" % i, [128, 512], BF16) for i in range(3)]
            onesr = self.sb(es, "onesr", [128, 64])
            rc = self.sb(es, "rc", [128, 512])
            bcs = self.sb(es, "bcs", [64, 512])
            ya = [self.sb(es, "ya%d" % i, [64, 512], BF16) for i in range(2)]
            sps = [self.ps(es, "sps%d" % i, [128, 512]) for i in range(3)]
            ops_ = [self.ps(es, "ops%d" % i, [128, 512]) for i in range(2)]
            bps = self.ps(es, "bps", [64, 512])
            P.op("pool", lambda e: e.memset(kT[64:128, :, :], 0.0), w=["kT"])
            for i in range(2):
                P.op("pool", lambda e, i=i: e.memset(qh[i][64:128, :], 0.0), w=["qh%d" % i])
            P.dma("sp", kT[0:64, :, :], self.kT_d.rearrange("(g d) s -> d g s", d=64), r=["qkT_d"], w=["kT"], stream="kT")
            P.op("pool", lambda e: e.memset(vx[:], 1.0), w=["vx"])
            P.op("pool", lambda e: e.memset(onesr[:], 1.0), w=["onesr"])
            for g in range(2):
                P.dma("sp", vx[:, :, g, 0:64], self.v_d[:, g * 64:(g + 1) * 64].rearrange("(t p) d -> p t d", p=128),
                      r=[("tm_d", id(self.v_d))], w=["vx"], stream="vx")
            its = [(hq, qb, kt) for hq in range(8) for qb in range(NB) for kt in range(NT)]
            N_ = len(its)
            deferred = {}

            def emit_qk(j):
                hq, qb, kt = its[j]
                g, qb_, b = hq // 4, hq % 2, j % 3
                if qb == 0 and kt == 0:
                    for h2 in ([0, 1] if hq == 0 else [hq + 1]):
                        if h2 < 8:
                            P.dma("sp", qh[h2 % 2][0:64, :], self.qT_d[h2 * 64:(h2 + 1) * 64, :], r=["qkT_d"], w=["qh%d" % (h2 % 2)],
                                  stream="qh%d" % (h2 % 2))
                P.op("pe", lambda e: e.matmul(sps[b][:], kT[:, g, kt * 128:(kt + 1) * 128], qh[qb_][:, qb * 512:(qb + 1) * 512],
                                              start=True, stop=True), r=["kT", "qh%d" % qb_], w=["sps%d" % b])

            def tail(hq, qb, ob):
                P.op("pe", lambda e: e.matmul(bps[:], onesr[64:65, :], rc[64:65, :], start=True, stop=True),
                     r=["onesr", "rc"], w=["bps"])
                P.op("act", lambda e: e.activation(out=bcs[:], in_=bps[:], func=AF.Copy), r=["bps"], w=["bcs"])
                P.op("dve", lambda e: e.tensor_tensor(out=ya[ob][:], in0=ops_[ob][0:64, :], in1=bcs[:], op=ALU.mult),
                     r=["ops%d" % ob, "bcs"], w=["ya%d" % ob])
                P.dma("sp", self.yaT_d[hq * 64:(hq + 1) * 64, qb * 512:(qb + 1) * 512], ya[ob][:], r=["ya%d" % ob],
                      w=["yaT_d"], stream="ya%d" % ob)

            emit_qk(0)
            emit_qk(1)
            for j in range(N_):
                hq, qb, kt = its[j]
                g, b = hq // 4, j % 3
                ob = (hq * NB + qb) % 2
                if j + 2 < N_:
                    emit_qk(j + 2)
                P.op("act", lambda e, b=b: e.activation(out=pT[b][:], in_=sps[b][:], func=AF.Exp),
                     r=["sps%d" % b], w=["pT%d" % b])
                P.op("pe", lambda e, b=b, kt=kt, g=g, ob=ob: e.matmul(ops_[ob][:, :], vx[:, kt, g, :], pT[b][:],
                                                          start=(kt == 0), stop=(kt == NT - 1)),
                     r=["vx", "pT%d" % b], w=["ops%d" % ob], inc=(kt == NT - 1))
                if kt == NT - 1:
                    P.op("dve", lambda e, ob=ob: e.reciprocal(out=rc[64:65, :], in_=ops_[ob][64:65, :]), r=["ops%d" % ob], w=["rc"])
                    deferred[min(j + 2, N_ - 1)] = (hq, qb, ob)
                if j in deferred:
                    tail(*deferred.pop(j))
            assert not deferred
            P.flush()

    def phase_attn_b(self, l):
        P = self.P
        S, NT = self.S, self.NT
        rows = S // GRID_W
        wr_ = min(8, rows)
        NBUF = 4
        with ExitStack() as es:
            qT = self.sb(es, "bqT", [64, 4, S], BF16)
            kT = self.sb(es, "bkT", [64, 4, S], BF16)
            v0 = self.sb(es, "bv0", [128, NT, 256], BF16)
            v1 = self.sb(es, "bv1", [128, NT, 256], BF16)
            bias = self.sb(es, "bbias", [64, 4, 960])
            sc = [self.sb(es, "bsc%d" % i, [64, 512]) for i in range(NBUF)]
            pr = [self.sb(es, "bpr%d" % i, [64, 512], BF16) for i in range(NBUF)]
            st = [self.sb(es, "bst%d" % i, [64, 4]) for i in range(NBUF)]
            dg = [self.sb(es, "bdg%d" % i, [64, 64], BF16) for i in range(NBUF)]
            pts = [self.sb(es, "bpts%d" % i, [128, 4, 64], BF16) for i in range(2)]
            yo = [self.sb(es, "byo%d" % i, [64, 4, 64], BF16) for i in range(2)]
            sp_ = [self.ps(es, "bsp%d" % i, [64, 512]) for i in range(NBUF)]
            ptp = [self.ps(es, "bptp%d" % i, [128, 4, 64]) for i in range(2)]
            op_ = [self.ps(es, "bop%d" % i, [64, 4, 64]) for i in range(2)]
            for h in range(4):
                P.dma("sp", qT[:, h, :], self.bqT_d[h * 64:(h + 1) * 64, :], r=[("fm_d", id(self.bqT_d))], w=["bqT"], stream="bqT")
                P.dma("sp", kT[:, h, :], self.bkT_d[h * 64:(h + 1) * 64, :], r=[("fm_d", id(self.bkT_d))], w=["bkT"], stream="bkT")
            P.dma("sp", v0[:], self.bv_d.rearrange("(t p) c -> p t c", p=128), r=[("tm_d", id(self.bv_d))], w=["bv0"], stream="bv0")
            P.dma("sp", v1[:, 0:NT - 1, :], self.bv_d[64:S - 64, :].rearrange("(t p) c -> p t c", p=128), r=[("tm_d", id(self.bv_d))],
                  w=["bv1"], stream="bv1")
            P.dma("sp", bias[:], self.bias_b[l].rearrange("h q k -> q h k"), w=["bbias"], stream="bbias")
            its = [(r, h) for r in range(rows) for h in range(4)]
            N_ = len(its)

            def geo(r):
                r0 = min(max(r - wr_ // 2, 0), rows - wr_)
                return r0, (r0 - r + 7) * 64, r0 * 64

            def s1(i):
                r, h = its[i]
                b = i % NBUF
                r0, d0, k0 = geo(r)
                P.op("pe", lambda e: e.matmul(sp_[b][:], qT[:, h, r * 64:(r + 1) * 64], kT[:, h, k0:k0 + 512], start=True, stop=True),
                     r=["bqT", "bkT"], w=["bsp%d" % b])
                P.op("dve", lambda e: e.tensor_tensor(out=sc[b][:], in0=sp_[b][:], in1=bias[:, h, d0:d0 + 512], op=ALU.add),
                     r=["bsp%d" % b, "bbias"], w=["bsc%d" % b])
                P.op("dve", lambda e: e.tensor_reduce(out=st[b][:, 0:1], in_=sc[b][:], axis=AX.X, op=ALU.max),
                     r=["bsc%d" % b], w=[("bst", b, 0)])
                P.op("dve", lambda e: e.tensor_scalar(out=st[b][:, 1:2], in0=st[b][:, 0:1], scalar1=-1.0, scalar2=None, op0=ALU.mult),
                     r=[("bst", b, 0)], w=[("bst", b, 1)])
                P.op("act", lambda e: e.activation(out=pr[b][:], in_=sc[b][:], func=AF.Exp, bias=st[b][:, 1:2], scale=1.0,
                                                   accum_out=st[b][:, 2:3]), r=["bsc%d" % b, ("bst", b, 1)], w=["bpr%d" % b, ("bst", b, 2)])

            def s1b(i):
                b = i % NBUF
                P.op("dve", lambda e: e.reciprocal(out=st[b][:, 3:4], in_=st[b][:, 2:3]), r=[("bst", b, 2)], w=[("bst", b, 3)])
                P.op("dve", lambda e: e.tensor_scalar(out=dg[b][:], in0=self.identb[0:64, 0:64], scalar1=st[b][:, 3:4], scalar2=None,
                                                      op0=ALU.mult), r=["identb", ("bst", b, 3)], w=["bdg%d" % b])

            def s2(i):
                b, pb = i % NBUF, i % 2
                for kc in range(4):
                    P.op("pe", lambda e, kc=kc: e.matmul(ptp[pb][:, kc, :], pr[b][:, kc * 128:(kc + 1) * 128], dg[b][:], start=True, stop=True),
                         r=["bpr%d" % b, "bdg%d" % b], w=["bptp%d" % pb], inc=(kc == 3))
                P.op("act", lambda e: e.activation(out=pts[pb][:], in_=ptp[pb][:], func=AF.Copy), r=["bptp%d" % pb], w=["bpts%d" % pb])

            def s3(i):
                r, h = its[i]
                pb, ob = i % 2, r % 2
                r0, d0, k0 = geo(r)
                vsrc, vres, t0 = (v0, "bv0", r0 // 2) if r0 % 2 == 0 else (v1, "bv1", (r0 - 1) // 2)
                for kc in range(4):
                    P.op("pe", lambda e, kc=kc: e.matmul(op_[ob][:, h, :], vsrc[:, t0 + kc, h * 64:(h + 1) * 64], pts[pb][:, kc, :],
                                                         start=(kc == 0), stop=(kc == 3)),
                         r=[vres, "bpts%d" % pb], w=["bop%d" % ob], inc=(kc == 3))
                if h == 3:
                    P.op("act", lambda e: e.activation(out=yo[ob][:], in_=op_[ob][:], func=AF.Copy), r=["bop%d" % ob], w=["byo%d" % ob])
                    P.dma("sp", self.ybT_d[:, r * 64:(r + 1) * 64].rearrange("(h d) q -> d h q", d=64), yo[ob][:], r=["byo%d" % ob],
                          w=["ybT_d"], stream="byo%d" % ob)

            for t in range(N_ + 4):
                if t < N_:
                    s1(t)
                if 0 <= t - 1 < N_:
                    s1b(t - 1)
                if 0 <= t - 3 < N_:
                    s2(t - 3)
                if 0 <= t - 4 < N_:
                    s3(t - 4)
            P.flush()

    def phase_gdn(self, l):
        P = self.P
        S, NT, NB = self.S, self.NT, self.NB
        nc = self.nc
        if not hasattr(self, "cn_d"):
            mk = lambda n, s: nc.dram_tensor(n, list(s), F32, kind=("ExternalOutput" if n in self.dbg else "Internal")).ap()
            self.cn_d = mk("cn_d", [768, S])
            self.ktok_d = mk("ktok_d", [S, 256])
            self.vtok_d = mk("vtok_d", [S, 256])
            self.gates_d = mk("gates_d", [S, 16])
            self.o_d = mk("o_d", [2, S, 256])
        with ExitStack() as es:
            cw = self.sb(es, "cw", [128, 6, 5])
            x = self.sb(es, "gx", [128, S + 4])
            y = self.sb(es, "gy", [128, S])
            sq = self.sb(es, "gsq", [128, 512], BF16)
            rs = self.sb(es, "grs", [128, 512])
            blk = self.sb(es, "gblk", [128, 128], BF16)
            tk = [self.sb(es, "gtk%d" % i, [128, 512]) for i in range(2)]
            gin = self.sb(es, "gin", [128, NT, 16])
            gout = self.sb(es, "gout", [128, NT, 16])
            ab = self.sb(es, "gab", [128, 2, 8])
            pss = self.ps(es, "gpss", [128, 512])
            ptr = [self.ps(es, "gptr%d" % i, [128, 4, 128]) for i in range(2)]
            P.dma("sp", cw[:], self.conv_w[l], w=["cw"], stream="cw")
            P.dma("pool", blk[:], self.c_blk, w=["gblk"], stream="gblk")
            P.dma("sp", ab[:], self.gdn_ab[l].partition_broadcast(128), w=["gab"], stream="gab")
            P.op("pool", lambda e: e.memset(x[:, 0:2], 0.0), w=["gxp"])
            P.op("pool", lambda e: e.memset(x[:, S + 2:S + 4], 0.0), w=["gxp"])
            ti = 0
            import os
            for ch in range(6 if os.environ.get("GDBG", "") != "gates" else 0):
                P.dma("sp", x[:, 2:S + 2], self.cT_d[ch * 128:(ch + 1) * 128, :], r=[("fm_d", id(self.cT_d))], w=["gx"], stream="gx")
                P.op("act", lambda e, ch=ch: e.activation(out=y[:], in_=x[:, 0:S], func=AF.Identity, scale=cw[:, ch, 0:1]),
                     r=["gx", "gxp", "cw"], w=["gy"])
                for k in range(1, 5):
                    P.op("dve", lambda e, ch=ch, k=k: e.scalar_tensor_tensor(out=y[:], in0=x[:, k:k + S], scalar=cw[:, ch, k:k + 1], in1=y[:],
                                                                             op0=ALU.mult, op1=ALU.add), r=["gx", "gxp", "cw", "gy"], w=["gy"])
                P.op("act", lambda e: e.activation(out=y[:], in_=y[:], func=AF.Silu), r=["gy"], w=["gy"])
                if ch < 4:
                    isq = ch < 2
                    for tb in range(NB):
                        tsl = slice(tb * 512, (tb + 1) * 512)
                        P.op("act", lambda e, tsl=tsl: e.activation(out=sq[:], in_=y[:, tsl], func=AF.Square), r=["gy"], w=["gsq"])
                        P.op("pe", lambda e: e.matmul(pss[:], blk[:], sq[:], start=True, stop=True), r=["gblk", "gsq"], w=["gpss"])
                        P.op("act", lambda e, isq=isq: e.activation(out=rs[:], in_=pss[:], func=AF.Sqrt, bias=(64e-6 if isq else 1e-6),
                                                                    scale=(64.0 if isq else 1.0)), r=["gpss"], w=["grs"])
                        P.op("dve", lambda e: e.reciprocal(out=rs[:], in_=rs[:]), r=["grs"], w=["grs"])
                        P.op("dve", lambda e, tsl=tsl: e.tensor_tensor(out=y[:, tsl], in0=y[:, tsl], in1=rs[:], op=ALU.mult),
                             r=["gy", "grs"], w=["gy"])
                P.dma("sp", self.cn_d[ch * 128:(ch + 1) * 128, :], y[:], r=["gy"], w=["cn_d"], stream="gy")
                if ch >= 2:
                    dst = self.ktok_d if ch < 4 else self.vtok_d
                    for tb in range(NB):
                        b = ti % 2
                        ti += 1
                        for j in range(4):
                            tt = tb * 4 + j
                            P.op("pe", lambda e, b=b, j=j, tt=tt: e.transpose(ptr[b][:, j, :], y[:, tt * 128:(tt + 1) * 128], self.ident[:]),
                                 r=["gy", "ident"], w=["gptr%d" % b], inc=(j == 3))
                        P.op("act", lambda e, b=b: e.activation(out=tk[b][:], in_=ptr[b][:].rearrange("p a b -> p (a b)"), func=AF.Copy),
                             r=["gptr%d" % b], w=["gtk%d" % b])
                        P.dma("sp", dst[tb * 512:(tb + 1) * 512, (ch % 2) * 128:(ch % 2 + 1) * 128].rearrange("(j p) c -> p j c", p=128),
                              tk[b][:].rearrange("p (j c) -> p j c", c=128), r=["gtk%d" % b], w=["kvtok_d"], stream="gtk%d" % b)
            if os.environ.get("GDBG", "") == "conv":
                P.flush()
                return
            P.dma("sp", gin[:], self.cz_d[:, 256:272].rearrange("(t p) c -> p t c", p=128), r=[("tm_d", id(self.cz_d))], w=["gin"], stream="gin")
            P.op("act", lambda e: e.activation(out=gout[:, :, 0:8], in_=gin[:, :, 0:8], func=AF.Sigmoid), r=["gin"], w=["gout_b"])
            P.op("dve", lambda e: e.tensor_tensor(out=gin[:, :, 8:16], in0=gin[:, :, 8:16], in1=bc(ab[:, 1:2, :], [128, NT, 8]), op=ALU.add),
                 r=["gin", "gab"], w=["gin2"])
            P.op("act", lambda e: e.activation(out=gin[:, :, 8:16], in_=gin[:, :, 8:16], func=AF.Exp), r=["gin2"], w=["gin2"])
            P.op("act", lambda e: e.activation(out=gin[:, :, 8:16], in_=gin[:, :, 8:16], func=AF.Ln, bias=1.0, scale=1.0), r=["gin2"], w=["gin2"])
            P.op("act", lambda e: e.activation(out=ab[:, 0, :], in_=ab[:, 0, :], func=AF.Exp), r=["gab"], w=["gab0"])
            P.op("dve", lambda e: e.scalar_tensor_tensor(out=gout[:, :, 8:16], in0=gin[:, :, 8:16], scalar=-1.0, in1=bc(ab[:, 0:1, :], [128, NT, 8]),
                                                         op0=ALU.mult, op1=ALU.mult), r=["gin2", "gab0"], w=["gout_g"])
            P.dma("sp", self.gates_d.rearrange("(t p) c -> p t c", p=128), gout[:], r=["gout_b", "gout_g"], w=["gates_d"], stream="gout")
            P.flush()
        if getattr(self, "gdn_stop", None) == "prep":
            return
        NC = S // 64
        with ExitStack() as es:
            cst = self.sb(es, "gcst", [128, 5, 4, 128])
            NEGM, NEGMT, STRICT, ID8 = cst[:, 0], cst[:, 1], cst[:, 2], cst[:, 3]
            CUM, ONES = cst[:, 4, 0, :], cst[:, 4, 1, :]
            T8 = lambda n: self.sb(es, n, [128, 4, 128])
            ld = [dict(KT=T8("gKT%d" % i), QT=T8("gQT%d" % i), Kt=T8("gKt%d" % i), Vt=T8("gVt%d" % i),
                       gb=self.sb(es, "ggb%d" % i, [128, 8])) for i in range(2)]
            TN = ("gdiag", "gD", "gDT", "geGr", "gta", "gtb", "gSB", "gqkTm", "gQgT", "gX0", "gX1", "gXT0", "gXT1",
                  "gPT", "grv", "grk", "gU", "gWT", "gVn", "gKd", "gO")
            WT_ = [{n: T8(n + "_%d" % p) for n in TN} for p in range(2)]
            SM = [self.sb(es, "gsm%d" % p, [128, 24]) for p in range(2)]
            St = T8("gS")
            psAA = self.ps(es, "gpsA", [128, 2, 8])
            BK = [[self.ps(es, "gpb%d_%d" % (p, i), [128, 4, 128]) for i in range(3)] for p in range(2)]
            fl = lambda t: t[:].rearrange("p c j -> p (c j)")
            P.dma("sp", cst[:], self.c_gdn, w=["gcst"], stream="gcst")
            P.op("pool", lambda e: e.memset(St[:], 0.0), w=["gS"])
            for i in range(2):
                for nm in ("KT", "QT", "Kt", "Vt"):
                    P.op("pool", lambda e, t=ld[i][nm]: e.memset(t[:], 0.0), w=["g" + nm + str(i)])

            def bcg(t, col0):
                return bc(t[:, col0:col0 + 4].unsqueeze(2), [128, 4, 128])

            def body(s_):
                a, b = s_, NC - 1 - s_
                p = s_ % 2
                L_ = ld[p]
                sfx = str(p)
                W = WT_[p]
                R = lambda n: n + "_" + sfx
                diagG, Dm, DTm, eGr, t_a, t_b, SBm, qkTm, QgT = (W[n] for n in ("gdiag", "gD", "gDT", "geGr", "gta", "gtb", "gSB", "gqkTm", "gQgT"))
                X, XT = [W["gX0"], W["gX1"]], [W["gXT0"], W["gXT1"]]
                PT, rv, rk, U, WTt, Vn, Kd, ot = (W[n] for n in ("gPT", "grv", "grk", "gU", "gWT", "gVn", "gKd", "gO"))
                sm = SM[p]
                psA = psAA[:, p, :]
                B0, B1, B2 = BK[p]
                rB0, rB1, rB2, rA = R("gB0"), R("gB1"), R("gB2"), R("gpsA")

                def mm8(ps, psres, lhs, lres, rhs, rres):
                    for c in range(4):
                        P.op("pe", lambda e, c=c: e.matmul(ps[:, c, :], lhs[:, c, :], rhs[:, c, :], start=True, stop=True),
                             r=[lres, rres], w=[psres], inc=(c == 3))

                ra, rb_ = slice(a * 64, (a + 1) * 64), slice(b * 64, (b + 1) * 64)
                lo, hi = slice(0, 64), slice(64, 128)
                for nm, src_, r0 in (("KT", self.cn_d, 256), ("QT", self.cn_d, 0)):
                    for pp, rr in ((lo, ra), (hi, rb_)):
                        P.dma("sp", L_[nm][pp, :, pp], src_[r0:r0 + 256, rr].rearrange("(h d) t -> d h t", d=64),
                              r=["cn_d"], w=["g" + nm + sfx], stream="g" + nm + sfx)
                for nm, src_ in (("Kt", self.ktok_d), ("Vt", self.vtok_d)):
                    for pp, rr in ((lo, ra), (hi, rb_)):
                        P.dma("sp", L_[nm][pp, :, pp], src_[rr, :].rearrange("t (h d) -> t h d", d=64),
                              r=["kvtok_d"], w=["g" + nm + sfx], stream="g" + nm + sfx)
                for pp, rr, c0, s0 in ((lo, ra, 0, 0), (hi, rb_, 0, 4), (lo, ra, 4, 8), (hi, rb_, 4, 12)):
                    P.dma("sp", L_["gb"][pp, c0:c0 + 4], self.gates_d[rr, s0:s0 + 4], r=["gates_d"], w=["ggb" + sfx], stream="ggb" + sfx)
                KT, QT, Kt, Vt, gb = L_["KT"], L_["QT"], L_["Kt"], L_["Vt"], L_["gb"]
                rKT, rQT, rKt, rVt, rgb = ("g" + n + sfx for n in ("KT", "QT", "Kt", "Vt", "gb"))
                P.op("pe", lambda e: e.matmul(psA[:, 0:4], CUM, gb[:, 4:8], start=True, stop=True), r=["gcst", rgb], w=[rA], inc=False)
                P.op("pe", lambda e: e.matmul(psA[:, 4:8], ONES, gb[:, 4:8], start=True, stop=True), r=["gcst", rgb], w=[rA])
                P.op("act", lambda e: e.activation(out=sm[:, 0:8], in_=psA, func=AF.Copy), r=[rA], w=[R("gsmG")])
                P.op("act", lambda e: e.activation(out=sm[:, 8:16], in_=sm[:, 0:8], func=AF.Exp), r=[R("gsmG")], w=[R("gsmE")])
                P.op("dve", lambda e: e.tensor_tensor(out=sm[:, 16:20], in0=sm[:, 4:8], in1=sm[:, 0:4], op=ALU.subtract), r=[R("gsmG")], w=[R("gsmK")])
                P.op("act", lambda e: e.activation(out=sm[:, 16:20], in_=sm[:, 16:20], func=AF.Exp), r=[R("gsmK")], w=[R("gsmK")])
                P.op("dve", lambda e: e.tensor_tensor(out=sm[:, 20:24], in0=gb[:, 0:4], in1=sm[:, 8:12], op=ALU.mult), r=[rgb, R("gsmE")], w=[R("gsmB")])
                Gb, eGtb, kdb, bkb = bcg(sm, 0), bcg(sm, 12), bcg(sm, 16), bcg(sm, 20)
                betab = bc(gb[:, 0:4].unsqueeze(2), [128, 4, 128])
                P.op("pool", lambda e: e.tensor_tensor(out=diagG[:], in0=ID8, in1=Gb, op=ALU.mult), r=["gcst", R("gsmG")], w=[R("gdiag")])
                P.op("pe", lambda e: e.matmul(fl(B0), ONES, fl(diagG), start=True, stop=True), r=["gcst", R("gdiag")], w=[rB0])
                P.op("dve", lambda e: e.scalar_tensor_tensor(out=t_a[:], in0=B0[:], scalar=-1.0, in1=NEGM, op0=ALU.mult, op1=ALU.add),
                     r=[rB0, "gcst"], w=[R("gta")])
                P.op("dve", lambda e: e.tensor_tensor(out=t_a[:], in0=t_a[:], in1=Gb, op=ALU.add), r=[R("gta"), R("gsmG")], w=[R("gta")])
                P.op("act", lambda e: e.activation(out=Dm[:], in_=t_a[:], func=AF.Exp), r=[R("gta")], w=[R("gD")])
                P.op("dve", lambda e: e.tensor_tensor(out=t_b[:], in0=B0[:], in1=NEGMT, op=ALU.add), r=[rB0, "gcst"], w=[R("gtb")])
                P.op("dve", lambda e: e.tensor_tensor(out=t_b[:], in0=t_b[:], in1=Gb, op=ALU.subtract), r=[R("gtb"), R("gsmG")], w=[R("gtb")])
                P.op("act", lambda e: e.activation(out=DTm[:], in_=t_b[:], func=AF.Exp), r=[R("gtb")], w=[R("gDT")])
                P.op("act", lambda e: e.activation(out=eGr[:], in_=B0[:], func=AF.Exp), r=[rB0], w=[R("geGr")])
                P.op("pool", lambda e: e.tensor_tensor(out=QgT[:], in0=QT[:], in1=eGr[:], op=ALU.mult), r=[rQT, R("geGr")], w=[R("gQgT")])
                mm8(B1, rB1, KT, rKT, KT, rKT)
                mm8(B2, rB2, KT, rKT, QT, rQT)
                P.op("pool", lambda e: e.tensor_tensor(out=SBm[:], in0=STRICT, in1=betab, op=ALU.mult), r=["gcst", rgb], w=[R("gSB")])
                P.op("dve", lambda e: e.tensor_tensor(out=t_a[:], in0=B1[:], in1=Dm[:], op=ALU.mult), r=[rB1, R("gD")], w=[R("gta")])
                P.op("pool", lambda e: e.tensor_tensor(out=X[0][:], in0=t_a[:], in1=SBm[:], op=ALU.mult), r=[R("gta"), R("gSB")], w=[R("gX0")])
                P.op("dve", lambda e: e.tensor_tensor(out=qkTm[:], in0=B2[:], in1=DTm[:], op=ALU.mult), r=[rB2, R("gDT")], w=[R("gqkTm")])
                mm8(B1, rB1, X[0], R("gX0"), cst[:, 3], "gcst")
                P.op("act", lambda e: e.activation(out=XT[0][:], in_=B1[:], func=AF.Copy), r=[rB1], w=[R("gXT0")])
                P.op("dve", lambda e: e.scalar_tensor_tensor(out=PT[:], in0=B1[:], scalar=-1.0, in1=ID8, op0=ALU.mult, op1=ALU.add),
                     r=[rB1, "gcst", R("gXT0")], w=[R("gPT")])
                for lv in range(1, 6):
                    ci, ni = (lv - 1) % 2, lv % 2
                    mm8(B0, rB0, XT[ci], R("gXT%d" % ci), X[ci], R("gX%d" % ci))
                    if lv < 5:
                        mm8(B2, rB2, X[ci], R("gX%d" % ci), XT[ci], R("gXT%d" % ci))
                    P.op("act", lambda e, ni=ni: e.activation(out=X[ni][:], in_=B0[:], func=AF.Copy), r=[rB0], w=[R("gX%d" % ni)])
                    if lv < 5:
                        P.op("dve", lambda e, ni=ni: e.tensor_copy(out=XT[ni][:], in_=B2[:]), r=[rB2], w=[R("gXT%d" % ni)])
                    mm8(B1, rB1, X[ni], R("gX%d" % ni), PT, R("gPT"))
                    P.op("dve", lambda e: e.tensor_tensor(out=PT[:], in0=PT[:], in1=B1[:], op=ALU.add), r=[R("gPT"), rB1], w=[R("gPT")])
                P.op("pool", lambda e: e.tensor_tensor(out=rv[:], in0=Vt[:], in1=betab, op=ALU.mult), r=[rVt, rgb], w=[R("grv")])
                P.op("pool", lambda e: e.tensor_tensor(out=rk[:], in0=Kt[:], in1=bkb, op=ALU.mult), r=[rKt, R("gsmB")], w=[R("grk")])
                P.op("pool", lambda e: e.tensor_tensor(out=Kd[:], in0=Kt[:], in1=kdb, op=ALU.mult), r=[rKt, R("gsmK")], w=[R("gKd")])
                mm8(B0, rB0, PT, R("gPT"), rv, R("grv"))
                P.op("act", lambda e: e.activation(out=U[:], in_=B0[:], func=AF.Copy), r=[rB0], w=[R("gU")])
                mm8(B2, rB2, rk, R("grk"), PT, R("gPT"))
                P.op("dve", lambda e: e.tensor_copy(out=WTt[:], in_=B2[:]), r=[rB2], w=[R("gWT")])
                mark.append(len(P.rec))
                mm8(B1, rB1, WTt, R("gWT"), St, "gS")
                P.op("dve", lambda e: e.scalar_tensor_tensor(out=Vn[:], in0=B1[:], scalar=-1.0, in1=U[:], op0=ALU.mult, op1=ALU.add),
                     r=[rB1, R("gU")], w=[R("gVn")])
                for c in range(4):
                    P.op("pe", lambda e, c=c: e.matmul(B0[:, c, :], QgT[:, c, :], St[:, c, :], start=True, stop=False), r=[R("gQgT"), "gS"], w=[rB0],
                         inc=False)
                    P.op("pe", lambda e, c=c: e.matmul(B0[:, c, :], qkTm[:, c, :], Vn[:, c, :], start=False, stop=True), r=[R("gqkTm"), R("gVn")],
                         w=[rB0], inc=(c == 3))
                mm8(B2, rB2, Kd, R("gKd"), Vn, R("gVn"))
                P.op("pool", lambda e: e.tensor_tensor(out=St[:], in0=St[:], in1=eGtb, op=ALU.mult), r=["gS", R("gsmE")], w=["gS"])
                P.op("dve", lambda e: e.tensor_tensor(out=St[:], in0=St[:], in1=B2[:], op=ALU.add), r=["gS", rB2], w=["gS"])
                P.op("act", lambda e: e.activation(out=ot[:], in_=B0[:], func=AF.Copy), r=[rB0], w=[R("gO")])
                P.dma("sp", self.o_d[0, ra, :].rearrange("t (h d) -> t h d", d=64), ot[0:64, :, 0:64], r=[R("gO")], w=["o_d"], stream="gO" + sfx)
                P.dma("sp", self.o_d[1, rb_, :].rearrange("t (h d) -> t h d", d=64), ot[64:128, :, 64:128], r=[R("gO")], w=["o_d"], stream="gO" + sfx)

            streams = [[], []]
            for s_ in range(NC):
                P.rec = []
                mark = []
                body(s_)
                items, P.rec = P.rec, None
                for k, it_ in enumerate(items):
                    streams[s_ % 2].append((s_, k >= mark[0], it_))
            per = len(streams[0]) // ((NC + 1) // 2)
            delay = per // 2
            merged = []
            i0 = i1 = 0
            n0, n1 = len(streams[0]), len(streams[1])
            t = 0
            while i0 < n0 or i1 < n1:
                if i0 < n0:
                    merged.append(streams[0][i0]); i0 += 1
                if t >= delay and i1 < n1:
                    merged.append(streams[1][i1]); i1 += 1
                t += 1
            last_rec = -1
            for s_, isrec, it_ in merged:
                if isrec:
                    assert s_ >= last_rec, "recurrence emitted out of chunk order"
                    last_rec = s_
                P.replay(it_)
            P.flush()
        if getattr(self, "gdn_stop", None) == "main":
            return
        with ExitStack() as es:
            gg = self.sb(es, "ggn", [128, 64])
            of = [self.sb(es, "gof%d" % i, [128, 4, 64]) for i in range(2)]
            obk = [self.sb(es, "gob%d" % i, [128, 4, 64]) for i in range(2)]
            zt = [self.sb(es, "gz%d" % i, [128, 4, 64]) for i in range(2)]
            sq2 = self.sb(es, "gsq2", [128, 4, 64])
            ms = self.sb(es, "gms", [128, 8])
            yb_ = [self.sb(es, "gyb%d" % i, [128, 2, 128], BF16) for i in range(2)]
            pt2 = [self.ps(es, "gpt2%d" % i, [128, 2, 128]) for i in range(2)]
            P.dma("sp", gg[:], self.gdn_g[l].partition_broadcast(128), w=["ggn"], stream="ggn")
            for tt in range(NT):
                b = tt % 2
                tr = slice(tt * 128, (tt + 1) * 128)
                P.dma("sp", of[b][:], self.o_d[0, tr, :].rearrange("t (h d) -> t h d", d=64), r=["o_d"], w=["gof%d" % b], stream="gof%d" % b)
                P.dma("sp", obk[b][:], self.o_d[1, tr, :].rearrange("t (h d) -> t h d", d=64), r=["o_d"], w=["gob%d" % b], stream="gob%d" % b)
                P.dma("sp", zt[b][:], self.cz_d[tr, 0:256].rearrange("t (h d) -> t h d", d=64), r=[("tm_d", id(self.cz_d))], w=["gz%d" % b],
                      stream="gz%d" % b)
                P.op("dve", lambda e, b=b: e.tensor_tensor(out=of[b][:], in0=of[b][:], in1=obk[b][:], op=ALU.add), r=["gof%d" % b, "gob%d" % b],
                     w=["gof%d" % b])
                P.op("act", lambda e, b=b: e.activation(out=sq2[:], in_=of[b][:], func=AF.Square), r=["gof%d" % b], w=["gsq2"])
                P.op("dve", lambda e: e.tensor_reduce(out=ms[:, 0:4], in_=sq2[:], axis=AX.X, op=ALU.add), r=["gsq2"], w=["gms"])
                P.op("act", lambda e: e.activation(out=ms[:, 4:8], in_=ms[:, 0:4], func=AF.Sqrt, bias=1e-6, scale=1.0 / 64), r=["gms"], w=["gms2"])
                P.op("dve", lambda e: e.reciprocal(out=ms[:, 4:8], in_=ms[:, 4:8]), r=["gms2"], w=["gms2"])
                P.op("dve", lambda e, b=b: e.tensor_tensor(out=of[b][:], in0=of[b][:], in1=bc(ms[:, 4:8].unsqueeze(2), [128, 4, 64]), op=ALU.mult),
                     r=["gof%d" % b, "gms2"], w=["gof%d" % b])
                P.op("pool", lambda e, b=b: e.tensor_tensor(out=of[b][:], in0=of[b][:], in1=bc(gg[:].unsqueeze(1), [128, 4, 64]), op=ALU.mult),
                     r=["gof%d" % b, "ggn"], w=["gof%d" % b])
                P.op("act", lambda e, b=b: e.activation(out=zt[b][:], in_=zt[b][:], func=AF.Silu), r=["gz%d" % b], w=["gz%d" % b])
                P.op("dve", lambda e, b=b: e.tensor_tensor(out=of[b][:], in0=of[b][:], in1=zt[b][:], op=ALU.mult), r=["gof%d" % b, "gz%d" % b],
                     w=["gof%d" % b])
                for j in range(2):
                    P.op("pe", lambda e, b=b, j=j: e.transpose(pt2[b][:, j, :], of[b][:, 2 * j:2 * j + 2, :].rearrange("p a d -> p (a d)"), self.ident[:]),
                         r=["gof%d" % b, "ident"], w=["gpt2%d" % b], inc=(j == 1))
                P.op("act", lambda e, b=b: e.activation(out=yb_[b][:], in_=pt2[b][:], func=AF.Copy), r=["gpt2%d" % b], w=["gyb%d" % b])
                P.dma("sp", self.ycT_d[:, tr].rearrange("(j p) t -> p j t", p=128), yb_[b][:], r=["gyb%d" % b], w=["ycT_d"], stream="gyb%d" % b)
            P.flush()

    def phase_merge(self, l):
        P = self.P
        S, NB, NT = self.S, self.NB, self.NT
        with ExitStack() as es:
            wa = self.sb(es, "wa", [128, 4, D], BF16)
            wb = self.sb(es, "wb", [128, 2, D], BF16)
            wc = self.sb(es, "wc", [128, 2, D], BF16)
            wo = self.sb(es, "wo", [128, 8, D], BF16)
            yT = self.sb(es, "yT", [128, 8, 512], BF16)
            gt = [self.sb(es, "gt%d" % i, [128, 3, 512], BF16) for i in range(2)]
            mixT = self.sb(es, "mixT", [128, 8, 512], BF16)
            m1 = self.sb(es, "m1", [128, 512])
            m2 = self.sb(es, "m2", [128, 512])
            m3 = self.sb(es, "m3", [128, 512])
            hold = [self.sb(es, "hold%d" % i, [128, D]) for i in range(2)]
            tsum = [self.sb(es, "tsum%d" % i, [128, D]) for i in range(2)]
            gb = self.sb(es, "gb1", [128, 2, D])
            pabc = [self.ps(es, "pabc%d" % i, [128, 512]) for i in range(3)]
            po = [self.ps(es, "po%d" % i, [128, 512]) for i in range(2)]
            tiles = self.ln_scratch(es, "l1")
            rt = self.router_setup(es) if not getattr(self, "no_router", False) else None
            P.dma("pool", wa[:], self.wa[l].rearrange("(kc p) n -> p kc n", p=128), w=["wa"], stream="wa")
            P.dma("pool", wb[:], self.wb[l].rearrange("(kc p) n -> p kc n", p=128), w=["wb"], stream="wb")
            P.dma("pool", wc[:], self.wc[l].rearrange("(kc p) n -> p kc n", p=128), w=["wc"], stream="wc")
            P.dma("pool", wo[:], self.wo[l].rearrange("(kc p) n -> p kc n", p=128), w=["wo"], stream="wo")
            P.dma("sp", gb[:], self.ln1[l].partition_broadcast(128), w=["gb1"], stream="gb1")
            if "B" not in self.parts:
                P.op("pool", lambda e: e.memset(yT[:, 4:6, :], 0.0), w=["yTb"])
            if "C" not in self.parts:
                P.op("pool", lambda e: e.memset(yT[:, 6:8, :], 0.0), w=["yTc"])
            for tb in range(NB):
                tsl = slice(tb * 512, (tb + 1) * 512)
                if "A" in self.parts:
                    P.dma("sp", yT[:, 0:4, :], self.yaT_d[:, tsl].rearrange("(kc p) s -> p kc s", p=128), r=["yaT_d"], w=["yTa"],
                          stream="yTa")
                else:
                    P.op("pool", lambda e: e.memset(yT[:, 0:4, :], 0.0), w=["yTa"])
                if "B" in self.parts:
                    P.dma("sp", yT[:, 4:6, :], self.ybT_d[:, tsl].rearrange("(kc p) s -> p kc s", p=128), r=["ybT_d"], w=["yTb"],
                          stream="yTb")
                if "C" in self.parts:
                    P.dma("sp", yT[:, 6:8, :], self.ycT_d[:, tsl].rearrange("(kc p) s -> p kc s", p=128), r=["ycT_d"], w=["yTc"],
                          stream="yTc")
                for dc in range(8):
                    b = dc % 2
                    P.dma("sp", gt[b][:], self.gT_d[:, tsl].rearrange("(t c p) s -> p t c s", p=128, t=3)[:, :, dc, :],
                          r=[("fm_d", id(self.gT_d))], w=["gt%d" % b], stream="gt%d" % b)
                    csl = slice(dc * 128, (dc + 1) * 128)
                    self.mm_group(pabc[0][:], [(wa[:, kc, csl], yT[:, kc, :]) for kc in range(4)], r=["wa", "yTa"], w=["pabc0"])
                    self.mm_group(pabc[1][:], [(wb[:, kc, csl], yT[:, 4 + kc, :]) for kc in range(2)], r=["wb", "yTb"], w=["pabc1"])
                    self.mm_group(pabc[2][:], [(wc[:, kc, csl], yT[:, 6 + kc, :]) for kc in range(2)], r=["wc", "yTc"], w=["pabc2"])
                    P.op("dve", lambda e, b=b: e.tensor_tensor(out=m1[:], in0=pabc[0][:], in1=gt[b][:, 0, :], op=ALU.mult),
                         r=["pabc0", "gt%d" % b], w=["m1"])
                    P.op("dve", lambda e, b=b: e.tensor_tensor(out=m2[:], in0=pabc[1][:], in1=gt[b][:, 1, :], op=ALU.mult),
                         r=["pabc1", "gt%d" % b], w=["m2"])
                    P.op("dve", lambda e, b=b: e.tensor_tensor(out=m3[:], in0=pabc[2][:], in1=gt[b][:, 2, :], op=ALU.mult),
                         r=["pabc2", "gt%d" % b], w=["m3"])
                    P.op("pool", lambda e: e.tensor_tensor(out=m1[:], in0=m1[:], in1=m2[:], op=ALU.add), r=["m1", "m2"], w=["m1"])
                    P.op("pool", lambda e, dc=dc: e.tensor_tensor(out=mixT[:, dc, :], in0=m1[:], in1=m3[:], op=ALU.add),
                         r=["m1", "m3"], w=[("mixT", dc)])
                for ti in range(4):
                    tt = tb * 4 + ti
                    hb = tt % 2
                    P.dma("sp", hold[hb][:], self.h_d[tt * 128:(tt + 1) * 128, :], r=[("h_d", tt)], w=["hold%d" % hb],
                          stream="hold%d" % hb)
                    for half in range(2):
                        self.mm_group(po[half][:], [(mixT[:, dc, ti * 128:(ti + 1) * 128], wo[:, dc, half * 512:(half + 1) * 512])
                                                    for dc in range(8)],
                                      r=["wo"] + [("mixT", dc) for dc in range(8)], w=["po%d" % half])
                        P.op("dve", lambda e, hb=hb, half=half: e.scalar_tensor_tensor(
                            out=tsum[hb][:, half * 512:(half + 1) * 512], in0=hold[hb][:, half * 512:(half + 1) * 512],
                            scalar=ALPHA, in1=po[half][:], op0=ALU.mult, op1=ALU.add),
                            r=["hold%d" % hb, "po%d" % half], w=["tsum%d" % hb])
                    self.ln_tile(tiles, tsum[hb], "tsum%d" % hb, gb, "gb1", self.h_d, tt, router=rt, pfx="l1")
            P.flush()

    def router_setup(self, es):
        P = self.P
        wr = self.sb(es, "wr", [128, KC, NE])
        wrh = self.sb(es, "wrh", [128, KC, NE], BF16)
        wrl = self.sb(es, "wrl", [128, KC, NE], BF16)
        rb = self.sb(es, "rb", [128, NE])
        hlo = self.sb(es, "hlo", [128, KC, 128], BF16)
        pl = self.ps(es, "pl", [128, NE])
        sc = self.sb(es, "r_sc", [128, NE])
        sel = self.sb(es, "r_sel", [128, NE])
        tmp = self.sb(es, "r_tmp", [128, NE])
        tmp2 = self.sb(es, "r_tmp2", [128, NE])
        g8 = self.sb(es, "r_g8", [128, 8, 4])
        oh1 = self.sb(es, "r_oh1", [128, NE])
        oh2 = self.sb(es, "r_oh2", [128, NE])
        sm = self.sb(es, "r_sm", [128, 8])
        P.dma("sp", wr[:], self.w_router.rearrange("(kc p) n -> p kc n", p=128), w=["wr"], stream="wr")
        P.dma("sp", rb[:], self.router_bias.partition_broadcast(128), w=["rb"], stream="rb")
        P.dma("pool", wrh[:], self.w_router.rearrange("(kc p) n -> p kc n", p=128), w=["wrh"], stream="wrh")
        P.op("dve", lambda e: e.tensor_tensor(out=wrl[:], in0=wr[:], in1=wrh[:], op=ALU.subtract), r=["wr", "wrh"], w=["wrl"])
        BIG = 1.0e4

        def router(tt, pT, pTres):
            tsl = slice(tt * 128, (tt + 1) * 128)
            P.op("dve", lambda e: e.tensor_tensor(out=hlo[:], in0=pT[:], in1=self.hT[:, :, tsl], op=ALU.subtract),
                 r=[pTres, ("hT", tt)], w=["hlo"])
            pairs = []
            for kc in range(KC):
                pairs += [(self.hT[:, kc, tsl], wrh[:, kc, :]), (self.hT[:, kc, tsl], wrl[:, kc, :]), (hlo[:, kc, :], wrh[:, kc, :])]
            self.mm_group(pl[:], pairs, r=["hlo", ("hT", tt), "wrh", "wrl"], w=["pl"])
            P.op("act", lambda e: e.activation(out=sc[:], in_=pl[:], func=AF.Sigmoid), r=["pl"], w=["r_sc"])
            P.op("dve", lambda e: e.tensor_tensor(out=sel[:], in0=sc[:], in1=rb[:], op=ALU.add), r=["r_sc", "rb"], w=["r_sel"])
            s3 = sel[:].rearrange("p (g k) -> p g k", k=4)
            t3 = tmp[:].rearrange("p (g k) -> p g k", k=4)
            P.op("dve", lambda e: e.tensor_reduce(out=sm[:, 0:8], in_=s3, axis=AX.X, op=ALU.max), r=["r_sel"], w=["r_sm"])
            P.op("dve", lambda e: e.tensor_tensor(out=t3, in0=s3, in1=bc(sm[:, 0:8].unsqueeze(2), [128, 8, 4]), op=ALU.is_equal),
                 r=["r_sel", "r_sm"], w=["r_tmp"])
            P.op("dve", lambda e: e.scalar_tensor_tensor(out=tmp[:], in0=tmp[:], scalar=-BIG, in1=sel[:], op0=ALU.mult, op1=ALU.add),
                 r=["r_tmp", "r_sel"], w=["r_tmp"])
            P.op("dve", lambda e: e.tensor_reduce(out=g8[:, :, 0], in_=t3, axis=AX.X, op=ALU.max), r=["r_tmp"], w=["r_g8"])
            P.op("dve", lambda e: e.tensor_tensor(out=g8[:, :, 1], in0=g8[:, :, 0], in1=sm[:, 0:8], op=ALU.add),
                 r=["r_g8", "r_sm"], w=["r_g8b"])
            P.op("dve", lambda e: e.tensor_reduce(out=sm[:, 0:1], in_=g8[:, :, 1], axis=AX.X, op=ALU.max), r=["r_g8b"], w=["r_sm1"])
            P.op("dve", lambda e: e.tensor_scalar(out=g8[:, :, 2], in0=g8[:, :, 1], scalar1=sm[:, 0:1], scalar2=None, op0=ALU.is_lt),
                 r=["r_g8b", "r_sm1"], w=["r_g8c"])
            P.op("dve", lambda e: e.scalar_tensor_tensor(out=t3, in0=bc(g8[:, :, 2:3], [128, 8, 4]), scalar=-BIG, in1=s3,
                                                         op0=ALU.mult, op1=ALU.add), r=["r_g8c", "r_sel"], w=["r_tmp"])
            P.op("dve", lambda e: e.tensor_reduce(out=sm[:, 1:2], in_=tmp[:], axis=AX.X, op=ALU.max), r=["r_tmp"], w=["r_sm2"])
            P.op("dve", lambda e: e.tensor_scalar(out=oh1[:], in0=tmp[:], scalar1=sm[:, 1:2], scalar2=None, op0=ALU.is_equal),
                 r=["r_tmp", "r_sm2"], w=["r_oh1"])
            P.op("dve", lambda e: e.scalar_tensor_tensor(out=tmp2[:], in0=oh1[:], scalar=-BIG, in1=tmp[:], op0=ALU.mult, op1=ALU.add),
                 r=["r_oh1", "r_tmp"], w=["r_tmp2"])
            P.op("dve", lambda e: e.tensor_reduce(out=sm[:, 2:3], in_=tmp2[:], axis=AX.X, op=ALU.max), r=["r_tmp2"], w=["r_sm3"])
            P.op("dve", lambda e: e.tensor_scalar(out=oh2[:], in0=tmp2[:], scalar1=sm[:, 2:3], scalar2=None, op0=ALU.is_equal),
                 r=["r_tmp2", "r_sm3"], w=["r_oh2"])
            P.op("dve", lambda e: e.tensor_tensor(out=oh1[:], in0=oh1[:], in1=oh2[:], op=ALU.add), r=["r_oh1", "r_oh2"], w=["r_oh1"])
            P.op("dve", lambda e: e.tensor_tensor(out=oh1[:], in0=oh1[:], in1=sc[:], op=ALU.mult), r=["r_oh1", "r_sc"], w=["r_oh1"])
            P.op("dve", lambda e: e.tensor_reduce(out=sm[:, 3:4], in_=oh1[:], axis=AX.X, op=ALU.add), r=["r_oh1"], w=["r_sm4"])
            P.op("dve", lambda e: e.reciprocal(out=sm[:, 4:5], in_=sm[:, 3:4]), r=["r_sm4"], w=["r_sm5"])
            P.op("dve", lambda e: e.tensor_scalar(out=self.comb[:, tt, :], in0=oh1[:], scalar1=sm[:, 4:5], scalar2=None, op0=ALU.mult),
                 r=["r_oh1", "r_sm5"], w=[("comb", tt)])
        return router

    def phase_moe_dense(self, l, last):
        P = self.P
        S, NB, NT = self.S, self.NB, self.NT
        G = min(8, NT)
        w1_l = self.w1[l].rearrange("e (kc p) n -> e p kc n", p=128)
        w3_l = self.w3[l].rearrange("e (kc p) n -> e p kc n", p=128)
        w2_l = self.w2[l].rearrange("e (kc p) n -> e p kc n", p=128)
        with ExitStack() as es:
            w1t = [self.sb(es, "w1t%d" % i, [128, KC, DE], BF16) for i in range(2)]
            w3t = [self.sb(es, "w3t%d" % i, [128, KC, DE], BF16) for i in range(2)]
            w2t = [self.sb(es, "w2t%d" % i, [128, 4, D], BF16) for i in range(2)]
            yacc = self.sb(es, "yacc", [128, G, D])
            hid = [self.sb(es, "hid%d" % i, [128, 4, 512], BF16) for i in range(2)]
            sl = [self.sb(es, "sl%d" % i, [128, 512], BF16) for i in range(2)]
            hold = [self.sb(es, "mhold%d" % i, [128, D]) for i in range(2)]
            gb = self.sb(es, "gb2", [128, 2, D])
            p1 = [self.ps(es, "p1_%d" % i, [128, 512]) for i in range(2)]
            p3 = [self.ps(es, "p3_%d" % i, [128, 512]) for i in range(2)]
            py = [self.ps(es, "py%d" % i, [128, 512]) for i in range(2)]
            tiles = self.ln_scratch(es, "l2")
            P.dma("sp", gb[:], self.ln2[l].partition_broadcast(128), w=["gb2"], stream="gb2")
            def record(fn):
                P.rec = []
                fn()
                items, P.rec = P.rec, None
                return items

            for grp in range(NT // G):
                P.op("pool", lambda e: e.memset(yacc[:], 0.0), w=["yacc"])
                units = [(ex, blk) for ex in range(NE) for blk in range(G // 4)]

                def stage1(ui):
                    ex, blk = units[ui]
                    b = ex % 2
                    tb = grp * (G // 4) + blk
                    hb = ui % 2
                    hres = [("hT", tb * 4 + i) for i in range(4)]
                    segs = []

                    def loads():
                        P.dma("pool", w1t[b][:], w1_l[ex], w=["w1t%d" % b], stream="w1t%d" % b)
                        P.dma("pool", w3t[b][:], w3_l[ex], w=["w3t%d" % b], stream="w3t%d" % b)
                        P.dma("pool", w2t[b][:], w2_l[ex], w=["w2t%d" % b], stream="w2t%d" % b)
                    pre = record(loads) if blk == 0 else []
                    for fc in range(4):
                        pb_ = fc % 2
                        fsl = slice(fc * 128, (fc + 1) * 128)

                        def s_a():
                            self.mm_group(p1[pb_][:], [(w1t[b][:, kc, fsl], self.hT[:, kc, tb * 512:(tb + 1) * 512]) for kc in range(KC)],
                                          r=["w1t%d" % b] + hres, w=["p1_%d" % pb_])
                            P.op("act", lambda e, pb_=pb_: e.activation(out=sl[pb_][:], in_=p1[pb_][:], func=AF.Silu),
                                 r=["p1_%d" % pb_], w=["sl%d" % pb_])

                        def s_b():
                            self.mm_group(p3[pb_][:], [(w3t[b][:, kc, fsl], self.hT[:, kc, tb * 512:(tb + 1) * 512]) for kc in range(KC)],
                                          r=["w3t%d" % b] + hres, w=["p3_%d" % pb_])
                            P.op("dve", lambda e, hb=hb, fc=fc, pb_=pb_: e.tensor_tensor(out=hid[hb][:, fc, :], in0=sl[pb_][:], in1=p3[pb_][:], op=ALU.mult),
                                 r=["sl%d" % pb_, "p3_%d" % pb_], w=[("hid", hb, fc)])
                        segs.append(pre + record(s_a))
                        pre = []
                        segs.append(record(s_b))
                    return segs

                def stage2(ui):
                    ex, blk = units[ui]
                    b = ex % 2
                    tb = grp * (G // 4) + blk
                    hb = ui % 2
                    segs = []
                    for ti in range(4):
                        gi = blk * 4 + ti
                        tt = tb * 4 + ti
                        for half in range(2):
                            hs = slice(half * 512, (half + 1) * 512)

                            def s_c():
                                self.mm_group(py[half][:], [(hid[hb][:, fc, ti * 128:(ti + 1) * 128], w2t[b][:, fc, hs]) for fc in range(4)],
                                              r=["w2t%d" % b] + [("hid", hb, fc) for fc in range(4)], w=["py%d" % half])
                                P.op("dve", lambda e, gi=gi, hs=hs, half=half, tt=tt, ex=ex: e.scalar_tensor_tensor(
                                    out=yacc[:, gi, hs], in0=py[half][:], scalar=self.comb[:, tt, ex:ex + 1], in1=yacc[:, gi, hs],
                                    op0=ALU.mult, op1=ALU.add), r=["py%d" % half, ("comb", tt), "yacc"], w=["yacc"])
                            segs.append(record(s_c))
                    return segs

                emit = lambda seg: [P.replay(it_) for it_ in seg]
                for seg in stage1(0):
                    emit(seg)
                for ui in range(len(units)):
                    s2 = stage2(ui)
                    s1 = stage1(ui + 1) if ui + 1 < len(units) else []
                    for k in range(8):
                        emit(s2[k])
                        if s1:
                            emit(s1[k])
                for gi in range(G):
                    tt = grp * G + gi
                    hb = tt % 2
                    P.dma("sp", hold[hb][:], self.h_d[tt * 128:(tt + 1) * 128, :], r=[("h_d", tt)], w=["mhold%d" % hb],
                          stream="mhold%d" % hb)
                    P.op("dve", lambda e, hb=hb, gi=gi: e.scalar_tensor_tensor(out=hold[hb][:], in0=hold[hb][:], scalar=ALPHA,
                                                                                 in1=yacc[:, gi, :], op0=ALU.mult, op1=ALU.add),
                         r=["mhold%d" % hb, "yacc"], w=["mhold%d" % hb])
                    self.ln_tile(tiles, hold[hb], "mhold%d" % hb, gb, "gb2", self.out if last else self.h_d, tt,
                                 hT_out=not last, pfx="l2")
            P.flush()

    def build(self, stop=None):
        P = self.P
        self.load_consts()
        self.phase_ln0()
        for l in range(self.L):
            if stop == "ln0":
                break
            self.phase_inproj(l)
            if stop == "inproj":
                break
            if "A" in self.parts:
                self.phase_attn_a(l)
            if "B" in self.parts:
                self.phase_attn_b(l)
            if "C" in self.parts:
                self.phase_gdn(l)
            if stop == "attn":
                break
            self.phase_merge(l)
            if stop == "merge":
                break
            self.phase_moe_dense(l, last=(l == self.L - 1))
        P.wait_all("sp", list(P.lastw.keys()))
        P.flush()
        self.es.close()
        P.close()
        return self.nc


def host_consts(S):
    c = {}
    c["c_ident"] = np.eye(128, dtype=np.float32)
    blk = np.zeros((128, 128), np.float32)
    blk[:64, :64] = 1.0
    blk[64:, 64:] = 1.0
    c["c_blk"] = blk
    R = np.zeros((64, 64), np.float32)
    for base in (0, 32):
        for i in range(16):
            R[base + i, base + 16 + i] = -1.0
            R[base + 16 + i, base + i] = 1.0
    RT = np.zeros((128, 128), np.float32)
    RT[:64, :64] = R.T
    RT[64:, 64:] = R.T
    c["c_rot"] = RT
    t = np.arange(S)
    row = (t // GRID_W).astype(np.float32)
    col = (t % GRID_W).astype(np.float32)
    inv = (10000.0 ** (-np.arange(0, 32, 2, dtype=np.float32) / 32)).astype(np.float32)
    ang_r = row[None, :] * inv[:, None]
    ang_c = col[None, :] * inv[:, None]
    cos64 = np.concatenate([np.cos(ang_r), np.cos(ang_r), np.cos(ang_c), np.cos(ang_c)], 0)
    sin64 = np.concatenate([np.sin(ang_r), np.sin(ang_r), np.sin(ang_c), np.sin(ang_c)], 0)
    c["c_cos"] = np.concatenate([cos64, cos64], 0).astype(np.float32)
    c["c_sin"] = np.concatenate([sin64, sin64], 0).astype(np.float32)
    g = np.zeros((128, 5, 4, 128), np.float32)
    i = np.arange(64)[:, None]
    j = np.arange(64)[None, :]
    g[:, 0] = NEG
    g[:, 1] = NEG
    lo, hi = slice(0, 64), slice(64, 128)
    for pp, fwd in ((lo, True), (hi, False)):
        allow = (i >= j) if fwd else (i <= j)
        for cc in range(4):
            g[pp, 0, cc, pp] = np.where(allow, 0.0, NEG)
            g[pp, 1, cc, pp] = np.where(allow.T, 0.0, NEG)
            g[pp, 2, cc, pp] = ((i > j) if fwd else (i < j)).astype(np.float32)
            g[pp, 3, cc, pp] = (i == j).astype(np.float32)
        g[pp, 4, 0, pp] = ((i <= j) if fwd else (i >= j)).astype(np.float32)
        g[pp, 4, 1, pp] = 1.0
    c["c_gdn"] = g
    return c


def host_layout(inp, L):
    f = lambda a: np.ascontiguousarray(np.asarray(a, dtype=np.float32))
    m = {}
    m["ln0"] = f(np.stack([inp["ln0_g"], inp["ln0_b"]], 0))
    m["w_in"] = f(inp["w_in"][:L])
    qg = np.asarray(inp["q_norm_g"], np.float32)[:L]
    kg = np.asarray(inp["k_norm_g"], np.float32)[:L]
    m["qkg"] = f(np.stack([np.concatenate([qg, qg], 1), np.concatenate([kg, kg], 1)], 2))
    m["wa"] = f(inp["w_branch_a"][:L])
    m["wb"] = f(inp["w_branch_b"][:L])
    m["wc"] = f(inp["w_branch_c"][:L])
    m["wo"] = f(inp["w_out"][:L])
    m["ln1"] = f(np.stack([inp["ln1_g"][:L], inp["ln1_b"][:L]], 1))
    m["ln2"] = f(np.stack([inp["ln2_g"][:L], inp["ln2_b"][:L]], 1))
    m["w_router"] = f(inp["w_router"])
    m["router_bias"] = f(inp["router_bias"])
    m["w1"] = f(inp["w1"][:L])
    m["w3"] = f(inp["w3"][:L])
    m["w2"] = f(inp["w2"][:L])
    rpb = np.asarray(inp["na_rpb"], np.float32)[:L]
    c = np.arange(64)
    cs = np.clip(c - 8, 0, 48)
    kc_ = np.arange(64)
    inwin = (kc_[None, :] >= cs[:, None]) & (kc_[None, :] < cs[:, None] + 16)
    dc = np.clip(kc_[None, :] - c[:, None] + 15, 0, 30)
    g = rpb[:, :, :, dc]
    g = np.where(inwin[None, None, None], g, np.float32(NEG))
    m["bias_b"] = f(np.transpose(g, (0, 1, 3, 2, 4)).reshape(L, 4, 64, 960))
    cwv = np.asarray(inp["conv_w"], np.float32)[:L]
    m["conv_w"] = f(np.transpose(cwv.reshape(L, 5, 6, 128), (0, 3, 2, 1)))
    m["gdn_ab"] = f(np.stack([np.asarray(inp["A_log"], np.float32)[:L].reshape(L, 8),
                              np.asarray(inp["dt_bias"], np.float32)[:L].reshape(L, 8)], 1))
    m["gdn_g"] = f(inp["gdn_norm_g"][:L])
    return m


_CACHE = {}


def kernel(**inputs):
    S = 4096
    L = DEPTH
    x = np.asarray(inputs["x"], dtype=np.float32)
    nb = x.shape[0]
    key = (S, L)
    if key not in _CACHE:
        _CACHE[key] = Builder(S, L).build()
    nc = _CACHE[key]
    shared = host_layout(inputs, L)
    shared.update(host_consts(S))
    in_maps = []
    for b in range(nb):
        mm = dict(shared)
        mm["x"] = np.ascontiguousarray(x[b])
        in_maps.append(mm)
    res = run_bass_kernel_spmd(nc, in_maps, core_ids=list(range(nb)))
    return np.stack([np.asarray(r["out"], dtype=np.float32) for r in res.results], 0)
```

```python
import math
import numpy as np
from contextlib import ExitStack
import concourse.bass as bass
import concourse.mybir as mybir
from concourse.bass_utils import run_bass_kernel_spmd

F32 = mybir.dt.float32
BF16 = mybir.dt.bfloat16
I32 = mybir.dt.int32
AF = mybir.ActivationFunctionType
ALU = mybir.AluOpType
AX = mybir.AxisListType

ENGS = ("pe", "act", "dve", "pool", "sp")

D = 1024
KC = 8
DEPTH = 4
GRID_W = 64
DIN = 5648
NE = 32
DE = 512
ALPHA = (2 * DEPTH) ** 0.25
C_AQ, C_AK, C_AV = 0, 512, 640
C_BQ, C_BK, C_BV = 768, 1024, 1280
C_CQ, C_CK, C_CV, C_CZ = 1536, 1792, 2048, 2304
C_CG = 2560
C_GATE = 2576
NEG = -30000.0


class Prog:
    def __init__(self, nc):
        self.nc = nc
        self.es = ExitStack()
        self.sem = {e: self.es.enter_context(nc.semaphore("s_" + e)) for e in ENGS}
        self.cnt = {e: 0 for e in ENGS}
        self.dsem = {}
        self.known = {e: {} for e in ENGS}
        self.lastw = {}
        self.readers = {}
        self.queue = {e: [] for e in ENGS}
        self.pending = {e: [] for e in ENGS}
        self.nops = 0

    def close(self):
        self.es.close()

    def _deps(self, eng, r, w):
        toks = []
        for k in r:
            t = self.lastw.get(k)
            if t is not None:
                toks.append(t)
        for k in w:
            t = self.lastw.get(k)
            if t is not None:
                toks.append(t)
            toks.extend(self.readers.get(k, ()))
        need = {}
        for t in toks:
            key, val = t[0], t[1]
            if eng == "pe" and key == "pe":
                continue
            if val is None:
                raise RuntimeError("dependency on unresolved (inc=False) op")
            if need.get(key, 0) < val:
                need[key] = val
        waits = []
        kn = self.known[eng]
        for key, val in need.items():
            if kn.get(key, 0) < val:
                kn[key] = val
                waits.append((key, val))
        return waits

    def _mark(self, tok, r, w):
        for k in w:
            self.lastw[k] = tok
            self.readers[k] = []
        for k in r:
            self.readers.setdefault(k, []).append(tok)

    rec = None

    def replay(self, item):
        kind, a, k = item
        (self.op if kind == "op" else self.dma)(*a, **k)

    def op(self, eng, fn, r=(), w=(), inc=True):
        if self.rec is not None:
            self.rec.append(("op", (eng, fn), dict(r=r, w=w, inc=inc)))
            return
        waits = self._deps(eng, r, w)
        if inc:
            self.cnt[eng] += 1
            tok = [eng, self.cnt[eng]]
            for p in self.pending[eng]:
                p[1] = self.cnt[eng]
            self.pending[eng] = []
        else:
            tok = [eng, None]
            self.pending[eng].append(tok)
        self._mark(tok, r, w)
        self.queue[eng].append((fn, waits, (eng, 1) if inc else None))
        self.nops += 1

    def dma(self, eng, out, in_, r=(), w=(), stream=None, **kw):
        assert stream is not None
        if self.rec is not None:
            self.rec.append(("dma", (eng, out, in_), dict(r=r, w=w, stream=stream, **kw)))
            return
        waits = self._deps(eng, r, w)
        if stream not in self.dsem:
            self.dsem[stream] = [self.es.enter_context(self.nc.semaphore("d%d" % len(self.dsem))), 0]
        ds = self.dsem[stream]
        ds[1] += 16
        tok = [("d", stream), ds[1]]
        self._mark(tok, r, w)
        self.queue[eng].append((lambda e, out=out, in_=in_, kw=kw: e.dma_start(out=out, in_=in_, **kw),
                                waits, (("d", stream), 16)))
        self.nops += 1

    def _semh(self, key):
        if isinstance(key, tuple):
            return self.dsem[key[1]][0]
        return self.sem[key]

    def wait_all(self, eng, keys):
        waits = self._deps(eng, keys, ())
        self.queue[eng].append((None, waits, None))

    def barrier(self):
        for e in ENGS:
            waits = []
            kn = self.known[e]
            for k2 in ENGS:
                if self.cnt[k2] > kn.get(k2, 0):
                    kn[k2] = self.cnt[k2]
                    waits.append((k2, self.cnt[k2]))
            for sname, ds in self.dsem.items():
                key = ("d", sname)
                if ds[1] > kn.get(key, 0):
                    kn[key] = ds[1]
                    waits.append((key, ds[1]))
            self.queue[e].append((None, waits, None))

    def flush(self):
        nc = self.nc
        self.barrier()
        q = self.queue
        self.queue = {e: [] for e in ENGS}
        for e in ENGS:
            if self.pending[e]:
                raise RuntimeError("unresolved inc=False ops at flush on " + e)

        def run(engine, items):
            for fn, waits, inc in items:
                for key, val in waits:
                    engine.wait_ge(self._semh(key), val)
                if fn is None:
                    continue
                ins = fn(engine)
                if inc is not None:
                    ins.then_inc(self._semh(inc[0]), inc[1])

        with nc.Block() as block:
            if q["sp"]:
                @block.sync
                def _(e):
                    run(e, q["sp"])
            if q["pe"]:
                @block.tensor
                def _(e):
                    run(e, q["pe"])
            if q["act"]:
                @block.scalar
                def _(e):
                    run(e, q["act"])
            if q["dve"]:
                @block.vector
                def _(e):
                    run(e, q["dve"])
            if q["pool"]:
                @block.gpsimd
                def _(e):
                    run(e, q["pool"])


def bc(ap, shape):
    return ap.to_broadcast(shape)


class Builder:
    def __init__(self, S, L, dbg=(), parts=("A", "B", "C"), moe="dense"):
        self.S, self.L = S, L
        self.NT = S // 128
        self.NB = S // 512
        self.parts = parts
        self.moe = moe
        nc = self.nc = bass.Bass("TRN2", target_bir_lowering=False)
        self.P = Prog(nc)
        self.dbg = set(dbg)
        dt_in = lambda n, s, d=F32: nc.dram_tensor(n, list(s), d, kind="ExternalInput").ap()
        self.x = dt_in("x", [S, D])
        self.ln0 = dt_in("ln0", [2, D])
        self.w_in = dt_in("w_in", [L, D, DIN])
        self.qkg = dt_in("qkg", [L, 128, 2])
        self.bias_b = dt_in("bias_b", [L, 4, 64, 960])
        self.conv_w = dt_in("conv_w", [L, 128, 6, 5])
        self.gdn_ab = dt_in("gdn_ab", [L, 2, 8])
        self.gdn_g = dt_in("gdn_g", [L, 64])
        self.wa = dt_in("wa", [L, 512, D])
        self.wb = dt_in("wb", [L, 256, D])
        self.wc = dt_in("wc", [L, 256, D])
        self.wo = dt_in("wo", [L, D, D])
        self.ln1 = dt_in("ln1", [L, 2, D])
        self.ln2 = dt_in("ln2", [L, 2, D])
        self.w_router = dt_in("w_router", [D, NE])
        self.router_bias = dt_in("router_bias", [NE])
        self.w1 = dt_in("w1", [L, NE, D, DE])
        self.w3 = dt_in("w3", [L, NE, D, DE])
        self.w2 = dt_in("w2", [L, NE, DE, D])
        self.c_ident = dt_in("c_ident", [128, 128])
        self.c_blk = dt_in("c_blk", [128, 128])
        self.c_rot = dt_in("c_rot", [128, 128])
        self.c_cos = dt_in("c_cos", [128, S])
        self.c_sin = dt_in("c_sin", [128, S])
        self.c_gdn = dt_in("c_gdn", [128, 5, 4, 128])
        okind = "ExternalOutput"
        self.out = nc.dram_tensor("out", [S, D], F32, kind=okind).ap()

        def scr(n, s, d=F32):
            k = "ExternalOutput" if n in self.dbg else "Internal"
            return nc.dram_tensor(n, list(s), d, kind=k).ap()
        self.h_d = scr("h_d", [S, D])
        self.qT_d = scr("qT_d", [512, S], BF16)
        self.kT_d = scr("kT_d", [128, S], BF16)
        self.v_d = scr("v_d", [S, 128], BF16)
        self.bqT_d = scr("bqT_d", [256, S], BF16)
        self.bkT_d = scr("bkT_d", [256, S], BF16)
        self.bv_d = scr("bv_d", [S, 256], BF16)
        self.cT_d = scr("cT_d", [768, S])
        self.cz_d = scr("cz_d", [S, 272])
        self.gT_d = scr("gT_d", [3072, S], BF16)
        self.yaT_d = scr("yaT_d", [512, S], BF16)
        self.ybT_d = scr("ybT_d", [256, S], BF16)
        self.ycT_d = scr("ycT_d", [256, S], BF16)
        self.es = ExitStack()
        self.hT = self.sb(self.es, "hT", [128, KC, S], BF16)
        self.comb = self.sb(self.es, "comb", [128, self.NT, NE])
        self.ident = self.sb(self.es, "ident", [128, 128])
        self.identb = self.sb(self.es, "identb", [128, 128], BF16)

    def sb(self, es, n, s, d=F32):
        self.uid = getattr(self, "uid", 0) + 1
        return es.enter_context(self.nc.sbuf_tensor("sb%d_%s" % (self.uid, n), list(s), d))

    def ps(self, es, n, s, d=F32):
        self.uid = getattr(self, "uid", 0) + 1
        return es.enter_context(self.nc.psum_tensor("ps%d_%s" % (self.uid, n), list(s), d))

    def mm_group(self, out_ap, pairs, r, w):
        P = self.P
        n = len(pairs)
        for i, (l, rh) in enumerate(pairs):
            P.op("pe", lambda e, l=l, rh=rh, i=i: e.matmul(out_ap, l, rh, start=(i == 0), stop=(i == n - 1)),
                 r=r, w=w, inc=(i == n - 1))

    def load_consts(self):
        P = self.P
        P.dma("sp", self.ident[:], self.c_ident, w=["ident"], stream="ident")
        P.dma("pool", self.identb[:], self.c_ident, w=["identb"], stream="identb")

    def ln_tile(self, es_tiles, t, tres, gb, gbres, dst_d, tt, hT_out=True, router=None, pfx="ln"):
        P = self.P
        st, mv, sd, xn, pT = (es_tiles[k] for k in ("st", "mv", "sd", "xn", "pT"))
        P.op("dve", lambda e: e.bn_stats(out=st[:, 0:6], in_=t[:, 0:512]), r=[tres], w=[pfx + "st"])
        P.op("dve", lambda e: e.bn_stats(out=st[:, 6:12], in_=t[:, 512:1024]), r=[tres], w=[pfx + "st"])
        P.op("dve", lambda e: e.bn_aggr(out=mv[:], in_=st[:]), r=[pfx + "st"], w=[pfx + "mv"])
        P.op("act", lambda e: e.activation(out=sd[:, 0:1], in_=mv[:, 1:2], func=AF.Sqrt, bias=1e-5, scale=1.0),
             r=[pfx + "mv"], w=[pfx + "sd"])
        P.op("dve", lambda e: e.reciprocal(out=sd[:, 1:2], in_=sd[:, 0:1]), r=[pfx + "sd"], w=[pfx + "sd1"])
        P.op("dve", lambda e: e.scalar_tensor_tensor(out=sd[:, 2:3], in0=mv[:, 0:1], scalar=-1.0, in1=sd[:, 1:2],
                                                     op0=ALU.mult, op1=ALU.mult),
             r=[pfx + "mv", pfx + "sd1"], w=[pfx + "sd2"])
        P.op("act", lambda e: e.activation(out=xn[:], in_=t[:], func=AF.Identity, bias=sd[:, 2:3], scale=sd[:, 1:2]),
             r=[tres, pfx + "sd1", pfx + "sd2"], w=[pfx + "xn"])
        P.op("dve", lambda e: e.tensor_tensor(out=xn[:], in0=xn[:], in1=gb[:, 0, :], op=ALU.mult),
             r=[pfx + "xn", gbres], w=[pfx + "xn"])
        P.op("pool", lambda e: e.tensor_tensor(out=xn[:], in0=xn[:], in1=gb[:, 1, :], op=ALU.add),
             r=[pfx + "xn", gbres], w=[pfx + "xn"])
        P.dma("sp", dst_d[tt * 128:(tt + 1) * 128, :], xn[:], r=[pfx + "xn"], w=[("h_d", tt) if dst_d is self.h_d else "out"],
              stream=pfx + "xn")
        if hT_out:
            for kc in range(KC):
                P.op("pe", lambda e, kc=kc: e.transpose(pT[:, kc, :], xn[:, kc * 128:(kc + 1) * 128], self.ident[:]),
                     r=[pfx + "xn", "ident"], w=[pfx + "pT"], inc=(kc == KC - 1))
            P.op("act", lambda e: e.activation(out=self.hT[:, :, tt * 128:(tt + 1) * 128], in_=pT[:], func=AF.Copy),
                 r=[pfx + "pT"], w=[("hT", tt)])
            if router is not None:
                router(tt, pT, pfx + "pT")

    def ln_scratch(self, es, pfx="ln"):
        return dict(st=self.sb(es, pfx + "st", [128, 12]), mv=self.sb(es, pfx + "mv", [128, 2]),
                    sd=self.sb(es, pfx + "sd", [128, 4]), xn=self.sb(es, pfx + "xn", [128, D]),
                    pT=self.ps(es, pfx + "pT", [128, KC, 128]))

    def phase_ln0(self):
        P = self.P
        with ExitStack() as es:
            tiles = self.ln_scratch(es)
            gb = self.sb(es, "gb0", [128, 2, D])
            xt = [self.sb(es, "x%d" % i, [128, D]) for i in range(2)]
            P.dma("sp", gb[:], self.ln0.partition_broadcast(128), w=["gb0"], stream="gb0")
            for tt in range(self.NT):
                b = tt % 2
                P.dma("sp", xt[b][:], self.x[tt * 128:(tt + 1) * 128, :], w=["xt%d" % b], stream="xt%d" % b)
                self.ln_tile(tiles, xt[b], "xt%d" % b, gb, "gb0", self.h_d if self.L > 0 else self.out, tt)
            P.flush()

    def phase_inproj(self, l):
        P = self.P
        S, NB, NT = self.S, self.NB, self.NT
        w_l = self.w_in[l].rearrange("(kc p) n -> p kc n", p=128)
        with ExitStack() as es:
            wt = [self.sb(es, "wi%d" % i, [128, KC, 512], BF16) for i in range(2)]
            stg = [self.sb(es, "stg%d" % i, [128, 512]) for i in range(2)]
            stgb = [self.sb(es, "stgb%d" % i, [128, 512], BF16) for i in range(2)]
            pa = [self.ps(es, "pa%d" % i, [128, 512]) for i in range(2)]
            pb = self.ps(es, "pb", [128, 512])
            pc = self.ps(es, "pc", [128, 512])
            blk = self.sb(es, "blk", [128, 128], BF16)
            rot = self.sb(es, "rot", [128, 128], BF16)
            cos = self.sb(es, "cos", [128, S])
            sin = self.sb(es, "sin", [128, S])
            qkg = self.sb(es, "qkg", [128, 2])
            sq = self.sb(es, "sq", [128, 512], BF16)
            rs = self.sb(es, "rs", [128, 512])
            qn = self.sb(es, "qn", [128, 512], BF16)
            t1 = self.sb(es, "t1", [128, 512])
            t2 = self.sb(es, "t2", [128, 512])
            P.dma("pool", blk[:], self.c_blk, w=["blk"], stream="blk")
            P.dma("pool", rot[:], self.c_rot, w=["rot"], stream="rot")
            P.dma("sp", cos[:], self.c_cos, w=["cos"], stream="cos")
            P.dma("sp", sin[:], self.c_sin, w=["sin"], stream="sin")
            P.dma("sp", qkg[:], self.qkg[l], w=["qkg"], stream="qkg")
            cnt = {"w": 0, "o": 0, "p": 0}

            def load_w(c0, n):
                b = cnt["w"] % 2
                cnt["w"] += 1
                P.dma("pool", wt[b][:, :, 0:n], w_l[:, :, c0:c0 + n], w=["wi%d" % b], stream="wi%d" % b)
                return wt[b], "wi%d" % b

            def fm_block(c0, n, evac):
                w, wres = load_w(c0, n)
                for j in range(n // 128):
                    for tb in range(NB):
                        pp = cnt["p"] % 2
                        cnt["p"] += 1
                        self.mm_group(pa[pp][:], [(w[:, kc, j * 128:(j + 1) * 128], self.hT[:, kc, tb * 512:(tb + 1) * 512])
                                                   for kc in range(KC)],
                                      r=[wres] + [("hT", tb * 4 + i) for i in range(4)], w=["pa%d" % pp])
                        evac(pa[pp], "pa%d" % pp, c0 + j * 128, tb)

            def out_stage(bf):
                b = cnt["o"] % 2
                cnt["o"] += 1
                return (stgb[b], "stgb%d" % b) if bf else (stg[b], "stg%d" % b)

            def evac_aqk(p, pres, col, tb):
                isq = col < C_AK
                gcol = 0 if isq else 1
                tsl = slice(tb * 512, (tb + 1) * 512)
                P.op("act", lambda e: e.activation(out=sq[:], in_=p[:], func=AF.Square), r=[pres], w=["sq"])
                P.op("pe", lambda e: e.matmul(pb[:], blk[:], sq[:], start=True, stop=True), r=["blk", "sq"], w=["pb"])
                P.op("act", lambda e: e.activation(out=rs[:], in_=pb[:], func=AF.Sqrt, bias=(64e-6 if isq else 1e-6),
                                                   scale=(1.0 if isq else 1.0 / 64)), r=["pb"], w=["rs"])
                P.op("dve", lambda e: e.reciprocal(out=rs[:], in_=rs[:]), r=["rs"], w=["rs"])
                P.op("dve", lambda e: e.scalar_tensor_tensor(out=qn[:], in0=p[:], scalar=qkg[:, gcol:gcol + 1], in1=rs[:],
                                                             op0=ALU.mult, op1=ALU.mult),
                     r=[pres, "rs", "qkg"], w=["qn"])
                P.op("pe", lambda e: e.matmul(pc[:], rot[:], qn[:], start=True, stop=True), r=["rot", "qn"], w=["pc"])
                P.op("pool", lambda e: e.tensor_tensor(out=t1[:], in0=qn[:], in1=cos[:, tsl], op=ALU.mult),
                     r=["qn", "cos"], w=["t1"])
                P.op("dve", lambda e: e.tensor_tensor(out=t2[:], in0=pc[:], in1=sin[:, tsl], op=ALU.mult),
                     r=["pc", "sin"], w=["t2"])
                o, ores = out_stage(True)
                P.op("pool", lambda e: e.tensor_tensor(out=o[:], in0=t1[:], in1=t2[:], op=ALU.add),
                     r=["t1", "t2"], w=[ores])
                dst = self.qT_d[col:col + 128, tsl] if isq else self.kT_d[:, tsl]
                P.dma("sp", dst, o[:], r=[ores], w=["qkT_d"], stream=ores)

            if "A" in self.parts:
                fm_block(C_AQ, 512, evac_aqk)
                fm_block(C_AK, 128, evac_aqk)

            def evac_simple(dst_d, row0, bf, func=AF.Copy, scale=1.0):
                def ev(p, pres, col, tb):
                    o, ores = out_stage(bf)
                    P.op("act", lambda e: e.activation(out=o[:], in_=p[:], func=func, scale=scale), r=[pres], w=[ores])
                    r0 = col - row0
                    P.dma("sp", dst_d[r0:r0 + 128, tb * 512:(tb + 1) * 512], o[:], r=[ores], w=[("fm_d", id(dst_d))],
                          stream=ores)
                return ev

            if "B" in self.parts:
                fm_block(C_BQ, 256, evac_simple(self.bqT_d, C_BQ, True, scale=0.125))
                fm_block(C_BK, 256, evac_simple(self.bkT_d, C_BK, True))
            if "C" in self.parts:
                fm_block(C_CQ, 512, evac_simple(self.cT_d, C_CQ, False))
                fm_block(C_CV, 256, evac_simple(self.cT_d, C_CQ, False))
            for g in range(6):
                fm_block(C_GATE + g * 512, 512, evac_simple(self.gT_d, C_GATE, True, func=AF.Sigmoid))

            def tm_block(c0, n, dst_d, bf):
                w, wres = load_w(c0, n)
                for tt in range(NT):
                    pp = cnt["p"] % 2
                    cnt["p"] += 1
                    self.mm_group(pa[pp][:, 0:n], [(self.hT[:, kc, tt * 128:(tt + 1) * 128], w[:, kc, 0:n]) for kc in range(KC)],
                                  r=[wres, ("hT", tt)], w=["pa%d" % pp])
                    o, ores = out_stage(bf)
                    P.op("act", lambda e, o=o, pp=pp: e.activation(out=o[:, 0:n], in_=pa[pp][:, 0:n], func=AF.Copy),
                         r=["pa%d" % pp], w=[ores])
                    P.dma("sp", dst_d[tt * 128:(tt + 1) * 128, :], o[:, 0:n], r=[ores], w=[("tm_d", id(dst_d))], stream=ores)

            if "A" in self.parts:
                tm_block(C_AV, 128, self.v_d, True)
            if "B" in self.parts:
                tm_block(C_BV, 256, self.bv_d, True)
            if "C" in self.parts:
                tm_block(C_CZ, 272, self.cz_d, False)
            P.flush()

    def phase_attn_a(self, l):
        P = self.P
        S, NB, NT = self.S, self.NB, self.NT
        with ExitStack() as es:
            qh = [self.sb(es, "qh%d" % i, [128, S], BF16) for i in range(2)]
            kT = self.sb(es, "kT", [128, 2, S], BF16)
            vx = self.sb(es, "vx", [128, NT, 2, 128], BF16)
            pT = [self.sb(es, "pT%d" % i, [128, 512], BF16) for i in range(3)]
            onesr = self.sb(es, "onesr", [128, 64])
            rc = self.sb(es, "rc", [128, 512])
            bcs = self.sb(es, "bcs", [64, 512])
            ya = [self.sb(es, "ya%d" % i, [64, 512], BF16) for i in range(2)]
            sps = [self.ps(es, "sps%d" % i, [128, 512]) for i in range(3)]
            ops_ = [self.ps(es, "ops%d" % i, [128, 512]) for i in range(2)]
            bps = self.ps(es, "bps", [64, 512])
            P.op("pool", lambda e: e.memset(kT[64:128, :, :], 0.0), w=["kT"])
            for i in range(2):
                P.op("pool", lambda e, i=i: e.memset(qh[i][64:128, :], 0.0), w=["qh%d" % i])
            P.dma("sp", kT[0:64, :, :], self.kT_d.rearrange("(g d) s -> d g s", d=64), r=["qkT_d"], w=["kT"], stream="kT")
            P.op("pool", lambda e: e.memset(vx[:], 1.0), w=["vx"])
            P.op("pool", lambda e: e.memset(onesr[:], 1.0), w=["onesr"])
            for g in range(2):
                P.dma("sp", vx[:, :, g, 0:64], self.v_d[:, g * 64:(g + 1) * 64].rearrange("(t p) d -> p t d", p=128),
                      r=[("tm_d", id(self.v_d))], w=["vx"], stream="vx")
            its = [(hq, qb, kt) for hq in range(8) for qb in range(NB) for kt in range(NT)]
            N_ = len(its)
            deferred = {}

            def emit_qk(j):
                hq, qb, kt = its[j]
                g, qb_, b = hq // 4, hq % 2, j % 3
                if qb == 0 and kt == 0:
                    for h2 in ([0, 1] if hq == 0 else [hq + 1]):
                        if h2 < 8:
                            P.dma("sp", qh[h2 % 2][0:64, :], self.qT_d[h2 * 64:(h2 + 1) * 64, :], r=["qkT_d"], w=["qh%d" % (h2 % 2)],
                                  stream="qh%d" % (h2 % 2))
                P.op("pe", lambda e: e.matmul(sps[b][:], kT[:, g, kt * 128:(kt + 1) * 128], qh[qb_][:, qb * 512:(qb + 1) * 512],
                                              start=True, stop=True), r=["kT", "qh%d" % qb_], w=["sps%d" % b])

            def tail(hq, qb, ob):
                P.op("pe", lambda e: e.matmul(bps[:], onesr[64:65, :], rc[64:65, :], start=True, stop=True),
                     r=["onesr", "rc"], w=["bps"])
                P.op("act", lambda e: e.activation(out=bcs[:], in_=bps[:], func=AF.Copy), r=["bps"], w=["bcs"])
                P.op("dve", lambda e: e.tensor_tensor(out=ya[ob][:], in0=ops_[ob][0:64, :], in1=bcs[:], op=ALU.mult),
                     r=["ops%d" % ob, "bcs"], w=["ya%d" % ob])
                P.dma("sp", self.yaT_d[hq * 64:(hq + 1) * 64, qb * 512:(qb + 1) * 512], ya[ob][:], r=["ya%d" % ob],
                      w=["yaT_d"], stream="ya%d" % ob)

            emit_qk(0)
            emit_qk(1)
            for j in range(N_):
                hq, qb, kt = its[j]
                g, b = hq // 4, j % 3
                ob = (hq * NB + qb) % 2
                if j + 2 < N_:
                    emit_qk(j + 2)
                P.op("act", lambda e, b=b: e.activation(out=pT[b][:], in_=sps[b][:], func=AF.Exp),
                     r=["sps%d" % b], w=["pT%d" % b])
                P.op("pe", lambda e, b=b, kt=kt, g=g, ob=ob: e.matmul(ops_[ob][:, :], vx[:, kt, g, :], pT[b][:],
                                                          start=(kt == 0), stop=(kt == NT - 1)),
                     r=["vx", "pT%d" % b], w=["ops%d" % ob], inc=(kt == NT - 1))
                if kt == NT - 1:
                    P.op("dve", lambda e, ob=ob: e.reciprocal(out=rc[64:65, :], in_=ops_[ob][64:65, :]), r=["ops%d" % ob], w=["rc"])
                    deferred[min(j + 2, N_ - 1)] = (hq, qb, ob)
                if j in deferred:
                    tail(*deferred.pop(j))
            assert not deferred
            P.flush()

    def phase_attn_b(self, l):
        P = self.P
        S, NT = self.S, self.NT
        rows = S // GRID_W
        wr_ = min(8, rows)
        NBUF = 4
        with ExitStack() as es:
            qT = self.sb(es, "bqT", [64, 4, S], BF16)
            kT = self.sb(es, "bkT", [64, 4, S], BF16)
            v0 = self.sb(es, "bv0", [128, NT, 256], BF16)
            v1 = self.sb(es, "bv1", [128, NT, 256], BF16)
            bias = self.sb(es, "bbias", [64, 4, 960])
            sc = [self.sb(es, "bsc%d" % i, [64, 512]) for i in range(NBUF)]
            pr = [self.sb(es, "bpr%d" % i, [64, 512], BF16) for i in range(NBUF)]
            st = [self.sb(es, "bst%d" % i, [64, 4]) for i in range(NBUF)]
            dg = [self.sb(es, "bdg%d" % i, [64, 64], BF16) for i in range(NBUF)]
            pts = [self.sb(es, "bpts%d" % i, [128, 4, 64], BF16) for i in range(2)]
            yo = [self.sb(es, "byo%d" % i, [64, 4, 64], BF16) for i in range(2)]
            sp_ = [self.ps(es, "bsp%d" % i, [64, 512]) for i in range(NBUF)]
            ptp = [self.ps(es, "bptp%d" % i, [128, 4, 64]) for i in range(2)]
            op_ = [self.ps(es, "bop%d" % i, [64, 4, 64]) for i in range(2)]
            for h in range(4):
                P.dma("sp", qT[:, h, :], self.bqT_d[h * 64:(h + 1) * 64, :], r=[("fm_d", id(self.bqT_d))], w=["bqT"], stream="bqT")
                P.dma("sp", kT[:, h, :], self.bkT_d[h * 64:(h + 1) * 64, :], r=[("fm_d", id(self.bkT_d))], w=["bkT"], stream="bkT")
            P.dma("sp", v0[:], self.bv_d.rearrange("(t p) c -> p t c", p=128), r=[("tm_d", id(self.bv_d))], w=["bv0"], stream="bv0")
            P.dma("sp", v1[:, 0:NT - 1, :], self.bv_d[64:S - 64, :].rearrange("(t p) c -> p t c", p=128), r=[("tm_d", id(self.bv_d))],
                  w=["bv1"], stream="bv1")
            P.dma("sp", bias[:], self.bias_b[l].rearrange("h q k -> q h k"), w=["bbias"], stream="bbias")
            its = [(r, h) for r in range(rows) for h in range(4)]
            N_ = len(its)

            def geo(r):
                r0 = min(max(r - wr_ // 2, 0), rows - wr_)
                return r0, (r0 - r + 7) * 64, r0 * 64

            def s1(i):
                r, h = its[i]
                b = i % NBUF
                r0, d0, k0 = geo(r)
                P.op("pe", lambda e: e.matmul(sp_[b][:], qT[:, h, r * 64:(r + 1) * 64], kT[:, h, k0:k0 + 512], start=True, stop=True),
                     r=["bqT", "bkT"], w=["bsp%d" % b])
                P.op("dve", lambda e: e.tensor_tensor(out=sc[b][:], in0=sp_[b][:], in1=bias[:, h, d0:d0 + 512], op=ALU.add),
                     r=["bsp%d" % b, "bbias"], w=["bsc%d" % b])
                P.op("dve", lambda e: e.tensor_reduce(out=st[b][:, 0:1], in_=sc[b][:], axis=AX.X, op=ALU.max),
                     r=["bsc%d" % b], w=[("bst", b, 0)])
                P.op("dve", lambda e: e.tensor_scalar(out=st[b][:, 1:2], in0=st[b][:, 0:1], scalar1=-1.0, scalar2=None, op0=ALU.mult),
                     r=[("bst", b, 0)], w=[("bst", b, 1)])
                P.op("act", lambda e: e.activation(out=pr[b][:], in_=sc[b][:], func=AF.Exp, bias=st[b][:, 1:2], scale=1.0,
                                                   accum_out=st[b][:, 2:3]), r=["bsc%d" % b, ("bst", b, 1)], w=["bpr%d" % b, ("bst", b, 2)])

            def s1b(i):
                b = i % NBUF
                P.op("dve", lambda e: e.reciprocal(out=st[b][:, 3:4], in_=st[b][:, 2:3]), r=[("bst", b, 2)], w=[("bst", b, 3)])
                P.op("dve", lambda e: e.tensor_scalar(out=dg[b][:], in0=self.identb[0:64, 0:64], scalar1=st[b][:, 3:4], scalar2=None,
                                                      op0=ALU.mult), r=["identb", ("bst", b, 3)], w=["bdg%d" % b])

            def s2(i):
                b, pb = i % NBUF, i % 2
                for kc in range(4):
                    P.op("pe", lambda e, kc=kc: e.matmul(ptp[pb][:, kc, :], pr[b][:, kc * 128:(kc + 1) * 128], dg[b][:], start=True, stop=True),
                         r=["bpr%d" % b, "bdg%d" % b], w=["bptp%d" % pb], inc=(kc == 3))
                P.op("act", lambda e: e.activation(out=pts[pb][:], in_=ptp[pb][:], func=AF.Copy), r=["bptp%d" % pb], w=["bpts%d" % pb])

            def s3(i):
                r, h = its[i]
                pb, ob = i % 2, r % 2
                r0, d0, k0 = geo(r)
                vsrc, vres, t0 = (v0, "bv0", r0 // 2) if r0 % 2 == 0 else (v1, "bv1", (r0 - 1) // 2)
                for kc in range(4):
                    P.op("pe", lambda e, kc=kc: e.matmul(op_[ob][:, h, :], vsrc[:, t0 + kc, h * 64:(h + 1) * 64], pts[pb][:, kc, :],
                                                         start=(kc == 0), stop=(kc == 3)),
                         r=[vres, "bpts%d" % pb], w=["bop%d" % ob], inc=(kc == 3))
                if h == 3:
                    P.op("act", lambda e: e.activation(out=yo[ob][:], in_=op_[ob][:], func=AF.Copy), r=["bop%d" % ob], w=["byo%d" % ob])
                    P.dma("sp", self.ybT_d[:, r * 64:(r + 1) * 64].rearrange("(h d) q -> d h q", d=64), yo[ob][:], r=["byo%d" % ob],
                          w=["ybT_d"], stream="byo%d" % ob)

            for t in range(N_ + 4):
                if t < N_:
                    s1(t)
                if 0 <= t - 1 < N_:
                    s1b(t - 1)
                if 0 <= t - 3 < N_:
                    s2(t - 3)
                if 0 <= t - 4 < N_:
                    s3(t - 4)
            P.flush()

    def phase_gdn(self, l):
        P = self.P
        S, NT, NB = self.S, self.NT, self.NB
        nc = self.nc
        if not hasattr(self, "cn_d"):
            mk = lambda n, s: nc.dram_tensor(n, list(s), F32, kind=("ExternalOutput" if n in self.dbg else "Internal")).ap()
            self.cn_d = mk("cn_d", [768, S])
            self.ktok_d = mk("ktok_d", [S, 256])
            self.vtok_d = mk("vtok_d", [S, 256])
            self.gates_d = mk("gates_d", [S, 16])
            self.o_d = mk("o_d", [2, S, 256])
        with ExitStack() as es:
            cw = self.sb(es, "cw", [128, 6, 5])
            x = self.sb(es, "gx", [128, S + 4])
            y = self.sb(es, "gy", [128, S])
            sq = self.sb(es, "gsq", [128, 512], BF16)
            rs = self.sb(es, "grs", [128, 512])
            blk = self.sb(es, "gblk", [128, 128], BF16)
            tk = [self.sb(es, "gtk%d" % i, [128, 512]) for i in range(2)]
            gin = self.sb(es, "gin", [128, NT, 16])
            gout = self.sb(es, "gout", [128, NT, 16])
            ab = self.sb(es, "gab", [128, 2, 8])
            pss = self.ps(es, "gpss", [128, 512])
            ptr = [self.ps(es, "gptr%d" % i, [128, 4, 128]) for i in range(2)]
            P.dma("sp", cw[:], self.conv_w[l], w=["cw"], stream="cw")
            P.dma("pool", blk[:], self.c_blk, w=["gblk"], stream="gblk")
            P.dma("sp", ab[:], self.gdn_ab[l].partition_broadcast(128), w=["gab"], stream="gab")
            P.op("pool", lambda e: e.memset(x[:, 0:2], 0.0), w=["gxp"])
            P.op("pool", lambda e: e.memset(x[:, S + 2:S + 4], 0.0), w=["gxp"])
            ti = 0
            import os
            for ch in range(6 if os.environ.get("GDBG", "") != "gates" else 0):
                P.dma("sp", x[:, 2:S + 2], self.cT_d[ch * 128:(ch + 1) * 128, :], r=[("fm_d", id(self.cT_d))], w=["gx"], stream="gx")
                P.op("act", lambda e, ch=ch: e.activation(out=y[:], in_=x[:, 0:S], func=AF.Identity, scale=cw[:, ch, 0:1]),
                     r=["gx", "gxp", "cw"], w=["gy"])
                for k in range(1, 5):
                    P.op("dve", lambda e, ch=ch, k=k: e.scalar_tensor_tensor(out=y[:], in0=x[:, k:k + S], scalar=cw[:, ch, k:k + 1], in1=y[:],
                                                                             op0=ALU.mult, op1=ALU.add), r=["gx", "gxp", "cw", "gy"], w=["gy"])
                P.op("act", lambda e: e.activation(out=y[:], in_=y[:], func=AF.Silu), r=["gy"], w=["gy"])
                if ch < 4:
                    isq = ch < 2
                    for tb in range(NB):
                        tsl = slice(tb * 512, (tb + 1) * 512)
                        P.op("act", lambda e, tsl=tsl: e.activation(out=sq[:], in_=y[:, tsl], func=AF.Square), r=["gy"], w=["gsq"])
                        P.op("pe", lambda e: e.matmul(pss[:], blk[:], sq[:], start=True, stop=True), r=["gblk", "gsq"], w=["gpss"])
                        P.op("act", lambda e, isq=isq: e.activation(out=rs[:], in_=pss[:], func=AF.Sqrt, bias=(64e-6 if isq else 1e-6),
                                                                    scale=(64.0 if isq else 1.0)), r=["gpss"], w=["grs"])
                        P.op("dve", lambda e: e.reciprocal(out=rs[:], in_=rs[:]), r=["grs"], w=["grs"])
                        P.op("dve", lambda e, tsl=tsl: e.tensor_tensor(out=y[:, tsl], in0=y[:, tsl], in1=rs[:], op=ALU.mult),
                             r=["gy", "grs"], w=["gy"])
                P.dma("sp", self.cn_d[ch * 128:(ch + 1) * 128, :], y[:], r=["gy"], w=["cn_d"], stream="gy")
                if ch >= 2:
                    dst = self.ktok_d if ch < 4 else self.vtok_d
                    for tb in range(NB):
                        b = ti % 2
                        ti += 1
                        for j in range(4):
                            tt = tb * 4 + j
                            P.op("pe", lambda e, b=b, j=j, tt=tt: e.transpose(ptr[b][:, j, :], y[:, tt * 128:(tt + 1) * 128], self.ident[:]),
                                 r=["gy", "ident"], w=["gptr%d" % b], inc=(j == 3))
                        P.op("act", lambda e, b=b: e.activation(out=tk[b][:], in_=ptr[b][:].rearrange("p a b -> p (a b)"), func=AF.Copy),
                             r=["gptr%d" % b], w=["gtk%d" % b])
                        P.dma("sp", dst[tb * 512:(tb + 1) * 512, (ch % 2) * 128:(ch % 2 + 1) * 128].rearrange("(j p) c -> p j c", p=128),
                              tk[b][:].rearrange("p (j c) -> p j c", c=128), r=["gtk%d" % b], w=["kvtok_d"], stream="gtk%d" % b)
            if os.environ.get("GDBG", "") == "conv":
                P.flush()
                return
            P.dma("sp", gin[:], self.cz_d[:, 256:272].rearrange("(t p) c -> p t c", p=128), r=[("tm_d", id(self.cz_d))], w=["gin"], stream="gin")
            P.op("act", lambda e: e.activation(out=gout[:, :, 0:8], in_=gin[:, :, 0:8], func=AF.Sigmoid), r=["gin"], w=["gout_b"])
            P.op("dve", lambda e: e.tensor_tensor(out=gin[:, :, 8:16], in0=gin[:, :, 8:16], in1=bc(ab[:, 1:2, :], [128, NT, 8]), op=ALU.add),
                 r=["gin", "gab"], w=["gin2"])
            P.op("act", lambda e: e.activation(out=gin[:, :, 8:16], in_=gin[:, :, 8:16], func=AF.Exp), r=["gin2"], w=["gin2"])
            P.op("act", lambda e: e.activation(out=gin[:, :, 8:16], in_=gin[:, :, 8:16], func=AF.Ln, bias=1.0, scale=1.0), r=["gin2"], w=["gin2"])
            P.op("act", lambda e: e.activation(out=ab[:, 0, :], in_=ab[:, 0, :], func=AF.Exp), r=["gab"], w=["gab0"])
            P.op("dve", lambda e: e.scalar_tensor_tensor(out=gout[:, :, 8:16], in0=gin[:, :, 8:16], scalar=-1.0, in1=bc(ab[:, 0:1, :], [128, NT, 8]),
                                                         op0=ALU.mult, op1=ALU.mult), r=["gin2", "gab0"], w=["gout_g"])
            P.dma("sp", self.gates_d.rearrange("(t p) c -> p t c", p=128), gout[:], r=["gout_b", "gout_g"], w=["gates_d"], stream="gout")
            P.flush()
        if getattr(self, "gdn_stop", None) == "prep":
            return
        NC = S // 64
        with ExitStack() as es:
            cst = self.sb(es, "gcst", [128, 5, 4, 128])
            NEGM, NEGMT, STRICT, ID8 = cst[:, 0], cst[:, 1], cst[:, 2], cst[:, 3]
            CUM, ONES = cst[:, 4, 0, :], cst[:, 4, 1, :]
            T8 = lambda n: self.sb(es, n, [128, 4, 128])
            ld = [dict(KT=T8("gKT%d" % i), QT=T8("gQT%d" % i), Kt=T8("gKt%d" % i), Vt=T8("gVt%d" % i),
                       gb=self.sb(es, "ggb%d" % i, [128, 8])) for i in range(2)]
            TN = ("gdiag", "gD", "gDT", "geGr", "gta", "gtb", "gSB", "gqkTm", "gQgT", "gX0", "gX1", "gXT0", "gXT1",
                  "gPT", "grv", "grk", "gU", "gWT", "gVn", "gKd", "gO")
            WT_ = [{n: T8(n + "_%d" % p) for n in TN} for p in range(2)]
            SM = [self.sb(es, "gsm%d" % p, [128, 24]) for p in range(2)]
            St = T8("gS")
            psAA = self.ps(es, "gpsA", [128, 2, 8])
            BK = [[self.ps(es, "gpb%d_%d" % (p, i), [128, 4, 128]) for i in range(3)] for p in range(2)]
            fl = lambda t: t[:].rearrange("p c j -> p (c j)")
            P.dma("sp", cst[:], self.c_gdn, w=["gcst"], stream="gcst")
            P.op("pool", lambda e: e.memset(St[:], 0.0), w=["gS"])
            for i in range(2):
                for nm in ("KT", "QT", "Kt", "Vt"):
                    P.op("pool", lambda e, t=ld[i][nm]: e.memset(t[:], 0.0), w=["g" + nm + str(i)])

            def bcg(t, col0):
                return bc(t[:, col0:col0 + 4].unsqueeze(2), [128, 4, 128])

            def body(s_):
                a, b = s_, NC - 1 - s_
                p = s_ % 2
                L_ = ld[p]
                sfx = str(p)
                W = WT_[p]
                R = lambda n: n + "_" + sfx
                diagG, Dm, DTm, eGr, t_a, t_b, SBm, qkTm, QgT = (W[n] for n in ("gdiag", "gD", "gDT", "geGr", "gta", "gtb", "gSB", "gqkTm", "gQgT"))
                X, XT = [W["gX0"], W["gX1"]], [W["gXT0"], W["gXT1"]]
                PT, rv, rk, U, WTt, Vn, Kd, ot = (W[n] for n in ("gPT", "grv", "grk", "gU", "gWT", "gVn", "gKd", "gO"))
                sm = SM[p]
                psA = psAA[:, p, :]
                B0, B1, B2 = BK[p]
                rB0, rB1, rB2, rA = R("gB0"), R("gB1"), R("gB2"), R("gpsA")

                def mm8(ps, psres, lhs, lres, rhs, rres):
                    for c in range(4):
                        P.op("pe", lambda e, c=c: e.matmul(ps[:, c, :], lhs[:, c, :], rhs[:, c, :], start=True, stop=True),
                             r=[lres, rres], w=[psres], inc=(c == 3))

                ra, rb_ = slice(a * 64, (a + 1) * 64), slice(b * 64, (b + 1) * 64)
                lo, hi = slice(0, 64), slice(64, 128)
                for nm, src_, r0 in (("KT", self.cn_d, 256), ("QT", self.cn_d, 0)):
                    for pp, rr in ((lo, ra), (hi, rb_)):
                        P.dma("sp", L_[nm][pp, :, pp], src_[r0:r0 + 256, rr].rearrange("(h d) t -> d h t", d=64),
                              r=["cn_d"], w=["g" + nm + sfx], stream="g" + nm + sfx)
                for nm, src_ in (("Kt", self.ktok_d), ("Vt", self.vtok_d)):
                    for pp, rr in ((lo, ra), (hi, rb_)):
                        P.dma("sp", L_[nm][pp, :, pp], src_[rr, :].rearrange("t (h d) -> t h d", d=64),
                              r=["kvtok_d"], w=["g" + nm + sfx], stream="g" + nm + sfx)
                for pp, rr, c0, s0 in ((lo, ra, 0, 0), (hi, rb_, 0, 4), (lo, ra, 4, 8), (hi, rb_, 4, 12)):
                    P.dma("sp", L_["gb"][pp, c0:c0 + 4], self.gates_d[rr, s0:s0 + 4], r=["gates_d"], w=["ggb" + sfx], stream="ggb" + sfx)
                KT, QT, Kt, Vt, gb = L_["KT"], L_["QT"], L_["Kt"], L_["Vt"], L_["gb"]
                rKT, rQT, rKt, rVt, rgb = ("g" + n + sfx for n in ("KT", "QT", "Kt", "Vt", "gb"))
                P.op("pe", lambda e: e.matmul(psA[:, 0:4], CUM, gb[:, 4:8], start=True, stop=True), r=["gcst", rgb], w=[rA], inc=False)
                P.op("pe", lambda e: e.matmul(psA[:, 4:8], ONES, gb[:, 4:8], start=True, stop=True), r=["gcst", rgb], w=[rA])
                P.op("act", lambda e: e.activation(out=sm[:, 0:8], in_=psA, func=AF.Copy), r=[rA], w=[R("gsmG")])
                P.op("act", lambda e: e.activation(out=sm[:, 8:16], in_=sm[:, 0:8], func=AF.Exp), r=[R("gsmG")], w=[R("gsmE")])
                P.op("dve", lambda e: e.tensor_tensor(out=sm[:, 16:20], in0=sm[:, 4:8], in1=sm[:, 0:4], op=ALU.subtract), r=[R("gsmG")], w=[R("gsmK")])
                P.op("act", lambda e: e.activation(out=sm[:, 16:20], in_=sm[:, 16:20], func=AF.Exp), r=[R("gsmK")], w=[R("gsmK")])
                P.op("dve", lambda e: e.tensor_tensor(out=sm[:, 20:24], in0=gb[:, 0:4], in1=sm[:, 8:12], op=ALU.mult), r=[rgb, R("gsmE")], w=[R("gsmB")])
                Gb, eGtb, kdb, bkb = bcg(sm, 0), bcg(sm, 12), bcg(sm, 16), bcg(sm, 20)
                betab = bc(gb[:, 0:4].unsqueeze(2), [128, 4, 128])
                P.op("pool", lambda e: e.tensor_tensor(out=diagG[:], in0=ID8, in1=Gb, op=ALU.mult), r=["gcst", R("gsmG")], w=[R("gdiag")])
                P.op("pe", lambda e: e.matmul(fl(B0), ONES, fl(diagG), start=True, stop=True), r=["gcst", R("gdiag")], w=[rB0])
                P.op("dve", lambda e: e.scalar_tensor_tensor(out=t_a[:], in0=B0[:], scalar=-1.0, in1=NEGM, op0=ALU.mult, op1=ALU.add),
                     r=[rB0, "gcst"], w=[R("gta")])
                P.op("dve", lambda e: e.tensor_tensor(out=t_a[:], in0=t_a[:], in1=Gb, op=ALU.add), r=[R("gta"), R("gsmG")], w=[R("gta")])
                P.op("act", lambda e: e.activation(out=Dm[:], in_=t_a[:], func=AF.Exp), r=[R("gta")], w=[R("gD")])
                P.op("dve", lambda e: e.tensor_tensor(out=t_b[:], in0=B0[:], in1=NEGMT, op=ALU.add), r=[rB0, "gcst"], w=[R("gtb")])
                P.op("dve", lambda e: e.tensor_tensor(out=t_b[:], in0=t_b[:], in1=Gb, op=ALU.subtract), r=[R("gtb"), R("gsmG")], w=[R("gtb")])
                P.op("act", lambda e: e.activation(out=DTm[:], in_=t_b[:], func=AF.Exp), r=[R("gtb")], w=[R("gDT")])
                P.op("act", lambda e: e.activation(out=eGr[:], in_=B0[:], func=AF.Exp), r=[rB0], w=[R("geGr")])
                P.op("pool", lambda e: e.tensor_tensor(out=QgT[:], in0=QT[:], in1=eGr[:], op=ALU.mult), r=[rQT, R("geGr")], w=[R("gQgT")])
                mm8(B1, rB1, KT, rKT, KT, rKT)
                mm8(B2, rB2, KT, rKT, QT, rQT)
                P.op("pool", lambda e: e.tensor_tensor(out=SBm[:], in0=STRICT, in1=betab, op=ALU.mult), r=["gcst", rgb], w=[R("gSB")])
                P.op("dve", lambda e: e.tensor_tensor(out=t_a[:], in0=B1[:], in1=Dm[:], op=ALU.mult), r=[rB1, R("gD")], w=[R("gta")])
                P.op("pool", lambda e: e.tensor_tensor(out=X[0][:], in0=t_a[:], in1=SBm[:], op=ALU.mult), r=[R("gta"), R("gSB")], w=[R("gX0")])
                P.op("dve", lambda e: e.tensor_tensor(out=qkTm[:], in0=B2[:], in1=DTm[:], op=ALU.mult), r=[rB2, R("gDT")], w=[R("gqkTm")])
                mm8(B1, rB1, X[0], R("gX0"), cst[:, 3], "gcst")
                P.op("act", lambda e: e.activation(out=XT[0][:], in_=B1[:], func=AF.Copy), r=[rB1], w=[R("gXT0")])
                P.op("dve", lambda e: e.scalar_tensor_tensor(out=PT[:], in0=B1[:], scalar=-1.0, in1=ID8, op0=ALU.mult, op1=ALU.add),
                     r=[rB1, "gcst", R("gXT0")], w=[R("gPT")])
                for lv in range(1, 6):
                    ci, ni = (lv - 1) % 2, lv % 2
                    mm8(B0, rB0, XT[ci], R("gXT%d" % ci), X[ci], R("gX%d" % ci))
                    if lv < 5:
                        mm8(B2, rB2, X[ci], R("gX%d" % ci), XT[ci], R("gXT%d" % ci))
                    P.op("act", lambda e, ni=ni: e.activation(out=X[ni][:], in_=B0[:], func=AF.Copy), r=[rB0], w=[R("gX%d" % ni)])
                    if lv < 5:
                        P.op("dve", lambda e, ni=ni: e.tensor_copy(out=XT[ni][:], in_=B2[:]), r=[rB2], w=[R("gXT%d" % ni)])
                    mm8(B1, rB1, X[ni], R("gX%d" % ni), PT, R("gPT"))
                    P.op("dve", lambda e: e.tensor_tensor(out=PT[:], in0=PT[:], in1=B1[:], op=ALU.add), r=[R("gPT"), rB1], w=[R("gPT")])
                P.op("pool", lambda e: e.tensor_tensor(out=rv[:], in0=Vt[:], in1=betab, op=ALU.mult), r=[rVt, rgb], w=[R("grv")])
                P.op("pool", lambda e: e.tensor_tensor(out=rk[:], in0=Kt[:], in1=bkb, op=ALU.mult), r=[rKt, R("gsmB")], w=[R("grk")])
                P.op("pool", lambda e: e.tensor_tensor(out=Kd[:], in0=Kt[:], in1=kdb, op=ALU.mult), r=[rKt, R("gsmK")], w=[R("gKd")])
                mm8(B0, rB0, PT, R("gPT"), rv, R("grv"))
                P.op("act", lambda e: e.activation(out=U[:], in_=B0[:], func=AF.Copy), r=[rB0], w=[R("gU")])
                mm8(B2, rB2, rk, R("grk"), PT, R("gPT"))
                P.op("dve", lambda e: e.tensor_copy(out=WTt[:], in_=B2[:]), r=[rB2], w=[R("gWT")])
                mark.append(len(P.rec))
                mm8(B1, rB1, WTt, R("gWT"), St, "gS")
                P.op("dve", lambda e: e.scalar_tensor_tensor(out=Vn[:], in0=B1[:], scalar=-1.0, in1=U[:], op0=ALU.mult, op1=ALU.add),
                     r=[rB1, R("gU")], w=[R("gVn")])
                for c in range(4):
                    P.op("pe", lambda e, c=c: e.matmul(B0[:, c, :], QgT[:, c, :], St[:, c, :], start=True, stop=False), r=[R("gQgT"), "gS"], w=[rB0],
                         inc=False)
                    P.op("pe", lambda e, c=c: e.matmul(B0[:, c, :], qkTm[:, c, :], Vn[:, c, :], start=False, stop=True), r=[R("gqkTm"), R("gVn")],
                         w=[rB0], inc=(c == 3))
                mm8(B2, rB2, Kd, R("gKd"), Vn, R("gVn"))
                P.op("pool", lambda e: e.tensor_tensor(out=St[:], in0=St[:], in1=eGtb, op=ALU.mult), r=["gS", R("gsmE")], w=["gS"])
                P.op("dve", lambda e: e.tensor_tensor(out=St[:], in0=St[:], in1=B2[:], op=ALU.add), r=["gS", rB2], w=["gS"])
                P.op("act", lambda e: e.activation(out=ot[:], in_=B0[:], func=AF.Copy), r=[rB0], w=[R("gO")])
                P.dma("sp", self.o_d[0, ra, :].rearrange("t (h d) -> t h d", d=64), ot[0:64, :, 0:64], r=[R("gO")], w=["o_d"], stream="gO" + sfx)
                P.dma("sp", self.o_d[1, rb_, :].rearrange("t (h d) -> t h d", d=64), ot[64:128, :, 64:128], r=[R("gO")], w=["o_d"], stream="gO" + sfx)

            streams = [[], []]
            for s_ in range(NC):
                P.rec = []
                mark = []
                body(s_)
                items, P.rec = P.rec, None
                for k, it_ in enumerate(items):
                    streams[s_ % 2].append((s_, k >= mark[0], it_))
            per = len(streams[0]) // ((NC + 1) // 2)
            delay = per // 2
            merged = []
            i0 = i1 = 0
            n0, n1 = len(streams[0]), len(streams[1])
            t = 0
            while i0 < n0 or i1 < n1:
                if i0 < n0:
                    merged.append(streams[0][i0]); i0 += 1
                if t >= delay and i1 < n1:
                    merged.append(streams[1][i1]); i1 += 1
                t += 1
            last_rec = -1
            for s_, isrec, it_ in merged:
                if isrec:
                    assert s_ >= last_rec, "recurrence emitted out of chunk order"
                    last_rec = s_
                P.replay(it_)
            P.flush()
        if getattr(self, "gdn_stop", None) == "main":
            return
        with ExitStack() as es:
            gg = self.sb(es, "ggn", [128, 64])
            of = [self.sb(es, "gof%d" % i, [128, 4, 64]) for i in range(2)]
            obk = [self.sb(es, "gob%d" % i, [128, 4, 64]) for i in range(2)]
            zt = [self.sb(es, "gz%d" % i, [128, 4, 64]) for i in range(2)]
            sq2 = self.sb(es, "gsq2", [128, 4, 64])
            ms = self.sb(es, "gms", [128, 8])
            yb_ = [self.sb(es, "gyb%d" % i, [128, 2, 128], BF16) for i in range(2)]
            pt2 = [self.ps(es, "gpt2%d" % i, [128, 2, 128]) for i in range(2)]
            P.dma("sp", gg[:], self.gdn_g[l].partition_broadcast(128), w=["ggn"], stream="ggn")
            for tt in range(NT):
                b = tt % 2
                tr = slice(tt * 128, (tt + 1) * 128)
                P.dma("sp", of[b][:], self.o_d[0, tr, :].rearrange("t (h d) -> t h d", d=64), r=["o_d"], w=["gof%d" % b], stream="gof%d" % b)
                P.dma("sp", obk[b][:], self.o_d[1, tr, :].rearrange("t (h d) -> t h d", d=64), r=["o_d"], w=["gob%d" % b], stream="gob%d" % b)
                P.dma("sp", zt[b][:], self.cz_d[tr, 0:256].rearrange("t (h d) -> t h d", d=64), r=[("tm_d", id(self.cz_d))], w=["gz%d" % b],
                      stream="gz%d" % b)
                P.op("dve", lambda e, b=b: e.tensor_tensor(out=of[b][:], in0=of[b][:], in1=obk[b][:], op=ALU.add), r=["gof%d" % b, "gob%d" % b],
                     w=["gof%d" % b])
                P.op("act", lambda e, b=b: e.activation(out=sq2[:], in_=of[b][:], func=AF.Square), r=["gof%d" % b], w=["gsq2"])
                P.op("dve", lambda e: e.tensor_reduce(out=ms[:, 0:4], in_=sq2[:], axis=AX.X, op=ALU.add), r=["gsq2"], w=["gms"])
                P.op("act", lambda e: e.activation(out=ms[:, 4:8], in_=ms[:, 0:4], func=AF.Sqrt, bias=1e-6, scale=1.0 / 64), r=["gms"], w=["gms2"])
                P.op("dve", lambda e: e.reciprocal(out=ms[:, 4:8], in_=ms[:, 4:8]), r=["gms2"], w=["gms2"])
                P.op("dve", lambda e, b=b: e.tensor_tensor(out=of[b][:], in0=of[b][:], in1=bc(ms[:, 4:8].unsqueeze(2), [128, 4, 64]), op=ALU.mult),
                     r=["gof%d" % b, "gms2"], w=["gof%d" % b])
                P.op("pool", lambda e, b=b: e.tensor_tensor(out=of[b][:], in0=of[b][:], in1=bc(gg[:].unsqueeze(1), [128, 4, 64]), op=ALU.mult),
                     r=["gof%d" % b, "ggn"], w=["gof%d" % b])
                P.op("act", lambda e, b=b: e.activation(out=zt[b][:], in_=zt[b][:], func=AF.Silu), r=["gz%d" % b], w=["gz%d" % b])
                P.op("dve", lambda e, b=b: e.tensor_tensor(out=of[b][:], in0=of[b][:], in1=zt[b][:], op=ALU.mult), r=["gof%d" % b, "gz%d" % b],
                     w=["gof%d" % b])
                for j in range(2):
                    P.op("pe", lambda e, b=b, j=j: e.transpose(pt2[b][:, j, :], of[b][:, 2 * j:2 * j + 2, :].rearrange("p a d -> p (a d)"), self.ident[:]),
                         r=["gof%d" % b, "ident"], w=["gpt2%d" % b], inc=(j == 1))
                P.op("act", lambda e, b=b: e.activation(out=yb_[b][:], in_=pt2[b][:], func=AF.Copy), r=["gpt2%d" % b], w=["gyb%d" % b])
                P.dma("sp", self.ycT_d[:, tr].rearrange("(j p) t -> p j t", p=128), yb_[b][:], r=["gyb%d" % b], w=["ycT_d"], stream="gyb%d" % b)
            P.flush()

    def phase_merge(self, l):
        P = self.P
        S, NB, NT = self.S, self.NB, self.NT
        with ExitStack() as es:
            wa = self.sb(es, "wa", [128, 4, D], BF16)
            wb = self.sb(es, "wb", [128, 2, D], BF16)
            wc = self.sb(es, "wc", [128, 2, D], BF16)
            wo = self.sb(es, "wo", [128, 8, D], BF16)
            yT = self.sb(es, "yT", [128, 8, 512], BF16)
            gt = [self.sb(es, "gt%d" % i, [128, 3, 512], BF16) for i in range(2)]
            mixT = self.sb(es, "mixT", [128, 8, 512], BF16)
            m1 = self.sb(es, "m1", [128, 512])
            m2 = self.sb(es, "m2", [128, 512])
            m3 = self.sb(es, "m3", [128, 512])
            hold = [self.sb(es, "hold%d" % i, [128, D]) for i in range(2)]
            tsum = [self.sb(es, "tsum%d" % i, [128, D]) for i in range(2)]
            gb = self.sb(es, "gb1", [128, 2, D])
            pabc = [self.ps(es, "pabc%d" % i, [128, 512]) for i in range(3)]
            po = [self.ps(es, "po%d" % i, [128, 512]) for i in range(2)]
            tiles = self.ln_scratch(es, "l1")
            rt = self.router_setup(es) if not getattr(self, "no_router", False) else None
            P.dma("pool", wa[:], self.wa[l].rearrange("(kc p) n -> p kc n", p=128), w=["wa"], stream="wa")
            P.dma("pool", wb[:], self.wb[l].rearrange("(kc p) n -> p kc n", p=128), w=["wb"], stream="wb")
            P.dma("pool", wc[:], self.wc[l].rearrange("(kc p) n -> p kc n", p=128), w=["wc"], stream="wc")
            P.dma("pool", wo[:], self.wo[l].rearrange("(kc p) n -> p kc n", p=128), w=["wo"], stream="wo")
            P.dma("sp", gb[:], self.ln1[l].partition_broadcast(128), w=["gb1"], stream="gb1")
            if "B" not in self.parts:
                P.op("pool", lambda e: e.memset(yT[:, 4:6, :], 0.0), w=["yTb"])
            if "C" not in self.parts:
                P.op("pool", lambda e: e.memset(yT[:, 6:8, :], 0.0), w=["yTc"])
            for tb in range(NB):
                tsl = slice(tb * 512, (tb + 1) * 512)
                if "A" in self.parts:
                    P.dma("sp", yT[:, 0:4, :], self.yaT_d[:, tsl].rearrange("(kc p) s -> p kc s", p=128), r=["yaT_d"], w=["yTa"],
                          stream="yTa")
                else:
                    P.op("pool", lambda e: e.memset(yT[:, 0:4, :], 0.0), w=["yTa"])
                if "B" in self.parts:
                    P.dma("sp", yT[:, 4:6, :], self.ybT_d[:, tsl].rearrange("(kc p) s -> p kc s", p=128), r=["ybT_d"], w=["yTb"],
                          stream="yTb")
                if "C" in self.parts:
                    P.dma("sp", yT[:, 6:8, :], self.ycT_d[:, tsl].rearrange("(kc p) s -> p kc s", p=128), r=["ycT_d"], w=["yTc"],
                          stream="yTc")
                for dc in range(8):
                    b = dc % 2
                    P.dma("sp", gt[b][:], self.gT_d[:, tsl].rearrange("(t c p) s -> p t c s", p=128, t=3)[:, :, dc, :],
                          r=[("fm_d", id(self.gT_d))], w=["gt%d" % b], stream="gt%d" % b)
                    csl = slice(dc * 128, (dc + 1) * 128)
                    self.mm_group(pabc[0][:], [(wa[:, kc, csl], yT[:, kc, :]) for kc in range(4)], r=["wa", "yTa"], w=["pabc0"])
                    self.mm_group(pabc[1][:], [(wb[:, kc, csl], yT[:, 4 + kc, :]) for kc in range(2)], r=["wb", "yTb"], w=["pabc1"])
                    self.mm_group(pabc[2][:], [(wc[:, kc, csl], yT[:, 6 + kc, :]) for kc in range(2)], r=["wc", "yTc"], w=["pabc2"])
                    P.op("dve", lambda e, b=b: e.tensor_tensor(out=m1[:], in0=pabc[0][:], in1=gt[b][:, 0, :], op=ALU.mult),
                         r=["pabc0", "gt%d" % b], w=["m1"])
                    P.op("dve", lambda e, b=b: e.tensor_tensor(out=m2[:], in0=pabc[1][:], in1=gt[b][:, 1, :], op=ALU.mult),
                         r=["pabc1", "gt%d" % b], w=["m2"])
                    P.op("dve", lambda e, b=b: e.tensor_tensor(out=m3[:], in0=pabc[2][:], in1=gt[b][:, 2, :], op=ALU.mult),
                         r=["pabc2", "gt%d" % b], w=["m3"])
                    P.op("pool", lambda e: e.tensor_tensor(out=m1[:], in0=m1[:], in1=m2[:], op=ALU.add), r=["m1", "m2"], w=["m1"])
                    P.op("pool", lambda e, dc=dc: e.tensor_tensor(out=mixT[:, dc, :], in0=m1[:], in1=m3[:], op=ALU.add),
                         r=["m1", "m3"], w=[("mixT", dc)])
                for ti in range(4):
                    tt = tb * 4 + ti
                    hb = tt % 2
                    P.dma("sp", hold[hb][:], self.h_d[tt * 128:(tt + 1) * 128, :], r=[("h_d", tt)], w=["hold%d" % hb],
                          stream="hold%d" % hb)
                    for half in range(2):
                        self.mm_group(po[half][:], [(mixT[:, dc, ti * 128:(ti + 1) * 128], wo[:, dc, half * 512:(half + 1) * 512])
                                                    for dc in range(8)],
                                      r=["wo"] + [("mixT", dc) for dc in range(8)], w=["po%d" % half])
                        P.op("dve", lambda e, hb=hb, half=half: e.scalar_tensor_tensor(
                            out=tsum[hb][:, half * 512:(half + 1) * 512], in0=hold[hb][:, half * 512:(half + 1) * 512],
                            scalar=ALPHA, in1=po[half][:], op0=ALU.mult, op1=ALU.add),
                            r=["hold%d" % hb, "po%d" % half], w=["tsum%d" % hb])
                    self.ln_tile(tiles, tsum[hb], "tsum%d" % hb, gb, "gb1", self.h_d, tt, router=rt, pfx="l1")
            P.flush()

    def router_setup(self, es):
        P = self.P
        wr = self.sb(es, "wr", [128, KC, NE])
        wrh = self.sb(es, "wrh", [128, KC, NE], BF16)
        wrl = self.sb(es, "wrl", [128, KC, NE], BF16)
        rb = self.sb(es, "rb", [128, NE])
        hlo = self.sb(es, "hlo", [128, KC, 128], BF16)
        pl = self.ps(es, "pl", [128, NE])
        sc = self.sb(es, "r_sc", [128, NE])
        sel = self.sb(es, "r_sel", [128, NE])
        tmp = self.sb(es, "r_tmp", [128, NE])
        tmp2 = self.sb(es, "r_tmp2", [128, NE])
        g8 = self.sb(es, "r_g8", [128, 8, 4])
        oh1 = self.sb(es, "r_oh1", [128, NE])
        oh2 = self.sb(es, "r_oh2", [128, NE])
        sm = self.sb(es, "r_sm", [128, 8])
        P.dma("sp", wr[:], self.w_router.rearrange("(kc p) n -> p kc n", p=128), w=["wr"], stream="wr")
        P.dma("sp", rb[:], self.router_bias.partition_broadcast(128), w=["rb"], stream="rb")
        P.dma("pool", wrh[:], self.w_router.rearrange("(kc p) n -> p kc n", p=128), w=["wrh"], stream="wrh")
        P.op("dve", lambda e: e.tensor_tensor(out=wrl[:], in0=wr[:], in1=wrh[:], op=ALU.subtract), r=["wr", "wrh"], w=["wrl"])
        BIG = 1.0e4

        def router(tt, pT, pTres):
            tsl = slice(tt * 128, (tt + 1) * 128)
            P.op("dve", lambda e: e.tensor_tensor(out=hlo[:], in0=pT[:], in1=self.hT[:, :, tsl], op=ALU.subtract),
                 r=[pTres, ("hT", tt)], w=["hlo"])
            pairs = []
            for kc in range(KC):
                pairs += [(self.hT[:, kc, tsl], wrh[:, kc, :]), (self.hT[:, kc, tsl], wrl[:, kc, :]), (hlo[:, kc, :], wrh[:, kc, :])]
            self.mm_group(pl[:], pairs, r=["hlo", ("hT", tt), "wrh", "wrl"], w=["pl"])
            P.op("act", lambda e: e.activation(out=sc[:], in_=pl[:], func=AF.Sigmoid), r=["pl"], w=["r_sc"])
            P.op("dve", lambda e: e.tensor_tensor(out=sel[:], in0=sc[:], in1=rb[:], op=ALU.add), r=["r_sc", "rb"], w=["r_sel"])
            s3 = sel[:].rearrange("p (g k) -> p g k", k=4)
            t3 = tmp[:].rearrange("p (g k) -> p g k", k=4)
            P.op("dve", lambda e: e.tensor_reduce(out=sm[:, 0:8], in_=s3, axis=AX.X, op=ALU.max), r=["r_sel"], w=["r_sm"])
            P.op("dve", lambda e: e.tensor_tensor(out=t3, in0=s3, in1=bc(sm[:, 0:8].unsqueeze(2), [128, 8, 4]), op=ALU.is_equal),
                 r=["r_sel", "r_sm"], w=["r_tmp"])
            P.op("dve", lambda e: e.scalar_tensor_tensor(out=tmp[:], in0=tmp[:], scalar=-BIG, in1=sel[:], op0=ALU.mult, op1=ALU.add),
                 r=["r_tmp", "r_sel"], w=["r_tmp"])
            P.op("dve", lambda e: e.tensor_reduce(out=g8[:, :, 0], in_=t3, axis=AX.X, op=ALU.max), r=["r_tmp"], w=["r_g8"])
            P.op("dve", lambda e: e.tensor_tensor(out=g8[:, :, 1], in0=g8[:, :, 0], in1=sm[:, 0:8], op=ALU.add),
                 r=["r_g8", "r_sm"], w=["r_g8b"])
            P.op("dve", lambda e: e.tensor_reduce(out=sm[:, 0:1], in_=g8[:, :, 1], axis=AX.X, op=ALU.max), r=["r_g8b"], w=["r_sm1"])
            P.op("dve", lambda e: e.tensor_scalar(out=g8[:, :, 2], in0=g8[:, :, 1], scalar1=sm[:, 0:1], scalar2=None, op0=ALU.is_lt),
                 r=["r_g8b", "r_sm1"], w=["r_g8c"])
            P.op("dve", lambda e: e.scalar_tensor_tensor(out=t3, in0=bc(g8[:, :, 2:3], [128, 8, 4]), scalar=-BIG, in1=s3,
                                                         op0=ALU.mult, op1=ALU.add), r=["r_g8c", "r_sel"], w=["r_tmp"])
            P.op("dve", lambda e: e.tensor_reduce(out=sm[:, 1:2], in_=tmp[:], axis=AX.X, op=ALU.max), r=["r_tmp"], w=["r_sm2"])
            P.op("dve", lambda e: e.tensor_scalar(out=oh1[:], in0=tmp[:], scalar1=sm[:, 1:2], scalar2=None, op0=ALU.is_equal),
                 r=["r_tmp", "r_sm2"], w=["r_oh1"])
            P.op("dve", lambda e: e.scalar_tensor_tensor(out=tmp2[:], in0=oh1[:], scalar=-BIG, in1=tmp[:], op0=ALU.mult, op1=ALU.add),
                 r=["r_oh1", "r_tmp"], w=["r_tmp2"])
            P.op("dve", lambda e: e.tensor_reduce(out=sm[:, 2:3], in_=tmp2[:], axis=AX.X, op=ALU.max), r=["r_tmp2"], w=["r_sm3"])
            P.op("dve", lambda e: e.tensor_scalar(out=oh2[:], in0=tmp2[:], scalar1=sm[:, 2:3], scalar2=None, op0=ALU.is_equal),
                 r=["r_tmp2", "r_sm3"], w=["r_oh2"])
            P.op("dve", lambda e: e.tensor_tensor(out=oh1[:], in0=oh1[:], in1=oh2[:], op=ALU.add), r=["r_oh1", "r_oh2"], w=["r_oh1"])
            P.op("dve", lambda e: e.tensor_tensor(out=oh1[:], in0=oh1[:], in1=sc[:], op=ALU.mult), r=["r_oh1", "r_sc"], w=["r_oh1"])
            P.op("dve", lambda e: e.tensor_reduce(out=sm[:, 3:4], in_=oh1[:], axis=AX.X, op=ALU.add), r=["r_oh1"], w=["r_sm4"])
            P.op("dve", lambda e: e.reciprocal(out=sm[:, 4:5], in_=sm[:, 3:4]), r=["r_sm4"], w=["r_sm5"])
            P.op("dve", lambda e: e.tensor_scalar(out=self.comb[:, tt, :], in0=oh1[:], scalar1=sm[:, 4:5], scalar2=None, op0=ALU.mult),
                 r=["r_oh1", "r_sm5"], w=[("comb", tt)])
        return router

    def phase_moe_dense(self, l, last):
        P = self.P
        S, NB, NT = self.S, self.NB, self.NT
        G = min(8, NT)
        w1_l = self.w1[l].rearrange("e (kc p) n -> e p kc n", p=128)
        w3_l = self.w3[l].rearrange("e (kc p) n -> e p kc n", p=128)
        w2_l = self.w2[l].rearrange("e (kc p) n -> e p kc n", p=128)
        with ExitStack() as es:
            w1t = [self.sb(es, "w1t%d" % i, [128, KC, DE], BF16) for i in range(2)]
            w3t = [self.sb(es, "w3t%d" % i, [128, KC, DE], BF16) for i in range(2)]
            w2t = [self.sb(es, "w2t%d" % i, [128, 4, D], BF16) for i in range(2)]
            yacc = self.sb(es, "yacc", [128, G, D])
            hid = [self.sb(es, "hid%d" % i, [128, 4, 512], BF16) for i in range(2)]
            sl = [self.sb(es, "sl%d" % i, [128, 512], BF16) for i in range(2)]
            hold = [self.sb(es, "mhold%d" % i, [128, D]) for i in range(2)]
            gb = self.sb(es, "gb2", [128, 2, D])
            p1 = [self.ps(es, "p1_%d" % i, [128, 512]) for i in range(2)]
            p3 = [self.ps(es, "p3_%d" % i, [128, 512]) for i in range(2)]
            py = [self.ps(es, "py%d" % i, [128, 512]) for i in range(2)]
            tiles = self.ln_scratch(es, "l2")
            P.dma("sp", gb[:], self.ln2[l].partition_broadcast(128), w=["gb2"], stream="gb2")
            def record(fn):
                P.rec = []
                fn()
                items, P.rec = P.rec, None
                return items

            for grp in range(NT // G):
                P.op("pool", lambda e: e.memset(yacc[:], 0.0), w=["yacc"])
                units = [(ex, blk) for ex in range(NE) for blk in range(G // 4)]

                def stage1(ui):
                    ex, blk = units[ui]
                    b = ex % 2
                    tb = grp * (G // 4) + blk
                    hb = ui % 2
                    hres = [("hT", tb * 4 + i) for i in range(4)]
                    segs = []

                    def loads():
                        P.dma("pool", w1t[b][:], w1_l[ex], w=["w1t%d" % b], stream="w1t%d" % b)
                        P.dma("pool", w3t[b][:], w3_l[ex], w=["w3t%d" % b], stream="w3t%d" % b)
                        P.dma("pool", w2t[b][:], w2_l[ex], w=["w2t%d" % b], stream="w2t%d" % b)
                    pre = record(loads) if blk == 0 else []
                    for fc in range(4):
                        pb_ = fc % 2
                        fsl = slice(fc * 128, (fc + 1) * 128)

                        def s_a():
                            self.mm_group(p1[pb_][:], [(w1t[b][:, kc, fsl], self.hT[:, kc, tb * 512:(tb + 1) * 512]) for kc in range(KC)],
                                          r=["w1t%d" % b] + hres, w=["p1_%d" % pb_])
                            P.op("act", lambda e, pb_=pb_: e.activation(out=sl[pb_][:], in_=p1[pb_][:], func=AF.Silu),
                                 r=["p1_%d" % pb_], w=["sl%d" % pb_])

                        def s_b():
                            self.mm_group(p3[pb_][:], [(w3t[b][:, kc, fsl], self.hT[:, kc, tb * 512:(tb + 1) * 512]) for kc in range(KC)],
                                          r=["w3t%d" % b] + hres, w=["p3_%d" % pb_])
                            P.op("dve", lambda e, hb=hb, fc=fc, pb_=pb_: e.tensor_tensor(out=hid[hb][:, fc, :], in0=sl[pb_][:], in1=p3[pb_][:], op=ALU.mult),
                                 r=["sl%d" % pb_, "p3_%d" % pb_], w=[("hid", hb, fc)])
                        segs.append(pre + record(s_a))
                        pre = []
                        segs.append(record(s_b))
                    return segs

                def stage2(ui):
                    ex, blk = units[ui]
                    b = ex % 2
                    tb = grp * (G // 4) + blk
                    hb = ui % 2
                    segs = []
                    for ti in range(4):
                        gi = blk * 4 + ti
                        tt = tb * 4 + ti
                        for half in range(2):
                            hs = slice(half * 512, (half + 1) * 512)

                            def s_c():
                                self.mm_group(py[half][:], [(hid[hb][:, fc, ti * 128:(ti + 1) * 128], w2t[b][:, fc, hs]) for fc in range(4)],
                                              r=["w2t%d" % b] + [("hid", hb, fc) for fc in range(4)], w=["py%d" % half])
                                P.op("dve", lambda e, gi=gi, hs=hs, half=half, tt=tt, ex=ex: e.scalar_tensor_tensor(
                                    out=yacc[:, gi, hs], in0=py[half][:], scalar=self.comb[:, tt, ex:ex + 1], in1=yacc[:, gi, hs],
                                    op0=ALU.mult, op1=ALU.add), r=["py%d" % half, ("comb", tt), "yacc"], w=["yacc"])
                            segs.append(record(s_c))
                    return segs

                emit = lambda seg: [P.replay(it_) for it_ in seg]
                for seg in stage1(0):
                    emit(seg)
                for ui in range(len(units)):
                    s2 = stage2(ui)
                    s1 = stage1(ui + 1) if ui + 1 < len(units) else []
                    for k in range(8):
                        emit(s2[k])
                        if s1:
                            emit(s1[k])
                for gi in range(G):
                    tt = grp * G + gi
                    hb = tt % 2
                    P.dma("sp", hold[hb][:], self.h_d[tt * 128:(tt + 1) * 128, :], r=[("h_d", tt)], w=["mhold%d" % hb],
                          stream="mhold%d" % hb)
                    P.op("dve", lambda e, hb=hb, gi=gi: e.scalar_tensor_tensor(out=hold[hb][:], in0=hold[hb][:], scalar=ALPHA,
                                                                                 in1=yacc[:, gi, :], op0=ALU.mult, op1=ALU.add),
                         r=["mhold%d" % hb, "yacc"], w=["mhold%d" % hb])
                    self.ln_tile(tiles, hold[hb], "mhold%d" % hb, gb, "gb2", self.out if last else self.h_d, tt,
                                 hT_out=not last, pfx="l2")
            P.flush()

    def build(self, stop=None):
        P = self.P
        self.load_consts()
        self.phase_ln0()
        for l in range(self.L):
            if stop == "ln0":
                break
            self.phase_inproj(l)
            if stop == "inproj":
                break
            if "A" in self.parts:
                self.phase_attn_a(l)
            if "B" in self.parts:
                self.phase_attn_b(l)
            if "C" in self.parts:
                self.phase_gdn(l)
            if stop == "attn":
                break
            self.phase_merge(l)
            if stop == "merge":
                break
            self.phase_moe_dense(l, last=(l == self.L - 1))
        P.wait_all("sp", list(P.lastw.keys()))
        P.flush()
        self.es.close()
        P.close()
        return self.nc


def host_consts(S):
    c = {}
    c["c_ident"] = np.eye(128, dtype=np.float32)
    blk = np.zeros((128, 128), np.float32)
    blk[:64, :64] = 1.0
    blk[64:, 64:] = 1.0
    c["c_blk"] = blk
    R = np.zeros((64, 64), np.float32)
    for base in (0, 32):
        for i in range(16):
            R[base + i, base + 16 + i] = -1.0
            R[base + 16 + i, base + i] = 1.0
    RT = np.zeros((128, 128), np.float32)
    RT[:64, :64] = R.T
    RT[64:, 64:] = R.T
    c["c_rot"] = RT
    t = np.arange(S)
    row = (t // GRID_W).astype(np.float32)
    col = (t % GRID_W).astype(np.float32)
    inv = (10000.0 ** (-np.arange(0, 32, 2, dtype=np.float32) / 32)).astype(np.float32)
    ang_r = row[None, :] * inv[:, None]
    ang_c = col[None, :] * inv[:, None]
    cos64 = np.concatenate([np.cos(ang_r), np.cos(ang_r), np.cos(ang_c), np.cos(ang_c)], 0)
    sin64 = np.concatenate([np.sin(ang_r), np.sin(ang_r), np.sin(ang_c), np.sin(ang_c)], 0)
    c["c_cos"] = np.concatenate([cos64, cos64], 0).astype(np.float32)
    c["c_sin"] = np.concatenate([sin64, sin64], 0).astype(np.float32)
    g = np.zeros((128, 5, 4, 128), np.float32)
    i = np.arange(64)[:, None]
    j = np.arange(64)[None, :]
    g[:, 0] = NEG
    g[:, 1] = NEG
    lo, hi = slice(0, 64), slice(64, 128)
    for pp, fwd in ((lo, True), (hi, False)):
        allow = (i >= j) if fwd else (i <= j)
        for cc in range(4):
            g[pp, 0, cc, pp] = np.where(allow, 0.0, NEG)
            g[pp, 1, cc, pp] = np.where(allow.T, 0.0, NEG)
            g[pp, 2, cc, pp] = ((i > j) if fwd else (i < j)).astype(np.float32)
            g[pp, 3, cc, pp] = (i == j).astype(np.float32)
        g[pp, 4, 0, pp] = ((i <= j) if fwd else (i >= j)).astype(np.float32)
        g[pp, 4, 1, pp] = 1.0
    c["c_gdn"] = g
    return c


def host_layout(inp, L):
    f = lambda a: np.ascontiguousarray(np.asarray(a, dtype=np.float32))
    m = {}
    m["ln0"] = f(np.stack([inp["ln0_g"], inp["ln0_b"]], 0))
    m["w_in"] = f(inp["w_in"][:L])
    qg = np.asarray(inp["q_norm_g"], np.float32)[:L]
    kg = np.asarray(inp["k_norm_g"], np.float32)[:L]
    m["qkg"] = f(np.stack([np.concatenate([qg, qg], 1), np.concatenate([kg, kg], 1)], 2))
    m["wa"] = f(inp["w_branch_a"][:L])
    m["wb"] = f(inp["w_branch_b"][:L])
    m["wc"] = f(inp["w_branch_c"][:L])
    m["wo"] = f(inp["w_out"][:L])
    m["ln1"] = f(np.stack([inp["ln1_g"][:L], inp["ln1_b"][:L]], 1))
    m["ln2"] = f(np.stack([inp["ln2_g"][:L], inp["ln2_b"][:L]], 1))
    m["w_router"] = f(inp["w_router"])
    m["router_bias"] = f(inp["router_bias"])
    m["w1"] = f(inp["w1"][:L])
    m["w3"] = f(inp["w3"][:L])
    m["w2"] = f(inp["w2"][:L])
    rpb = np.asarray(inp["na_rpb"], np.float32)[:L]
    c = np.arange(64)
    cs = np.clip(c - 8, 0, 48)
    kc_ = np.arange(64)
    inwin = (kc_[None, :] >= cs[:, None]) & (kc_[None, :] < cs[:, None] + 16)
    dc = np.clip(kc_[None, :] - c[:, None] + 15, 0, 30)
    g = rpb[:, :, :, dc]
    g = np.where(inwin[None, None, None], g, np.float32(NEG))
    m["bias_b"] = f(np.transpose(g, (0, 1, 3, 2, 4)).reshape(L, 4, 64, 960))
    cwv = np.asarray(inp["conv_w"], np.float32)[:L]
    m["conv_w"] = f(np.transpose(cwv.reshape(L, 5, 6, 128), (0, 3, 2, 1)))
    m["gdn_ab"] = f(np.stack([np.asarray(inp["A_log"], np.float32)[:L].reshape(L, 8),
                              np.asarray(inp["dt_bias"], np.float32)[:L].reshape(L, 8)], 1))
    m["gdn_g"] = f(inp["gdn_norm_g"][:L])
    return m


_CACHE = {}


def kernel(**inputs):
    S = 4096
    L = DEPTH
    x = np.asarray(inputs["x"], dtype=np.float32)
    nb = x.shape[0]
    key = (S, L)
    if key not in _CACHE:
        _CACHE[key] = Builder(S, L).build()
    nc = _CACHE[key]
    shared = host_layout(inputs, L)
    shared.update(host_consts(S))
    in_maps = []
    for b in range(nb):
        mm = dict(shared)
        mm["x"] = np.ascontiguousarray(x[b])
        in_maps.append(mm)
    res = run_bass_kernel_spmd(nc, in_maps, core_ids=list(range(nb)))
    return np.stack([np.asarray(r["out"], dtype=np.float32) for r in res.results], 0)
```
